# Optimizing a Trainium2 kernel written in Bass

```python
import jax, jax.numpy as jnp
from jax import lax
import numpy as np

D_MODEL = 2048
BATCH = 16
SEQ = 2048
DEPTH = 4

HEAD_DIM = 128
MIX_WIDTH = D_MODEL
N_HEADS_NSA = MIX_WIDTH // (2 * HEAD_DIM)
N_KV_NSA = 2
GQA_GROUP = N_HEADS_NSA // N_KV_NSA
N_HEADS_FOX = MIX_WIDTH // (2 * HEAD_DIM)
NSA_WIDTH = N_HEADS_NSA * HEAD_DIM
FOX_WIDTH = N_HEADS_FOX * HEAD_DIM
ROPE_DIM = HEAD_DIM // 4
ROPE_THETA = 500000.0
CMP_BLOCK = 32
CMP_STRIDE = 16
CMP_HIDDEN = 256
SLC_BLOCK = 64
SLC_TOPK = 8
N_LOCAL_SLC = 2
WINDOW = 512
Q_BLOCK = 128
SLC_Q_CHUNK = 32
D_FF = 5632
EPS = 1e-6
NEG_INF = -1e30
FORCED_SCORE = 1e9
N_NSA_BRANCHES = 3
COLS_NSA_Q = NSA_WIDTH
COLS_NSA_KV = N_NSA_BRANCHES * 2 * N_KV_NSA * HEAD_DIM
COLS_NSA_GATE = N_NSA_BRANCHES * N_HEADS_NSA
COLS_FOX_QKV = 3 * FOX_WIDTH
COLS_FOX_F = N_HEADS_FOX
PROJ_COLS = COLS_NSA_Q + COLS_NSA_KV + COLS_NSA_GATE + COLS_FOX_QKV + COLS_FOX_F
SPLITS = (COLS_NSA_Q,
          COLS_NSA_Q + COLS_NSA_KV,
          COLS_NSA_Q + COLS_NSA_KV + COLS_NSA_GATE,
          COLS_NSA_Q + COLS_NSA_KV + COLS_NSA_GATE + COLS_FOX_QKV)

kernel_name = "hymba_nsa_fox_macaron"


def rms_norm(x, g):
    xf = x.astype(jnp.float32)
    y = xf * lax.rsqrt(jnp.mean(xf * xf, axis=-1, keepdims=True) + EPS)
    return (y * g.astype(jnp.float32)).astype(x.dtype)


def swiglu(h, w_gate, w_up, w_down):
    return (jax.nn.silu(h @ w_gate) * (h @ w_up)) @ w_down


def rope_tables(pos):
    inv = ROPE_THETA ** (-jnp.arange(0, ROPE_DIM, 2, dtype=jnp.float32) / ROPE_DIM)
    ang = pos.astype(jnp.float32)[:, None] * inv[None, :]
    return jnp.cos(ang), jnp.sin(ang)


def partial_rope(x, cos, sin):
    half = ROPE_DIM // 2
    shape = (1, cos.shape[0]) + (1,) * (x.ndim - 3) + (half,)
    c = cos.reshape(shape).astype(x.dtype)
    s = sin.reshape(shape).astype(x.dtype)
    x1, x2, xp = x[..., :half], x[..., half:ROPE_DIM], x[..., ROPE_DIM:]
    return jnp.concatenate([x1 * c - x2 * s, x2 * c + x1 * s, xp], axis=-1)


def masked_probs(s, mask):
    s = jnp.where(mask, s, NEG_INF)
    m = jnp.max(s, axis=-1, keepdims=True)
    p = jnp.where(mask, jnp.exp(s - m), 0.0)
    return p / jnp.maximum(jnp.sum(p, axis=-1, keepdims=True), 1e-30)


def to_blocks(a, size):
    return a.reshape((a.shape[0], a.shape[1] // size, size) + a.shape[2:]).swapaxes(0, 1)


def from_blocks(a):
    a = a.swapaxes(0, 1)
    return a.reshape((a.shape[0], a.shape[1] * a.shape[2]) + a.shape[3:])


def compress_blocks(k, cmp_idx, pos_emb, w1, w2):
    B = k.shape[0]
    n_cmp = cmp_idx.shape[0]
    blocks = k[:, cmp_idx] + pos_emb[None, None, :, None, :]
    blocks = blocks.transpose(0, 1, 3, 2, 4).reshape(B, n_cmp, N_KV_NSA, CMP_BLOCK * HEAD_DIM)
    return jax.nn.gelu(blocks @ w1) @ w2


def nsa_attention(q, k_c, v_c, k_s, v_s, k_w, v_w, gates, k_norm, cmp_pos_emb, cmp_w1, cmp_w2):
    B, T = q.shape[:2]
    scale = HEAD_DIM ** -0.5
    pos = jnp.arange(T)
    cos, sin = rope_tables(pos)

    n_cmp = (T - CMP_BLOCK) // CMP_STRIDE + 1
    cmp_start = np.arange(n_cmp) * CMP_STRIDE
    cmp_idx = cmp_start[:, None] + np.arange(CMP_BLOCK)[None, :]
    cmp_end = cmp_start + CMP_BLOCK - 1
    kc = compress_blocks(k_c, cmp_idx, cmp_pos_emb[0], cmp_w1[0], cmp_w2[0])
    vc = compress_blocks(v_c, cmp_idx, cmp_pos_emb[1], cmp_w1[1], cmp_w2[1])
    cos_c, sin_c = rope_tables(jnp.asarray(cmp_end))
    kc = partial_rope(rms_norm(kc, k_norm[0]), cos_c, sin_c)
    s_c = jnp.einsum('btkgd,bckd->bkgtc', q, kc, preferred_element_type=jnp.float32) * scale
    p_c = masked_probs(s_c, jnp.asarray(cmp_end)[None, :] <= pos[:, None])
    o_cmp = jnp.einsum('bkgtc,bckd->btkgd', p_c.astype(vc.dtype), vc)

    n_slc = T // SLC_BLOCK
    top_n = min(SLC_TOPK, n_slc)
    slc_start = np.arange(n_slc) * SLC_BLOCK
    overlap = ((cmp_start[:, None] < slc_start[None, :] + SLC_BLOCK)
               & (cmp_start[:, None] + CMP_BLOCK > slc_start[None, :])).astype(np.float32)
    imp = jnp.einsum('bkgtc,cj->btkj', p_c, jnp.asarray(overlap))
    t_blk = pos // SLC_BLOCK
    j = jnp.arange(n_slc)
    causal_blk = j[None, :] <= t_blk[:, None]
    forced = (j[None, :] == 0) | (causal_blk & (j[None, :] > t_blk[:, None] - N_LOCAL_SLC))
    score = jnp.where(forced[None, :, None, :], FORCED_SCORE,
                      jnp.where(causal_blk[None, :, None, :], imp, NEG_INF))
    _, sel = lax.top_k(score, top_n)

    s_norm = rms_norm(k_s, k_norm[1])
    k_s = partial_rope(s_norm, cos, sin)
    kb = k_s.reshape(B, n_slc, SLC_BLOCK, N_KV_NSA, HEAD_DIM).transpose(0, 3, 1, 2, 4)
    vb = v_s.reshape(B, n_slc, SLC_BLOCK, N_KV_NSA, HEAD_DIM).transpose(0, 3, 1, 2, 4)
    b_idx = jnp.arange(B)[:, None, None, None]
    h_idx = jnp.arange(N_KV_NSA)[None, None, :, None]
    n_keys = top_n * SLC_BLOCK

    def slc_chunk(args):
        q_c, sel_c, t_c = args
        kg = kb[b_idx, h_idx, sel_c].reshape(B, SLC_Q_CHUNK, N_KV_NSA, n_keys, HEAD_DIM)
        vg = vb[b_idx, h_idx, sel_c].reshape(B, SLC_Q_CHUNK, N_KV_NSA, n_keys, HEAD_DIM)
        kpos = (sel_c[..., None] * SLC_BLOCK + jnp.arange(SLC_BLOCK)).reshape(B, SLC_Q_CHUNK, N_KV_NSA, n_keys)
        mask = (kpos <= t_c[None, :, None, None])[:, :, :, None, :]
        s = jnp.einsum('bckgd,bckjd->bckgj', q_c, kg, preferred_element_type=jnp.float32) * scale
        p = masked_probs(s, mask)
        return jnp.einsum('bckgj,bckjd->bckgd', p.astype(vg.dtype), vg)

    o_slc = from_blocks(lax.map(slc_chunk, (to_blocks(q, SLC_Q_CHUNK), to_blocks(sel, SLC_Q_CHUNK),
                                            pos.reshape(-1, SLC_Q_CHUNK))))

    k_w = partial_rope(rms_norm(k_w, k_norm[2]), cos, sin)
    kp = jnp.pad(k_w, ((0, 0), (WINDOW, 0), (0, 0), (0, 0)))
    vp = jnp.pad(v_w, ((0, 0), (WINDOW, 0), (0, 0), (0, 0)))
    band = WINDOW + Q_BLOCK

    def win_block(args):
        q_b, qb = args
        start = qb * Q_BLOCK
        k_b = lax.dynamic_slice_in_dim(kp, start, band, axis=1)
        v_b = lax.dynamic_slice_in_dim(vp, start, band, axis=1)
        kpos = start - WINDOW + jnp.arange(band)
        tq = start + jnp.arange(Q_BLOCK)
        mask = ((kpos[None, :] <= tq[:, None]) & (kpos[None, :] > tq[:, None] - WINDOW)
                & (kpos[None, :] >= 0))
        s = jnp.einsum('bqkgd,bskd->bkgqs', q_b, k_b, preferred_element_type=jnp.float32) * scale
        p = masked_probs(s, mask)
        return jnp.einsum('bkgqs,bskd->bqkgd', p.astype(v_b.dtype), v_b)

    o_win = from_blocks(lax.map(win_block, (to_blocks(q, Q_BLOCK), jnp.arange(T // Q_BLOCK))))

    return gates[..., 0:1] * o_cmp + gates[..., 1:2] * o_slc + gates[..., 2:3] * o_win


def forgetting_attention(q, k, v, log_f):
    T = q.shape[1]
    scale = HEAD_DIM ** -0.5
    c = jnp.cumsum(log_f, axis=1)
    c_keys = c.transpose(0, 2, 1)
    pos = jnp.arange(T)

    def block(args):
        q_b, c_b, t_b = args
        s = jnp.einsum('bqhd,bshd->bhqs', q_b, k, preferred_element_type=jnp.float32) * scale
        s = s + c_b.transpose(0, 2, 1)[..., None] - c_keys[:, :, None, :]
        p = masked_probs(s, pos[None, :] <= t_b[:, None])
        return jnp.einsum('bhqs,bshd->bqhd', p.astype(v.dtype), v)

    return from_blocks(lax.map(block, (to_blocks(q, Q_BLOCK), to_blocks(c, Q_BLOCK),
                                       pos.reshape(-1, Q_BLOCK))))


def hybrid_mixer(h, w_in, nsa_q_norm, nsa_k_norm, cmp_pos_emb, cmp_w1, cmp_w2, nsa_out_norm,
                 fox_q_norm, fox_k_norm, fox_forget_bias, fox_out_norm, w_out):
    B, T, _ = h.shape
    proj = h @ w_in
    q_n, kv_n, gate_n, fox_qkv, fox_f = jnp.split(proj, SPLITS, axis=-1)

    cos, sin = rope_tables(jnp.arange(T))
    q = q_n.reshape(B, T, N_KV_NSA, GQA_GROUP, HEAD_DIM)
    q = partial_rope(rms_norm(q, nsa_q_norm), cos, sin)
    kv = kv_n.reshape(B, T, N_NSA_BRANCHES, 2, N_KV_NSA, HEAD_DIM)
    gates = jax.nn.sigmoid(gate_n.astype(jnp.float32)).astype(h.dtype)
    gates = gates.reshape(B, T, N_KV_NSA, GQA_GROUP, N_NSA_BRANCHES)
    o_nsa = nsa_attention(q, kv[:, :, 0, 0], kv[:, :, 0, 1], kv[:, :, 1, 0], kv[:, :, 1, 1],
                          kv[:, :, 2, 0], kv[:, :, 2, 1], gates, nsa_k_norm,
                          cmp_pos_emb, cmp_w1, cmp_w2)
    o_nsa = rms_norm(o_nsa.reshape(B, T, NSA_WIDTH), nsa_out_norm)

    fqkv = fox_qkv.reshape(B, T, 3, N_HEADS_FOX, HEAD_DIM)
    fq = rms_norm(fqkv[:, :, 0], fox_q_norm)
    fk = rms_norm(fqkv[:, :, 1], fox_k_norm)
    log_f = jax.nn.log_sigmoid(fox_f.astype(jnp.float32) + fox_forget_bias.astype(jnp.float32))
    o_fox = forgetting_attention(fq, fk, fqkv[:, :, 2], log_f)
    o_fox = rms_norm(o_fox.reshape(B, T, FOX_WIDTH), fox_out_norm)

    return jnp.concatenate([o_nsa, o_fox], axis=-1) @ w_out


def setup_inputs(seed: int = 0) -> dict:
    key = jax.random.key(seed)
    ks = iter(jax.random.split(key, 32))

    def nrm(shape, scale):
        return scale * jax.random.normal(next(ks), shape, jnp.float32)

    def gain(shape):
        return 1.0 + 0.02 * jax.random.normal(next(ks), shape, jnp.float32)

    L = DEPTH
    return {
        "x": nrm((BATCH, SEQ, D_MODEL), 1.0),
        "ffn1_norm": gain((L, D_MODEL)),
        "ffn1_w_gate": nrm((L, D_MODEL, D_FF), D_MODEL ** -0.5),
        "ffn1_w_up": nrm((L, D_MODEL, D_FF), D_MODEL ** -0.5),
        "ffn1_w_down": nrm((L, D_FF, D_MODEL), D_FF ** -0.5),
        "mix_norm": gain((L, D_MODEL)),
        "w_in": nrm((L, D_MODEL, PROJ_COLS), D_MODEL ** -0.5),
        "nsa_q_norm": gain((L, HEAD_DIM)),
        "nsa_k_norm": gain((L, N_NSA_BRANCHES, HEAD_DIM)),
        "cmp_pos_emb": nrm((L, 2, CMP_BLOCK, HEAD_DIM), 0.1),
        "cmp_w1": nrm((L, 2, CMP_BLOCK * HEAD_DIM, CMP_HIDDEN), (CMP_BLOCK * HEAD_DIM) ** -0.5),
        "cmp_w2": nrm((L, 2, CMP_HIDDEN, HEAD_DIM), CMP_HIDDEN ** -0.5),
        "nsa_out_norm": gain((L, NSA_WIDTH)),
        "fox_q_norm": gain((L, HEAD_DIM)),
        "fox_k_norm": gain((L, HEAD_DIM)),
        "fox_forget_bias": jnp.linspace(1.0, 5.0, N_HEADS_FOX, dtype=jnp.float32)[None, :]
                           + nrm((L, N_HEADS_FOX), 0.1),
        "fox_out_norm": gain((L, FOX_WIDTH)),
        "w_out": nrm((L, MIX_WIDTH, D_MODEL), MIX_WIDTH ** -0.5),
        "ffn2_norm": gain((L, D_MODEL)),
        "ffn2_w_gate": nrm((L, D_MODEL, D_FF), D_MODEL ** -0.5),
        "ffn2_w_up": nrm((L, D_MODEL, D_FF), D_MODEL ** -0.5),
        "ffn2_w_down": nrm((L, D_FF, D_MODEL), D_FF ** -0.5),
    }


def reference(x, ffn1_norm, ffn1_w_gate, ffn1_w_up, ffn1_w_down, mix_norm, w_in, nsa_q_norm,
              nsa_k_norm, cmp_pos_emb, cmp_w1, cmp_w2, nsa_out_norm, fox_q_norm, fox_k_norm,
              fox_forget_bias, fox_out_norm, w_out, ffn2_norm, ffn2_w_gate, ffn2_w_up, ffn2_w_down):
    for i in range(DEPTH):
        x = x + 0.5 * swiglu(rms_norm(x, ffn1_norm[i]), ffn1_w_gate[i], ffn1_w_up[i], ffn1_w_down[i])
        x = x + hybrid_mixer(rms_norm(x, mix_norm[i]), w_in[i], nsa_q_norm[i], nsa_k_norm[i],
                             cmp_pos_emb[i], cmp_w1[i], cmp_w2[i], nsa_out_norm[i],
                             fox_q_norm[i], fox_k_norm[i], fox_forget_bias[i], fox_out_norm[i], w_out[i])
        x = x + 0.5 * swiglu(rms_norm(x, ffn2_norm[i]), ffn2_w_gate[i], ffn2_w_up[i], ffn2_w_down[i])
    return x
```

```python
import numpy as np
import concourse.bass as bass
import concourse.mybir as mybir
from concourse.bass_utils import run_bass_kernel_spmd

F32 = mybir.dt.float32
BF16 = mybir.dt.bfloat16
U8 = mybir.dt.uint8
AF = mybir.ActivationFunctionType
ALU = mybir.AluOpType

D = 2048
T = 2048
DEPTH = 4
HD = 128
DFF = 5632
DC = D // 128
FC = DFF // 128
TT = 512
NTT = T // TT
EPS = 1e-6
SCALE = HD ** -0.5
NEG = -32768.0
NCMP = 127
VW = 72

PK = {}
_off = 0
def _add(name, n):
    global _off
    PK[name] = (_off, n)
    _off += n
for _f in (1, 2):
    _add(f"wg{_f}", D * DFF)
    _add(f"wu{_f}", D * DFF)
    _add(f"wd{_f}", D * DFF)
_add("wfm1", 16 * 128 * 16 * 128)
_add("wtm", 3 * 128 * 16 * 512)
_add("wfm2", 16 * 128 * 16 * 128)
_add("wout", 16 * 128 * 16 * 128)
_add("cw1", 2 * 128 * 32 * 256)
_add("cw2", 2 * 128 * 2 * 128)
_add("wf", 128 * 16 * 8)
_add("wgt", 128 * 16 * 24)
_add("pos", 128 * 2 * 32)
NPK = ((_off + 2047) // 2048) * 2048
NPKR = NPK // 2048

CB = {}
_c = 0
def _addc(name, n):
    global _c
    CB[name] = (_c, n)
    _c += n
_addc("ident", 128)
_addc("ones", 128)
_addc("rt", 32)
_addc("ovl", 32)
_addc("cb", 4 * 512)
_addc("wb", 4 * 512)
_addc("cmask", 2048)
_addc("ex", 16 * 128)
_addc("esel", 24 * 128)
NCB = _c


class Prog:
    NS = 8

    def __init__(self, nc):
        self.nc = nc
        self.ops = []
        self.deps = []
        self.last_w = {}
        self.readers = {}
        self.streams = {k: [] for k in ("pe", "act", "dve", "pool", "sp")}
        self.groups = {}
        self.last_op = {k: None for k in self.streams}
        self.recent_dma = {"sp": [], "pool": []}
        self.cur_barrier = None

    def _record(self, stream, fn, kind, reads, writes, group=None, is_barrier=False):
        i = len(self.ops)
        d = set()
        lw = self.last_w
        rd = self.readers
        for k in reads:
            w = lw.get(k)
            if w is not None:
                d.add(w)
            r = rd.get(k)
            if r is None:
                r = rd[k] = [{}, []]
            if kind == "c":
                r[0][stream] = i
            else:
                r[1].append(i)
        for k in writes:
            w = lw.get(k)
            if w is not None:
                d.add(w)
            r = rd.get(k)
            if r is not None:
                d.update(r[0].values())
                d.update(r[1])
                del rd[k]
            lw[k] = i
        if is_barrier:
            for st, li in self.last_op.items():
                if li is not None:
                    d.add(li)
            for q, lst in self.recent_dma.items():
                d.update(lst)
        elif self.cur_barrier is not None:
            d.add(self.cur_barrier)
        d.discard(i)
        self.ops.append((stream, fn, kind, group))
        self.deps.append(d)
        self.streams[stream].append(i)
        if kind == "c":
            self.last_op[stream] = i
        elif group is None:
            lst = self.recent_dma[stream]
            lst.append(i)
            if len(lst) > self.NS:
                lst.pop(0)
        if group is not None:
            self.groups[group] = self.groups.get(group, 0) + 1
        if is_barrier:
            self.cur_barrier = i
        return i

    def op(self, stream, fn, reads=(), writes=()):
        return self._record(stream, fn, "c", reads, writes)

    def dma(self, q, out, in_, reads=(), writes=(), group=None):
        return self._record(q, lambda e, o=out, i=in_: e.dma_start(out=o, in_=i), "d",
                            reads, writes, group)

    def barrier(self):
        self._record("sp", lambda e: e.nop(), "c", [], [], is_barrier=True)

    def emit(self, block, sems_ctx):
        nc = self.nc
        ops = self.ops
        n = len(ops)
        dsem = {}
        qcount = {"sp": 0, "pool": 0}
        qhist = {"sp": [], "pool": []}
        extra = {}
        for i in range(n):
            st, fn, kind, group = ops[i]
            if kind != "d":
                continue
            if group is not None:
                dsem[i] = (("g", group), 16 * self.groups[group])
            else:
                c = qcount[st]
                dsem[i] = (("s", st, c % self.NS), 16 * (c // self.NS + 1))
                if c >= self.NS:
                    extra[i] = qhist[st][c - self.NS]
                qhist[st].append(i)
                qcount[st] = c + 1
        sig = [False] * n
        for i in range(n):
            st = ops[i][0]
            for d in self.deps[i]:
                sd, _, kd, _ = ops[d]
                if kd == "c" and not (sd == "pe" and st == "pe"):
                    sig[d] = True
        cnt = {k: 0 for k in self.streams}
        sval = [0] * n
        for i in range(n):
            st, fn, kind, group = ops[i]
            if kind == "c" and sig[i]:
                cnt[st] += 1
                sval[i] = cnt[st]
        semkeys = set(("e", k) for k in self.streams)
        for i in dsem:
            semkeys.add(dsem[i][0])
        sem = {}
        for k in sorted(semkeys, key=str):
            sem[k] = sems_ctx.enter_context(nc.semaphore("s_" + "_".join(str(x) for x in k)))
        waits = [None] * n
        waited = {k: {} for k in self.streams}
        for st in self.streams:
            wd = waited[st]
            for i in self.streams[st]:
                need = {}
                dl = self.deps[i]
                if i in extra:
                    dl = set(dl)
                    dl.add(extra[i])
                for d in dl:
                    sd, _, kd, _ = ops[d]
                    if kd == "d":
                        k, v = dsem[d]
                    else:
                        if sd == "pe" and st == "pe":
                            continue
                        k, v = ("e", sd), sval[d]
                    if need.get(k, 0) < v:
                        need[k] = v
                w = []
                for k, v in need.items():
                    if wd.get(k, 0) < v:
                        wd[k] = v
                        w.append((sem[k], v))
                waits[i] = w
        self.n_waits = sum(len(w) for w in waits)
        self.n_sig = sum(sig)

        def run_stream(st, eng):
            for i in self.streams[st]:
                _, fn, kind, group = ops[i]
                for s, v in waits[i]:
                    eng.wait_ge(s, v)
                ins = fn(eng)
                if kind == "d":
                    ins.then_inc(sem[dsem[i][0]], 16)
                elif sig[i]:
                    ins.then_inc(sem[("e", st)], 1)

        @block.tensor
        def _(e):
            run_stream("pe", e)

        @block.scalar
        def _(e):
            run_stream("act", e)

        @block.vector
        def _(e):
            run_stream("dve", e)

        @block.gpsimd
        def _(e):
            run_stream("pool", e)

        @block.sync
        def _(e):
            run_stream("sp", e)


class Arena:
    def __init__(self, ap, size):
        self.ap = ap
        self.size = size
        self.off = 0

    def take(self, nbytes, dtype, pattern=None, **kw):
        rb = (nbytes + 63) // 64 * 64
        assert self.off + rb <= self.size, ("arena overflow", self.off, rb, self.size)
        v = self.ap[:, self.off:self.off + nbytes].bitcast(dtype)
        self.off += rb
        if pattern:
            v = v.rearrange(pattern, **kw)
        return v

    def mark(self):
        return self.off

    def reset(self, m):
        self.off = m


class Builder:
    def __init__(self, nseq, layers, dbg=None):
        self.nseq = nseq
        self.layers = layers
        self.dbg = dbg or {}
        nc = bass.Bass("TRN2", target_bir_lowering=False)
        self.nc = nc
        self.P = Prog(nc)
        L = len(layers)
        self.L = L
        self.xin = nc.dram_tensor("xin", [nseq, DC, 128, T], F32, kind="ExternalInput")
        self.xs = nc.dram_tensor("xout", [nseq, DC, 128, T], F32, kind="ExternalOutput")
        self.wsrc = nc.dram_tensor("wsrc", [L * NPKR, 2048], F32, kind="ExternalInput")
        self.wpk = [nc.dram_tensor(f"wpk{i}", [NPKR, 2048], BF16, kind="Internal") for i in range(L)]
        self.vec = nc.dram_tensor("vec", [128, L * VW], F32, kind="ExternalInput")
        self.cbf = nc.dram_tensor("cbf", [128, NCB], F32, kind="ExternalInput")
        self.ropec = nc.dram_tensor("ropec", [32, T], F32, kind="ExternalInput")
        self.ropes = nc.dram_tensor("ropes", [32, T], F32, kind="ExternalInput")
        self.ropekc = nc.dram_tensor("ropekc", [32, 2, 128], F32, kind="ExternalInput")
        self.selc = nc.dram_tensor("selc", [128, 2, 16, 32], F32, kind="ExternalInput")
        sk = "ExternalOutput" if self.dbg.get("dump") else "Internal"
        self.s_fm = nc.dram_tensor("s_fm", [16, 128, T], BF16, kind=sk)
        self.s_tm = nc.dram_tensor("s_tm", [12, 128, 16, 128], BF16, kind=sk)
        self.s_kc = nc.dram_tensor("s_kc", [2, 128, 128], BF16, kind=sk)
        self.s_vc = nc.dram_tensor("s_vc", [2, 128, 128], BF16, kind=sk)
        self.s_ab = nc.dram_tensor("s_ab", [8, 2, 6, T], BF16, kind=sk)
        ASZ = 206 * 1024
        self.arena_t = nc.alloc_sbuf_tensor("arena", [128, ASZ], U8)
        self.A = Arena(self.arena_t.ap(), ASZ)
        self.ps = nc.alloc_psum_tensor("ps", [128, 8, 512], F32).ap()
        A = self.A
        self.cb_sb = A.take(NCB * 2, BF16)
        self.vec_sb = A.take(L * VW * 4, F32)
        self.base_mark = A.mark()
        self.wslot_ctr = 0

    def cview(self, name, rows=128):
        o, n = CB[name]
        return self.cb_sb[0:rows, o:o + n]

    def vcol(self, li, c, rows=128, n=1):
        return self.vec_sb[0:rows, li * VW + c: li * VW + c + n]

    def wview(self, li, name):
        o, n = PK[name]
        flat = self.wpk[li].ap().rearrange("r c -> (r c)")
        return flat[o:o + n]

    def prologue(self):
        P = self.P
        P.dma("pool", self.cb_sb, self.cbf.ap(), writes=["cb"])
        P.dma("sp", self.vec_sb, self.vec.ap(), writes=["vec"])
        self.convert(0)

    def convert(self, li):
        P = self.P
        CH = 4096
        r0 = li * NPKR
        r = 0
        last = None
        while r < NPKR:
            n = min(CH, NPKR - r)
            last = P.dma("pool", self.wpk[li].ap()[r:r + n, :], self.wsrc.ap()[r0 + r:r0 + r + n, :],
                         writes=[], group=f"conv{li}")
            r += n
        P.last_w[("w", li)] = last

    def norm_tile(self, xt, h, sq, tmp, rs, li, gcol, xkey, hkey, psb):
        P = self.P
        ones = self.cview("ones")
        psn = self.ps[:, psb, :]
        pk = ("ps", psb)
        for dc in range(DC):
            sl = dc % 2
            P.op("act", lambda e, o=sq[:, sl, :], i=xt[:, dc, :]: e.activation(out=o, in_=i, func=AF.Square),
                 reads=[(xkey, dc)], writes=[("sq", sl)])
            P.op("pe", lambda e, r=sq[:, sl, :], s=(dc == 0), t=(dc == DC - 1): e.matmul(psn, lhsT=ones, rhs=r, start=s, stop=t),
                 reads=[("sq", sl), "cb"], writes=[pk])
        P.op("act", lambda e: e.activation(out=tmp, in_=psn, func=AF.Sqrt, bias=EPS, scale=1.0 / D),
             reads=[pk], writes=["ntmp"])
        P.op("dve", lambda e: e.reciprocal(out=rs, in_=tmp), reads=["ntmp"], writes=["nrs"])
        for dc in range(DC):
            P.op("dve", lambda e, o=h[:, dc, :], i=xt[:, dc, :], g=self.vcol(li, gcol + dc):
                 e.scalar_tensor_tensor(out=o, in0=i, scalar=g, in1=rs, op0=ALU.mult, op1=ALU.mult),
                 reads=[(xkey, dc), "nrs", "vec"], writes=[(hkey, dc)])

    def ffn_tile(self, li, f, xt, h, sq, tmp, rs, aT, sg, wring, xkey):
        P = self.P
        NSLOT = wring.shape[1]
        self.norm_tile(xt, h, sq, tmp, rs, li, {1: 0, 2: 32}[f], xkey, "h", 0)
        wg = self.wview(li, f"wg{f}").rearrange("(g p x) -> g p x", g=11, p=128)
        wu = self.wview(li, f"wu{f}").rearrange("(g p x) -> g p x", g=11, p=128)
        wd = self.wview(li, f"wd{f}").rearrange("(g p x) -> g p x", g=16, p=128)
        wkey = ("w", li)
        it = 0
        for fg in range(11):
            sa = self.wslot_ctr % NSLOT; self.wslot_ctr += 1
            sb_ = self.wslot_ctr % NSLOT; self.wslot_ctr += 1
            P.dma("sp", wring[:, sa, :], wg[fg], reads=[wkey], writes=[("ws", sa)])
            P.dma("sp", wring[:, sb_, :], wu[fg], reads=[wkey], writes=[("ws", sb_)])
            wa = wring[:, sa, :].rearrange("p (c x) -> p c x", c=DC)
            wb = wring[:, sb_, :].rearrange("p (c x) -> p c x", c=DC)
            for j in range(4):
                fc = fg * 4 + j
                bg = 1 + (it % 2)
                bu = 3 + (it % 2)
                sgs = it % 2
                it += 1
                psg = self.ps[:, bg, :]
                psu = self.ps[:, bu, :]
                for dc in range(DC):
                    P.op("pe", lambda e, o=psg, w=wa[:, dc, j * 128:(j + 1) * 128], r=h[:, dc, :], s=(dc == 0), t=(dc == DC - 1):
                         e.matmul(o, lhsT=w, rhs=r, start=s, stop=t),
                         reads=[("ws", sa), ("h", dc)], writes=[("ps", bg)])
                for dc in range(DC):
                    P.op("pe", lambda e, o=psu, w=wb[:, dc, j * 128:(j + 1) * 128], r=h[:, dc, :], s=(dc == 0), t=(dc == DC - 1):
                         e.matmul(o, lhsT=w, rhs=r, start=s, stop=t),
                         reads=[("ws", sb_), ("h", dc)], writes=[("ps", bu)])
                P.op("act", lambda e, o=sg[:, sgs, :], i=psg: e.activation(out=o, in_=i, func=AF.Silu),
                     reads=[("ps", bg)], writes=[("sg", sgs)])
                P.op("dve", lambda e, o=aT[:, fc, :], a=psu, b=sg[:, sgs, :]: e.tensor_tensor(out=o, in0=a, in1=b, op=ALU.mult),
                     reads=[("ps", bu), ("sg", sgs)], writes=[("aT", fc)])
        for dco in range(DC):
            s = self.wslot_ctr % NSLOT; self.wslot_ctr += 1
            P.dma("sp", wring[:, s, 0:FC * 128], wd[dco], reads=[wkey], writes=[("ws", s)])
            w = wring[:, s, 0:FC * 128].rearrange("p (c x) -> p c x", c=FC)
            bd = 5 + (dco % 2)
            psd = self.ps[:, bd, :]
            for fc in range(FC):
                P.op("pe", lambda e, o=psd, ww=w[:, fc, :], r=aT[:, fc, :], s_=(fc == 0), t_=(fc == FC - 1):
                     e.matmul(o, lhsT=ww, rhs=r, start=s_, stop=t_),
                     reads=[("ws", s), ("aT", fc)], writes=[("ps", bd)])
            P.op("dve", lambda e, o=xt[:, dco, :], a=psd: e.scalar_tensor_tensor(out=o, in0=a, scalar=0.5, in1=o, op0=ALU.mult, op1=ALU.add),
                 reads=[("ps", bd)], writes=[(xkey, dco)])

    def ffn_phase(self, s, jobs, src_is_input):
        P = self.P
        A = self.A
        P.barrier()
        A.reset(self.base_mark)
        xt = A.take(DC * TT * 4, F32, "p (c t) -> p c t", c=DC)
        h = A.take(DC * TT * 2, BF16, "p (c t) -> p c t", c=DC)
        sq = A.take(2 * TT * 2, BF16, "p (c t) -> p c t", c=2)
        tmp = A.take(TT * 4, F32)
        rs = A.take(TT * 4, F32)
        aT = A.take(FC * TT * 2, BF16, "p (c t) -> p c t", c=FC)
        sg = A.take(2 * TT * 2, BF16, "p (c t) -> p c t", c=2)
        NSLOT = 4
        wring = A.take(NSLOT * 16384, BF16, "p (s x) -> p s x", s=NSLOT)
        for tt in range(NTT):
            src = self.xin if src_is_input else self.xs
            P.dma("sp", xt, src.ap()[s, :, :, tt * TT:(tt + 1) * TT].rearrange("c p t -> p c t"),
                  reads=[("X", s, tt)], writes=[("xt", dc) for dc in range(DC)])
            for (li, f) in jobs:
                self.ffn_tile(li, f, xt, h, sq, tmp, rs, aT, sg, wring, "xt")
            P.dma("sp", self.xs.ap()[s, :, :, tt * TT:(tt + 1) * TT].rearrange("c p t -> p c t"), xt,
                  reads=[("xt", dc) for dc in range(DC)], writes=[("X", s, tt)])

    def build(self):
        P = self.P
        self.prologue()
        L = self.L
        mode = self.dbg.get("mode", "full")
        for s in range(self.nseq):
            first = True
            for li in range(L):
                jobs = []
                if li > 0:
                    jobs.append((li - 1, 2))
                jobs.append((li, 1))
                if mode in ("full", "ffn"):
                    self.ffn_phase(s, jobs, src_is_input=first)
                    first = False
                if s == 0 and li + 1 < L:
                    self.convert(li + 1)
                if mode in ("full", "mixer"):
                    self.mixer_phase(s, li, src_is_input=first)
                    first = False
            if mode in ("full", "ffn"):
                self.ffn_phase(s, [(L - 1, 2)], src_is_input=False)
        keys = [("X", s, tt) for s in range(self.nseq) for tt in range(NTT)]
        P.op("sp", lambda e: e.nop(), reads=keys)
        from contextlib import ExitStack
        with ExitStack() as es:
            es.enter_context(self.nc.allow_low_precision("bf16 matmul operands by design; accumulation is fp32"))
            block = es.enter_context(self.nc.Block())
            P.emit(block, es)
        return self.nc

    def mixer_phase(self, s, li, src_is_input):
        P, A = self.P, self.A
        P.barrier()
        A.reset(self.base_mark)
        M = {}
        M["xt"] = A.take(DC * TT * 4, F32, "p (c t) -> p c t", c=DC)
        M["h"] = A.take(DC * TT * 2, BF16, "p (c t) -> p c t", c=DC)
        M["sq"] = A.take(2 * TT * 2, BF16, "p (c t) -> p c t", c=2)
        M["tmp"] = A.take(TT * 4, F32)
        M["rs"] = A.take(TT * 4, F32)
        cm0 = A.mark()
        M["cfull"] = A.take(T * 4, F32)
        M["src"] = self.xin if src_is_input else self.xs
        cm = A.mark()
        if self.dbg.get("skip1") is None:
            self.pass1a(s, li, M)
            P.barrier()
            A.reset(cm)
            self.pass1b(s, li, M)
        if self.dbg.get("only1"):
            return
        P.barrier()
        A.reset(cm0)
        self.pass2(s, li, M)

    def load_x(self, M, s, tt):
        self.P.dma("sp", M["xt"], M["src"].ap()[s, :, :, tt * TT:(tt + 1) * TT].rearrange("c p t -> p c t"),
                   reads=[("X", s, tt)], writes=[("xt", dc) for dc in range(DC)])

    def head_norm(self, ps_in, pskey, n, gain, W, out_bf, outkey, rope=None, sumbank=3, rotbank=4):
        P = self.P
        ones = self.cview("ones")
        a = self.hn_ctr % 2
        self.hn_ctr += 1
        hsq = W["hsq"][:, a, 0:n]
        pss = self.ps[:, sumbank, 0:n]
        P.op("act", lambda e: e.activation(out=hsq, in_=ps_in, func=AF.Square), reads=[pskey], writes=[("hsq", a)])
        P.op("pe", lambda e: e.matmul(pss, lhsT=ones, rhs=hsq, start=True, stop=True),
             reads=[("hsq", a), "cb"], writes=[("ps", sumbank)])
        hl = W["hl"][:, 0:n]
        hr = W["hr"][:, 0:n]
        P.op("act", lambda e: e.activation(out=hl, in_=pss, func=AF.Ln, bias=EPS, scale=1.0 / HD),
             reads=[("ps", sumbank)], writes=["hl"])
        P.op("act", lambda e: e.activation(out=hr, in_=hl, func=AF.Exp, scale=-0.5), reads=["hl"], writes=["hr"])
        if rope is None:
            P.op("dve", lambda e: e.scalar_tensor_tensor(out=out_bf, in0=ps_in, scalar=gain, in1=hr, op0=ALU.mult, op1=ALU.mult),
                 reads=[pskey, "hr", "vec"], writes=[outkey])
            return
        C, S, rkey = rope
        kn = W["kn"][:, a, 0:n]
        P.op("dve", lambda e: e.scalar_tensor_tensor(out=kn, in0=ps_in, scalar=gain, in1=hr, op0=ALU.mult, op1=ALU.mult),
             reads=[pskey, "hr", "vec"], writes=[("kn", a)])
        knb = W["knb"][0:32, 0:n]
        P.op("pool", lambda e: e.tensor_copy(out=knb, in_=kn[0:32, :]), reads=[("kn", a)], writes=["knb"])
        psr = self.ps[0:32, rotbank, 0:n]
        rt = self.cview("rt", 32)
        P.op("pe", lambda e: e.matmul(psr, lhsT=rt, rhs=knb, start=True, stop=True), reads=["knb", "cb"], writes=[("ps", rotbank)])
        t1 = W["t1"][0:32, 0:n]
        t2 = W["t2"][0:32, 0:n]
        P.op("dve", lambda e: e.tensor_tensor(out=t1, in0=psr, in1=S, op=ALU.mult), reads=[("ps", rotbank), rkey], writes=["t1"])
        P.op("pool", lambda e: e.tensor_tensor(out=t2, in0=kn[0:32, :], in1=C, op=ALU.mult), reads=[("kn", a), rkey], writes=["t2"])
        P.op("pool", lambda e: e.tensor_tensor(out=kn[0:32, :], in0=t1, in1=t2, op=ALU.add), reads=["t1", "t2"], writes=[("kn", a)])
        P.op("act", lambda e: e.activation(out=out_bf, in_=kn, func=AF.Copy), reads=[("kn", a)], writes=[outkey])

    def head_temps(self, A):
        W = {}
        W["hsq"] = A.take(2 * TT * 2, BF16, "p (c t) -> p c t", c=2)
        W["hl"] = A.take(TT * 4, F32)
        W["hr"] = A.take(TT * 4, F32)
        W["kn"] = A.take(2 * TT * 4, F32, "p (c t) -> p c t", c=2)
        W["t1"] = A.take(TT * 4, F32)
        W["t2"] = A.take(TT * 4, F32)
        W["knb"] = A.take(TT * 2, BF16)
        W["rc"] = A.take(TT * 4, F32)
        W["rsn"] = A.take(TT * 4, F32)
        return W

    def pass1a(self, s, li, M):
        P, A = self.P, self.A
        self.hn_ctr = 0
        xt, h = M["xt"], M["h"]
        NS1 = 3
        wring = A.take(NS1 * 16384, BF16, "p (s x) -> p s x", s=NS1)
        W = self.head_temps(A)
        stage = A.take(4 * TT * 2, BF16, "p (c t) -> p c t", c=4)
        lf1 = A.take(TT * 4, F32)
        lf2 = A.take(TT * 4, F32)
        ones8 = A.take(TT * 4, F32)
        negb = A.take(64, F32)
        wfs = A.take(DC * 8 * 2, BF16, "p (c x) -> p c x", c=DC)
        wkey = ("w", li)
        cfull = M["cfull"]
        wfm1 = self.wview(li, "wfm1").rearrange("(g p x) -> g p x", g=16, p=128)
        wtm = self.wview(li, "wtm").rearrange("(g p x) -> g p x", g=3, p=128)
        P.dma("sp", wfs, self.wview(li, "wf").rearrange("(p c x) -> p c x", p=128, c=DC), reads=[wkey], writes=["wfs"])
        P.op("dve", lambda e: e.memset(ones8, 1.0), writes=["ones8"])
        P.op("dve", lambda e: e.tensor_scalar(out=negb[0:8, 0:1], in0=self.vcol(li, 70, rows=8), scalar1=-1.0, scalar2=None, op0=ALU.mult),
             reads=["vec"], writes=["negb"])
        sctr = 0
        stg = 0
        for tt in range(NTT):
            tsl = slice(tt * TT, (tt + 1) * TT)
            self.load_x(M, s, tt)
            self.norm_tile(xt, h, M["sq"], M["tmp"], M["rs"], li, 16, "xt", "h", 0)
            P.dma("sp", W["rc"][0:32, :], self.ropec.ap()[:, tsl], writes=["rope"])
            P.dma("sp", W["rsn"][0:32, :], self.ropes.ap()[:, tsl], writes=["rope"])
            for cc in range(16):
                sl = sctr % NS1; sctr += 1
                P.dma("sp", wring[:, sl, 0:2048], wfm1[cc], reads=[wkey], writes=[("ws", sl)])
                w = wring[:, sl, 0:2048].rearrange("p (c x) -> p c x", c=DC)
                pb = 1 + cc % 2
                psp = self.ps[:, pb, :]
                for dc in range(DC):
                    P.op("pe", lambda e, o=psp, ww=w[:, dc, :], r=h[:, dc, :], s_=(dc == 0), t_=(dc == DC - 1):
                         e.matmul(o, lhsT=ww, rhs=r, start=s_, stop=t_),
                         reads=[("ws", sl), ("h", dc)], writes=[("ps", pb)])
                st = stg % 4; stg += 1
                so = stage[:, st, :]
                if cc < 4:
                    P.op("dve", lambda e, o=so, i=psp: e.tensor_copy(out=o, in_=i), reads=[("ps", pb)], writes=[("stage", st)])
                elif cc < 8:
                    gain = self.vcol(li, 49 + (1 if cc < 6 else 2))
                    self.head_norm(psp, ("ps", pb), TT, gain, W, so, ("stage", st),
                                   rope=(W["rc"][0:32, :], W["rsn"][0:32, :], "rope"))
                else:
                    self.head_norm(psp, ("ps", pb), TT, self.vcol(li, 53), W, so, ("stage", st))
                P.dma("sp", self.s_fm.ap()[cc, :, tsl], so, reads=[("stage", st)], writes=[("s_fm", cc)])
            psf = self.ps[0:8, 5, :]
            for dc in range(DC):
                P.op("pe", lambda e, ww=wfs[:, dc, :], r=h[:, dc, :], s_=(dc == 0), t_=(dc == DC - 1):
                     e.matmul(psf, lhsT=ww, rhs=r, start=s_, stop=t_), reads=["wfs", ("h", dc)], writes=[("ps", 5)])
            P.op("act", lambda e: e.activation(out=lf1[0:8, :], in_=psf, func=AF.Exp, bias=negb[0:8, 0:1], scale=-1.0),
                 reads=[("ps", 5), "negb"], writes=["lf1"])
            P.op("act", lambda e: e.activation(out=lf2[0:8, :], in_=lf1[0:8, :], func=AF.Ln, bias=1.0, scale=1.0),
                 reads=["lf1"], writes=["lf2"])
            P.op("dve", lambda e: e.tensor_scalar(out=lf1[0:8, :], in0=lf2[0:8, :], scalar1=-1.0 / SCALE, scalar2=None, op0=ALU.mult),
                 reads=["lf2"], writes=["lf1"])
            init = 0.0 if tt == 0 else cfull[0:8, tt * TT - 1:tt * TT]
            P.op("dve", lambda e, o=cfull[0:8, tsl], i=init: e.tensor_tensor_scan(out=o, data0=ones8[0:8, :], data1=lf1[0:8, :],
                                                                                   initial=i, op0=ALU.mult, op1=ALU.add),
                 reads=["lf1", "ones8", "cfull"], writes=["cfull"])
            n2 = 0
            for cg in range(3):
                sl = sctr % NS1; sctr += 1
                P.dma("sp", wring[:, sl, :], wtm[cg], reads=[wkey], writes=[("ws", sl)])
                w = wring[:, sl, :].rearrange("p (c x) -> p c x", c=DC)
                for tk in range(4):
                    pb = 6 + n2 % 2; n2 += 1
                    psp = self.ps[:, pb, :]
                    for dc in range(DC):
                        P.op("pe", lambda e, o=psp, l_=h[:, dc, tk * 128:(tk + 1) * 128], r=w[:, dc, :], s_=(dc == 0), t_=(dc == DC - 1):
                             e.matmul(o, lhsT=l_, rhs=r, start=s_, stop=t_),
                             reads=[("ws", sl), ("h", dc)], writes=[("ps", pb)])
                    st = stg % 4; stg += 1
                    so = stage[:, st, :]
                    if n2 % 2:
                        P.op("act", lambda e, o=so, i=psp: e.activation(out=o, in_=i, func=AF.Copy), reads=[("ps", pb)], writes=[("stage", st)])
                    else:
                        P.op("dve", lambda e, o=so, i=psp: e.tensor_copy(out=o, in_=i), reads=[("ps", pb)], writes=[("stage", st)])
                    kt = tt * 4 + tk
                    P.dma("sp", self.s_tm.ap()[cg * 4:(cg + 1) * 4, :, kt, :].rearrange("g p d -> p g d"),
                          so.rearrange("p (g d) -> p g d", g=4), reads=[("stage", st)], writes=[("s_tm", cg)])

    def pass1b(self, s, li, M):
        P, A = self.P, self.A
        wkey = ("w", li)
        W = self.head_temps(A)
        kcraw = A.take(4 * T * 2, BF16, "p (c t) -> p c t", c=4)
        cw1 = A.take(2 * 32 * 256 * 2, BF16, "p (k l j) -> p k l j", k=2, l=32)
        cw2 = A.take(2 * 2 * 128 * 2, BF16, "p (k c x) -> p k c x", k=2, c=2)
        posb = A.take(64 * 2, BF16, "p (k l) -> p k l", k=2)
        hid = A.take(2 * 128 * 2, BF16, "p (c x) -> p c x", c=2)
        bias = A.take(16, F32)
        stage = A.take(2 * 128 * 2, BF16, "p (c x) -> p c x", c=2)
        rkc = A.take(2 * 128 * 4, F32, "p (c x) -> p c x", c=2)
        c3 = A.take(3 * T * 2, BF16, "p (c t) -> p c t", c=3)
        n3 = A.take(3 * T * 2, BF16, "p (c t) -> p c t", c=3)
        o3 = A.take(3 * T * 2, BF16, "p (c t) -> p c t", c=3)
        r1 = A.take(T * 4, F32)
        cfull = M["cfull"]
        P.dma("sp", kcraw, self.s_fm.ap()[0:4, :, :].rearrange("c p t -> p c t"), reads=[("s_fm", c) for c in range(4)], writes=["kcraw"])
        P.dma("sp", cw1, self.wview(li, "cw1").rearrange("(k p l j) -> p k l j", k=2, p=128, l=32), reads=[wkey], writes=["cw1"])
        P.dma("sp", cw2, self.wview(li, "cw2").rearrange("(k p c x) -> p k c x", k=2, p=128, c=2), reads=[wkey], writes=["cw2"])
        P.dma("sp", posb, self.wview(li, "pos").rearrange("(p k l) -> p k l", p=128, k=2), reads=[wkey], writes=["posb"])
        P.dma("sp", rkc[0:32, :, :], self.ropekc.ap(), writes=["rkc"])
        cf = cfull[0:8, :]
        P.op("dve", lambda e: e.tensor_copy(out=c3[0:8, 0, :], in_=cf), reads=["cfull"], writes=["c3"])
        P.op("dve", lambda e: e.tensor_tensor(out=r1[0:8, :], in0=cf, in1=c3[0:8, 0, :], op=ALU.subtract), reads=["c3", "cfull"], writes=["r1"])
        P.op("dve", lambda e: e.tensor_copy(out=c3[0:8, 1, :], in_=r1[0:8, :]), reads=["r1"], writes=["c3"])
        P.op("dve", lambda e: e.tensor_tensor(out=r1[0:8, :], in0=r1[0:8, :], in1=c3[0:8, 1, :], op=ALU.subtract), reads=["c3"], writes=["r1"])
        P.op("dve", lambda e: e.tensor_copy(out=c3[0:8, 2, :], in_=r1[0:8, :]), reads=["r1"], writes=["c3"])
        P.op("dve", lambda e: e.tensor_scalar(out=n3[0:8, :, :], in0=c3[0:8, :, :], scalar1=-1.0, scalar2=None, op0=ALU.mult), reads=["c3"], writes=["n3"])
        P.op("pool", lambda e: e.memset(o3[0:8, :, :], 1.0), writes=["o3"])
        ab = self.s_ab.ap()
        P.dma("sp", ab[:, 0, 0:3, :], o3[0:8, :, :], reads=["o3"], writes=["s_ab"])
        P.dma("sp", ab[:, 0, 3:6, :], c3[0:8, :, :], reads=["c3"], writes=["s_ab"])
        P.dma("sp", ab[:, 1, 0:3, :], n3[0:8, :, :], reads=["n3"], writes=["s_ab"])
        P.dma("sp", ab[:, 1, 3:6, :], o3[0:8, :, :], reads=["o3"], writes=["s_ab"])
        self.hn_ctr = 0
        nq = 0
        for kv in range(2):
            for jc in range(2):
                for l in range(32):
                    P.op("pe", lambda e, o=self.ps[:, 0, kv * 2 + jc:kv * 2 + jc + 1], ww=cw1[:, kv, l, jc * 128:(jc + 1) * 128], r=posb[:, kv, l:l + 1],
                         s_=(l == 0), t_=(l == 31): e.matmul(o, lhsT=ww, rhs=r, start=s_, stop=t_),
                         reads=["cw1", "posb"], writes=[("ps", 0)])
            P.op("dve", lambda e, o=bias[:, kv * 2:kv * 2 + 2], i=self.ps[:, 0, kv * 2:kv * 2 + 2]: e.tensor_copy(out=o, in_=i),
                 reads=[("ps", 0)], writes=["cbias"])
            for kvh in range(2):
                raw = kcraw[:, kv * 2 + kvh, :]
                for jc in range(2):
                    pb = 1 + jc
                    psh = self.ps[:, pb, 0:NCMP]
                    for l in range(32):
                        P.op("pe", lambda e, o=psh, ww=cw1[:, kv, l, jc * 128:(jc + 1) * 128], r=raw[:, l:l + 16 * (NCMP - 1) + 1:16],
                             s_=(l == 0), t_=(l == 31): e.matmul(o, lhsT=ww, rhs=r, start=s_, stop=t_),
                             reads=["cw1", "kcraw"], writes=[("ps", pb)])
                    P.op("act", lambda e, o=hid[:, jc, 0:NCMP], i=psh, b=bias[:, kv * 2 + jc:kv * 2 + jc + 1]:
                         e.activation(out=o, in_=i, func=AF.Gelu_apprx_tanh, bias=b),
                         reads=[("ps", pb), "cbias"], writes=[("hid", jc)])
                st = nq % 2; nq += 1
                if kv == 0:
                    psk = self.ps[:, 5, 0:NCMP]
                    for jc in range(2):
                        P.op("pe", lambda e, ww=cw2[:, 0, jc, :], r=hid[:, jc, 0:NCMP], s_=(jc == 0), t_=(jc == 1):
                             e.matmul(psk, lhsT=ww, rhs=r, start=s_, stop=t_), reads=["cw2", ("hid", jc)], writes=[("ps", 5)])
                    self.head_norm(psk, ("ps", 5), NCMP, self.vcol(li, 49), W, stage[:, st, 0:NCMP], ("stage", st),
                                   rope=(rkc[0:32, 0, 0:NCMP], rkc[0:32, 1, 0:NCMP], "rkc"))
                    P.dma("sp", self.s_kc.ap()[kvh, :, 0:NCMP], stage[:, st, 0:NCMP], reads=[("stage", st)], writes=["s_kc"])
                else:
                    psv = self.ps[0:NCMP, 6, 0:128]
                    for jc in range(2):
                        P.op("pe", lambda e, l_=hid[:, jc, 0:NCMP], r=cw2[:, 1, jc, :], s_=(jc == 0), t_=(jc == 1):
                             e.matmul(psv, lhsT=l_, rhs=r, start=s_, stop=t_), reads=["cw2", ("hid", jc)], writes=[("ps", 6)])
                    P.op("dve", lambda e, o=stage[0:NCMP, st, :]: e.tensor_copy(out=o, in_=psv), reads=[("ps", 6)], writes=[("stage", st)])
                    P.dma("sp", self.s_vc.ap()[kvh, 0:NCMP, :], stage[0:NCMP, st, :], reads=[("stage", st)], writes=["s_vc"])

    def attn(self, q_ap, qkey, tiles, lo):
        P = self.P
        ones = self.cview("ones")
        lb, ob = lo
        n = len(tiles)
        last = None
        for i, t in enumerate(tiles):
            sb = self.sctr % 2; self.sctr += 1
            nk = t["nk"]
            pss = self.ps[0:nk, sb, :]
            mm = [(t["k"], q_ap, list(t["kkeys"]) + [qkey])] + list(t["extras"])
            for m, (l_, r_, keys) in enumerate(mm):
                P.op("pe", lambda e, o=pss, l_=l_, r_=r_, s_=(m == 0), t_=(m == len(mm) - 1): e.matmul(o, lhsT=l_, rhs=r_, start=s_, stop=t_),
                     reads=list(keys) + ["cb"], writes=[("ps", sb)])
            p = self.pctr % 3; self.pctr += 1
            ptile = self.pt[0:nk, p, :]
            P.op("act", lambda e, o=ptile, i_=pss: e.activation(out=o, in_=i_, func=AF.Exp, scale=SCALE),
                 reads=[("ps", sb)], writes=[("pt", p)])
            P.op("pe", lambda e, l_=ones[0:nk, :], r_=ptile, s_=(i == 0), t_=(i == n - 1): e.matmul(self.ps[:, lb, :], lhsT=l_, rhs=r_, start=s_, stop=t_),
                 reads=[("pt", p), "cb"], writes=[("ps", lb)])
            P.op("pe", lambda e, l_=t["v"], r_=ptile, s_=(i == 0), t_=(i == n - 1): e.matmul(self.ps[:, ob, :], lhsT=l_, rhs=r_, start=s_, stop=t_),
                 reads=[("pt", p)] + list(t["vkeys"]), writes=[("ps", ob)])
            last = (ptile, ("pt", p))
        return last

    def pass2(self, s, li, M):
        P, A = self.P, self.A
        self.hn_ctr = 0
        self.sctr = 0
        self.pctr = 0
        xt, h = M["xt"], M["h"]
        o_sb = h
        wkey = ("w", li)
        W = self.head_temps(A)
        NS2 = 4
        wring = A.take(NS2 * 4096, BF16, "p (s x) -> p s x", s=NS2)
        qT = A.take(8 * TT * 2, BF16, "p (c t) -> p c t", c=8)
        fqT = A.take(8 * TT * 2, BF16, "p (c t) -> p c t", c=8)
        gsb = A.take(TT * 2, BF16)
        gtmp = A.take(TT * 4, F32)
        wgs = A.take(DC * 24 * 2, BF16, "p (c x) -> p c x", c=DC)
        NKV = 6
        kvr = A.take(NKV * 4096, BF16, "p (s x) -> p s x", s=NKV)
        abA = A.take(2 * TT * 2, BF16, "p (c t) -> p c t", c=2)
        kcs = A.take(2 * 2 * 128 * 2, BF16, "p (a b x) -> p a b x", a=2, b=2)
        self.pt = A.take(3 * TT * 2, BF16, "p (c t) -> p c t", c=3)
        acc = A.take(4 * TT * 4, F32, "p (c t) -> p c t", c=4)
        rl = A.take(2 * TT * 4, F32, "p (c t) -> p c t", c=2)
        wg = A.take(TT * 4, F32)
        of = A.take(2 * TT * 4, F32, "p (c t) -> p c t", c=2)
        osq = A.take(2 * TT * 2, BF16, "p (c t) -> p c t", c=2)
        phat = A.take(2 * TT * 2, BF16, "p (c t) -> p c t", c=2)
        score = A.take(4 * 32 * 4, F32, "p (c x) -> p c x", c=4)
        mx8 = A.take(4 * 8 * 4, F32, "p (c x) -> p c x", c=4)
        selb = A.take(4 * 32 * 2, BF16, "p (c x) -> p c x", c=4)
        selbT = A.take(TT * 2, BF16)
        selct = A.take(2 * 4 * 32 * 4, F32, "p (a c x) -> p a c x", a=2, c=4)
        ssq = A.take(2 * TT * 4, F32, "p (c t) -> p c t", c=2)
        rstd = A.take(2 * TT * 4, F32, "p (c t) -> p c t", c=2)
        otmp = A.take(2 * TT * 4, F32, "p (c t) -> p c t", c=2)
        ident = self.cview("ident")
        ones = self.cview("ones")
        ovl = self.cview("ovl")
        cbv = self.cview("cb").rearrange("p (k n) -> p k n", k=4)
        wbv = self.cview("wb").rearrange("p (k n) -> p k n", k=4)
        cmask = self.cview("cmask")
        exv = self.cview("ex", 32).rearrange("p (k n) -> p k n", k=16)
        eselv = self.cview("esel", 32).rearrange("p (k n) -> p k n", k=24)
        wfm2 = self.wview(li, "wfm2").rearrange("(g p x) -> g p x", g=16, p=128)
        wout = self.wview(li, "wout").rearrange("(g p x) -> g p x", g=16, p=128)
        P.dma("sp", wgs, self.wview(li, "wgt").rearrange("(p c x) -> p c x", p=128, c=DC), reads=[wkey], writes=["wgs"])
        P.dma("sp", kcs[:, :, 0, :], self.s_kc.ap().rearrange("k p x -> p k x"), reads=["s_kc"], writes=["kcs"])
        P.dma("sp", kcs[:, :, 1, :], self.s_vc.ap().rearrange("k p x -> p k x"), reads=["s_vc"], writes=["kcs"])
        sctr = 0
        kvc = 0
        rctr = 0
        yctr = 0
        actr = 0
        zctr = 0
        loc = 0
        for j in range(NTT):
            tsl = slice(j * TT, (j + 1) * TT)
            nkt = 4 * (j + 1)
            nk = nkt * 128
            self.load_x(M, s, j)
            self.norm_tile(xt, h, M["sq"], M["tmp"], M["rs"], li, 16, "xt", "h", 0)
            P.dma("sp", W["rc"][0:32, :], self.ropec.ap()[:, tsl], writes=["rope"])
            P.dma("sp", W["rsn"][0:32, :], self.ropes.ap()[:, tsl], writes=["rope"])
            P.dma("sp", selct, self.selc.ap()[:, :, 4 * j:4 * j + 4, :], writes=["selct"])
            for cc in range(16):
                sl = sctr % NS2; sctr += 1
                P.dma("sp", wring[:, sl, :], wfm2[cc], reads=[wkey], writes=[("ws", sl)])
                w = wring[:, sl, :].rearrange("p (c x) -> p c x", c=DC)
                pb = 1 + cc % 2
                psp = self.ps[:, pb, :]
                for dc in range(DC):
                    P.op("pe", lambda e, o=psp, ww=w[:, dc, :], r=h[:, dc, :], s_=(dc == 0), t_=(dc == DC - 1):
                         e.matmul(o, lhsT=ww, rhs=r, start=s_, stop=t_),
                         reads=[("ws", sl), ("h", dc)], writes=[("ps", pb)])
                if cc < 8:
                    self.head_norm(psp, ("ps", pb), TT, self.vcol(li, 48), W, qT[:, cc, :], ("qT", cc),
                                   rope=(W["rc"][0:32, :], W["rsn"][0:32, :], "rope"))
                else:
                    self.head_norm(psp, ("ps", pb), TT, self.vcol(li, 52), W, fqT[:, cc - 8, :], ("fqT", cc - 8))
            psg = self.ps[0:24, 5, :]
            for dc in range(DC):
                P.op("pe", lambda e, ww=wgs[:, dc, :], r=h[:, dc, :], s_=(dc == 0), t_=(dc == DC - 1):
                     e.matmul(psg, lhsT=ww, rhs=r, start=s_, stop=t_), reads=["wgs", ("h", dc)], writes=[("ps", 5)])
            P.op("act", lambda e: e.activation(out=gtmp[0:24, :], in_=psg, func=AF.Exp, scale=-1.0), reads=[("ps", 5)], writes=["gtmp"])
            P.op("dve", lambda e: e.tensor_scalar(out=gtmp[0:24, :], in0=gtmp[0:24, :], scalar1=1.0, scalar2=None, op0=ALU.add), reads=["gtmp"], writes=["gtmp"])
            P.op("dve", lambda e: e.reciprocal(out=gsb[0:24, :], in_=gtmp[0:24, :]), reads=["gtmp"], writes=["gsb"])
            for kvh in range(2):
                ch = []
                for _ in range(4):
                    ch.append(kvc % NKV); kvc += 1
                c_ks, c_kw, c_vs, c_vw = ch
                P.dma("sp", kvr[:, c_ks, 0:nk], self.s_fm.ap()[4 + kvh, :, 0:nk], reads=[("s_fm", 4 + kvh)], writes=[("kv", c_ks)])
                P.dma("sp", kvr[:, c_kw, 0:nk], self.s_fm.ap()[6 + kvh, :, 0:nk], reads=[("s_fm", 6 + kvh)], writes=[("kv", c_kw)])
                P.dma("sp", kvr[:, c_vs, 0:nk], self.s_tm.ap()[0 + kvh, :, 0:nkt, :].rearrange("p k d -> p (k d)"), reads=[("s_tm", 0)], writes=[("kv", c_vs)])
                P.dma("sp", kvr[:, c_vw, 0:nk], self.s_tm.ap()[2 + kvh, :, 0:nkt, :].rearrange("p k d -> p (k d)"), reads=[("s_tm", 0)], writes=[("kv", c_vw)])
                ksT = kvr[:, c_ks, :]
                kwT = kvr[:, c_kw, :]
                vs = kvr[:, c_vs, :].rearrange("p (k d) -> p k d", d=128)
                vw = kvr[:, c_vw, :].rearrange("p (k d) -> p k d", d=128)
                kcT = kcs[:, kvh, 0, 0:NCMP]
                vc = kcs[0:NCMP, kvh, 1, :]

                def fin_branch(g, b, lo, first):
                    nonlocal rctr, yctr
                    lb, ob = lo
                    x_ = rctr % 2; rctr += 1
                    rlb = rl[:, x_, :]
                    r = ((kvh * 4 + g) * 3 + b)
                    P.op("dve", lambda e: e.tensor_scalar(out=rlb, in0=self.ps[:, lb, :], scalar1=1e-30, scalar2=None, op0=ALU.max),
                         reads=[("ps", lb)], writes=[("rl", x_)])
                    P.op("dve", lambda e: e.reciprocal(out=rlb, in_=rlb), reads=[("rl", x_)], writes=[("rl", x_)])
                    P.op("pe", lambda e: e.matmul(self.ps[:, 7, :], lhsT=eselv[0:24, r, :], rhs=gsb[0:24, :], start=True, stop=True),
                         reads=["gsb", "cb"], writes=[("ps", 7)])
                    P.op("dve", lambda e: e.tensor_tensor(out=wg, in0=self.ps[:, 7, :], in1=rlb, op=ALU.mult),
                         reads=[("ps", 7), ("rl", x_)], writes=["wg"])
                    if first:
                        P.op("dve", lambda e: e.tensor_tensor(out=acc[:, g, :], in0=self.ps[:, ob, :], in1=wg, op=ALU.mult),
                             reads=[("ps", ob), "wg"], writes=[("acc", g)])
                    else:
                        y_ = yctr % 2; yctr += 1
                        P.op("dve", lambda e: e.tensor_tensor(out=otmp[:, y_, :], in0=self.ps[:, ob, :], in1=wg, op=ALU.mult),
                             reads=[("ps", ob), "wg"], writes=[("otmp", y_)])
                        P.op("pool", lambda e: e.tensor_tensor(out=acc[:, g, :], in0=acc[:, g, :], in1=otmp[:, y_, :], op=ALU.add),
                             reads=[("otmp", y_)], writes=[("acc", g)])
                    return x_

                for g in range(4):
                    hq = kvh * 4 + g
                    lo = (3, 4) if loc % 2 == 0 else (5, 6); loc += 1
                    tiles = [dict(k=kcT, kkeys=["kcs"], nk=NCMP, extras=[(ident[0:NCMP, 0:NCMP], cmask[0:NCMP, tsl], [])],
                                  v=vc, vkeys=["kcs"])]
                    ptile, pkey = self.attn(qT[:, hq, :], ("qT", hq), tiles, lo)
                    x_ = fin_branch(g, 0, lo, True)
                    z_ = zctr % 2; zctr += 1
                    P.op("pool", lambda e, o=phat[0:NCMP, z_, :], a=ptile, b=rl[0:NCMP, x_, :]: e.tensor_tensor(out=o, in0=a, in1=b, op=ALU.mult),
                         reads=[pkey, ("rl", x_)], writes=[("phat", z_)])
                    for tk in range(4):
                        P.op("pe", lambda e, o=self.ps[:, 2, tk * 32:(tk + 1) * 32], l_=phat[0:NCMP, z_, tk * 128:(tk + 1) * 128], r_=ovl[0:NCMP, :],
                             s_=(g == 0 and tk == 0), t_=(g == 3): e.matmul(o, lhsT=l_, rhs=r_, start=s_, stop=t_, skip_group_check=True),
                             reads=[("phat", z_), "cb"], writes=[("ps", 2)])
                imp = self.ps[:, 2, 0:128].rearrange("p (c x) -> p c x", c=4)
                P.op("dve", lambda e: e.tensor_tensor(out=score, in0=imp, in1=selct[:, 0, :, :], op=ALU.mult),
                     reads=[("ps", 2), "selct"], writes=["score"])
                P.op("dve", lambda e: e.tensor_tensor(out=score, in0=score, in1=selct[:, 1, :, :], op=ALU.add),
                     reads=["selct"], writes=["score"])
                for tk in range(4):
                    P.op("dve", lambda e, o=mx8[:, tk, :], i_=score[:, tk, :]: e.max(out=o, in_=i_), reads=["score"], writes=[("mx8", tk)])
                for tk in range(4):
                    P.op("dve", lambda e, o=selb[:, tk, :], i_=score[:, tk, :], th=mx8[:, tk, 7:8]:
                         e.tensor_scalar(out=o, in0=i_, scalar1=th, scalar2=NEG, op0=ALU.is_lt, op1=ALU.mult),
                         reads=["score", ("mx8", tk)], writes=[("selb", tk)])
                for tk in range(4):
                    P.op("pe", lambda e, o=self.ps[0:32, 2, tk * 128:(tk + 1) * 128], l_=selb[:, tk, :]:
                         e.matmul(o, lhsT=l_, rhs=ident, start=True, stop=True),
                         reads=[("selb", tk), "cb"], writes=[("ps", 2)])
                P.op("dve", lambda e: e.tensor_copy(out=selbT[0:32, :], in_=self.ps[0:32, 2, :]), reads=[("ps", 2)], writes=["selbT"])
                for g in range(4):
                    hq = kvh * 4 + g
                    lo = (3, 4) if loc % 2 == 0 else (5, 6); loc += 1
                    tiles = []
                    for kt in range(nkt):
                        ex = [(exv[0:32, kt, :], selbT[0:32, :], ["selbT"])]
                        if kt >= 4 * j:
                            ex.append((ident, cbv[:, kt - 4 * j, :], []))
                        tiles.append(dict(k=ksT[:, kt * 128:(kt + 1) * 128], kkeys=[("kv", c_ks)], nk=128, extras=ex,
                                          v=vs[:, kt, :], vkeys=[("kv", c_vs)]))
                    self.attn(qT[:, hq, :], ("qT", hq), tiles, lo)
                    fin_branch(g, 1, lo, False)
                for g in range(4):
                    hq = kvh * 4 + g
                    lo = (3, 4) if loc % 2 == 0 else (5, 6); loc += 1
                    tiles = []
                    for kt in range(max(0, 4 * j - 4), nkt):
                        if kt >= 4 * j:
                            ex = [(ident, cbv[:, kt - 4 * j, :], [])]
                        else:
                            ex = [(ident, wbv[:, kt - (4 * j - 4), :], [])]
                        tiles.append(dict(k=kwT[:, kt * 128:(kt + 1) * 128], kkeys=[("kv", c_kw)], nk=128, extras=ex,
                                          v=vw[:, kt, :], vkeys=[("kv", c_vw)]))
                    self.attn(qT[:, hq, :], ("qT", hq), tiles, lo)
                    fin_branch(g, 2, lo, False)
                for g in range(4):
                    hq = kvh * 4 + g
                    a_ = actr % 2; actr += 1
                    P.op("pool", lambda e, o=osq[:, a_, :], i_=acc[:, g, :]: e.tensor_tensor(out=o, in0=i_, in1=i_, op=ALU.mult),
                         reads=[("acc", g)], writes=[("osq", a_)])
                    P.op("pe", lambda e, r_=osq[:, a_, :]: e.matmul(self.ps[:, 7, :], lhsT=ones, rhs=r_, start=True, stop=True),
                         reads=[("osq", a_), "cb"], writes=[("ps", 7)])
                    if hq == 0:
                        P.op("dve", lambda e: e.tensor_copy(out=ssq[:, 0, :], in_=self.ps[:, 7, :]), reads=[("ps", 7)], writes=[("ssq", 0)])
                    else:
                        P.op("dve", lambda e: e.tensor_tensor(out=ssq[:, 0, :], in0=self.ps[:, 7, :], in1=ssq[:, 0, :], op=ALU.add),
                             reads=[("ps", 7)], writes=[("ssq", 0)])
                    P.op("dve", lambda e, o=o_sb[:, hq, :], i_=acc[:, g, :], gc=self.vcol(li, 54 + hq):
                         e.tensor_scalar(out=o, in0=i_, scalar1=gc, scalar2=None, op0=ALU.mult),
                         reads=[("acc", g), "vec"], writes=[("h", hq)])
            for hf in range(8):
                ch = []
                for _ in range(3):
                    ch.append(kvc % NKV); kvc += 1
                c_k, c_v, c_b = ch
                a_ = hf % 2
                P.dma("sp", kvr[:, c_k, 0:nk], self.s_fm.ap()[8 + hf, :, 0:nk], reads=[("s_fm", 8 + hf)], writes=[("kv", c_k)])
                P.dma("sp", kvr[:, c_v, 0:nk], self.s_tm.ap()[4 + hf, :, 0:nkt, :].rearrange("p k d -> p (k d)"), reads=[("s_tm", 1), ("s_tm", 2)], writes=[("kv", c_v)])
                P.dma("sp", kvr[0:6, c_b, 0:nk], self.s_ab.ap()[hf, 1, :, 0:nk], reads=["s_ab"], writes=[("kv", c_b)])
                P.dma("sp", abA[0:6, a_, :], self.s_ab.ap()[hf, 0, :, tsl], reads=["s_ab"], writes=[("abA", a_)])
                fk = kvr[:, c_k, :]
                fv = kvr[:, c_v, :].rearrange("p (k d) -> p k d", d=128)
                Bc = kvr[0:6, c_b, :]
                lo = (3, 4) if loc % 2 == 0 else (5, 6); loc += 1
                tiles = []
                for kt in range(nkt):
                    ex = [(Bc[:, kt * 128:(kt + 1) * 128], abA[0:6, a_, :], [("kv", c_b), ("abA", a_)])]
                    if kt >= 4 * j:
                        ex.append((ident, cbv[:, kt - 4 * j, :], []))
                    tiles.append(dict(k=fk[:, kt * 128:(kt + 1) * 128], kkeys=[("kv", c_k)], nk=128, extras=ex,
                                      v=fv[:, kt, :], vkeys=[("kv", c_v)]))
                self.attn(fqT[:, hf, :], ("fqT", hf), tiles, lo)
                lb, ob = lo
                x_ = rctr % 2; rctr += 1
                rlb = rl[:, x_, :]
                P.op("dve", lambda e, o=rlb, i_=self.ps[:, lb, :]: e.reciprocal(out=o, in_=i_), reads=[("ps", lb)], writes=[("rl", x_)])
                f_ = hf % 2
                P.op("dve", lambda e, o=of[:, f_, :], a=self.ps[:, ob, :], b=rlb: e.tensor_tensor(out=o, in0=a, in1=b, op=ALU.mult),
                     reads=[("ps", ob), ("rl", x_)], writes=[("of", f_)])
                q_ = actr % 2; actr += 1
                P.op("pool", lambda e, o=osq[:, q_, :], i_=of[:, f_, :]: e.tensor_tensor(out=o, in0=i_, in1=i_, op=ALU.mult),
                     reads=[("of", f_)], writes=[("osq", q_)])
                P.op("pe", lambda e, r_=osq[:, q_, :]: e.matmul(self.ps[:, 7, :], lhsT=ones, rhs=r_, start=True, stop=True),
                     reads=[("osq", q_), "cb"], writes=[("ps", 7)])
                if hf == 0:
                    P.op("dve", lambda e: e.tensor_copy(out=ssq[:, 1, :], in_=self.ps[:, 7, :]), reads=[("ps", 7)], writes=[("ssq", 1)])
                else:
                    P.op("dve", lambda e: e.tensor_tensor(out=ssq[:, 1, :], in0=self.ps[:, 7, :], in1=ssq[:, 1, :], op=ALU.add),
                         reads=[("ps", 7)], writes=[("ssq", 1)])
                P.op("dve", lambda e, o=o_sb[:, 8 + hf, :], i_=of[:, f_, :], gc=self.vcol(li, 62 + hf):
                     e.tensor_scalar(out=o, in0=i_, scalar1=gc, scalar2=None, op0=ALU.mult),
                     reads=[("of", f_), "vec"], writes=[("h", 8 + hf)])
            for k in range(2):
                P.op("act", lambda e, o=rstd[:, k, :], i_=ssq[:, k, :]: e.activation(out=o, in_=i_, func=AF.Ln, bias=EPS, scale=1.0 / 1024.0),
                     reads=[("ssq", k)], writes=[("rstd", k)])
                P.op("act", lambda e, o=rstd[:, k, :]: e.activation(out=o, in_=o, func=AF.Exp, scale=-0.5),
                     reads=[("rstd", k)], writes=[("rstd", k)])
            self.load_x(M, s, j)
            for dco in range(DC):
                sl = sctr % NS2; sctr += 1
                P.dma("sp", wring[:, sl, :], wout[dco], reads=[wkey], writes=[("ws", sl)])
                w = wring[:, sl, :].rearrange("p (c x) -> p c x", c=16)
                bn, bf = (0, 1) if dco % 2 == 0 else (2, 3)
                for half, bnk in ((0, bn), (1, bf)):
                    for kk in range(8):
                        kc = half * 8 + kk
                        P.op("pe", lambda e, o=self.ps[:, bnk, :], ww=w[:, kc, :], r=o_sb[:, kc, :], s_=(kk == 0), t_=(kk == 7):
                             e.matmul(o, lhsT=ww, rhs=r, start=s_, stop=t_),
                             reads=[("ws", sl), ("h", kc)], writes=[("ps", bnk)])
                for half, bnk in ((0, bn), (1, bf)):
                    y_ = yctr % 2; yctr += 1
                    P.op("dve", lambda e, o=otmp[:, y_, :], a=self.ps[:, bnk, :], b=rstd[:, half, :]: e.tensor_tensor(out=o, in0=a, in1=b, op=ALU.mult),
                         reads=[("ps", bnk), ("rstd", half)], writes=[("otmp", y_)])
                    P.op("pool", lambda e, o=xt[:, dco, :], b=otmp[:, y_, :]: e.tensor_tensor(out=o, in0=o, in1=b, op=ALU.add),
                         reads=[("otmp", y_)], writes=[("xt", dco)])
            P.dma("sp", self.xs.ap()[s, :, :, tsl].rearrange("c p t -> p c t"), xt,
                  reads=[("xt", dc) for dc in range(DC)], writes=[("X", s, j)])


def _pack_layer(w, li):
    out = np.zeros(NPK, np.float32)

    def put(name, arr):
        o, n = PK[name]
        a = np.ascontiguousarray(arr, dtype=np.float32).reshape(-1)
        assert a.size == n, (name, a.size, n)
        out[o:o + n] = a

    for f in (1, 2):
        wg = w[f"ffn{f}_w_gate"][li]
        wu = w[f"ffn{f}_w_up"][li]
        wd = w[f"ffn{f}_w_down"][li]
        put(f"wg{f}", wg.reshape(DC, 128, 11, 512).transpose(2, 1, 0, 3))
        put(f"wu{f}", wu.reshape(DC, 128, 11, 512).transpose(2, 1, 0, 3))
        put(f"wd{f}", wd.reshape(FC, 128, DC, 128).transpose(2, 1, 0, 3))
    win = w["w_in"][li]
    kv0 = 1024
    def kvcol(branch, typ, kvh):
        return kv0 + ((branch * 2 + typ) * 2 + kvh) * 128
    fq0 = 2584
    fk0 = fq0 + 1024
    fv0 = fq0 + 2048
    cols1 = ([kvcol(0, 0, 0), kvcol(0, 0, 1), kvcol(0, 1, 0), kvcol(0, 1, 1),
              kvcol(1, 0, 0), kvcol(1, 0, 1), kvcol(2, 0, 0), kvcol(2, 0, 1)]
             + [fk0 + h * 128 for h in range(8)])
    def fm(cols):
        blk = np.stack([win[:, c:c + 128] for c in cols], 0)
        return blk.reshape(len(cols), DC, 128, 128).transpose(0, 2, 1, 3)
    put("wfm1", fm(cols1))
    tmcols = np.concatenate([np.arange(kvcol(1, 1, 0), kvcol(1, 1, 0) + 256),
                             np.arange(kvcol(2, 1, 0), kvcol(2, 1, 0) + 256),
                             np.arange(fv0, fv0 + 1024)])
    wt = win[:, tmcols]
    put("wtm", wt.reshape(DC, 128, 3, 512).transpose(2, 1, 0, 3))
    cols2 = [h * 128 for h in range(8)] + [fq0 + h * 128 for h in range(8)]
    put("wfm2", fm(cols2))
    wo = w["w_out"][li]
    put("wout", wo.reshape(16, 128, DC, 128).transpose(2, 1, 0, 3))
    cw1 = w["cmp_w1"][li]
    put("cw1", cw1.reshape(2, 32, 128, 256).transpose(0, 2, 1, 3))
    cw2 = w["cmp_w2"][li]
    put("cw2", cw2.reshape(2, 2, 128, 128).transpose(0, 2, 1, 3))
    put("wf", win[:, 5656:5664].reshape(DC, 128, 8).transpose(1, 0, 2))
    put("wgt", win[:, 2560:2584].reshape(DC, 128, 24).transpose(1, 0, 2))
    pos = w["cmp_pos_emb"][li]
    put("pos", pos.transpose(2, 0, 1))
    return out


def _vec_pack(w, layers):
    v = np.zeros((128, len(layers) * VW), np.float32)
    for i, li in enumerate(layers):
        b = i * VW
        v[:, b + 0:b + 16] = w["ffn1_norm"][li].reshape(DC, 128).T
        v[:, b + 16:b + 32] = w["mix_norm"][li].reshape(DC, 128).T
        v[:, b + 32:b + 48] = w["ffn2_norm"][li].reshape(DC, 128).T
        v[:, b + 48] = w["nsa_q_norm"][li]
        v[:, b + 49:b + 52] = w["nsa_k_norm"][li].T
        v[:, b + 52] = w["fox_q_norm"][li]
        v[:, b + 53] = w["fox_k_norm"][li]
        v[:, b + 54:b + 62] = w["nsa_out_norm"][li].reshape(8, 128).T
        v[:, b + 62:b + 70] = w["fox_out_norm"][li].reshape(8, 128).T
        v[0:8, b + 70] = w["fox_forget_bias"][li]
    return v


def _consts():
    cb = np.zeros((128, NCB), np.float32)
    def put(name, arr):
        o, n = CB[name]
        cb[:arr.shape[0], o:o + n] = arr.reshape(arr.shape[0], -1)
    put("ident", np.eye(128, dtype=np.float32))
    put("ones", np.ones((128, 128), np.float32))
    rt = np.zeros((32, 32), np.float32)
    for i in range(16):
        rt[16 + i, i] = -1.0
        rt[i, 16 + i] = 1.0
    put("rt", rt)
    cstart = np.arange(NCMP) * 16
    sstart = np.arange(32) * 64
    ovl = ((cstart[:, None] < sstart[None, :] + 64) & (cstart[:, None] + 32 > sstart[None, :])).astype(np.float32)
    put("ovl", ovl)
    p = np.arange(128)[:, None]
    n = np.arange(512)[None, :]
    cbm = np.stack([np.where(128 * k + p <= n, 0.0, NEG) for k in range(4)], 1)
    wbm = np.stack([np.where(128 * k + p > n, 0.0, NEG) for k in range(4)], 1)
    put("cb", cbm.astype(np.float32))
    put("wb", wbm.astype(np.float32))
    cend = cstart + 31
    t = np.arange(T)[None, :]
    put("cmask", np.where(cend[:, None] <= t, 0.0, NEG).astype(np.float32))
    j = np.arange(32)[:, None, None]
    kt = np.arange(16)[None, :, None]
    pp = np.arange(128)[None, None, :]
    put("ex", (j == 2 * kt + pp // 64).astype(np.float32))
    r = np.arange(32)[:, None, None]
    rr = np.arange(24)[None, :, None]
    put("esel", np.broadcast_to((r == rr), (32, 24, 128)).astype(np.float32))
    inv = (np.float32(500000.0) ** (-np.arange(0, 32, 2, dtype=np.float32) / np.float32(32))).astype(np.float32)
    def tables(pos):
        ang = pos.astype(np.float32)[:, None] * inv[None, :]
        c = np.cos(ang).astype(np.float32).T
        s_ = np.sin(ang).astype(np.float32).T
        return np.concatenate([c, c], 0), np.concatenate([s_, s_], 0)
    rc, rs = tables(np.arange(T))
    kc_c, kc_s = tables(cend)
    ropekc = np.zeros((32, 2, 128), np.float32)
    ropekc[:, 0, :NCMP] = kc_c
    ropekc[:, 1, :NCMP] = kc_s
    tok = np.arange(T)
    tb = tok // 64
    jj = np.arange(32)[None, :]
    causal = jj <= tb[:, None]
    forced = (jj == 0) | (causal & (jj > tb[:, None] - 2))
    mmul = (causal & ~forced).astype(np.float32)
    badd = np.where(forced, 1e9, np.where(causal, 0.0, -1e30)).astype(np.float32)
    selc = np.stack([mmul.reshape(16, 128, 32).transpose(1, 0, 2), badd.reshape(16, 128, 32).transpose(1, 0, 2)], 1)
    return cb, rc, rs, ropekc, np.ascontiguousarray(selc)


_CACHE = {}


def _get_prog(nseq, layers_key, dbg_key=None):
    key = (nseq, layers_key, dbg_key)
    if key not in _CACHE:
        b = Builder(nseq, list(layers_key), dict(dbg_key or ()))
        _CACHE[key] = b.build()
    return _CACHE[key]


def kernel(**inputs):
    x = np.asarray(inputs["x"], np.float32)
    B = x.shape[0]
    ncores = 8
    nseq = B // ncores
    w = {k: np.asarray(v) for k, v in inputs.items() if k != "x"}
    layers = tuple(range(DEPTH))
    wsrc = np.concatenate([_pack_layer(w, li) for li in layers]).reshape(len(layers) * NPKR, 2048)
    vec = _vec_pack(w, layers)
    cb, rc, rs, ropekc, selc = _consts()
    nc = _get_prog(nseq, layers)
    in_maps = []
    for c in range(ncores):
        xc = x[c * nseq:(c + 1) * nseq]
        xT = np.ascontiguousarray(xc.transpose(0, 2, 1)).reshape(nseq, DC, 128, T)
        in_maps.append({"xin": xT, "wsrc": wsrc, "vec": vec, "cbf": cb, "ropec": rc, "ropes": rs,
                        "ropekc": ropekc, "selc": selc})
    res = run_bass_kernel_spmd(nc, in_maps, core_ids=list(range(ncores)))
    outs = []
    for c in range(ncores):
        y = res.results[c]["xout"].reshape(nseq, D, T).transpose(0, 2, 1)
        outs.append(y)
    return np.ascontiguousarray(np.concatenate(outs, 0), dtype=np.float32)
```

```python
import numpy as np
import concourse.bass as bass
import concourse.mybir as mybir
from concourse.bass_utils import run_bass_kernel_spmd

F32 = mybir.dt.float32
BF16 = mybir.dt.bfloat16
U8 = mybir.dt.uint8
AF = mybir.ActivationFunctionType
ALU = mybir.AluOpType

D = 2048
T = 2048
DEPTH = 4
HD = 128
DFF = 5632
DC = D // 128
FC = DFF // 128
TT = 512
NTT = T // TT
EPS = 1e-6
SCALE = HD ** -0.5
NEG = -32768.0
NCMP = 127
VW = 72

PK = {}
_off = 0
def _add(name, n):
    global _off
    PK[name] = (_off, n)
    _off += n
for _f in (1, 2):
    _add(f"wg{_f}", D * DFF)
    _add(f"wu{_f}", D * DFF)
    _add(f"wd{_f}", D * DFF)
_add("wfm1", 16 * 128 * 16 * 128)
_add("wtm", 3 * 128 * 16 * 512)
_add("wfm2", 16 * 128 * 16 * 128)
_add("wout", 16 * 128 * 16 * 128)
_add("cw1", 2 * 128 * 32 * 256)
_add("cw2", 2 * 128 * 2 * 128)
_add("wf", 128 * 16 * 8)
_add("wgt", 128 * 16 * 24)
_add("pos", 128 * 2 * 32)
NPK = ((_off + 2047) // 2048) * 2048
NPKR = NPK // 2048

CB = {}
_c = 0
def _addc(name, n):
    global _c
    CB[name] = (_c, n)
    _c += n
_addc("ident", 128)
_addc("ones", 128)
_addc("rt", 32)
_addc("ovl", 32)
_addc("cb", 4 * 512)
_addc("wb", 4 * 512)
_addc("cmask", 2048)
_addc("ex", 16 * 128)
_addc("esel", 24 * 128)
NCB = _c


class Prog:
    NS = 8

    def __init__(self, nc):
        self.nc = nc
        self.ops = []
        self.deps = []
        self.last_w = {}
        self.readers = {}
        self.streams = {k: [] for k in ("pe", "act", "dve", "pool", "sp")}
        self.groups = {}
        self.last_op = {k: None for k in self.streams}
        self.recent_dma = {"sp": [], "pool": []}
        self.cur_barrier = None

    def _record(self, stream, fn, kind, reads, writes, group=None, is_barrier=False):
        i = len(self.ops)
        d = set()
        lw = self.last_w
        rd = self.readers
        for k in reads:
            w = lw.get(k)
            if w is not None:
                d.add(w)
            r = rd.get(k)
            if r is None:
                r = rd[k] = [{}, []]
            if kind == "c":
                r[0][stream] = i
            else:
                r[1].append(i)
        for k in writes:
            w = lw.get(k)
            if w is not None:
                d.add(w)
            r = rd.get(k)
            if r is not None:
                d.update(r[0].values())
                d.update(r[1])
                del rd[k]
            lw[k] = i
        if is_barrier:
            for st, li in self.last_op.items():
                if li is not None:
                    d.add(li)
            for q, lst in self.recent_dma.items():
                d.update(lst)
        elif self.cur_barrier is not None:
            d.add(self.cur_barrier)
        d.discard(i)
        self.ops.append((stream, fn, kind, group))
        self.deps.append(d)
        self.streams[stream].append(i)
        if kind == "c":
            self.last_op[stream] = i
        elif group is None:
            lst = self.recent_dma[stream]
            lst.append(i)
            if len(lst) > self.NS:
                lst.pop(0)
        if group is not None:
            self.groups[group] = self.groups.get(group, 0) + 1
        if is_barrier:
            self.cur_barrier = i
        return i

    def op(self, stream, fn, reads=(), writes=()):
        return self._record(stream, fn, "c", reads, writes)

    def dma(self, q, out, in_, reads=(), writes=(), group=None):
        return self._record(q, lambda e, o=out, i=in_: e.dma_start(out=o, in_=i), "d",
                            reads, writes, group)

    def barrier(self):
        self._record("sp", lambda e: e.nop(), "c", [], [], is_barrier=True)

    def emit(self, block, sems_ctx):
        nc = self.nc
        ops = self.ops
        n = len(ops)
        dsem = {}
        qcount = {"sp": 0, "pool": 0}
        qhist = {"sp": [], "pool": []}
        extra = {}
        for i in range(n):
            st, fn, kind, group = ops[i]
            if kind != "d":
                continue
            if group is not None:
                dsem[i] = (("g", group), 16 * self.groups[group])
            else:
                c = qcount[st]
                dsem[i] = (("s", st, c % self.NS), 16 * (c // self.NS + 1))
                if c >= self.NS:
                    extra[i] = qhist[st][c - self.NS]
                qhist[st].append(i)
                qcount[st] = c + 1
        sig = [False] * n
        for i in range(n):
            st = ops[i][0]
            for d in self.deps[i]:
                sd, _, kd, _ = ops[d]
                if kd == "c" and not (sd == "pe" and st == "pe"):
                    sig[d] = True
        cnt = {k: 0 for k in self.streams}
        sval = [0] * n
        for i in range(n):
            st, fn, kind, group = ops[i]
            if kind == "c" and sig[i]:
                cnt[st] += 1
                sval[i] = cnt[st]
        semkeys = set(("e", k) for k in self.streams)
        for i in dsem:
            semkeys.add(dsem[i][0])
        sem = {}
        for k in sorted(semkeys, key=str):
            sem[k] = sems_ctx.enter_context(nc.semaphore("s_" + "_".join(str(x) for x in k)))
        waits = [None] * n
        waited = {k: {} for k in self.streams}
        for st in self.streams:
            wd = waited[st]
            for i in self.streams[st]:
                need = {}
                dl = self.deps[i]
                if i in extra:
                    dl = set(dl)
                    dl.add(extra[i])
                for d in dl:
                    sd, _, kd, _ = ops[d]
                    if kd == "d":
                        k, v = dsem[d]
                    else:
                        if sd == "pe" and st == "pe":
                            continue
                        k, v = ("e", sd), sval[d]
                    if need.get(k, 0) < v:
                        need[k] = v
                w = []
                for k, v in need.items():
                    if wd.get(k, 0) < v:
                        wd[k] = v
                        w.append((sem[k], v))
                waits[i] = w
        self.n_waits = sum(len(w) for w in waits)
        self.n_sig = sum(sig)

        def run_stream(st, eng):
            for i in self.streams[st]:
                _, fn, kind, group = ops[i]
                for s, v in waits[i]:
                    eng.wait_ge(s, v)
                ins = fn(eng)
                if kind == "d":
                    ins.then_inc(sem[dsem[i][0]], 16)
                elif sig[i]:
                    ins.then_inc(sem[("e", st)], 1)

        @block.tensor
        def _(e):
            run_stream("pe", e)

        @block.scalar
        def _(e):
            run_stream("act", e)

        @block.vector
        def _(e):
            run_stream("dve", e)

        @block.gpsimd
        def _(e):
            run_stream("pool", e)

        @block.sync
        def _(e):
            run_stream("sp", e)


class Arena:
    def __init__(self, ap, size):
        self.ap = ap
        self.size = size
        self.off = 0

    def take(self, nbytes, dtype, pattern=None, **kw):
        rb = (nbytes + 63) // 64 * 64
        assert self.off + rb <= self.size, ("arena overflow", self.off, rb, self.size)
        v = self.ap[:, self.off:self.off + nbytes].bitcast(dtype)
        self.off += rb
        if pattern:
            v = v.rearrange(pattern, **kw)
        return v

    def mark(self):
        return self.off

    def reset(self, m):
        self.off = m


class Builder:
    def __init__(self, nseq, layers, dbg=None):
        self.nseq = nseq
        self.layers = layers
        self.dbg = dbg or {}
        nc = bass.Bass("TRN2", target_bir_lowering=False)
        self.nc = nc
        self.P = Prog(nc)
        L = len(layers)
        self.L = L
        self.xin = nc.dram_tensor("xin", [nseq, DC, 128, T], F32, kind="ExternalInput")
        self.xs = nc.dram_tensor("xout", [nseq, DC, 128, T], F32, kind="ExternalOutput")
        self.wsrc = nc.dram_tensor("wsrc", [L * NPKR, 2048], F32, kind="ExternalInput")
        self.wpk = [nc.dram_tensor(f"wpk{i}", [NPKR, 2048], BF16, kind="Internal") for i in range(L)]
        self.vec = nc.dram_tensor("vec", [128, L * VW], F32, kind="ExternalInput")
        self.cbf = nc.dram_tensor("cbf", [128, NCB], F32, kind="ExternalInput")
        self.ropec = nc.dram_tensor("ropec", [32, T], F32, kind="ExternalInput")
        self.ropes = nc.dram_tensor("ropes", [32, T], F32, kind="ExternalInput")
        self.ropekc = nc.dram_tensor("ropekc", [32, 2, 128], F32, kind="ExternalInput")
        self.selc = nc.dram_tensor("selc", [128, 2, 16, 32], F32, kind="ExternalInput")
        sk = "ExternalOutput" if self.dbg.get("dump") else "Internal"
        self.s_fm = nc.dram_tensor("s_fm", [16, 128, T], BF16, kind=sk)
        self.s_tm = nc.dram_tensor("s_tm", [12, 128, 16, 128], BF16, kind=sk)
        self.s_kc = nc.dram_tensor("s_kc", [2, 128, 128], BF16, kind=sk)
        self.s_vc = nc.dram_tensor("s_vc", [2, 128, 128], BF16, kind=sk)
        self.s_ab = nc.dram_tensor("s_ab", [8, 2, 6, T], BF16, kind=sk)
        ASZ = 206 * 1024
        self.arena_t = nc.alloc_sbuf_tensor("arena", [128, ASZ], U8)
        self.A = Arena(self.arena_t.ap(), ASZ)
        self.ps = nc.alloc_psum_tensor("ps", [128, 8, 512], F32).ap()
        A = self.A
        self.cb_sb = A.take(NCB * 2, BF16)
        self.vec_sb = A.take(L * VW * 4, F32)
        self.base_mark = A.mark()
        self.wslot_ctr = 0

    def cview(self, name, rows=128):
        o, n = CB[name]
        return self.cb_sb[0:rows, o:o + n]

    def vcol(self, li, c, rows=128, n=1):
        return self.vec_sb[0:rows, li * VW + c: li * VW + c + n]

    def wview(self, li, name):
        o, n = PK[name]
        flat = self.wpk[li].ap().rearrange("r c -> (r c)")
        return flat[o:o + n]

    def prologue(self):
        P = self.P
        P.dma("pool", self.cb_sb, self.cbf.ap(), writes=["cb"])
        P.dma("sp", self.vec_sb, self.vec.ap(), writes=["vec"])
        self.convert(0)

    def convert(self, li):
        P = self.P
        CH = 4096
        r0 = li * NPKR
        r = 0
        last = None
        while r < NPKR:
            n = min(CH, NPKR - r)
            last = P.dma("pool", self.wpk[li].ap()[r:r + n, :], self.wsrc.ap()[r0 + r:r0 + r + n, :],
                         writes=[], group=f"conv{li}")
            r += n
        P.last_w[("w", li)] = last

    def norm_tile(self, xt, h, sq, tmp, rs, li, gcol, xkey, hkey, psb, sqkey="sqa"):
        P = self.P
        ones = self.cview("ones")
        psn = self.ps[:, psb, :]
        pk = ("ps", psb)
        for c in range(4):
            P.op("act", lambda e, o=sq[:, 4 * c:4 * c + 4, :], i=xt[:, 4 * c:4 * c + 4, :]: e.activation(out=o, in_=i, func=AF.Square),
                 reads=[(xkey, 4 * c + k) for k in range(4)], writes=[(sqkey, 4 * c + k) for k in range(4)])
        for dc in range(DC):
            P.op("pe", lambda e, r=sq[:, dc, :], s=(dc == 0), t=(dc == DC - 1): e.matmul(psn, lhsT=ones, rhs=r, start=s, stop=t),
                 reads=[(sqkey, dc), "cb"], writes=[pk])
        P.op("act", lambda e: e.activation(out=tmp, in_=psn, func=AF.Sqrt, bias=EPS, scale=1.0 / D),
             reads=[pk], writes=["ntmp"])
        P.op("dve", lambda e: e.reciprocal(out=rs, in_=tmp), reads=["ntmp"], writes=["nrs"])
        for dc in range(DC):
            P.op("dve", lambda e, o=h[:, dc, :], i=xt[:, dc, :], g=self.vcol(li, gcol + dc):
                 e.scalar_tensor_tensor(out=o, in0=i, scalar=g, in1=rs, op0=ALU.mult, op1=ALU.mult),
                 reads=[(xkey, dc), "nrs", "vec"], writes=[(hkey, dc)])

    def ffn_tile(self, li, f, xt, h, sq, tmp, rs, aT, sg, wring, xkey):
        P = self.P
        NSLOT = wring.shape[1]
        self.norm_tile(xt, h, sq, tmp, rs, li, {1: 0, 2: 32}[f], xkey, "h", 0)
        wg = self.wview(li, f"wg{f}").rearrange("(g p x) -> g p x", g=11, p=128)
        wu = self.wview(li, f"wu{f}").rearrange("(g p x) -> g p x", g=11, p=128)
        wd = self.wview(li, f"wd{f}").rearrange("(g p x) -> g p x", g=16, p=128)
        wkey = ("w", li)
        it = 0
        for fg in range(11):
            sa = self.wslot_ctr % NSLOT; self.wslot_ctr += 1
            sb_ = self.wslot_ctr % NSLOT; self.wslot_ctr += 1
            P.dma("sp", wring[:, sa, :], wg[fg], reads=[wkey], writes=[("ws", sa)])
            P.dma("sp", wring[:, sb_, :], wu[fg], reads=[wkey], writes=[("ws", sb_)])
            wa = wring[:, sa, :].rearrange("p (c x) -> p c x", c=DC)
            wb = wring[:, sb_, :].rearrange("p (c x) -> p c x", c=DC)
            for j in range(4):
                fc = fg * 4 + j
                bg = 1 + (it % 2)
                bu = 3 + (it % 2)
                sgs = it % 2
                it += 1
                psg = self.ps[:, bg, :]
                psu = self.ps[:, bu, :]
                for dc in range(DC):
                    P.op("pe", lambda e, o=psg, w=wa[:, dc, j * 128:(j + 1) * 128], r=h[:, dc, :], s=(dc == 0), t=(dc == DC - 1):
                         e.matmul(o, lhsT=w, rhs=r, start=s, stop=t),
                         reads=[("ws", sa), ("h", dc)], writes=[("ps", bg)])
                for dc in range(DC):
                    P.op("pe", lambda e, o=psu, w=wb[:, dc, j * 128:(j + 1) * 128], r=h[:, dc, :], s=(dc == 0), t=(dc == DC - 1):
                         e.matmul(o, lhsT=w, rhs=r, start=s, stop=t),
                         reads=[("ws", sb_), ("h", dc)], writes=[("ps", bu)])
                P.op("act", lambda e, o=sg[:, sgs, :], i=psg: e.activation(out=o, in_=i, func=AF.Silu),
                     reads=[("ps", bg)], writes=[("sg", sgs)])
                P.op("dve", lambda e, o=aT[:, fc, :], a=psu, b=sg[:, sgs, :]: e.tensor_tensor(out=o, in0=a, in1=b, op=ALU.mult),
                     reads=[("ps", bu), ("sg", sgs)], writes=[("aT", fc)])
        for dco in range(DC):
            s = self.wslot_ctr % NSLOT; self.wslot_ctr += 1
            P.dma("sp", wring[:, s, 0:FC * 128], wd[dco], reads=[wkey], writes=[("ws", s)])
            w = wring[:, s, 0:FC * 128].rearrange("p (c x) -> p c x", c=FC)
            bd = 5 + (dco % 2)
            psd = self.ps[:, bd, :]
            for fc in range(FC):
                P.op("pe", lambda e, o=psd, ww=w[:, fc, :], r=aT[:, fc, :], s_=(fc == 0), t_=(fc == FC - 1):
                     e.matmul(o, lhsT=ww, rhs=r, start=s_, stop=t_),
                     reads=[("ws", s), ("aT", fc)], writes=[("ps", bd)])
            P.op("dve", lambda e, o=xt[:, dco, :], a=psd: e.scalar_tensor_tensor(out=o, in0=a, scalar=0.5, in1=o, op0=ALU.mult, op1=ALU.add),
                 reads=[("ps", bd)], writes=[(xkey, dco)])

    def ffn_phase(self, s, jobs, src_is_input):
        P = self.P
        A = self.A
        P.barrier()
        A.reset(self.base_mark)
        xt = A.take(DC * TT * 4, F32, "p (c t) -> p c t", c=DC)
        h = A.take(DC * TT * 2, BF16, "p (c t) -> p c t", c=DC)
        sq = A.take(DC * TT * 2, BF16, "p (c t) -> p c t", c=DC)
        tmp = A.take(TT * 4, F32)
        rs = A.take(TT * 4, F32)
        aT = A.take(FC * TT * 2, BF16, "p (c t) -> p c t", c=FC)
        sg = A.take(2 * TT * 2, BF16, "p (c t) -> p c t", c=2)
        NSLOT = 4
        wring = A.take(NSLOT * 16384, BF16, "p (s x) -> p s x", s=NSLOT)
        for tt in range(NTT):
            src = self.xin if src_is_input else self.xs
            P.dma("sp", xt, src.ap()[s, :, :, tt * TT:(tt + 1) * TT].rearrange("c p t -> p c t"),
                  reads=[("X", s, tt)], writes=[("xt", dc) for dc in range(DC)])
            for (li, f) in jobs:
                self.ffn_tile(li, f, xt, h, sq, tmp, rs, aT, sg, wring, "xt")
            P.dma("sp", self.xs.ap()[s, :, :, tt * TT:(tt + 1) * TT].rearrange("c p t -> p c t"), xt,
                  reads=[("xt", dc) for dc in range(DC)], writes=[("X", s, tt)])

    def build(self):
        P = self.P
        self.prologue()
        L = self.L
        mode = self.dbg.get("mode", "full")
        for s in range(self.nseq):
            first = True
            for li in range(L):
                jobs = []
                if li > 0:
                    jobs.append((li - 1, 2))
                jobs.append((li, 1))
                if mode in ("full", "ffn"):
                    self.ffn_phase(s, jobs, src_is_input=first)
                    first = False
                if s == 0 and li + 1 < L:
                    self.convert(li + 1)
                if mode in ("full", "mixer"):
                    self.mixer_phase(s, li, src_is_input=first)
                    first = False
            if mode in ("full", "ffn"):
                self.ffn_phase(s, [(L - 1, 2)], src_is_input=False)
        keys = [("X", s, tt) for s in range(self.nseq) for tt in range(NTT)]
        P.op("sp", lambda e: e.nop(), reads=keys)
        from contextlib import ExitStack
        with ExitStack() as es:
            es.enter_context(self.nc.allow_low_precision("bf16 matmul operands by design; accumulation is fp32"))
            block = es.enter_context(self.nc.Block())
            P.emit(block, es)
        return self.nc

    def mixer_phase(self, s, li, src_is_input):
        P, A = self.P, self.A
        P.barrier()
        A.reset(self.base_mark)
        M = {}
        M["xt"] = A.take(DC * TT * 4, F32, "p (c t) -> p c t", c=DC)
        M["h"] = A.take(DC * TT * 2, BF16, "p (c t) -> p c t", c=DC)
        M["tmp"] = A.take(TT * 4, F32)
        M["rs"] = A.take(TT * 4, F32)
        cm0 = A.mark()
        M["cfull"] = A.take(T * 4, F32)
        M["src"] = self.xin if src_is_input else self.xs
        cm = A.mark()
        if self.dbg.get("skip1") is None:
            self.pass1a(s, li, M)
            P.barrier()
            A.reset(cm)
            self.pass1b(s, li, M)
        if self.dbg.get("only1"):
            return
        P.barrier()
        A.reset(cm0)
        self.pass2(s, li, M)

    def load_x(self, M, s, tt):
        self.P.dma("sp", M["xt"], M["src"].ap()[s, :, :, tt * TT:(tt + 1) * TT].rearrange("c p t -> p c t"),
                   reads=[("X", s, tt)], writes=[("xt", dc) for dc in range(DC)])

    def head_norm(self, ps_in, pskey, n, gain, W, out_bf, outkey, rope=None, sumbank=3, rotbank=4):
        st = self.head_norm_B(ps_in, pskey, n, gain, W, out_bf, outkey, rope, sumbank)
        self.head_norm_C(st, rotbank)

    def head_norm_B(self, ps_in, pskey, n, gain, W, out_bf, outkey, rope=None, sumbank=3):
        P = self.P
        ones = self.cview("ones")
        a = self.hn_ctr % 2
        self.hn_ctr += 1
        hsq = W["hsq"][:, a, 0:n]
        pss = self.ps[:, sumbank, 0:n]
        P.op("act", lambda e: e.activation(out=hsq, in_=ps_in, func=AF.Square), reads=[pskey], writes=[("hsq", a)])
        P.op("pe", lambda e: e.matmul(pss, lhsT=ones, rhs=hsq, start=True, stop=True),
             reads=[("hsq", a), "cb"], writes=[("ps", sumbank)])
        hl = W["hl"][:, 0:n]
        hr = W["hr"][:, 0:n]
        P.op("act", lambda e: e.activation(out=hl, in_=pss, func=AF.Ln, bias=EPS, scale=1.0 / HD),
             reads=[("ps", sumbank)], writes=["hl"])
        P.op("act", lambda e: e.activation(out=hr, in_=hl, func=AF.Exp, scale=-0.5), reads=["hl"], writes=["hr"])
        if rope is None:
            P.op("dve", lambda e: e.scalar_tensor_tensor(out=out_bf, in0=ps_in, scalar=gain, in1=hr, op0=ALU.mult, op1=ALU.mult),
                 reads=[pskey, "hr", "vec"], writes=[outkey])
            return None
        kn = W["kn"][:, a, 0:n]
        P.op("dve", lambda e: e.scalar_tensor_tensor(out=kn, in0=ps_in, scalar=gain, in1=hr, op0=ALU.mult, op1=ALU.mult),
             reads=[pskey, "hr", "vec"], writes=[("kn", a)])
        knb = W["knb"][0:32, a, 0:n]
        P.op("pool", lambda e: e.tensor_copy(out=knb, in_=kn[0:32, :]), reads=[("kn", a)], writes=[("knb", a)])
        return (a, n, kn, knb, rope, out_bf, outkey, W)

    def head_norm_C(self, st, rotbank=4):
        if st is None:
            return
        P = self.P
        a, n, kn, knb, rope, out_bf, outkey, W = st
        C, S, rkey = rope
        psr = self.ps[0:32, rotbank, 0:n]
        rt = self.cview("rt", 32)
        P.op("pe", lambda e: e.matmul(psr, lhsT=rt, rhs=knb, start=True, stop=True), reads=[("knb", a), "cb"], writes=[("ps", rotbank)])
        t1 = W["t1"][0:32, 0:n]
        t2 = W["t2"][0:32, 0:n]
        P.op("dve", lambda e: e.tensor_tensor(out=t1, in0=psr, in1=S, op=ALU.mult), reads=[("ps", rotbank), rkey], writes=["t1"])
        P.op("pool", lambda e: e.tensor_tensor(out=t2, in0=kn[0:32, :], in1=C, op=ALU.mult), reads=[("kn", a), rkey], writes=["t2"])
        P.op("pool", lambda e: e.tensor_tensor(out=kn[0:32, :], in0=t1, in1=t2, op=ALU.add), reads=["t1", "t2"], writes=[("kn", a)])
        P.op("act", lambda e: e.activation(out=out_bf, in_=kn, func=AF.Copy), reads=[("kn", a)], writes=[outkey])

    def head_pipeline(self, heads):
        n = len(heads)
        stB = [None] * n
        res = [None] * n
        for step in range(n + 2):
            if step < n:
                res[step] = heads[step]["A"]()
            i = step - 1
            if 0 <= i < n:
                hd = heads[i]
                if hd.get("B") is not None:
                    stB[i] = self.head_norm_B(res[i][0], res[i][1], **hd["B"])
                elif hd.get("raw") is not None:
                    hd["raw"](res[i][0], res[i][1])
            i = step - 2
            if 0 <= i < n:
                self.head_norm_C(stB[i])
                if heads[i].get("post") is not None:
                    heads[i]["post"]()

    def head_temps(self, A):
        W = {}
        W["hsq"] = A.take(2 * TT * 2, BF16, "p (c t) -> p c t", c=2)
        W["hl"] = A.take(TT * 4, F32)
        W["hr"] = A.take(TT * 4, F32)
        W["kn"] = A.take(2 * TT * 4, F32, "p (c t) -> p c t", c=2)
        W["t1"] = A.take(TT * 4, F32)
        W["t2"] = A.take(TT * 4, F32)
        W["knb"] = A.take(2 * TT * 2, BF16, "p (c t) -> p c t", c=2)
        W["rc"] = A.take(TT * 4, F32)
        W["rsn"] = A.take(TT * 4, F32)
        return W

    def pass1a(self, s, li, M):
        P, A = self.P, self.A
        self.hn_ctr = 0
        xt, h = M["xt"], M["h"]
        NS1 = 3
        wring = A.take(NS1 * 16384, BF16, "p (s x) -> p s x", s=NS1)
        W = self.head_temps(A)
        M["sq"] = A.take(DC * TT * 2, BF16, "p (c t) -> p c t", c=DC)
        stage = A.take(4 * TT * 2, BF16, "p (c t) -> p c t", c=4)
        lf1 = A.take(TT * 4, F32)
        lf2 = A.take(TT * 4, F32)
        ones8 = A.take(TT * 4, F32)
        negb = A.take(64, F32)
        wfs = A.take(DC * 8 * 2, BF16, "p (c x) -> p c x", c=DC)
        wkey = ("w", li)
        cfull = M["cfull"]
        wfm1 = self.wview(li, "wfm1").rearrange("(g p x) -> g p x", g=16, p=128)
        wtm = self.wview(li, "wtm").rearrange("(g p x) -> g p x", g=3, p=128)
        P.dma("sp", wfs, self.wview(li, "wf").rearrange("(p c x) -> p c x", p=128, c=DC), reads=[wkey], writes=["wfs"])
        P.op("dve", lambda e: e.memset(ones8, 1.0), writes=["ones8"])
        P.op("dve", lambda e: e.tensor_scalar(out=negb[0:8, 0:1], in0=self.vcol(li, 70, rows=8), scalar1=-1.0, scalar2=None, op0=ALU.mult),
             reads=["vec"], writes=["negb"])
        sctr = 0
        stg = 0
        for tt in range(NTT):
            tsl = slice(tt * TT, (tt + 1) * TT)
            self.load_x(M, s, tt)
            self.norm_tile(xt, h, M["sq"], M["tmp"], M["rs"], li, 16, "xt", "h", 0)
            P.dma("sp", W["rc"][0:32, :], self.ropec.ap()[:, tsl], writes=["rope"])
            P.dma("sp", W["rsn"][0:32, :], self.ropes.ap()[:, tsl], writes=["rope"])
            heads = []
            PB = (1, 2, 6, 7)
            for cc in range(16):
                def A_(cc=cc):
                    nonlocal sctr
                    sl = sctr % NS1; sctr += 1
                    P.dma("sp", wring[:, sl, 0:2048], wfm1[cc], reads=[wkey], writes=[("ws", sl)])
                    w = wring[:, sl, 0:2048].rearrange("p (c x) -> p c x", c=DC)
                    pb = PB[cc % 4]
                    psp = self.ps[:, pb, :]
                    for dc in range(DC):
                        P.op("pe", lambda e, o=psp, ww=w[:, dc, :], r=h[:, dc, :], s_=(dc == 0), t_=(dc == DC - 1):
                             e.matmul(o, lhsT=ww, rhs=r, start=s_, stop=t_),
                             reads=[("ws", sl), ("h", dc)], writes=[("ps", pb)])
                    return psp, ("ps", pb)
                st = stg % 4; stg += 1
                so = stage[:, st, :]
                hd = dict(A=A_)
                if cc < 4:
                    hd["raw"] = (lambda psp, pk, so=so, st=st: P.op("dve", lambda e: e.tensor_copy(out=so, in_=psp), reads=[pk], writes=[("stage", st)]))
                elif cc < 8:
                    hd["B"] = dict(n=TT, gain=self.vcol(li, 49 + (1 if cc < 6 else 2)), W=W, out_bf=so, outkey=("stage", st),
                                   rope=(W["rc"][0:32, :], W["rsn"][0:32, :], "rope"))
                else:
                    hd["B"] = dict(n=TT, gain=self.vcol(li, 53), W=W, out_bf=so, outkey=("stage", st))
                hd["post"] = (lambda cc=cc, so=so, st=st: P.dma("sp", self.s_fm.ap()[cc, :, tsl], so, reads=[("stage", st)], writes=[("s_fm", cc)]))
                heads.append(hd)
            self.head_pipeline(heads)
            psf = self.ps[0:8, 5, :]
            for dc in range(DC):
                P.op("pe", lambda e, ww=wfs[:, dc, :], r=h[:, dc, :], s_=(dc == 0), t_=(dc == DC - 1):
                     e.matmul(psf, lhsT=ww, rhs=r, start=s_, stop=t_), reads=["wfs", ("h", dc)], writes=[("ps", 5)])
            P.op("act", lambda e: e.activation(out=lf1[0:8, :], in_=psf, func=AF.Exp, bias=negb[0:8, 0:1], scale=-1.0),
                 reads=[("ps", 5), "negb"], writes=["lf1"])
            P.op("act", lambda e: e.activation(out=lf2[0:8, :], in_=lf1[0:8, :], func=AF.Ln, bias=1.0, scale=1.0),
                 reads=["lf1"], writes=["lf2"])
            P.op("dve", lambda e: e.tensor_scalar(out=lf1[0:8, :], in0=lf2[0:8, :], scalar1=-1.0 / SCALE, scalar2=None, op0=ALU.mult),
                 reads=["lf2"], writes=["lf1"])
            init = 0.0 if tt == 0 else cfull[0:8, tt * TT - 1:tt * TT]
            P.op("dve", lambda e, o=cfull[0:8, tsl], i=init: e.tensor_tensor_scan(out=o, data0=ones8[0:8, :], data1=lf1[0:8, :],
                                                                                   initial=i, op0=ALU.mult, op1=ALU.add),
                 reads=["lf1", "ones8", "cfull"], writes=["cfull"])
            n2 = 0
            for cg in range(3):
                sl = sctr % NS1; sctr += 1
                P.dma("sp", wring[:, sl, :], wtm[cg], reads=[wkey], writes=[("ws", sl)])
                w = wring[:, sl, :].rearrange("p (c x) -> p c x", c=DC)
                for tk in range(4):
                    pb = 6 + n2 % 2; n2 += 1
                    psp = self.ps[:, pb, :]
                    for dc in range(DC):
                        P.op("pe", lambda e, o=psp, l_=h[:, dc, tk * 128:(tk + 1) * 128], r=w[:, dc, :], s_=(dc == 0), t_=(dc == DC - 1):
                             e.matmul(o, lhsT=l_, rhs=r, start=s_, stop=t_),
                             reads=[("ws", sl), ("h", dc)], writes=[("ps", pb)])
                    st = stg % 4; stg += 1
                    so = stage[:, st, :]
                    if n2 % 2:
                        P.op("act", lambda e, o=so, i=psp: e.activation(out=o, in_=i, func=AF.Copy), reads=[("ps", pb)], writes=[("stage", st)])
                    else:
                        P.op("dve", lambda e, o=so, i=psp: e.tensor_copy(out=o, in_=i), reads=[("ps", pb)], writes=[("stage", st)])
                    kt = tt * 4 + tk
                    P.dma("sp", self.s_tm.ap()[cg * 4:(cg + 1) * 4, :, kt, :].rearrange("g p d -> p g d"),
                          so.rearrange("p (g d) -> p g d", g=4), reads=[("stage", st)], writes=[("s_tm", cg)])

    def pass1b(self, s, li, M):
        P, A = self.P, self.A
        wkey = ("w", li)
        W = self.head_temps(A)
        kcraw = A.take(4 * T * 2, BF16, "p (c t) -> p c t", c=4)
        cw1 = A.take(2 * 32 * 256 * 2, BF16, "p (k l j) -> p k l j", k=2, l=32)
        cw2 = A.take(2 * 2 * 128 * 2, BF16, "p (k c x) -> p k c x", k=2, c=2)
        posb = A.take(64 * 2, BF16, "p (k l) -> p k l", k=2)
        hid = A.take(2 * 128 * 2, BF16, "p (c x) -> p c x", c=2)
        bias = A.take(16, F32)
        stage = A.take(2 * 128 * 2, BF16, "p (c x) -> p c x", c=2)
        rkc = A.take(2 * 128 * 4, F32, "p (c x) -> p c x", c=2)
        c3 = A.take(3 * T * 2, BF16, "p (c t) -> p c t", c=3)
        n3 = A.take(3 * T * 2, BF16, "p (c t) -> p c t", c=3)
        o3 = A.take(3 * T * 2, BF16, "p (c t) -> p c t", c=3)
        r1 = A.take(T * 4, F32)
        cfull = M["cfull"]
        P.dma("sp", kcraw, self.s_fm.ap()[0:4, :, :].rearrange("c p t -> p c t"), reads=[("s_fm", c) for c in range(4)], writes=["kcraw"])
        P.dma("sp", cw1, self.wview(li, "cw1").rearrange("(k p l j) -> p k l j", k=2, p=128, l=32), reads=[wkey], writes=["cw1"])
        P.dma("sp", cw2, self.wview(li, "cw2").rearrange("(k p c x) -> p k c x", k=2, p=128, c=2), reads=[wkey], writes=["cw2"])
        P.dma("sp", posb, self.wview(li, "pos").rearrange("(p k l) -> p k l", p=128, k=2), reads=[wkey], writes=["posb"])
        P.dma("sp", rkc[0:32, :, :], self.ropekc.ap(), writes=["rkc"])
        cf = cfull[0:8, :]
        P.op("dve", lambda e: e.tensor_copy(out=c3[0:8, 0, :], in_=cf), reads=["cfull"], writes=["c3"])
        P.op("dve", lambda e: e.tensor_tensor(out=r1[0:8, :], in0=cf, in1=c3[0:8, 0, :], op=ALU.subtract), reads=["c3", "cfull"], writes=["r1"])
        P.op("dve", lambda e: e.tensor_copy(out=c3[0:8, 1, :], in_=r1[0:8, :]), reads=["r1"], writes=["c3"])
        P.op("dve", lambda e: e.tensor_tensor(out=r1[0:8, :], in0=r1[0:8, :], in1=c3[0:8, 1, :], op=ALU.subtract), reads=["c3"], writes=["r1"])
        P.op("dve", lambda e: e.tensor_copy(out=c3[0:8, 2, :], in_=r1[0:8, :]), reads=["r1"], writes=["c3"])
        P.op("dve", lambda e: e.tensor_scalar(out=n3[0:8, :, :], in0=c3[0:8, :, :], scalar1=-1.0, scalar2=None, op0=ALU.mult), reads=["c3"], writes=["n3"])
        P.op("pool", lambda e: e.memset(o3[0:8, :, :], 1.0), writes=["o3"])
        ab = self.s_ab.ap()
        P.dma("sp", ab[:, 0, 0:3, :], o3[0:8, :, :], reads=["o3"], writes=["s_ab"])
        P.dma("sp", ab[:, 0, 3:6, :], c3[0:8, :, :], reads=["c3"], writes=["s_ab"])
        P.dma("sp", ab[:, 1, 0:3, :], n3[0:8, :, :], reads=["n3"], writes=["s_ab"])
        P.dma("sp", ab[:, 1, 3:6, :], o3[0:8, :, :], reads=["o3"], writes=["s_ab"])
        self.hn_ctr = 0
        nq = 0
        for kv in range(2):
            for jc in range(2):
                for l in range(32):
                    P.op("pe", lambda e, o=self.ps[:, 0, kv * 2 + jc:kv * 2 + jc + 1], ww=cw1[:, kv, l, jc * 128:(jc + 1) * 128], r=posb[:, kv, l:l + 1],
                         s_=(l == 0), t_=(l == 31): e.matmul(o, lhsT=ww, rhs=r, start=s_, stop=t_),
                         reads=["cw1", "posb"], writes=[("ps", 0)])
            P.op("dve", lambda e, o=bias[:, kv * 2:kv * 2 + 2], i=self.ps[:, 0, kv * 2:kv * 2 + 2]: e.tensor_copy(out=o, in_=i),
                 reads=[("ps", 0)], writes=["cbias"])
            for kvh in range(2):
                raw = kcraw[:, kv * 2 + kvh, :]
                for jc in range(2):
                    pb = 1 + jc
                    psh = self.ps[:, pb, 0:NCMP]
                    for l in range(32):
                        P.op("pe", lambda e, o=psh, ww=cw1[:, kv, l, jc * 128:(jc + 1) * 128], r=raw[:, l:l + 16 * (NCMP - 1) + 1:16],
                             s_=(l == 0), t_=(l == 31): e.matmul(o, lhsT=ww, rhs=r, start=s_, stop=t_),
                             reads=["cw1", "kcraw"], writes=[("ps", pb)])
                    P.op("act", lambda e, o=hid[:, jc, 0:NCMP], i=psh, b=bias[:, kv * 2 + jc:kv * 2 + jc + 1]:
                         e.activation(out=o, in_=i, func=AF.Gelu_apprx_tanh, bias=b),
                         reads=[("ps", pb), "cbias"], writes=[("hid", jc)])
                st = nq % 2; nq += 1
                if kv == 0:
                    psk = self.ps[:, 5, 0:NCMP]
                    for jc in range(2):
                        P.op("pe", lambda e, ww=cw2[:, 0, jc, :], r=hid[:, jc, 0:NCMP], s_=(jc == 0), t_=(jc == 1):
                             e.matmul(psk, lhsT=ww, rhs=r, start=s_, stop=t_), reads=["cw2", ("hid", jc)], writes=[("ps", 5)])
                    self.head_norm(psk, ("ps", 5), NCMP, self.vcol(li, 49), W, stage[:, st, 0:NCMP], ("stage", st),
                                   rope=(rkc[0:32, 0, 0:NCMP], rkc[0:32, 1, 0:NCMP], "rkc"))
                    P.dma("sp", self.s_kc.ap()[kvh, :, 0:NCMP], stage[:, st, 0:NCMP], reads=[("stage", st)], writes=["s_kc"])
                else:
                    psv = self.ps[0:NCMP, 6, 0:128]
                    for jc in range(2):
                        P.op("pe", lambda e, l_=hid[:, jc, 0:NCMP], r=cw2[:, 1, jc, :], s_=(jc == 0), t_=(jc == 1):
                             e.matmul(psv, lhsT=l_, rhs=r, start=s_, stop=t_), reads=["cw2", ("hid", jc)], writes=[("ps", 6)])
                    P.op("dve", lambda e, o=stage[0:NCMP, st, :]: e.tensor_copy(out=o, in_=psv), reads=[("ps", 6)], writes=[("stage", st)])
                    P.dma("sp", self.s_vc.ap()[kvh, 0:NCMP, :], stage[0:NCMP, st, :], reads=[("stage", st)], writes=["s_vc"])

    def attn_multi(self, jobs):
        P = self.P
        ones = self.cview("ones")
        flat = [(ji, ti) for ji, job in enumerate(jobs) for ti in range(len(job["tiles"]))]

        def emit_qk(ji, ti):
            job = jobs[ji]
            t = job["tiles"][ti]
            sb = self.sctr % 2; self.sctr += 1
            nk = t["nk"]
            pss = self.ps[0:nk, sb, :]
            mm = [(t["k"], job["q"], list(t["kkeys"]) + [job["qkey"]])] + list(t["extras"])
            for m, (l_, r_, keys) in enumerate(mm):
                P.op("pe", lambda e, o=pss, l_=l_, r_=r_, s_=(m == 0), t_=(m == len(mm) - 1): e.matmul(o, lhsT=l_, rhs=r_, start=s_, stop=t_),
                     reads=list(keys) + ["cb"], writes=[("ps", sb)])
            return pss, sb, nk

        def emit_rest(ji, ti, info):
            job = jobs[ji]
            t = job["tiles"][ti]
            n = len(job["tiles"])
            pss, sb, nk = info
            lb, ob = job["lo"]
            p = self.pctr % 3; self.pctr += 1
            ptile = self.pt[0:nk, p, :]
            P.op("act", lambda e, o=ptile, i_=pss: e.activation(out=o, in_=i_, func=AF.Exp, scale=SCALE),
                 reads=[("ps", sb)], writes=[("pt", p)])
            P.op("pe", lambda e, l_=ones[0:nk, :], r_=ptile, s_=(ti == 0), t_=(ti == n - 1): e.matmul(self.ps[:, lb, :], lhsT=l_, rhs=r_, start=s_, stop=t_),
                 reads=[("pt", p), "cb"], writes=[("ps", lb)])
            P.op("pe", lambda e, l_=t["v"], r_=ptile, s_=(ti == 0), t_=(ti == n - 1): e.matmul(self.ps[:, ob, :], lhsT=l_, rhs=r_, start=s_, stop=t_),
                 reads=[("pt", p)] + list(t["vkeys"]), writes=[("ps", ob)])
            if ti == n - 1 and job.get("fin") is not None:
                job["fin"](ptile, ("pt", p))

        pending = None
        for (ji, ti) in flat:
            if ti == 0 and jobs[ji].get("pre") is not None:
                jobs[ji]["pre"]()
            cur = emit_qk(ji, ti)
            if pending is not None:
                emit_rest(*pending)
            pending = (ji, ti, cur)
        if pending is not None:
            emit_rest(*pending)

    def pass2(self, s, li, M):
        P, A = self.P, self.A
        self.hn_ctr = 0
        self.sctr = 0
        self.pctr = 0
        xt, h = M["xt"], M["h"]
        o_sb = h
        wkey = ("w", li)
        W = self.head_temps(A)
        NS2 = 4
        wring = A.take(NS2 * 4096, BF16, "p (s x) -> p s x", s=NS2)
        qall = A.take(16 * TT * 2, BF16, "p (c t) -> p c t", c=16)
        gsb = A.take(TT * 2, BF16)
        gtmp = A.take(TT * 4, F32)
        wgs = A.take(DC * 24 * 2, BF16, "p (c x) -> p c x", c=DC)
        NKV = 6
        kvr = A.take(NKV * 4096, BF16, "p (s x) -> p s x", s=NKV)
        abA = A.take(2 * TT * 2, BF16, "p (c t) -> p c t", c=2)
        kcs = A.take(2 * 2 * 128 * 2, BF16, "p (a b x) -> p a b x", a=2, b=2)
        self.pt = A.take(3 * TT * 2, BF16, "p (c t) -> p c t", c=3)
        acc = A.take(4 * TT * 4, F32, "p (c t) -> p c t", c=4)
        rl = A.take(2 * TT * 4, F32, "p (c t) -> p c t", c=2)
        wg = A.take(TT * 4, F32)
        of = A.take(2 * TT * 4, F32, "p (c t) -> p c t", c=2)
        osq = A.take(2 * TT * 2, BF16, "p (c t) -> p c t", c=2)
        phat = A.take(2 * TT * 2, BF16, "p (c t) -> p c t", c=2)
        score = A.take(4 * 32 * 4, F32, "p (c x) -> p c x", c=4)
        mx8 = A.take(4 * 8 * 4, F32, "p (c x) -> p c x", c=4)
        selb = A.take(4 * 32 * 2, BF16, "p (c x) -> p c x", c=4)
        selbT = A.take(TT * 2, BF16)
        selct = A.take(2 * 4 * 32 * 4, F32, "p (a c x) -> p a c x", a=2, c=4)
        ssq = A.take(2 * TT * 4, F32, "p (c t) -> p c t", c=2)
        rstd = A.take(2 * TT * 4, F32, "p (c t) -> p c t", c=2)
        otmp = A.take(2 * TT * 4, F32, "p (c t) -> p c t", c=2)
        ident = self.cview("ident")
        ones = self.cview("ones")
        ovl = self.cview("ovl")
        cbv = self.cview("cb").rearrange("p (k n) -> p k n", k=4)
        wbv = self.cview("wb").rearrange("p (k n) -> p k n", k=4)
        cmask = self.cview("cmask")
        exv = self.cview("ex", 32).rearrange("p (k n) -> p k n", k=16)
        eselv = self.cview("esel", 32).rearrange("p (k n) -> p k n", k=24)
        wfm2 = self.wview(li, "wfm2").rearrange("(g p x) -> g p x", g=16, p=128)
        wout = self.wview(li, "wout").rearrange("(g p x) -> g p x", g=16, p=128)
        P.dma("sp", wgs, self.wview(li, "wgt").rearrange("(p c x) -> p c x", p=128, c=DC), reads=[wkey], writes=["wgs"])
        P.dma("sp", kcs[:, :, 0, :], self.s_kc.ap().rearrange("k p x -> p k x"), reads=["s_kc"], writes=["kcs"])
        P.dma("sp", kcs[:, :, 1, :], self.s_vc.ap().rearrange("k p x -> p k x"), reads=["s_vc"], writes=["kcs"])
        sctr = 0
        kvc = 0
        rctr = 0
        yctr = 0
        actr = 0
        zctr = 0
        loc = 0
        for j in range(NTT):
            tsl = slice(j * TT, (j + 1) * TT)
            nkt = 4 * (j + 1)
            nk = nkt * 128
            self.load_x(M, s, j)
            self.norm_tile(xt, h, qall, M["tmp"], M["rs"], li, 16, "xt", "h", 0, sqkey="qa")
            P.dma("sp", W["rc"][0:32, :], self.ropec.ap()[:, tsl], writes=["rope"])
            P.dma("sp", W["rsn"][0:32, :], self.ropes.ap()[:, tsl], writes=["rope"])
            P.dma("sp", selct, self.selc.ap()[:, :, 4 * j:4 * j + 4, :], writes=["selct"])
            heads = []
            PB = (1, 2, 5, 6)
            for cc in range(16):
                def A_(cc=cc):
                    nonlocal sctr
                    sl = sctr % NS2; sctr += 1
                    P.dma("sp", wring[:, sl, :], wfm2[cc], reads=[wkey], writes=[("ws", sl)])
                    w = wring[:, sl, :].rearrange("p (c x) -> p c x", c=DC)
                    pb = PB[cc % 4]
                    psp = self.ps[:, pb, :]
                    for dc in range(DC):
                        P.op("pe", lambda e, o=psp, ww=w[:, dc, :], r=h[:, dc, :], s_=(dc == 0), t_=(dc == DC - 1):
                             e.matmul(o, lhsT=ww, rhs=r, start=s_, stop=t_),
                             reads=[("ws", sl), ("h", dc)], writes=[("ps", pb)])
                    return psp, ("ps", pb)
                if cc < 8:
                    B = dict(n=TT, gain=self.vcol(li, 48), W=W, out_bf=qall[:, cc, :], outkey=("qa", cc),
                             rope=(W["rc"][0:32, :], W["rsn"][0:32, :], "rope"))
                else:
                    B = dict(n=TT, gain=self.vcol(li, 52), W=W, out_bf=qall[:, cc, :], outkey=("qa", cc))
                heads.append(dict(A=A_, B=B))
            self.head_pipeline(heads)
            psg = self.ps[0:24, 7, :]
            for dc in range(DC):
                P.op("pe", lambda e, ww=wgs[:, dc, :], r=h[:, dc, :], s_=(dc == 0), t_=(dc == DC - 1):
                     e.matmul(psg, lhsT=ww, rhs=r, start=s_, stop=t_), reads=["wgs", ("h", dc)], writes=[("ps", 7)])
            P.op("act", lambda e: e.activation(out=gtmp[0:24, :], in_=psg, func=AF.Exp, scale=-1.0), reads=[("ps", 7)], writes=["gtmp"])
            P.op("dve", lambda e: e.tensor_scalar(out=gtmp[0:24, :], in0=gtmp[0:24, :], scalar1=1.0, scalar2=None, op0=ALU.add), reads=["gtmp"], writes=["gtmp"])
            P.op("dve", lambda e: e.reciprocal(out=gsb[0:24, :], in_=gtmp[0:24, :]), reads=["gtmp"], writes=["gsb"])
            for kvh in range(2):
                ch = []
                for _ in range(4):
                    ch.append(kvc % NKV); kvc += 1
                c_ks, c_kw, c_vs, c_vw = ch
                P.dma("sp", kvr[:, c_ks, 0:nk], self.s_fm.ap()[4 + kvh, :, 0:nk], reads=[("s_fm", 4 + kvh)], writes=[("kv", c_ks)])
                P.dma("sp", kvr[:, c_vs, 0:nk], self.s_tm.ap()[0 + kvh, :, 0:nkt, :].rearrange("p k d -> p (k d)"), reads=[("s_tm", 0)], writes=[("kv", c_vs)])
                P.dma("sp", kvr[:, c_kw, 0:nk], self.s_fm.ap()[6 + kvh, :, 0:nk], reads=[("s_fm", 6 + kvh)], writes=[("kv", c_kw)])
                P.dma("sp", kvr[:, c_vw, 0:nk], self.s_tm.ap()[2 + kvh, :, 0:nkt, :].rearrange("p k d -> p (k d)"), reads=[("s_tm", 0)], writes=[("kv", c_vw)])
                ksT = kvr[:, c_ks, :]
                kwT = kvr[:, c_kw, :]
                vs = kvr[:, c_vs, :].rearrange("p (k d) -> p k d", d=128)
                vw = kvr[:, c_vw, :].rearrange("p (k d) -> p k d", d=128)
                kcT = kcs[:, kvh, 0, 0:NCMP]
                vc = kcs[0:NCMP, kvh, 1, :]

                def fin_branch(g, b, lo, first, kvh=kvh):
                    nonlocal rctr, yctr
                    lb, ob = lo
                    x_ = rctr % 2; rctr += 1
                    rlb = rl[:, x_, :]
                    r = ((kvh * 4 + g) * 3 + b)
                    P.op("dve", lambda e: e.tensor_scalar(out=rlb, in0=self.ps[:, lb, :], scalar1=1e-30, scalar2=None, op0=ALU.max),
                         reads=[("ps", lb)], writes=[("rl", x_)])
                    P.op("dve", lambda e: e.reciprocal(out=rlb, in_=rlb), reads=[("rl", x_)], writes=[("rl", x_)])
                    P.op("pe", lambda e: e.matmul(self.ps[:, 7, :], lhsT=eselv[0:24, r, :], rhs=gsb[0:24, :], start=True, stop=True),
                         reads=["gsb", "cb"], writes=[("ps", 7)])
                    P.op("dve", lambda e: e.tensor_tensor(out=wg, in0=self.ps[:, 7, :], in1=rlb, op=ALU.mult),
                         reads=[("ps", 7), ("rl", x_)], writes=["wg"])
                    if first:
                        P.op("dve", lambda e: e.tensor_tensor(out=acc[:, g, :], in0=self.ps[:, ob, :], in1=wg, op=ALU.mult),
                             reads=[("ps", ob), "wg"], writes=[("acc", g)])
                    else:
                        y_ = yctr % 2; yctr += 1
                        P.op("dve", lambda e: e.tensor_tensor(out=otmp[:, y_, :], in0=self.ps[:, ob, :], in1=wg, op=ALU.mult),
                             reads=[("ps", ob), "wg"], writes=[("otmp", y_)])
                        P.op("pool", lambda e: e.tensor_tensor(out=acc[:, g, :], in0=acc[:, g, :], in1=otmp[:, y_, :], op=ALU.add),
                             reads=[("otmp", y_)], writes=[("acc", g)])
                    return x_

                jobs = []
                for g in range(4):
                    hq = kvh * 4 + g
                    lo = (3, 4) if loc % 2 == 0 else (5, 6); loc += 1
                    tiles = [dict(k=kcT, kkeys=["kcs"], nk=NCMP, extras=[(ident[0:NCMP, 0:NCMP], cmask[0:NCMP, tsl], [])],
                                  v=vc, vkeys=["kcs"])]

                    def fin_cmp(ptile, pkey, g=g, lo=lo):
                        nonlocal zctr
                        x_ = fin_branch(g, 0, lo, True)
                        z_ = zctr % 2; zctr += 1
                        P.op("pool", lambda e, o=phat[0:NCMP, z_, :], a=ptile, b=rl[0:NCMP, x_, :]: e.tensor_tensor(out=o, in0=a, in1=b, op=ALU.mult),
                             reads=[pkey, ("rl", x_)], writes=[("phat", z_)])
                        for tk in range(4):
                            P.op("pe", lambda e, o=self.ps[:, 2, tk * 32:(tk + 1) * 32], l_=phat[0:NCMP, z_, tk * 128:(tk + 1) * 128], r_=ovl[0:NCMP, :],
                                 s_=(g == 0 and tk == 0), t_=(g == 3): e.matmul(o, lhsT=l_, rhs=r_, start=s_, stop=t_, skip_group_check=True),
                                 reads=[("phat", z_), "cb"], writes=[("ps", 2)])
                    jobs.append(dict(q=qall[:, hq, :], qkey=("qa", hq), tiles=tiles, lo=lo, fin=fin_cmp))
                self.attn_multi(jobs)
                imp = self.ps[:, 2, 0:128].rearrange("p (c x) -> p c x", c=4)
                P.op("dve", lambda e: e.tensor_tensor(out=score, in0=imp, in1=selct[:, 0, :, :], op=ALU.mult),
                     reads=[("ps", 2), "selct"], writes=["score"])
                P.op("dve", lambda e: e.tensor_tensor(out=score, in0=score, in1=selct[:, 1, :, :], op=ALU.add),
                     reads=["selct"], writes=["score"])
                for tk in range(4):
                    P.op("dve", lambda e, o=mx8[:, tk, :], i_=score[:, tk, :]: e.max(out=o, in_=i_), reads=["score"], writes=[("mx8", tk)])
                for tk in range(4):
                    P.op("dve", lambda e, o=selb[:, tk, :], i_=score[:, tk, :], th=mx8[:, tk, 7:8]:
                         e.tensor_scalar(out=o, in0=i_, scalar1=th, scalar2=NEG, op0=ALU.is_lt, op1=ALU.mult),
                         reads=["score", ("mx8", tk)], writes=[("selb", tk)])
                for tk in range(4):
                    P.op("pe", lambda e, o=self.ps[0:32, 2, tk * 128:(tk + 1) * 128], l_=selb[:, tk, :]:
                         e.matmul(o, lhsT=l_, rhs=ident, start=True, stop=True),
                         reads=[("selb", tk), "cb"], writes=[("ps", 2)])
                P.op("dve", lambda e: e.tensor_copy(out=selbT[0:32, :], in_=self.ps[0:32, 2, :]), reads=[("ps", 2)], writes=["selbT"])
                jobs = []
                for g in range(4):
                    hq = kvh * 4 + g
                    lo = (3, 4) if loc % 2 == 0 else (5, 6); loc += 1
                    tiles = []
                    for kt in range(nkt):
                        ex = [(exv[0:32, kt, :], selbT[0:32, :], ["selbT"])]
                        if kt >= 4 * j:
                            ex.append((ident, cbv[:, kt - 4 * j, :], []))
                        tiles.append(dict(k=ksT[:, kt * 128:(kt + 1) * 128], kkeys=[("kv", c_ks)], nk=128, extras=ex,
                                          v=vs[:, kt, :], vkeys=[("kv", c_vs)]))
                    jobs.append(dict(q=qall[:, hq, :], qkey=("qa", hq), tiles=tiles, lo=lo,
                                     fin=(lambda ptile, pkey, g=g, lo=lo: fin_branch(g, 1, lo, False))))
                for g in range(4):
                    hq = kvh * 4 + g
                    lo = (3, 4) if loc % 2 == 0 else (5, 6); loc += 1
                    tiles = []
                    for kt in range(max(0, 4 * j - 4), nkt):
                        if kt >= 4 * j:
                            ex = [(ident, cbv[:, kt - 4 * j, :], [])]
                        else:
                            ex = [(ident, wbv[:, kt - (4 * j - 4), :], [])]
                        tiles.append(dict(k=kwT[:, kt * 128:(kt + 1) * 128], kkeys=[("kv", c_kw)], nk=128, extras=ex,
                                          v=vw[:, kt, :], vkeys=[("kv", c_vw)]))
                    jobs.append(dict(q=qall[:, hq, :], qkey=("qa", hq), tiles=tiles, lo=lo,
                                     fin=(lambda ptile, pkey, g=g, lo=lo: fin_branch(g, 2, lo, False))))
                self.attn_multi(jobs)
                for g in range(4):
                    hq = kvh * 4 + g
                    a_ = actr % 2; actr += 1
                    P.op("pool", lambda e, o=osq[:, a_, :], i_=acc[:, g, :]: e.tensor_tensor(out=o, in0=i_, in1=i_, op=ALU.mult),
                         reads=[("acc", g)], writes=[("osq", a_)])
                    P.op("pe", lambda e, r_=osq[:, a_, :]: e.matmul(self.ps[:, 7, :], lhsT=ones, rhs=r_, start=True, stop=True),
                         reads=[("osq", a_), "cb"], writes=[("ps", 7)])
                    if hq == 0:
                        P.op("dve", lambda e: e.tensor_copy(out=ssq[:, 0, :], in_=self.ps[:, 7, :]), reads=[("ps", 7)], writes=[("ssq", 0)])
                    else:
                        P.op("dve", lambda e: e.tensor_tensor(out=ssq[:, 0, :], in0=self.ps[:, 7, :], in1=ssq[:, 0, :], op=ALU.add),
                             reads=[("ps", 7)], writes=[("ssq", 0)])
                    P.op("dve", lambda e, o=o_sb[:, hq, :], i_=acc[:, g, :], gc=self.vcol(li, 54 + hq):
                         e.tensor_scalar(out=o, in0=i_, scalar1=gc, scalar2=None, op0=ALU.mult),
                         reads=[("acc", g), "vec"], writes=[("h", hq)])
            jobs = []
            for hf in range(8):
                ch = []
                for _ in range(3):
                    ch.append(kvc % NKV); kvc += 1
                c_k, c_v, c_b = ch
                a_ = hf % 2
                def pre_fox(hf=hf, c_k=c_k, c_v=c_v, c_b=c_b, a_=a_):
                    P.dma("sp", kvr[:, c_k, 0:nk], self.s_fm.ap()[8 + hf, :, 0:nk], reads=[("s_fm", 8 + hf)], writes=[("kv", c_k)])
                    P.dma("sp", kvr[0:6, c_b, 0:nk], self.s_ab.ap()[hf, 1, :, 0:nk], reads=["s_ab"], writes=[("kv", c_b)])
                    P.dma("sp", abA[0:6, a_, :], self.s_ab.ap()[hf, 0, :, tsl], reads=["s_ab"], writes=[("abA", a_)])
                    P.dma("sp", kvr[:, c_v, 0:nk], self.s_tm.ap()[4 + hf, :, 0:nkt, :].rearrange("p k d -> p (k d)"), reads=[("s_tm", 1), ("s_tm", 2)], writes=[("kv", c_v)])
                fk = kvr[:, c_k, :]
                fv = kvr[:, c_v, :].rearrange("p (k d) -> p k d", d=128)
                Bc = kvr[0:6, c_b, :]
                lo = (3, 4) if loc % 2 == 0 else (5, 6); loc += 1
                tiles = []
                for kt in range(nkt):
                    ex = [(Bc[:, kt * 128:(kt + 1) * 128], abA[0:6, a_, :], [("kv", c_b), ("abA", a_)])]
                    if kt >= 4 * j:
                        ex.append((ident, cbv[:, kt - 4 * j, :], []))
                    tiles.append(dict(k=fk[:, kt * 128:(kt + 1) * 128], kkeys=[("kv", c_k)], nk=128, extras=ex,
                                      v=fv[:, kt, :], vkeys=[("kv", c_v)]))

                def fin_fox(ptile, pkey, hf=hf, lo=lo):
                    nonlocal rctr, actr
                    lb, ob = lo
                    x_ = rctr % 2; rctr += 1
                    rlb = rl[:, x_, :]
                    P.op("dve", lambda e, o=rlb, i_=self.ps[:, lb, :]: e.reciprocal(out=o, in_=i_), reads=[("ps", lb)], writes=[("rl", x_)])
                    f_ = hf % 2
                    P.op("dve", lambda e, o=of[:, f_, :], a=self.ps[:, ob, :], b=rlb: e.tensor_tensor(out=o, in0=a, in1=b, op=ALU.mult),
                         reads=[("ps", ob), ("rl", x_)], writes=[("of", f_)])
                    q_ = actr % 2; actr += 1
                    P.op("pool", lambda e, o=osq[:, q_, :], i_=of[:, f_, :]: e.tensor_tensor(out=o, in0=i_, in1=i_, op=ALU.mult),
                         reads=[("of", f_)], writes=[("osq", q_)])
                    P.op("pe", lambda e, r_=osq[:, q_, :]: e.matmul(self.ps[:, 7, :], lhsT=ones, rhs=r_, start=True, stop=True),
                         reads=[("osq", q_), "cb"], writes=[("ps", 7)])
                    if hf == 0:
                        P.op("dve", lambda e: e.tensor_copy(out=ssq[:, 1, :], in_=self.ps[:, 7, :]), reads=[("ps", 7)], writes=[("ssq", 1)])
                    else:
                        P.op("dve", lambda e: e.tensor_tensor(out=ssq[:, 1, :], in0=self.ps[:, 7, :], in1=ssq[:, 1, :], op=ALU.add),
                             reads=[("ps", 7)], writes=[("ssq", 1)])
                    P.op("dve", lambda e, o=o_sb[:, 8 + hf, :], i_=of[:, f_, :], gc=self.vcol(li, 62 + hf):
                         e.tensor_scalar(out=o, in0=i_, scalar1=gc, scalar2=None, op0=ALU.mult),
                         reads=[("of", f_), "vec"], writes=[("h", 8 + hf)])
                jobs.append(dict(q=qall[:, 8 + hf, :], qkey=("qa", 8 + hf), tiles=tiles, lo=lo, fin=fin_fox, pre=pre_fox))
            self.attn_multi(jobs)
            for k in range(2):
                P.op("act", lambda e, o=rstd[:, k, :], i_=ssq[:, k, :]: e.activation(out=o, in_=i_, func=AF.Ln, bias=EPS, scale=1.0 / 1024.0),
                     reads=[("ssq", k)], writes=[("rstd", k)])
                P.op("act", lambda e, o=rstd[:, k, :]: e.activation(out=o, in_=o, func=AF.Exp, scale=-0.5),
                     reads=[("rstd", k)], writes=[("rstd", k)])
            for kc in range(16):
                P.op("dve" if kc % 2 == 0 else "pool", lambda e, o=o_sb[:, kc, :], b=rstd[:, kc // 8, :]: e.tensor_tensor(out=o, in0=o, in1=b, op=ALU.mult),
                     reads=[("rstd", kc // 8)], writes=[("h", kc)])
            self.load_x(M, s, j)
            for dco in range(DC):
                sl = sctr % NS2; sctr += 1
                P.dma("sp", wring[:, sl, :], wout[dco], reads=[wkey], writes=[("ws", sl)])
                w = wring[:, sl, :].rearrange("p (c x) -> p c x", c=16)
                bnk = dco % 2
                for kc in range(16):
                    P.op("pe", lambda e, o=self.ps[:, bnk, :], ww=w[:, kc, :], r=o_sb[:, kc, :], s_=(kc == 0), t_=(kc == 15):
                         e.matmul(o, lhsT=ww, rhs=r, start=s_, stop=t_),
                         reads=[("ws", sl), ("h", kc)], writes=[("ps", bnk)])
                P.op("dve", lambda e, o=xt[:, dco, :], a=self.ps[:, bnk, :]: e.tensor_tensor(out=o, in0=a, in1=o, op=ALU.add),
                     reads=[("ps", bnk)], writes=[("xt", dco)])
            P.dma("sp", self.xs.ap()[s, :, :, tsl].rearrange("c p t -> p c t"), xt,
                  reads=[("xt", dc) for dc in range(DC)], writes=[("X", s, j)])


def _pack_layer(w, li):
    out = np.zeros(NPK, np.float32)

    def put(name, arr):
        o, n = PK[name]
        a = np.ascontiguousarray(arr, dtype=np.float32).reshape(-1)
        assert a.size == n, (name, a.size, n)
        out[o:o + n] = a

    for f in (1, 2):
        wg = w[f"ffn{f}_w_gate"][li]
        wu = w[f"ffn{f}_w_up"][li]
        wd = w[f"ffn{f}_w_down"][li]
        put(f"wg{f}", wg.reshape(DC, 128, 11, 512).transpose(2, 1, 0, 3))
        put(f"wu{f}", wu.reshape(DC, 128, 11, 512).transpose(2, 1, 0, 3))
        put(f"wd{f}", wd.reshape(FC, 128, DC, 128).transpose(2, 1, 0, 3))
    win = w["w_in"][li]
    kv0 = 1024
    def kvcol(branch, typ, kvh):
        return kv0 + ((branch * 2 + typ) * 2 + kvh) * 128
    fq0 = 2584
    fk0 = fq0 + 1024
    fv0 = fq0 + 2048
    cols1 = ([kvcol(0, 0, 0), kvcol(0, 0, 1), kvcol(0, 1, 0), kvcol(0, 1, 1),
              kvcol(1, 0, 0), kvcol(1, 0, 1), kvcol(2, 0, 0), kvcol(2, 0, 1)]
             + [fk0 + h * 128 for h in range(8)])
    def fm(cols):
        blk = np.stack([win[:, c:c + 128] for c in cols], 0)
        return blk.reshape(len(cols), DC, 128, 128).transpose(0, 2, 1, 3)
    put("wfm1", fm(cols1))
    tmcols = np.concatenate([np.arange(kvcol(1, 1, 0), kvcol(1, 1, 0) + 256),
                             np.arange(kvcol(2, 1, 0), kvcol(2, 1, 0) + 256),
                             np.arange(fv0, fv0 + 1024)])
    wt = win[:, tmcols]
    put("wtm", wt.reshape(DC, 128, 3, 512).transpose(2, 1, 0, 3))
    cols2 = [h * 128 for h in range(8)] + [fq0 + h * 128 for h in range(8)]
    put("wfm2", fm(cols2))
    wo = w["w_out"][li]
    put("wout", wo.reshape(16, 128, DC, 128).transpose(2, 1, 0, 3))
    cw1 = w["cmp_w1"][li]
    put("cw1", cw1.reshape(2, 32, 128, 256).transpose(0, 2, 1, 3))
    cw2 = w["cmp_w2"][li]
    put("cw2", cw2.reshape(2, 2, 128, 128).transpose(0, 2, 1, 3))
    put("wf", win[:, 5656:5664].reshape(DC, 128, 8).transpose(1, 0, 2))
    put("wgt", win[:, 2560:2584].reshape(DC, 128, 24).transpose(1, 0, 2))
    pos = w["cmp_pos_emb"][li]
    put("pos", pos.transpose(2, 0, 1))
    return out


def _vec_pack(w, layers):
    v = np.zeros((128, len(layers) * VW), np.float32)
    for i, li in enumerate(layers):
        b = i * VW
        v[:, b + 0:b + 16] = w["ffn1_norm"][li].reshape(DC, 128).T
        v[:, b + 16:b + 32] = w["mix_norm"][li].reshape(DC, 128).T
        v[:, b + 32:b + 48] = w["ffn2_norm"][li].reshape(DC, 128).T
        v[:, b + 48] = w["nsa_q_norm"][li]
        v[:, b + 49:b + 52] = w["nsa_k_norm"][li].T
        v[:, b + 52] = w["fox_q_norm"][li]
        v[:, b + 53] = w["fox_k_norm"][li]
        v[:, b + 54:b + 62] = w["nsa_out_norm"][li].reshape(8, 128).T
        v[:, b + 62:b + 70] = w["fox_out_norm"][li].reshape(8, 128).T
        v[0:8, b + 70] = w["fox_forget_bias"][li]
    return v


def _consts():
    cb = np.zeros((128, NCB), np.float32)
    def put(name, arr):
        o, n = CB[name]
        cb[:arr.shape[0], o:o + n] = arr.reshape(arr.shape[0], -1)
    put("ident", np.eye(128, dtype=np.float32))
    put("ones", np.ones((128, 128), np.float32))
    rt = np.zeros((32, 32), np.float32)
    for i in range(16):
        rt[16 + i, i] = -1.0
        rt[i, 16 + i] = 1.0
    put("rt", rt)
    cstart = np.arange(NCMP) * 16
    sstart = np.arange(32) * 64
    ovl = ((cstart[:, None] < sstart[None, :] + 64) & (cstart[:, None] + 32 > sstart[None, :])).astype(np.float32)
    put("ovl", ovl)
    p = np.arange(128)[:, None]
    n = np.arange(512)[None, :]
    cbm = np.stack([np.where(128 * k + p <= n, 0.0, NEG) for k in range(4)], 1)
    wbm = np.stack([np.where(128 * k + p > n, 0.0, NEG) for k in range(4)], 1)
    put("cb", cbm.astype(np.float32))
    put("wb", wbm.astype(np.float32))
    cend = cstart + 31
    t = np.arange(T)[None, :]
    put("cmask", np.where(cend[:, None] <= t, 0.0, NEG).astype(np.float32))
    j = np.arange(32)[:, None, None]
    kt = np.arange(16)[None, :, None]
    pp = np.arange(128)[None, None, :]
    put("ex", (j == 2 * kt + pp // 64).astype(np.float32))
    r = np.arange(32)[:, None, None]
    rr = np.arange(24)[None, :, None]
    put("esel", np.broadcast_to((r == rr), (32, 24, 128)).astype(np.float32))
    inv = (np.float32(500000.0) ** (-np.arange(0, 32, 2, dtype=np.float32) / np.float32(32))).astype(np.float32)
    def tables(pos):
        ang = pos.astype(np.float32)[:, None] * inv[None, :]
        c = np.cos(ang).astype(np.float32).T
        s_ = np.sin(ang).astype(np.float32).T
        return np.concatenate([c, c], 0), np.concatenate([s_, s_], 0)
    rc, rs = tables(np.arange(T))
    kc_c, kc_s = tables(cend)
    ropekc = np.zeros((32, 2, 128), np.float32)
    ropekc[:, 0, :NCMP] = kc_c
    ropekc[:, 1, :NCMP] = kc_s
    tok = np.arange(T)
    tb = tok // 64
    jj = np.arange(32)[None, :]
    causal = jj <= tb[:, None]
    forced = (jj == 0) | (causal & (jj > tb[:, None] - 2))
    mmul = (causal & ~forced).astype(np.float32)
    badd = np.where(forced, 1e9, np.where(causal, 0.0, -1e30)).astype(np.float32)
    selc = np.stack([mmul.reshape(16, 128, 32).transpose(1, 0, 2), badd.reshape(16, 128, 32).transpose(1, 0, 2)], 1)
    return cb, rc, rs, ropekc, np.ascontiguousarray(selc)


_CACHE = {}


def _get_prog(nseq, layers_key, dbg_key=None):
    key = (nseq, layers_key, dbg_key)
    if key not in _CACHE:
        b = Builder(nseq, list(layers_key), dict(dbg_key or ()))
        _CACHE[key] = b.build()
    return _CACHE[key]


def kernel(**inputs):
    x = np.asarray(inputs["x"], np.float32)
    B = x.shape[0]
    ncores = 8
    nseq = B // ncores
    w = {k: np.asarray(v) for k, v in inputs.items() if k != "x"}
    layers = tuple(range(DEPTH))
    wsrc = np.concatenate([_pack_layer(w, li) for li in layers]).reshape(len(layers) * NPKR, 2048)
    vec = _vec_pack(w, layers)
    cb, rc, rs, ropekc, selc = _consts()
    nc = _get_prog(nseq, layers)
    in_maps = []
    for c in range(ncores):
        xc = x[c * nseq:(c + 1) * nseq]
        xT = np.ascontiguousarray(xc.transpose(0, 2, 1)).reshape(nseq, DC, 128, T)
        in_maps.append({"xin": xT, "wsrc": wsrc, "vec": vec, "cbf": cb, "ropec": rc, "ropes": rs,
                        "ropekc": ropekc, "selc": selc})
    res = run_bass_kernel_spmd(nc, in_maps, core_ids=list(range(ncores)))
    outs = []
    for c in range(ncores):
        y = res.results[c]["xout"].reshape(nseq, D, T).transpose(0, 2, 1)
        outs.append(y)
    return np.ascontiguousarray(np.concatenate(outs, 0), dtype=np.float32)
```

```python
import numpy as np
import concourse.bass as bass
import concourse.mybir as mybir
from concourse.bass_utils import run_bass_kernel_spmd

F32 = mybir.dt.float32
BF16 = mybir.dt.bfloat16
U8 = mybir.dt.uint8
AF = mybir.ActivationFunctionType
ALU = mybir.AluOpType

D = 2048
T = 2048
DEPTH = 4
HD = 128
DFF = 5632
DC = D // 128
FC = DFF // 128
TT = 512
NTT = T // TT
EPS = 1e-6
SCALE = HD ** -0.5
NEG = -32768.0
NCMP = 127
VW = 72

PK = {}
_off = 0
def _add(name, n):
    global _off
    PK[name] = (_off, n)
    _off += n
for _f in (1, 2):
    _add(f"wg{_f}", D * DFF)
    _add(f"wu{_f}", D * DFF)
    _add(f"wd{_f}", D * DFF)
_add("wfm1", 16 * 128 * 16 * 128)
_add("wtm", 3 * 128 * 16 * 512)
_add("wfm2", 16 * 128 * 16 * 128)
_add("wout", 16 * 128 * 16 * 128)
_add("cw1", 2 * 128 * 32 * 256)
_add("cw2", 2 * 128 * 2 * 128)
_add("wf", 128 * 16 * 8)
_add("wgt", 128 * 16 * 24)
_add("pos", 128 * 2 * 32)
NPK = ((_off + 2047) // 2048) * 2048
NPKR = NPK // 2048

CB = {}
_c = 0
def _addc(name, n):
    global _c
    CB[name] = (_c, n)
    _c += n
_addc("ident", 128)
_addc("ones", 128)
_addc("rt", 32)
_addc("ovl", 32)
_addc("cb", 4 * 512)
_addc("wb", 4 * 512)
_addc("cmask", 2048)
_addc("ex", 16 * 128)
_addc("esel", 24 * 128)
NCB = _c


class Prog:
    NS = 8

    def __init__(self, nc):
        self.nc = nc
        self.ops = []
        self.deps = []
        self.last_w = {}
        self.readers = {}
        self.streams = {k: [] for k in ("pe", "act", "dve", "pool", "sp")}
        self.groups = {}
        self.last_op = {k: None for k in self.streams}
        self.recent_dma = {"sp": [], "pool": [], "act": []}
        self.cur_barrier = None

    def _record(self, stream, fn, kind, reads, writes, group=None, is_barrier=False):
        i = len(self.ops)
        d = set()
        lw = self.last_w
        rd = self.readers
        for k in reads:
            w = lw.get(k)
            if w is not None:
                d.add(w)
            r = rd.get(k)
            if r is None:
                r = rd[k] = [{}, []]
            if kind == "c":
                r[0][stream] = i
            else:
                r[1].append(i)
        for k in writes:
            w = lw.get(k)
            if w is not None:
                d.add(w)
            r = rd.get(k)
            if r is not None:
                d.update(r[0].values())
                d.update(r[1])
                del rd[k]
            lw[k] = i
        if is_barrier:
            for st, li in self.last_op.items():
                if li is not None:
                    d.add(li)
            for q, lst in self.recent_dma.items():
                d.update(lst)
        elif self.cur_barrier is not None:
            d.add(self.cur_barrier)
        d.discard(i)
        self.ops.append((stream, fn, kind, group))
        self.deps.append(d)
        self.streams[stream].append(i)
        if kind == "c":
            self.last_op[stream] = i
        elif group is None:
            lst = self.recent_dma[stream]
            lst.append(i)
            if len(lst) > self.NS:
                lst.pop(0)
        if group is not None:
            self.groups[group] = self.groups.get(group, 0) + 1
        if is_barrier:
            self.cur_barrier = i
        return i

    def op(self, stream, fn, reads=(), writes=()):
        return self._record(stream, fn, "c", reads, writes)

    def dma(self, q, out, in_, reads=(), writes=(), group=None):
        return self._record(q, lambda e, o=out, i=in_: e.dma_start(out=o, in_=i), "d",
                            reads, writes, group)

    def barrier(self):
        self._record("sp", lambda e: e.nop(), "c", [], [], is_barrier=True)

    def emit(self, block, sems_ctx):
        nc = self.nc
        ops = self.ops
        n = len(ops)
        dsem = {}
        qcount = {"sp": 0, "pool": 0, "act": 0}
        qhist = {"sp": [], "pool": [], "act": []}
        extra = {}
        for i in range(n):
            st, fn, kind, group = ops[i]
            if kind != "d":
                continue
            if group is not None:
                dsem[i] = (("g", group), 16 * self.groups[group])
            else:
                c = qcount[st]
                dsem[i] = (("s", st, c % self.NS), 16 * (c // self.NS + 1))
                if c >= self.NS:
                    extra[i] = qhist[st][c - self.NS]
                qhist[st].append(i)
                qcount[st] = c + 1
        sig = [False] * n
        for i in range(n):
            st = ops[i][0]
            for d in self.deps[i]:
                sd, _, kd, _ = ops[d]
                if kd == "c" and not (sd == "pe" and st == "pe"):
                    sig[d] = True
        cnt = {k: 0 for k in self.streams}
        sval = [0] * n
        for i in range(n):
            st, fn, kind, group = ops[i]
            if kind == "c" and sig[i]:
                cnt[st] += 1
                sval[i] = cnt[st]
        semkeys = set(("e", k) for k in self.streams)
        for i in dsem:
            semkeys.add(dsem[i][0])
        sem = {}
        for k in sorted(semkeys, key=str):
            sem[k] = sems_ctx.enter_context(nc.semaphore("s_" + "_".join(str(x) for x in k)))
        waits = [None] * n
        waited = {k: {} for k in self.streams}
        for st in self.streams:
            wd = waited[st]
            for i in self.streams[st]:
                need = {}
                dl = self.deps[i]
                if i in extra:
                    dl = set(dl)
                    dl.add(extra[i])
                for d in dl:
                    sd, _, kd, _ = ops[d]
                    if kd == "d":
                        k, v = dsem[d]
                    else:
                        if sd == "pe" and st == "pe":
                            continue
                        k, v = ("e", sd), sval[d]
                    if need.get(k, 0) < v:
                        need[k] = v
                w = []
                for k, v in need.items():
                    if wd.get(k, 0) < v:
                        wd[k] = v
                        w.append((sem[k], v))
                waits[i] = w
        self.n_waits = sum(len(w) for w in waits)
        self.n_sig = sum(sig)

        def run_stream(st, eng):
            for i in self.streams[st]:
                _, fn, kind, group = ops[i]
                for s, v in waits[i]:
                    eng.wait_ge(s, v)
                ins = fn(eng)
                if kind == "d":
                    ins.then_inc(sem[dsem[i][0]], 16)
                elif sig[i]:
                    ins.then_inc(sem[("e", st)], 1)

        @block.tensor
        def _(e):
            run_stream("pe", e)

        @block.scalar
        def _(e):
            run_stream("act", e)

        @block.vector
        def _(e):
            run_stream("dve", e)

        @block.gpsimd
        def _(e):
            run_stream("pool", e)

        @block.sync
        def _(e):
            run_stream("sp", e)


class Arena:
    def __init__(self, ap, size):
        self.ap = ap
        self.size = size
        self.off = 0

    def take(self, nbytes, dtype, pattern=None, **kw):
        rb = (nbytes + 63) // 64 * 64
        assert self.off + rb <= self.size, ("arena overflow", self.off, rb, self.size)
        v = self.ap[:, self.off:self.off + nbytes].bitcast(dtype)
        self.off += rb
        if pattern:
            v = v.rearrange(pattern, **kw)
        return v

    def mark(self):
        return self.off

    def reset(self, m):
        self.off = m


class Builder:
    def __init__(self, nseq, layers, dbg=None):
        self.nseq = nseq
        self.layers = layers
        self.dbg = dbg or {}
        nc = bass.Bass("TRN2", target_bir_lowering=False)
        self.nc = nc
        self.P = Prog(nc)
        L = len(layers)
        self.L = L
        self.xin = nc.dram_tensor("xin", [nseq, DC, 128, T], F32, kind="ExternalInput")
        self.xs = nc.dram_tensor("xout", [nseq, DC, 128, T], F32, kind="ExternalOutput")
        self.wsrc = nc.dram_tensor("wsrc", [L * NPKR, 2048], F32, kind="ExternalInput")
        self.wpk = [nc.dram_tensor(f"wpk{i}", [NPKR, 2048], BF16, kind="Internal") for i in range(L)]
        self.vec = nc.dram_tensor("vec", [128, L * VW], F32, kind="ExternalInput")
        self.cbf = nc.dram_tensor("cbf", [128, NCB], F32, kind="ExternalInput")
        self.ropec = nc.dram_tensor("ropec", [32, T], F32, kind="ExternalInput")
        self.ropes = nc.dram_tensor("ropes", [32, T], F32, kind="ExternalInput")
        self.ropekc = nc.dram_tensor("ropekc", [32, 2, 128], F32, kind="ExternalInput")
        self.selc = nc.dram_tensor("selc", [128, 2, 16, 32], F32, kind="ExternalInput")
        sk = "ExternalOutput" if self.dbg.get("dump") else "Internal"
        self.s_fm = nc.dram_tensor("s_fm", [16, 128, T], BF16, kind=sk)
        self.s_tm = nc.dram_tensor("s_tm", [12, 128, 16, 128], BF16, kind=sk)
        self.s_kc = nc.dram_tensor("s_kc", [2, 128, 128], BF16, kind=sk)
        self.s_vc = nc.dram_tensor("s_vc", [2, 128, 128], BF16, kind=sk)
        self.s_ab = nc.dram_tensor("s_ab", [8, 2, 6, T], BF16, kind=sk)
        ASZ = 206 * 1024
        self.arena_t = nc.alloc_sbuf_tensor("arena", [128, ASZ], U8)
        self.A = Arena(self.arena_t.ap(), ASZ)
        self.ps = nc.alloc_psum_tensor("ps", [128, 8, 512], F32).ap()
        A = self.A
        self.cb_sb = A.take(NCB * 2, BF16)
        self.vec_sb = A.take(L * VW * 4, F32)
        self.base_mark = A.mark()
        self.wslot_ctr = 0

    def cview(self, name, rows=128):
        o, n = CB[name]
        return self.cb_sb[0:rows, o:o + n]

    def vcol(self, li, c, rows=128, n=1):
        return self.vec_sb[0:rows, li * VW + c: li * VW + c + n]

    def wview(self, li, name):
        o, n = PK[name]
        flat = self.wpk[li].ap().rearrange("r c -> (r c)")
        return flat[o:o + n]

    def prologue(self):
        P = self.P
        P.dma("pool", self.cb_sb, self.cbf.ap(), writes=["cb"])
        P.dma("sp", self.vec_sb, self.vec.ap(), writes=["vec"])
        self.convert(0)

    CONV_CH = 4096

    def conv_chunks(self, li):
        if li == 0:
            return [(r, min(self.CONV_CH, NPKR - r)) for r in range(0, NPKR, self.CONV_CH)]
        return [(0, NPKR)]

    def convert(self, li):
        P = self.P
        r0 = li * NPKR
        for k, (r, n) in enumerate(self.conv_chunks(li)):
            last = None
            rr = r
            while rr < r + n:
                m = min(4096, r + n - rr)
                last = P.dma("pool", self.wpk[li].ap()[rr:rr + m, :], self.wsrc.ap()[r0 + rr:r0 + rr + m, :],
                             writes=[], group=f"conv{li}_{k}")
                rr += m
            P.last_w[("w", li, k)] = last

    def wkeys(self, li, name):
        o, n = PK[name]
        lo, hi = o // 2048, (o + n - 1) // 2048
        return [("w", li, k) for k, (r, m) in enumerate(self.conv_chunks(li)) if r <= hi and r + m - 1 >= lo]

    def norm_tile(self, xt, h, sq, tmp, rs, li, gcol, xkey, hkey, psb, sqkey="sqa"):
        P = self.P
        ones = self.cview("ones")
        psn = self.ps[:, psb, :]
        pk = ("ps", psb)
        for c in range(4):
            P.op("act", lambda e, o=sq[:, 4 * c:4 * c + 4, :], i=xt[:, 4 * c:4 * c + 4, :]: e.activation(out=o, in_=i, func=AF.Square),
                 reads=[(xkey, 4 * c + k) for k in range(4)], writes=[(sqkey, 4 * c + k) for k in range(4)])
        for dc in range(DC):
            P.op("pe", lambda e, r=sq[:, dc, :], s=(dc == 0), t=(dc == DC - 1): e.matmul(psn, lhsT=ones, rhs=r, start=s, stop=t),
                 reads=[(sqkey, dc), "cb"], writes=[pk])
        P.op("act", lambda e: e.activation(out=tmp, in_=psn, func=AF.Sqrt, bias=EPS, scale=1.0 / D),
             reads=[pk], writes=["ntmp"])
        P.op("dve", lambda e: e.reciprocal(out=rs, in_=tmp), reads=["ntmp"], writes=["nrs"])
        for dc in range(DC):
            P.op("dve", lambda e, o=h[:, dc, :], i=xt[:, dc, :], g=self.vcol(li, gcol + dc):
                 e.scalar_tensor_tensor(out=o, in0=i, scalar=g, in1=rs, op0=ALU.mult, op1=ALU.mult),
                 reads=[(xkey, dc), "nrs", "vec"], writes=[(hkey, dc)])

    def ffn_tile(self, li, f, xt, h, sq, tmp, rs, aT, sg, wring, xkey):
        P = self.P
        NSLOT = wring.shape[1]
        self.norm_tile(xt, h, sq, tmp, rs, li, {1: 0, 2: 32}[f], xkey, "h", 0)
        wg = self.wview(li, f"wg{f}").rearrange("(g p x) -> g p x", g=11, p=128)
        wu = self.wview(li, f"wu{f}").rearrange("(g p x) -> g p x", g=11, p=128)
        wd = self.wview(li, f"wd{f}").rearrange("(g p x) -> g p x", g=16, p=128)
        it = 0
        for fg in range(11):
            sa = self.wslot_ctr % NSLOT; self.wslot_ctr += 1
            sb_ = self.wslot_ctr % NSLOT; self.wslot_ctr += 1
            P.dma("sp", wring[:, sa, :], wg[fg], reads=self.wkeys(li, f"wg{f}"), writes=[("ws", sa)])
            P.dma("sp", wring[:, sb_, :], wu[fg], reads=self.wkeys(li, f"wu{f}"), writes=[("ws", sb_)])
            wa = wring[:, sa, :].rearrange("p (c x) -> p c x", c=DC)
            wb = wring[:, sb_, :].rearrange("p (c x) -> p c x", c=DC)
            for j in range(4):
                fc = fg * 4 + j
                bg = 1 + (it % 2)
                bu = 3 + (it % 2)
                sgs = it % 2
                it += 1
                psg = self.ps[:, bg, :]
                psu = self.ps[:, bu, :]
                for dc in range(DC):
                    P.op("pe", lambda e, o=psg, w=wa[:, dc, j * 128:(j + 1) * 128], r=h[:, dc, :], s=(dc == 0), t=(dc == DC - 1):
                         e.matmul(o, lhsT=w, rhs=r, start=s, stop=t),
                         reads=[("ws", sa), ("h", dc)], writes=[("ps", bg)])
                for dc in range(DC):
                    P.op("pe", lambda e, o=psu, w=wb[:, dc, j * 128:(j + 1) * 128], r=h[:, dc, :], s=(dc == 0), t=(dc == DC - 1):
                         e.matmul(o, lhsT=w, rhs=r, start=s, stop=t),
                         reads=[("ws", sb_), ("h", dc)], writes=[("ps", bu)])
                P.op("act", lambda e, o=sg[:, sgs, :], i=psg: e.activation(out=o, in_=i, func=AF.Silu),
                     reads=[("ps", bg)], writes=[("sg", sgs)])
                P.op("dve", lambda e, o=aT[:, fc, :], a=psu, b=sg[:, sgs, :]: e.tensor_tensor(out=o, in0=a, in1=b, op=ALU.mult),
                     reads=[("ps", bu), ("sg", sgs)], writes=[("aT", fc)])
        for dco in range(DC):
            s = self.wslot_ctr % NSLOT; self.wslot_ctr += 1
            P.dma("sp", wring[:, s, 0:FC * 128], wd[dco], reads=self.wkeys(li, f"wd{f}"), writes=[("ws", s)])
            w = wring[:, s, 0:FC * 128].rearrange("p (c x) -> p c x", c=FC)
            bd = 5 + (dco % 2)
            psd = self.ps[:, bd, :]
            for fc in range(FC):
                P.op("pe", lambda e, o=psd, ww=w[:, fc, :], r=aT[:, fc, :], s_=(fc == 0), t_=(fc == FC - 1):
                     e.matmul(o, lhsT=ww, rhs=r, start=s_, stop=t_),
                     reads=[("ws", s), ("aT", fc)], writes=[("ps", bd)])
            P.op("dve", lambda e, o=xt[:, dco, :], a=psd: e.scalar_tensor_tensor(out=o, in0=a, scalar=0.5, in1=o, op0=ALU.mult, op1=ALU.add),
                 reads=[("ps", bd)], writes=[(xkey, dco)])

    def ffn_phase(self, s, jobs, src_is_input):
        P = self.P
        A = self.A
        P.barrier()
        A.reset(self.base_mark)
        xt = A.take(DC * TT * 4, F32, "p (c t) -> p c t", c=DC)
        h = A.take(DC * TT * 2, BF16, "p (c t) -> p c t", c=DC)
        sq = A.take(DC * TT * 2, BF16, "p (c t) -> p c t", c=DC)
        tmp = A.take(TT * 4, F32)
        rs = A.take(TT * 4, F32)
        aT = A.take(FC * TT * 2, BF16, "p (c t) -> p c t", c=FC)
        sg = A.take(2 * TT * 2, BF16, "p (c t) -> p c t", c=2)
        NSLOT = 4
        wring = A.take(NSLOT * 16384, BF16, "p (s x) -> p s x", s=NSLOT)
        for tt in range(NTT):
            src = self.xin if src_is_input else self.xs
            P.dma("sp", xt, src.ap()[s, :, :, tt * TT:(tt + 1) * TT].rearrange("c p t -> p c t"),
                  reads=[("X", s, tt, dc) for dc in range(DC)], writes=[("xt", dc) for dc in range(DC)])
            for (li, f) in jobs:
                self.ffn_tile(li, f, xt, h, sq, tmp, rs, aT, sg, wring, "xt")
            P.dma("sp", self.xs.ap()[s, :, :, tt * TT:(tt + 1) * TT].rearrange("c p t -> p c t"), xt,
                  reads=[("xt", dc) for dc in range(DC)], writes=[("X", s, tt, dc) for dc in range(DC)])

    def build(self):
        P = self.P
        self.prologue()
        L = self.L
        mode = self.dbg.get("mode", "full")
        for s in range(self.nseq):
            first = True
            for li in range(L):
                jobs = []
                if li > 0:
                    jobs.append((li - 1, 2))
                jobs.append((li, 1))
                if mode in ("full", "ffn"):
                    self.ffn_phase(s, jobs, src_is_input=first)
                    first = False
                if s == 0 and li + 1 < L:
                    self.convert(li + 1)
                if mode in ("full", "mixer"):
                    self.mixer_phase(s, li, src_is_input=first)
                    first = False
            if mode in ("full", "ffn"):
                self.ffn_phase(s, [(L - 1, 2)], src_is_input=False)
        keys = [("X", s, tt, dc) for s in range(self.nseq) for tt in range(NTT) for dc in range(DC)]
        P.op("sp", lambda e: e.nop(), reads=keys)
        from contextlib import ExitStack
        with ExitStack() as es:
            es.enter_context(self.nc.allow_low_precision("bf16 matmul operands by design; accumulation is fp32"))
            block = es.enter_context(self.nc.Block())
            P.emit(block, es)
        return self.nc

    def mixer_phase(self, s, li, src_is_input):
        P, A = self.P, self.A
        P.barrier()
        A.reset(self.base_mark)
        M = {}
        M["xt"] = A.take(DC * TT * 4, F32, "p (c t) -> p c t", c=DC)
        M["h"] = A.take(DC * TT * 2, BF16, "p (c t) -> p c t", c=DC)
        M["tmp"] = A.take(TT * 4, F32)
        M["rs"] = A.take(TT * 4, F32)
        cm0 = A.mark()
        M["cfull"] = A.take(T * 4, F32)
        M["src"] = self.xin if src_is_input else self.xs
        cm = A.mark()
        if self.dbg.get("skip1") is None:
            self.pass1a(s, li, M)
            P.barrier()
            A.reset(cm)
            self.pass1b(s, li, M)
        if self.dbg.get("only1"):
            return
        P.barrier()
        A.reset(cm0)
        self.pass2(s, li, M)

    def load_x(self, M, s, tt):
        self.P.dma("sp", M["xt"], M["src"].ap()[s, :, :, tt * TT:(tt + 1) * TT].rearrange("c p t -> p c t"),
                   reads=[("X", s, tt, dc) for dc in range(DC)], writes=[("xt", dc) for dc in range(DC)])

    def head_norm(self, ps_in, pskey, n, gain, W, out_bf, outkey, rope=None, sumbank=3, rotbank=4):
        st = self.head_norm_B(ps_in, pskey, n, gain, W, out_bf, outkey, rope, sumbank)
        self.head_norm_C(st, rotbank)

    def head_norm_B(self, ps_in, pskey, n, gain, W, out_bf, outkey, rope=None, sumbank=3):
        P = self.P
        ones = self.cview("ones")
        a = self.hn_ctr % 2
        self.hn_ctr += 1
        hsq = W["hsq"][:, a, 0:n]
        pss = self.ps[:, sumbank, 0:n]
        P.op("act", lambda e: e.activation(out=hsq, in_=ps_in, func=AF.Square), reads=[pskey], writes=[("hsq", a)])
        P.op("pe", lambda e: e.matmul(pss, lhsT=ones, rhs=hsq, start=True, stop=True),
             reads=[("hsq", a), "cb"], writes=[("ps", sumbank)])
        hl = W["hl"][:, 0:n]
        hr = W["hr"][:, 0:n]
        P.op("act", lambda e: e.activation(out=hl, in_=pss, func=AF.Ln, bias=EPS, scale=1.0 / HD),
             reads=[("ps", sumbank)], writes=["hl"])
        P.op("act", lambda e: e.activation(out=hr, in_=hl, func=AF.Exp, scale=-0.5), reads=["hl"], writes=["hr"])
        if rope is None:
            P.op("dve", lambda e: e.scalar_tensor_tensor(out=out_bf, in0=ps_in, scalar=gain, in1=hr, op0=ALU.mult, op1=ALU.mult),
                 reads=[pskey, "hr", "vec"], writes=[outkey])
            return None
        kn = W["kn"][:, a, 0:n]
        P.op("dve", lambda e: e.scalar_tensor_tensor(out=kn, in0=ps_in, scalar=gain, in1=hr, op0=ALU.mult, op1=ALU.mult),
             reads=[pskey, "hr", "vec"], writes=[("kn", a)])
        knb = W["knb"][0:32, a, 0:n]
        P.op("pool", lambda e: e.tensor_copy(out=knb, in_=kn[0:32, :]), reads=[("kn", a)], writes=[("knb", a)])
        return (a, n, kn, knb, rope, out_bf, outkey, W)

    def head_norm_C(self, st, rotbank=4):
        if st is None:
            return
        P = self.P
        a, n, kn, knb, rope, out_bf, outkey, W = st
        C, S, rkey = rope
        psr = self.ps[0:32, rotbank, 0:n]
        rt = self.cview("rt", 32)
        P.op("pe", lambda e: e.matmul(psr, lhsT=rt, rhs=knb, start=True, stop=True), reads=[("knb", a), "cb"], writes=[("ps", rotbank)])
        t1 = W["t1"][0:32, 0:n]
        t2 = W["t2"][0:32, 0:n]
        P.op("dve", lambda e: e.tensor_tensor(out=t1, in0=psr, in1=S, op=ALU.mult), reads=[("ps", rotbank), rkey], writes=["t1"])
        P.op("pool", lambda e: e.tensor_tensor(out=t2, in0=kn[0:32, :], in1=C, op=ALU.mult), reads=[("kn", a), rkey], writes=["t2"])
        P.op("pool", lambda e: e.tensor_tensor(out=kn[0:32, :], in0=t1, in1=t2, op=ALU.add), reads=["t1", "t2"], writes=[("kn", a)])
        P.op("act", lambda e: e.activation(out=out_bf, in_=kn, func=AF.Copy), reads=[("kn", a)], writes=[outkey])

    def head_pipeline(self, heads):
        n = len(heads)
        stB = [None] * n
        res = [None] * n
        for step in range(n + 2):
            if step < n:
                res[step] = heads[step]["A"]()
            i = step - 1
            if 0 <= i < n:
                hd = heads[i]
                if hd.get("B") is not None:
                    stB[i] = self.head_norm_B(res[i][0], res[i][1], **hd["B"])
                elif hd.get("raw") is not None:
                    hd["raw"](res[i][0], res[i][1])
            i = step - 2
            if 0 <= i < n:
                self.head_norm_C(stB[i])
                if heads[i].get("post") is not None:
                    heads[i]["post"]()

    def head_temps(self, A):
        W = {}
        W["hsq"] = A.take(2 * TT * 2, BF16, "p (c t) -> p c t", c=2)
        W["hl"] = A.take(TT * 4, F32)
        W["hr"] = A.take(TT * 4, F32)
        W["kn"] = A.take(2 * TT * 4, F32, "p (c t) -> p c t", c=2)
        W["t1"] = A.take(TT * 4, F32)
        W["t2"] = A.take(TT * 4, F32)
        W["knb"] = A.take(2 * TT * 2, BF16, "p (c t) -> p c t", c=2)
        W["rc"] = A.take(TT * 4, F32)
        W["rsn"] = A.take(TT * 4, F32)
        return W

    def pass1a(self, s, li, M):
        P, A = self.P, self.A
        self.hn_ctr = 0
        xt, h = M["xt"], M["h"]
        NS1 = 3
        wring = A.take(NS1 * 16384, BF16, "p (s x) -> p s x", s=NS1)
        W = self.head_temps(A)
        M["sq"] = A.take(DC * TT * 2, BF16, "p (c t) -> p c t", c=DC)
        stage = A.take(4 * TT * 2, BF16, "p (c t) -> p c t", c=4)
        lf1 = A.take(TT * 4, F32)
        lf2 = A.take(TT * 4, F32)
        ones8 = A.take(TT * 4, F32)
        negb = A.take(64, F32)
        wfs = A.take(DC * 8 * 2, BF16, "p (c x) -> p c x", c=DC)
        wkey = None
        cfull = M["cfull"]
        wfm1 = self.wview(li, "wfm1").rearrange("(g p x) -> g p x", g=16, p=128)
        wtm = self.wview(li, "wtm").rearrange("(g p x) -> g p x", g=3, p=128)
        P.dma("sp", wfs, self.wview(li, "wf").rearrange("(p c x) -> p c x", p=128, c=DC), reads=self.wkeys(li, "wf"), writes=["wfs"])
        P.op("dve", lambda e: e.memset(ones8, 1.0), writes=["ones8"])
        P.op("dve", lambda e: e.tensor_scalar(out=negb[0:8, 0:1], in0=self.vcol(li, 70, rows=8), scalar1=-1.0, scalar2=None, op0=ALU.mult),
             reads=["vec"], writes=["negb"])
        sctr = 0
        stg = 0
        for tt in range(NTT):
            tsl = slice(tt * TT, (tt + 1) * TT)
            if tt == 0:
                self.load_x(M, s, tt)
            self.norm_tile(xt, h, M["sq"], M["tmp"], M["rs"], li, 16, "xt", "h", 0)
            if tt + 1 < NTT:
                self.load_x(M, s, tt + 1)
            P.dma("sp", W["rc"][0:32, :], self.ropec.ap()[:, tsl], writes=["rope"])
            P.dma("sp", W["rsn"][0:32, :], self.ropes.ap()[:, tsl], writes=["rope"])
            heads = []
            PB = (1, 2, 6, 7)
            for cc in range(16):
                def A_(cc=cc):
                    nonlocal sctr
                    sl = sctr % NS1; sctr += 1
                    P.dma("sp", wring[:, sl, 0:2048], wfm1[cc], reads=self.wkeys(li, "wfm1"), writes=[("ws", sl)])
                    w = wring[:, sl, 0:2048].rearrange("p (c x) -> p c x", c=DC)
                    pb = PB[cc % 4]
                    psp = self.ps[:, pb, :]
                    for dc in range(DC):
                        P.op("pe", lambda e, o=psp, ww=w[:, dc, :], r=h[:, dc, :], s_=(dc == 0), t_=(dc == DC - 1):
                             e.matmul(o, lhsT=ww, rhs=r, start=s_, stop=t_),
                             reads=[("ws", sl), ("h", dc)], writes=[("ps", pb)])
                    return psp, ("ps", pb)
                st = stg % 4; stg += 1
                so = stage[:, st, :]
                hd = dict(A=A_)
                if cc < 4:
                    hd["raw"] = (lambda psp, pk, so=so, st=st: P.op("dve", lambda e: e.tensor_copy(out=so, in_=psp), reads=[pk], writes=[("stage", st)]))
                elif cc < 8:
                    hd["B"] = dict(n=TT, gain=self.vcol(li, 49 + (1 if cc < 6 else 2)), W=W, out_bf=so, outkey=("stage", st),
                                   rope=(W["rc"][0:32, :], W["rsn"][0:32, :], "rope"))
                else:
                    hd["B"] = dict(n=TT, gain=self.vcol(li, 53), W=W, out_bf=so, outkey=("stage", st))
                hd["post"] = (lambda cc=cc, so=so, st=st: P.dma("act", self.s_fm.ap()[cc, :, tsl], so, reads=[("stage", st)], writes=[("s_fm", cc)]))
                heads.append(hd)
            self.head_pipeline(heads)
            psf = self.ps[0:8, 5, :]
            for dc in range(DC):
                P.op("pe", lambda e, ww=wfs[:, dc, :], r=h[:, dc, :], s_=(dc == 0), t_=(dc == DC - 1):
                     e.matmul(psf, lhsT=ww, rhs=r, start=s_, stop=t_), reads=["wfs", ("h", dc)], writes=[("ps", 5)])
            P.op("act", lambda e: e.activation(out=lf1[0:8, :], in_=psf, func=AF.Exp, bias=negb[0:8, 0:1], scale=-1.0),
                 reads=[("ps", 5), "negb"], writes=["lf1"])
            P.op("act", lambda e: e.activation(out=lf2[0:8, :], in_=lf1[0:8, :], func=AF.Ln, bias=1.0, scale=1.0),
                 reads=["lf1"], writes=["lf2"])
            P.op("dve", lambda e: e.tensor_scalar(out=lf1[0:8, :], in0=lf2[0:8, :], scalar1=-1.0 / SCALE, scalar2=None, op0=ALU.mult),
                 reads=["lf2"], writes=["lf1"])
            init = 0.0 if tt == 0 else cfull[0:8, tt * TT - 1:tt * TT]
            P.op("dve", lambda e, o=cfull[0:8, tsl], i=init: e.tensor_tensor_scan(out=o, data0=ones8[0:8, :], data1=lf1[0:8, :],
                                                                                   initial=i, op0=ALU.mult, op1=ALU.add),
                 reads=["lf1", "ones8", "cfull"], writes=["cfull"])
            n2 = 0
            for cg in range(3):
                sl = sctr % NS1; sctr += 1
                P.dma("sp", wring[:, sl, :], wtm[cg], reads=self.wkeys(li, "wtm"), writes=[("ws", sl)])
                w = wring[:, sl, :].rearrange("p (c x) -> p c x", c=DC)
                for tk in range(4):
                    pb = 6 + n2 % 2; n2 += 1
                    psp = self.ps[:, pb, :]
                    for dc in range(DC):
                        P.op("pe", lambda e, o=psp, l_=h[:, dc, tk * 128:(tk + 1) * 128], r=w[:, dc, :], s_=(dc == 0), t_=(dc == DC - 1):
                             e.matmul(o, lhsT=l_, rhs=r, start=s_, stop=t_),
                             reads=[("ws", sl), ("h", dc)], writes=[("ps", pb)])
                    st = stg % 4; stg += 1
                    so = stage[:, st, :]
                    if n2 % 2:
                        P.op("act", lambda e, o=so, i=psp: e.activation(out=o, in_=i, func=AF.Copy), reads=[("ps", pb)], writes=[("stage", st)])
                    else:
                        P.op("dve", lambda e, o=so, i=psp: e.tensor_copy(out=o, in_=i), reads=[("ps", pb)], writes=[("stage", st)])
                    kt = tt * 4 + tk
                    P.dma("act", self.s_tm.ap()[cg * 4:(cg + 1) * 4, :, kt, :].rearrange("g p d -> p g d"),
                          so.rearrange("p (g d) -> p g d", g=4), reads=[("stage", st)], writes=[("s_tm", cg)])

    def pass1b(self, s, li, M):
        P, A = self.P, self.A
        wkey = ("w", li)
        W = self.head_temps(A)
        kcraw = A.take(4 * T * 2, BF16, "p (c t) -> p c t", c=4)
        cw1 = A.take(2 * 32 * 256 * 2, BF16, "p (k l j) -> p k l j", k=2, l=32)
        cw2 = A.take(2 * 2 * 128 * 2, BF16, "p (k c x) -> p k c x", k=2, c=2)
        posb = A.take(64 * 2, BF16, "p (k l) -> p k l", k=2)
        hid = A.take(2 * 128 * 2, BF16, "p (c x) -> p c x", c=2)
        bias = A.take(16, F32)
        stage = A.take(2 * 128 * 2, BF16, "p (c x) -> p c x", c=2)
        rkc = A.take(2 * 128 * 4, F32, "p (c x) -> p c x", c=2)
        c3 = A.take(3 * T * 2, BF16, "p (c t) -> p c t", c=3)
        n3 = A.take(3 * T * 2, BF16, "p (c t) -> p c t", c=3)
        o3 = A.take(3 * T * 2, BF16, "p (c t) -> p c t", c=3)
        r1 = A.take(T * 4, F32)
        cfull = M["cfull"]
        P.dma("sp", kcraw, self.s_fm.ap()[0:4, :, :].rearrange("c p t -> p c t"), reads=[("s_fm", c) for c in range(4)], writes=["kcraw"])
        P.dma("sp", cw1, self.wview(li, "cw1").rearrange("(k p l j) -> p k l j", k=2, p=128, l=32), reads=self.wkeys(li, "cw1"), writes=["cw1"])
        P.dma("sp", cw2, self.wview(li, "cw2").rearrange("(k p c x) -> p k c x", k=2, p=128, c=2), reads=self.wkeys(li, "cw2"), writes=["cw2"])
        P.dma("sp", posb, self.wview(li, "pos").rearrange("(p k l) -> p k l", p=128, k=2), reads=self.wkeys(li, "pos"), writes=["posb"])
        P.dma("sp", rkc[0:32, :, :], self.ropekc.ap(), writes=["rkc"])
        cf = cfull[0:8, :]
        P.op("dve", lambda e: e.tensor_copy(out=c3[0:8, 0, :], in_=cf), reads=["cfull"], writes=["c3"])
        P.op("dve", lambda e: e.tensor_tensor(out=r1[0:8, :], in0=cf, in1=c3[0:8, 0, :], op=ALU.subtract), reads=["c3", "cfull"], writes=["r1"])
        P.op("dve", lambda e: e.tensor_copy(out=c3[0:8, 1, :], in_=r1[0:8, :]), reads=["r1"], writes=["c3"])
        P.op("dve", lambda e: e.tensor_tensor(out=r1[0:8, :], in0=r1[0:8, :], in1=c3[0:8, 1, :], op=ALU.subtract), reads=["c3"], writes=["r1"])
        P.op("dve", lambda e: e.tensor_copy(out=c3[0:8, 2, :], in_=r1[0:8, :]), reads=["r1"], writes=["c3"])
        P.op("dve", lambda e: e.tensor_scalar(out=n3[0:8, :, :], in0=c3[0:8, :, :], scalar1=-1.0, scalar2=None, op0=ALU.mult), reads=["c3"], writes=["n3"])
        P.op("pool", lambda e: e.memset(o3[0:8, :, :], 1.0), writes=["o3"])
        ab = self.s_ab.ap()
        P.dma("sp", ab[:, 0, 0:3, :], o3[0:8, :, :], reads=["o3"], writes=["s_ab"])
        P.dma("sp", ab[:, 0, 3:6, :], c3[0:8, :, :], reads=["c3"], writes=["s_ab"])
        P.dma("sp", ab[:, 1, 0:3, :], n3[0:8, :, :], reads=["n3"], writes=["s_ab"])
        P.dma("sp", ab[:, 1, 3:6, :], o3[0:8, :, :], reads=["o3"], writes=["s_ab"])
        self.hn_ctr = 0
        nq = 0
        for kv in range(2):
            for jc in range(2):
                for l in range(32):
                    P.op("pe", lambda e, o=self.ps[:, 0, kv * 2 + jc:kv * 2 + jc + 1], ww=cw1[:, kv, l, jc * 128:(jc + 1) * 128], r=posb[:, kv, l:l + 1],
                         s_=(l == 0), t_=(l == 31): e.matmul(o, lhsT=ww, rhs=r, start=s_, stop=t_),
                         reads=["cw1", "posb"], writes=[("ps", 0)])
            P.op("dve", lambda e, o=bias[:, kv * 2:kv * 2 + 2], i=self.ps[:, 0, kv * 2:kv * 2 + 2]: e.tensor_copy(out=o, in_=i),
                 reads=[("ps", 0)], writes=["cbias"])
            for kvh in range(2):
                raw = kcraw[:, kv * 2 + kvh, :]
                for jc in range(2):
                    pb = 1 + jc
                    psh = self.ps[:, pb, 0:NCMP]
                    for l in range(32):
                        P.op("pe", lambda e, o=psh, ww=cw1[:, kv, l, jc * 128:(jc + 1) * 128], r=raw[:, l:l + 16 * (NCMP - 1) + 1:16],
                             s_=(l == 0), t_=(l == 31): e.matmul(o, lhsT=ww, rhs=r, start=s_, stop=t_),
                             reads=["cw1", "kcraw"], writes=[("ps", pb)])
                    P.op("act", lambda e, o=hid[:, jc, 0:NCMP], i=psh, b=bias[:, kv * 2 + jc:kv * 2 + jc + 1]:
                         e.activation(out=o, in_=i, func=AF.Gelu_apprx_tanh, bias=b),
                         reads=[("ps", pb), "cbias"], writes=[("hid", jc)])
                st = nq % 2; nq += 1
                if kv == 0:
                    psk = self.ps[:, 5, 0:NCMP]
                    for jc in range(2):
                        P.op("pe", lambda e, ww=cw2[:, 0, jc, :], r=hid[:, jc, 0:NCMP], s_=(jc == 0), t_=(jc == 1):
                             e.matmul(psk, lhsT=ww, rhs=r, start=s_, stop=t_), reads=["cw2", ("hid", jc)], writes=[("ps", 5)])
                    self.head_norm(psk, ("ps", 5), NCMP, self.vcol(li, 49), W, stage[:, st, 0:NCMP], ("stage", st),
                                   rope=(rkc[0:32, 0, 0:NCMP], rkc[0:32, 1, 0:NCMP], "rkc"))
                    P.dma("sp", self.s_kc.ap()[kvh, :, 0:NCMP], stage[:, st, 0:NCMP], reads=[("stage", st)], writes=["s_kc"])
                else:
                    psv = self.ps[0:NCMP, 6, 0:128]
                    for jc in range(2):
                        P.op("pe", lambda e, l_=hid[:, jc, 0:NCMP], r=cw2[:, 1, jc, :], s_=(jc == 0), t_=(jc == 1):
                             e.matmul(psv, lhsT=l_, rhs=r, start=s_, stop=t_), reads=["cw2", ("hid", jc)], writes=[("ps", 6)])
                    P.op("dve", lambda e, o=stage[0:NCMP, st, :]: e.tensor_copy(out=o, in_=psv), reads=[("ps", 6)], writes=[("stage", st)])
                    P.dma("sp", self.s_vc.ap()[kvh, 0:NCMP, :], stage[0:NCMP, st, :], reads=[("stage", st)], writes=["s_vc"])

    def attn_multi(self, jobs):
        P = self.P
        ones = self.cview("ones")
        flat = [(ji, ti) for ji, job in enumerate(jobs) for ti in range(len(job["tiles"]))]

        def emit_qk(ji, ti):
            job = jobs[ji]
            t = job["tiles"][ti]
            sb = self.sctr % 2; self.sctr += 1
            nk = t["nk"]
            pss = self.ps[0:nk, sb, :]
            mm = [(t["k"], job["q"], list(t["kkeys"]) + [job["qkey"]])] + list(t["extras"])
            for m, (l_, r_, keys) in enumerate(mm):
                P.op("pe", lambda e, o=pss, l_=l_, r_=r_, s_=(m == 0), t_=(m == len(mm) - 1): e.matmul(o, lhsT=l_, rhs=r_, start=s_, stop=t_),
                     reads=list(keys) + ["cb"], writes=[("ps", sb)])
            return pss, sb, nk

        def emit_rest(ji, ti, info):
            job = jobs[ji]
            t = job["tiles"][ti]
            n = len(job["tiles"])
            pss, sb, nk = info
            lb, ob = job["lo"]
            p = self.pctr % 3; self.pctr += 1
            ptile = self.pt[0:nk, p, :]
            P.op("act", lambda e, o=ptile, i_=pss: e.activation(out=o, in_=i_, func=AF.Exp, scale=SCALE),
                 reads=[("ps", sb)], writes=[("pt", p)])
            P.op("pe", lambda e, l_=ones[0:nk, :], r_=ptile, s_=(ti == 0), t_=(ti == n - 1): e.matmul(self.ps[:, lb, :], lhsT=l_, rhs=r_, start=s_, stop=t_),
                 reads=[("pt", p), "cb"], writes=[("ps", lb)])
            P.op("pe", lambda e, l_=t["v"], r_=ptile, s_=(ti == 0), t_=(ti == n - 1): e.matmul(self.ps[:, ob, :], lhsT=l_, rhs=r_, start=s_, stop=t_),
                 reads=[("pt", p)] + list(t["vkeys"]), writes=[("ps", ob)])
            if ti == n - 1 and job.get("fin") is not None:
                d = job["fin"](ptile, ("pt", p))
                for f in deferred:
                    f()
                del deferred[:]
                if d is not None:
                    deferred.append(d)

        deferred = []
        pending = None
        for (ji, ti) in flat:
            if ti == 0 and jobs[ji].get("pre") is not None:
                jobs[ji]["pre"]()
            cur = emit_qk(ji, ti)
            if pending is not None:
                emit_rest(*pending)
            pending = (ji, ti, cur)
        if pending is not None:
            emit_rest(*pending)
        for f in deferred:
            f()

    def pass2(self, s, li, M):
        P, A = self.P, self.A
        self.hn_ctr = 0
        self.sctr = 0
        self.pctr = 0
        xt, h = M["xt"], M["h"]
        o_sb = h
        wkey = ("w", li)
        W = self.head_temps(A)
        NS2 = 4
        wring = A.take(NS2 * 4096, BF16, "p (s x) -> p s x", s=NS2)
        qall = A.take(16 * TT * 2, BF16, "p (c t) -> p c t", c=16)
        gsb = A.take(TT * 2, BF16)
        xst = A.take(3 * TT * 4, F32, "p (c t) -> p c t", c=3)
        wgs = A.take(DC * 24 * 2, BF16, "p (c x) -> p c x", c=DC)
        NKV = 6
        kvr = A.take(NKV * 4096, BF16, "p (s x) -> p s x", s=NKV)
        abA = A.take(2 * TT * 2, BF16, "p (c t) -> p c t", c=2)
        kcs = A.take(2 * 2 * 128 * 2, BF16, "p (a b x) -> p a b x", a=2, b=2)
        self.pt = A.take(3 * TT * 2, BF16, "p (c t) -> p c t", c=3)
        acc = A.take(4 * TT * 4, F32, "p (c t) -> p c t", c=4)
        rl = A.take(2 * TT * 4, F32, "p (c t) -> p c t", c=2)
        wg = A.take(TT * 4, F32)
        gtmp = wg
        of = A.take(2 * TT * 4, F32, "p (c t) -> p c t", c=2)
        osq = A.take(2 * TT * 2, BF16, "p (c t) -> p c t", c=2)
        phat = A.take(2 * TT * 2, BF16, "p (c t) -> p c t", c=2)
        score = A.take(4 * 32 * 4, F32, "p (c x) -> p c x", c=4)
        mx8 = A.take(4 * 8 * 4, F32, "p (c x) -> p c x", c=4)
        selb = A.take(4 * 32 * 2, BF16, "p (c x) -> p c x", c=4)
        selbT = A.take(TT * 2, BF16)
        selct = A.take(2 * 4 * 32 * 4, F32, "p (a c x) -> p a c x", a=2, c=4)
        ssq = A.take(2 * TT * 4, F32, "p (c t) -> p c t", c=2)
        rstd = A.take(2 * TT * 4, F32, "p (c t) -> p c t", c=2)
        otmp = A.take(2 * TT * 4, F32, "p (c t) -> p c t", c=2)
        ident = self.cview("ident")
        ones = self.cview("ones")
        ovl = self.cview("ovl")
        cbv = self.cview("cb").rearrange("p (k n) -> p k n", k=4)
        wbv = self.cview("wb").rearrange("p (k n) -> p k n", k=4)
        cmask = self.cview("cmask")
        exv = self.cview("ex", 32).rearrange("p (k n) -> p k n", k=16)
        eselv = self.cview("esel", 32).rearrange("p (k n) -> p k n", k=24)
        wfm2 = self.wview(li, "wfm2").rearrange("(g p x) -> g p x", g=16, p=128)
        wout = self.wview(li, "wout").rearrange("(g p x) -> g p x", g=16, p=128)
        P.dma("sp", wgs, self.wview(li, "wgt").rearrange("(p c x) -> p c x", p=128, c=DC), reads=self.wkeys(li, "wgt"), writes=["wgs"])
        P.dma("sp", kcs[:, :, 0, :], self.s_kc.ap().rearrange("k p x -> p k x"), reads=["s_kc"], writes=["kcs"])
        P.dma("sp", kcs[:, :, 1, :], self.s_vc.ap().rearrange("k p x -> p k x"), reads=["s_vc"], writes=["kcs"])
        sctr = 0
        kvc = 0
        rctr = 0
        yctr = 0
        actr = 0
        zctr = 0
        loc = 0
        for j in range(NTT):
            tsl = slice(j * TT, (j + 1) * TT)
            nkt = 4 * (j + 1)
            nk = nkt * 128
            if j == 0:
                self.load_x(M, s, j)
            self.norm_tile(xt, h, qall, M["tmp"], M["rs"], li, 16, "xt", "h", 0, sqkey="qa")
            if j + 1 < NTT:
                self.load_x(M, s, j + 1)
            P.dma("sp", W["rc"][0:32, :], self.ropec.ap()[:, tsl], writes=["rope"])
            P.dma("sp", W["rsn"][0:32, :], self.ropes.ap()[:, tsl], writes=["rope"])
            P.dma("sp", selct, self.selc.ap()[:, :, 4 * j:4 * j + 4, :], writes=["selct"])
            heads = []
            PB = (1, 2, 5, 6)
            for cc in range(16):
                def A_(cc=cc):
                    nonlocal sctr
                    sl = sctr % NS2; sctr += 1
                    P.dma("sp", wring[:, sl, :], wfm2[cc], reads=self.wkeys(li, "wfm2"), writes=[("ws", sl)])
                    w = wring[:, sl, :].rearrange("p (c x) -> p c x", c=DC)
                    pb = PB[cc % 4]
                    psp = self.ps[:, pb, :]
                    for dc in range(DC):
                        P.op("pe", lambda e, o=psp, ww=w[:, dc, :], r=h[:, dc, :], s_=(dc == 0), t_=(dc == DC - 1):
                             e.matmul(o, lhsT=ww, rhs=r, start=s_, stop=t_),
                             reads=[("ws", sl), ("h", dc)], writes=[("ps", pb)])
                    return psp, ("ps", pb)
                if cc < 8:
                    B = dict(n=TT, gain=self.vcol(li, 48), W=W, out_bf=qall[:, cc, :], outkey=("qa", cc),
                             rope=(W["rc"][0:32, :], W["rsn"][0:32, :], "rope"))
                else:
                    B = dict(n=TT, gain=self.vcol(li, 52), W=W, out_bf=qall[:, cc, :], outkey=("qa", cc))
                heads.append(dict(A=A_, B=B))
            self.head_pipeline(heads)
            psg = self.ps[0:24, 7, :]
            for dc in range(DC):
                P.op("pe", lambda e, ww=wgs[:, dc, :], r=h[:, dc, :], s_=(dc == 0), t_=(dc == DC - 1):
                     e.matmul(psg, lhsT=ww, rhs=r, start=s_, stop=t_), reads=["wgs", ("h", dc)], writes=[("ps", 7)])
            P.op("act", lambda e: e.activation(out=gtmp[0:24, :], in_=psg, func=AF.Exp, scale=-1.0), reads=[("ps", 7)], writes=["wg"])
            P.op("dve", lambda e: e.tensor_scalar(out=gtmp[0:24, :], in0=gtmp[0:24, :], scalar1=1.0, scalar2=None, op0=ALU.add), reads=["wg"], writes=["wg"])
            P.op("dve", lambda e: e.reciprocal(out=gsb[0:24, :], in_=gtmp[0:24, :]), reads=["wg"], writes=["gsb"])
            for kvh in range(2):
                ch = []
                for _ in range(4):
                    ch.append(kvc % NKV); kvc += 1
                c_ks, c_kw, c_vs, c_vw = ch
                P.dma("sp", kvr[:, c_ks, 0:nk], self.s_fm.ap()[4 + kvh, :, 0:nk], reads=[("s_fm", 4 + kvh)], writes=[("kv", c_ks)])
                P.dma("sp", kvr[:, c_vs, 0:nk], self.s_tm.ap()[0 + kvh, :, 0:nkt, :].rearrange("p k d -> p (k d)"), reads=[("s_tm", 0)], writes=[("kv", c_vs)])
                P.dma("sp", kvr[:, c_kw, 0:nk], self.s_fm.ap()[6 + kvh, :, 0:nk], reads=[("s_fm", 6 + kvh)], writes=[("kv", c_kw)])
                P.dma("sp", kvr[:, c_vw, 0:nk], self.s_tm.ap()[2 + kvh, :, 0:nkt, :].rearrange("p k d -> p (k d)"), reads=[("s_tm", 0)], writes=[("kv", c_vw)])
                ksT = kvr[:, c_ks, :]
                kwT = kvr[:, c_kw, :]
                vs = kvr[:, c_vs, :].rearrange("p (k d) -> p k d", d=128)
                vw = kvr[:, c_vw, :].rearrange("p (k d) -> p k d", d=128)
                kcT = kcs[:, kvh, 0, 0:NCMP]
                vc = kcs[0:NCMP, kvh, 1, :]

                def fin_branch(g, b, lo, first, kvh=kvh):
                    nonlocal rctr, yctr
                    lb, ob = lo
                    x_ = rctr % 2; rctr += 1
                    rlb = rl[:, x_, :]
                    r = ((kvh * 4 + g) * 3 + b)
                    P.op("pe", lambda e: e.matmul(self.ps[:, 7, :], lhsT=eselv[0:24, r, :], rhs=gsb[0:24, :], start=True, stop=True),
                         reads=["gsb", "cb"], writes=[("ps", 7)])
                    P.op("dve", lambda e: e.tensor_scalar(out=rlb, in0=self.ps[:, lb, :], scalar1=1e-30, scalar2=None, op0=ALU.max),
                         reads=[("ps", lb)], writes=[("rl", x_)])
                    P.op("dve", lambda e: e.reciprocal(out=rlb, in_=rlb), reads=[("rl", x_)], writes=[("rl", x_)])
                    P.op("dve", lambda e: e.tensor_tensor(out=wg, in0=self.ps[:, 7, :], in1=rlb, op=ALU.mult),
                         reads=[("ps", 7), ("rl", x_)], writes=["wg"])
                    if first:
                        P.op("dve", lambda e: e.tensor_tensor(out=acc[:, g, :], in0=self.ps[:, ob, :], in1=wg, op=ALU.mult),
                             reads=[("ps", ob), "wg"], writes=[("acc", g)])
                    else:
                        y_ = yctr % 2; yctr += 1
                        P.op("dve", lambda e: e.tensor_tensor(out=otmp[:, y_, :], in0=self.ps[:, ob, :], in1=wg, op=ALU.mult),
                             reads=[("ps", ob), "wg"], writes=[("otmp", y_)])
                        P.op("pool", lambda e: e.tensor_tensor(out=acc[:, g, :], in0=acc[:, g, :], in1=otmp[:, y_, :], op=ALU.add),
                             reads=[("otmp", y_)], writes=[("acc", g)])
                    return x_

                jobs = []
                for g in range(4):
                    hq = kvh * 4 + g
                    lo = (3, 4) if loc % 2 == 0 else (5, 6); loc += 1
                    tiles = [dict(k=kcT, kkeys=["kcs"], nk=NCMP, extras=[(ident[0:NCMP, 0:NCMP], cmask[0:NCMP, tsl], [])],
                                  v=vc, vkeys=["kcs"])]

                    def fin_cmp(ptile, pkey, g=g, lo=lo):
                        nonlocal zctr
                        x_ = fin_branch(g, 0, lo, True)
                        z_ = zctr % 2; zctr += 1
                        P.op("pool", lambda e, o=phat[0:NCMP, z_, :], a=ptile, b=rl[0:NCMP, x_, :]: e.tensor_tensor(out=o, in0=a, in1=b, op=ALU.mult),
                             reads=[pkey, ("rl", x_)], writes=[("phat", z_)])
                        def dfr():
                            for tk in range(4):
                                P.op("pe", lambda e, o=self.ps[:, 2, tk * 32:(tk + 1) * 32], l_=phat[0:NCMP, z_, tk * 128:(tk + 1) * 128], r_=ovl[0:NCMP, :],
                                     s_=(g == 0 and tk == 0), t_=(g == 3): e.matmul(o, lhsT=l_, rhs=r_, start=s_, stop=t_, skip_group_check=True),
                                     reads=[("phat", z_), "cb"], writes=[("ps", 2)])
                        return dfr
                    jobs.append(dict(q=qall[:, hq, :], qkey=("qa", hq), tiles=tiles, lo=lo, fin=fin_cmp))
                self.attn_multi(jobs)
                imp = self.ps[:, 2, 0:128].rearrange("p (c x) -> p c x", c=4)
                P.op("dve", lambda e: e.tensor_tensor(out=score, in0=imp, in1=selct[:, 0, :, :], op=ALU.mult),
                     reads=[("ps", 2), "selct"], writes=["score"])
                P.op("dve", lambda e: e.tensor_tensor(out=score, in0=score, in1=selct[:, 1, :, :], op=ALU.add),
                     reads=["selct"], writes=["score"])
                for tk in range(4):
                    P.op("dve", lambda e, o=mx8[:, tk, :], i_=score[:, tk, :]: e.max(out=o, in_=i_), reads=["score"], writes=[("mx8", tk)])
                for tk in range(4):
                    P.op("dve", lambda e, o=selb[:, tk, :], i_=score[:, tk, :], th=mx8[:, tk, 7:8]:
                         e.tensor_scalar(out=o, in0=i_, scalar1=th, scalar2=NEG, op0=ALU.is_lt, op1=ALU.mult),
                         reads=["score", ("mx8", tk)], writes=[("selb", tk)])
                for tk in range(4):
                    P.op("pe", lambda e, o=self.ps[0:32, 2, tk * 128:(tk + 1) * 128], l_=selb[:, tk, :]:
                         e.matmul(o, lhsT=l_, rhs=ident, start=True, stop=True),
                         reads=[("selb", tk), "cb"], writes=[("ps", 2)])
                P.op("dve", lambda e: e.tensor_copy(out=selbT[0:32, :], in_=self.ps[0:32, 2, :]), reads=[("ps", 2)], writes=["selbT"])
                jobs = []
                for g in range(4):
                    hq = kvh * 4 + g
                    lo = (3, 4) if loc % 2 == 0 else (5, 6); loc += 1
                    tiles = []
                    for kt in range(nkt):
                        ex = [(exv[0:32, kt, :], selbT[0:32, :], ["selbT"])]
                        if kt >= 4 * j:
                            ex.append((ident, cbv[:, kt - 4 * j, :], []))
                        tiles.append(dict(k=ksT[:, kt * 128:(kt + 1) * 128], kkeys=[("kv", c_ks)], nk=128, extras=ex,
                                          v=vs[:, kt, :], vkeys=[("kv", c_vs)]))
                    jobs.append(dict(q=qall[:, hq, :], qkey=("qa", hq), tiles=tiles, lo=lo,
                                     fin=(lambda ptile, pkey, g=g, lo=lo: (fin_branch(g, 1, lo, False), None)[1])))
                for g in range(4):
                    hq = kvh * 4 + g
                    lo = (3, 4) if loc % 2 == 0 else (5, 6); loc += 1
                    tiles = []
                    for kt in range(max(0, 4 * j - 4), nkt):
                        if kt >= 4 * j:
                            ex = [(ident, cbv[:, kt - 4 * j, :], [])]
                        else:
                            ex = [(ident, wbv[:, kt - (4 * j - 4), :], [])]
                        tiles.append(dict(k=kwT[:, kt * 128:(kt + 1) * 128], kkeys=[("kv", c_kw)], nk=128, extras=ex,
                                          v=vw[:, kt, :], vkeys=[("kv", c_vw)]))
                    jobs.append(dict(q=qall[:, hq, :], qkey=("qa", hq), tiles=tiles, lo=lo,
                                     fin=(lambda ptile, pkey, g=g, lo=lo: (fin_branch(g, 2, lo, False), None)[1])))
                self.attn_multi(jobs)
                sl_of = {}
                def fsq(g):
                    nonlocal actr
                    a_ = actr % 2; actr += 1
                    sl_of[g] = a_
                    P.op("pool", lambda e, o=osq[:, a_, :], i_=acc[:, g, :]: e.tensor_tensor(out=o, in0=i_, in1=i_, op=ALU.mult),
                         reads=[("acc", g)], writes=[("osq", a_)])
                    P.op("dve", lambda e, o=o_sb[:, kvh * 4 + g, :], i_=acc[:, g, :], gc=self.vcol(li, 54 + kvh * 4 + g):
                         e.tensor_scalar(out=o, in0=i_, scalar1=gc, scalar2=None, op0=ALU.mult),
                         reads=[("acc", g), "vec"], writes=[("h", kvh * 4 + g)])
                def fsum(g):
                    a_ = sl_of[g]
                    hq = kvh * 4 + g
                    P.op("pe", lambda e, r_=osq[:, a_, :]: e.matmul(self.ps[:, 7, :], lhsT=ones, rhs=r_, start=True, stop=True),
                         reads=[("osq", a_), "cb"], writes=[("ps", 7)])
                    if hq == 0:
                        P.op("dve", lambda e: e.tensor_copy(out=ssq[:, 0, :], in_=self.ps[:, 7, :]), reads=[("ps", 7)], writes=[("ssq", 0)])
                    else:
                        P.op("dve", lambda e: e.tensor_tensor(out=ssq[:, 0, :], in0=self.ps[:, 7, :], in1=ssq[:, 0, :], op=ALU.add),
                             reads=[("ps", 7)], writes=[("ssq", 0)])
                fsq(0)
                for g in range(4):
                    if g + 1 < 4:
                        fsq(g + 1)
                    fsum(g)
            jobs = []
            for hf in range(8):
                ch = []
                for _ in range(3):
                    ch.append(kvc % NKV); kvc += 1
                c_k, c_v, c_b = ch
                a_ = hf % 2
                def pre_fox(hf=hf, c_k=c_k, c_v=c_v, c_b=c_b, a_=a_):
                    P.dma("sp", kvr[:, c_k, 0:nk], self.s_fm.ap()[8 + hf, :, 0:nk], reads=[("s_fm", 8 + hf)], writes=[("kv", c_k)])
                    P.dma("sp", kvr[0:6, c_b, 0:nk], self.s_ab.ap()[hf, 1, :, 0:nk], reads=["s_ab"], writes=[("kv", c_b)])
                    P.dma("sp", abA[0:6, a_, :], self.s_ab.ap()[hf, 0, :, tsl], reads=["s_ab"], writes=[("abA", a_)])
                    P.dma("sp", kvr[:, c_v, 0:nk], self.s_tm.ap()[4 + hf, :, 0:nkt, :].rearrange("p k d -> p (k d)"), reads=[("s_tm", 1), ("s_tm", 2)], writes=[("kv", c_v)])
                fk = kvr[:, c_k, :]
                fv = kvr[:, c_v, :].rearrange("p (k d) -> p k d", d=128)
                Bc = kvr[0:6, c_b, :]
                lo = (3, 4) if loc % 2 == 0 else (5, 6); loc += 1
                tiles = []
                for kt in range(nkt):
                    ex = [(Bc[:, kt * 128:(kt + 1) * 128], abA[0:6, a_, :], [("kv", c_b), ("abA", a_)])]
                    if kt >= 4 * j:
                        ex.append((ident, cbv[:, kt - 4 * j, :], []))
                    tiles.append(dict(k=fk[:, kt * 128:(kt + 1) * 128], kkeys=[("kv", c_k)], nk=128, extras=ex,
                                      v=fv[:, kt, :], vkeys=[("kv", c_v)]))

                def fin_fox(ptile, pkey, hf=hf, lo=lo):
                    nonlocal rctr, actr
                    lb, ob = lo
                    x_ = rctr % 2; rctr += 1
                    rlb = rl[:, x_, :]
                    P.op("dve", lambda e, o=rlb, i_=self.ps[:, lb, :]: e.reciprocal(out=o, in_=i_), reads=[("ps", lb)], writes=[("rl", x_)])
                    f_ = hf % 2
                    P.op("dve", lambda e, o=of[:, f_, :], a=self.ps[:, ob, :], b=rlb: e.tensor_tensor(out=o, in0=a, in1=b, op=ALU.mult),
                         reads=[("ps", ob), ("rl", x_)], writes=[("of", f_)])
                    q_ = actr % 2; actr += 1
                    P.op("pool", lambda e, o=osq[:, q_, :], i_=of[:, f_, :]: e.tensor_tensor(out=o, in0=i_, in1=i_, op=ALU.mult),
                         reads=[("of", f_)], writes=[("osq", q_)])
                    P.op("dve", lambda e, o=o_sb[:, 8 + hf, :], i_=of[:, f_, :], gc=self.vcol(li, 62 + hf):
                         e.tensor_scalar(out=o, in0=i_, scalar1=gc, scalar2=None, op0=ALU.mult),
                         reads=[("of", f_), "vec"], writes=[("h", 8 + hf)])

                    def dfr():
                        P.op("pe", lambda e, r_=osq[:, q_, :]: e.matmul(self.ps[:, 7, :], lhsT=ones, rhs=r_, start=True, stop=True),
                             reads=[("osq", q_), "cb"], writes=[("ps", 7)])
                        if hf == 0:
                            P.op("dve", lambda e: e.tensor_copy(out=ssq[:, 1, :], in_=self.ps[:, 7, :]), reads=[("ps", 7)], writes=[("ssq", 1)])
                        else:
                            P.op("dve", lambda e: e.tensor_tensor(out=ssq[:, 1, :], in0=self.ps[:, 7, :], in1=ssq[:, 1, :], op=ALU.add),
                                 reads=[("ps", 7)], writes=[("ssq", 1)])
                    return dfr
                jobs.append(dict(q=qall[:, 8 + hf, :], qkey=("qa", 8 + hf), tiles=tiles, lo=lo, fin=fin_fox, pre=pre_fox))
            self.attn_multi(jobs)
            for k in range(2):
                P.op("act", lambda e, o=rstd[:, k, :], i_=ssq[:, k, :]: e.activation(out=o, in_=i_, func=AF.Ln, bias=EPS, scale=1.0 / 1024.0),
                     reads=[("ssq", k)], writes=[("rstd", k)])
                P.op("act", lambda e, o=rstd[:, k, :]: e.activation(out=o, in_=o, func=AF.Exp, scale=-0.5),
                     reads=[("rstd", k)], writes=[("rstd", k)])
            for kc in range(16):
                P.op("dve" if kc % 2 == 0 else "pool", lambda e, o=o_sb[:, kc, :], b=rstd[:, kc // 8, :]: e.tensor_tensor(out=o, in0=o, in1=b, op=ALU.mult),
                     reads=[("rstd", kc // 8)], writes=[("h", kc)])
            def issue(dco):
                nonlocal sctr
                sl = sctr % NS2; sctr += 1
                k = dco % 3
                P.dma("sp", wring[:, sl, :], wout[dco], reads=self.wkeys(li, "wout"), writes=[("ws", sl)])
                P.dma("sp", xst[:, k, :], M["src"].ap()[s, dco, :, tsl], reads=[("X", s, j, dco)], writes=[("xst", k)])
                return sl
            slots = {0: issue(0)}
            for dco in range(DC):
                if dco + 1 < DC:
                    slots[dco + 1] = issue(dco + 1)
                sl = slots[dco]
                k = dco % 3
                w = wring[:, sl, :].rearrange("p (c x) -> p c x", c=16)
                bnk = dco % 2
                for kc in range(16):
                    P.op("pe", lambda e, o=self.ps[:, bnk, :], ww=w[:, kc, :], r=o_sb[:, kc, :], s_=(kc == 0), t_=(kc == 15):
                         e.matmul(o, lhsT=ww, rhs=r, start=s_, stop=t_),
                         reads=[("ws", sl), ("h", kc)], writes=[("ps", bnk)])
                P.op("dve", lambda e, o=xst[:, k, :], a=self.ps[:, bnk, :]: e.tensor_tensor(out=o, in0=a, in1=o, op=ALU.add),
                     reads=[("ps", bnk)], writes=[("xst", k)])
                P.dma("sp", self.xs.ap()[s, dco, :, tsl], xst[:, k, :], reads=[("xst", k)], writes=[("X", s, j, dco)])


def _pack_layer(w, li):
    out = np.zeros(NPK, np.float32)

    def put(name, arr):
        o, n = PK[name]
        a = np.ascontiguousarray(arr, dtype=np.float32).reshape(-1)
        assert a.size == n, (name, a.size, n)
        out[o:o + n] = a

    for f in (1, 2):
        wg = w[f"ffn{f}_w_gate"][li]
        wu = w[f"ffn{f}_w_up"][li]
        wd = w[f"ffn{f}_w_down"][li]
        put(f"wg{f}", wg.reshape(DC, 128, 11, 512).transpose(2, 1, 0, 3))
        put(f"wu{f}", wu.reshape(DC, 128, 11, 512).transpose(2, 1, 0, 3))
        put(f"wd{f}", wd.reshape(FC, 128, DC, 128).transpose(2, 1, 0, 3))
    win = w["w_in"][li]
    kv0 = 1024
    def kvcol(branch, typ, kvh):
        return kv0 + ((branch * 2 + typ) * 2 + kvh) * 128
    fq0 = 2584
    fk0 = fq0 + 1024
    fv0 = fq0 + 2048
    cols1 = ([kvcol(0, 0, 0), kvcol(0, 0, 1), kvcol(0, 1, 0), kvcol(0, 1, 1),
              kvcol(1, 0, 0), kvcol(1, 0, 1), kvcol(2, 0, 0), kvcol(2, 0, 1)]
             + [fk0 + h * 128 for h in range(8)])
    def fm(cols):
        blk = np.stack([win[:, c:c + 128] for c in cols], 0)
        return blk.reshape(len(cols), DC, 128, 128).transpose(0, 2, 1, 3)
    put("wfm1", fm(cols1))
    tmcols = np.concatenate([np.arange(kvcol(1, 1, 0), kvcol(1, 1, 0) + 256),
                             np.arange(kvcol(2, 1, 0), kvcol(2, 1, 0) + 256),
                             np.arange(fv0, fv0 + 1024)])
    wt = win[:, tmcols]
    put("wtm", wt.reshape(DC, 128, 3, 512).transpose(2, 1, 0, 3))
    cols2 = [h * 128 for h in range(8)] + [fq0 + h * 128 for h in range(8)]
    put("wfm2", fm(cols2))
    wo = w["w_out"][li]
    put("wout", wo.reshape(16, 128, DC, 128).transpose(2, 1, 0, 3))
    cw1 = w["cmp_w1"][li]
    put("cw1", cw1.reshape(2, 32, 128, 256).transpose(0, 2, 1, 3))
    cw2 = w["cmp_w2"][li]
    put("cw2", cw2.reshape(2, 2, 128, 128).transpose(0, 2, 1, 3))
    put("wf", win[:, 5656:5664].reshape(DC, 128, 8).transpose(1, 0, 2))
    put("wgt", win[:, 2560:2584].reshape(DC, 128, 24).transpose(1, 0, 2))
    pos = w["cmp_pos_emb"][li]
    put("pos", pos.transpose(2, 0, 1))
    return out


def _vec_pack(w, layers):
    v = np.zeros((128, len(layers) * VW), np.float32)
    for i, li in enumerate(layers):
        b = i * VW
        v[:, b + 0:b + 16] = w["ffn1_norm"][li].reshape(DC, 128).T
        v[:, b + 16:b + 32] = w["mix_norm"][li].reshape(DC, 128).T
        v[:, b + 32:b + 48] = w["ffn2_norm"][li].reshape(DC, 128).T
        v[:, b + 48] = w["nsa_q_norm"][li]
        v[:, b + 49:b + 52] = w["nsa_k_norm"][li].T
        v[:, b + 52] = w["fox_q_norm"][li]
        v[:, b + 53] = w["fox_k_norm"][li]
        v[:, b + 54:b + 62] = w["nsa_out_norm"][li].reshape(8, 128).T
        v[:, b + 62:b + 70] = w["fox_out_norm"][li].reshape(8, 128).T
        v[0:8, b + 70] = w["fox_forget_bias"][li]
    return v


def _consts():
    cb = np.zeros((128, NCB), np.float32)
    def put(name, arr):
        o, n = CB[name]
        cb[:arr.shape[0], o:o + n] = arr.reshape(arr.shape[0], -1)
    put("ident", np.eye(128, dtype=np.float32))
    put("ones", np.ones((128, 128), np.float32))
    rt = np.zeros((32, 32), np.float32)
    for i in range(16):
        rt[16 + i, i] = -1.0
        rt[i, 16 + i] = 1.0
    put("rt", rt)
    cstart = np.arange(NCMP) * 16
    sstart = np.arange(32) * 64
    ovl = ((cstart[:, None] < sstart[None, :] + 64) & (cstart[:, None] + 32 > sstart[None, :])).astype(np.float32)
    put("ovl", ovl)
    p = np.arange(128)[:, None]
    n = np.arange(512)[None, :]
    cbm = np.stack([np.where(128 * k + p <= n, 0.0, NEG) for k in range(4)], 1)
    wbm = np.stack([np.where(128 * k + p > n, 0.0, NEG) for k in range(4)], 1)
    put("cb", cbm.astype(np.float32))
    put("wb", wbm.astype(np.float32))
    cend = cstart + 31
    t = np.arange(T)[None, :]
    put("cmask", np.where(cend[:, None] <= t, 0.0, NEG).astype(np.float32))
    j = np.arange(32)[:, None, None]
    kt = np.arange(16)[None, :, None]
    pp = np.arange(128)[None, None, :]
    put("ex", (j == 2 * kt + pp // 64).astype(np.float32))
    r = np.arange(32)[:, None, None]
    rr = np.arange(24)[None, :, None]
    put("esel", np.broadcast_to((r == rr), (32, 24, 128)).astype(np.float32))
    inv = (np.float32(500000.0) ** (-np.arange(0, 32, 2, dtype=np.float32) / np.float32(32))).astype(np.float32)
    def tables(pos):
        ang = pos.astype(np.float32)[:, None] * inv[None, :]
        c = np.cos(ang).astype(np.float32).T
        s_ = np.sin(ang).astype(np.float32).T
        return np.concatenate([c, c], 0), np.concatenate([s_, s_], 0)
    rc, rs = tables(np.arange(T))
    kc_c, kc_s = tables(cend)
    ropekc = np.zeros((32, 2, 128), np.float32)
    ropekc[:, 0, :NCMP] = kc_c
    ropekc[:, 1, :NCMP] = kc_s
    tok = np.arange(T)
    tb = tok // 64
    jj = np.arange(32)[None, :]
    causal = jj <= tb[:, None]
    forced = (jj == 0) | (causal & (jj > tb[:, None] - 2))
    mmul = (causal & ~forced).astype(np.float32)
    badd = np.where(forced, 1e9, np.where(causal, 0.0, -1e30)).astype(np.float32)
    selc = np.stack([mmul.reshape(16, 128, 32).transpose(1, 0, 2), badd.reshape(16, 128, 32).transpose(1, 0, 2)], 1)
    return cb, rc, rs, ropekc, np.ascontiguousarray(selc)


_CACHE = {}


def _get_prog(nseq, layers_key, dbg_key=None):
    key = (nseq, layers_key, dbg_key)
    if key not in _CACHE:
        b = Builder(nseq, list(layers_key), dict(dbg_key or ()))
        _CACHE[key] = b.build()
    return _CACHE[key]


def kernel(**inputs):
    x = np.asarray(inputs["x"], np.float32)
    B = x.shape[0]
    ncores = 8
    nseq = B // ncores
    w = {k: np.asarray(v) for k, v in inputs.items() if k != "x"}
    layers = tuple(range(DEPTH))
    wsrc = np.concatenate([_pack_layer(w, li) for li in layers]).reshape(len(layers) * NPKR, 2048)
    vec = _vec_pack(w, layers)
    cb, rc, rs, ropekc, selc = _consts()
    nc = _get_prog(nseq, layers)
    in_maps = []
    for c in range(ncores):
        xc = x[c * nseq:(c + 1) * nseq]
        xT = np.ascontiguousarray(xc.transpose(0, 2, 1)).reshape(nseq, DC, 128, T)
        in_maps.append({"xin": xT, "wsrc": wsrc, "vec": vec, "cbf": cb, "ropec": rc, "ropes": rs,
                        "ropekc": ropekc, "selc": selc})
    res = run_bass_kernel_spmd(nc, in_maps, core_ids=list(range(ncores)))
    outs = []
    for c in range(ncores):
        y = res.results[c]["xout"].reshape(nseq, D, T).transpose(0, 2, 1)
        outs.append(y)
    return np.ascontiguousarray(np.concatenate(outs, 0), dtype=np.float32)
```

```python
import numpy as np
import concourse.bass as bass
import concourse.mybir as mybir
from concourse.bass_utils import run_bass_kernel_spmd

F32 = mybir.dt.float32
BF16 = mybir.dt.bfloat16
U8 = mybir.dt.uint8
AF = mybir.ActivationFunctionType
ALU = mybir.AluOpType

D = 2048
T = 2048
DEPTH = 4
HD = 128
DFF = 5632
DC = D // 128
FC = DFF // 128
TT = 512
NTT = T // TT
EPS = 1e-6
SCALE = HD ** -0.5
NEG = -32768.0
NCMP = 127
VW = 72

PK = {}
_off = 0
def _add(name, n):
    global _off
    PK[name] = (_off, n)
    _off += n
for _f in (1, 2):
    _add(f"wg{_f}", D * DFF)
    _add(f"wu{_f}", D * DFF)
    _add(f"wd{_f}", D * DFF)
_add("wfm1", 16 * 128 * 16 * 128)
_add("wtm", 3 * 128 * 16 * 512)
_add("wfm2", 16 * 128 * 16 * 128)
_add("wout", 16 * 128 * 16 * 128)
_add("cw1", 2 * 128 * 32 * 256)
_add("cw2", 2 * 128 * 2 * 128)
_add("wf", 128 * 16 * 8)
_add("wgt", 128 * 16 * 24)
_add("pos", 128 * 2 * 32)
NPK = ((_off + 2047) // 2048) * 2048
NPKR = NPK // 2048

CB = {}
_c = 0
def _addc(name, n):
    global _c
    CB[name] = (_c, n)
    _c += n
_addc("ident", 128)
_addc("ones", 128)
_addc("rt", 32)
_addc("ovl", 32)
_addc("cb", 4 * 512)
_addc("wb", 4 * 512)
_addc("cmask", 2048)
_addc("ex", 16 * 128)
_addc("esel", 24 * 128)
NCB = _c


class Prog:
    NS = 8

    def __init__(self, nc):
        self.nc = nc
        self.ops = []
        self.deps = []
        self.last_w = {}
        self.readers = {}
        self.streams = {k: [] for k in ("pe", "act", "dve", "pool", "sp")}
        self.groups = {}
        self.last_op = {k: None for k in self.streams}
        self.recent_dma = {"sp": [], "pool": [], "act": []}
        self.cur_barrier = None

    def _record(self, stream, fn, kind, reads, writes, group=None, is_barrier=False):
        i = len(self.ops)
        d = set()
        lw = self.last_w
        rd = self.readers
        for k in reads:
            w = lw.get(k)
            if w is not None:
                d.add(w)
            r = rd.get(k)
            if r is None:
                r = rd[k] = [{}, []]
            if kind == "c":
                r[0][stream] = i
            else:
                r[1].append(i)
        for k in writes:
            w = lw.get(k)
            if w is not None:
                d.add(w)
            r = rd.get(k)
            if r is not None:
                d.update(r[0].values())
                d.update(r[1])
                del rd[k]
            lw[k] = i
        if is_barrier:
            for st, li in self.last_op.items():
                if li is not None:
                    d.add(li)
            for q, lst in self.recent_dma.items():
                d.update(lst)
        elif self.cur_barrier is not None:
            d.add(self.cur_barrier)
        d.discard(i)
        self.ops.append((stream, fn, kind, group))
        self.deps.append(d)
        self.streams[stream].append(i)
        if kind == "c":
            self.last_op[stream] = i
        elif group is None:
            lst = self.recent_dma[stream]
            lst.append(i)
            if len(lst) > self.NS:
                lst.pop(0)
        if group is not None:
            self.groups[group] = self.groups.get(group, 0) + 1
        if is_barrier:
            self.cur_barrier = i
        return i

    def op(self, stream, fn, reads=(), writes=()):
        return self._record(stream, fn, "c", reads, writes)

    def dma(self, q, out, in_, reads=(), writes=(), group=None):
        return self._record(q, lambda e, o=out, i=in_: e.dma_start(out=o, in_=i), "d",
                            reads, writes, group)

    def barrier(self):
        self._record("sp", lambda e: e.nop(), "c", [], [], is_barrier=True)

    def emit(self, block, sems_ctx):
        nc = self.nc
        ops = self.ops
        n = len(ops)
        dsem = {}
        qcount = {"sp": 0, "pool": 0, "act": 0}
        qhist = {"sp": [], "pool": [], "act": []}
        extra = {}
        for i in range(n):
            st, fn, kind, group = ops[i]
            if kind != "d":
                continue
            if group is not None:
                dsem[i] = (("g", group), 16 * self.groups[group])
            else:
                c = qcount[st]
                dsem[i] = (("s", st, c % self.NS), 16 * (c // self.NS + 1))
                if c >= self.NS:
                    extra[i] = qhist[st][c - self.NS]
                qhist[st].append(i)
                qcount[st] = c + 1
        sig = [False] * n
        for i in range(n):
            st = ops[i][0]
            for d in self.deps[i]:
                sd, _, kd, _ = ops[d]
                if kd == "c" and not (sd == "pe" and st == "pe"):
                    sig[d] = True
        cnt = {k: 0 for k in self.streams}
        sval = [0] * n
        for i in range(n):
            st, fn, kind, group = ops[i]
            if kind == "c" and sig[i]:
                cnt[st] += 1
                sval[i] = cnt[st]
        semkeys = set(("e", k) for k in self.streams)
        for i in dsem:
            semkeys.add(dsem[i][0])
        sem = {}
        for k in sorted(semkeys, key=str):
            sem[k] = sems_ctx.enter_context(nc.semaphore("s_" + "_".join(str(x) for x in k)))
        waits = [None] * n
        waited = {k: {} for k in self.streams}
        for st in self.streams:
            wd = waited[st]
            for i in self.streams[st]:
                need = {}
                dl = self.deps[i]
                if i in extra:
                    dl = set(dl)
                    dl.add(extra[i])
                for d in dl:
                    sd, _, kd, _ = ops[d]
                    if kd == "d":
                        k, v = dsem[d]
                    else:
                        if sd == "pe" and st == "pe":
                            continue
                        k, v = ("e", sd), sval[d]
                    if need.get(k, 0) < v:
                        need[k] = v
                w = []
                for k, v in need.items():
                    if wd.get(k, 0) < v:
                        wd[k] = v
                        w.append((sem[k], v))
                waits[i] = w
        self.n_waits = sum(len(w) for w in waits)
        self.n_sig = sum(sig)

        def run_stream(st, eng):
            for i in self.streams[st]:
                _, fn, kind, group = ops[i]
                for s, v in waits[i]:
                    eng.wait_ge(s, v)
                ins = fn(eng)
                if kind == "d":
                    ins.then_inc(sem[dsem[i][0]], 16)
                elif sig[i]:
                    ins.then_inc(sem[("e", st)], 1)

        @block.tensor
        def _(e):
            run_stream("pe", e)

        @block.scalar
        def _(e):
            run_stream("act", e)

        @block.vector
        def _(e):
            run_stream("dve", e)

        @block.gpsimd
        def _(e):
            run_stream("pool", e)

        @block.sync
        def _(e):
            run_stream("sp", e)


class Arena:
    def __init__(self, ap, size):
        self.ap = ap
        self.size = size
        self.off = 0

    def take(self, nbytes, dtype, pattern=None, **kw):
        rb = (nbytes + 63) // 64 * 64
        assert self.off + rb <= self.size, ("arena overflow", self.off, rb, self.size)
        v = self.ap[:, self.off:self.off + nbytes].bitcast(dtype)
        self.off += rb
        if pattern:
            v = v.rearrange(pattern, **kw)
        return v

    def mark(self):
        return self.off

    def reset(self, m):
        self.off = m


class Builder:
    def __init__(self, nseq, layers, dbg=None):
        self.nseq = nseq
        self.layers = layers
        self.dbg = dbg or {}
        nc = bass.Bass("TRN2", target_bir_lowering=False)
        self.nc = nc
        self.P = Prog(nc)
        L = len(layers)
        self.L = L
        self.xin = nc.dram_tensor("xin", [nseq, DC, 128, T], F32, kind="ExternalInput")
        self.xs = nc.dram_tensor("xout", [nseq, DC, 128, T], F32, kind="ExternalOutput")
        self.wsrc = nc.dram_tensor("wsrc", [L * NPKR, 2048], F32, kind="ExternalInput")
        self.wpk = [nc.dram_tensor(f"wpk{i}", [NPKR, 2048], BF16, kind="Internal") for i in range(L)]
        self.vec = nc.dram_tensor("vec", [128, L * VW], F32, kind="ExternalInput")
        self.cbf = nc.dram_tensor("cbf", [128, NCB], F32, kind="ExternalInput")
        self.ropec = nc.dram_tensor("ropec", [32, T], F32, kind="ExternalInput")
        self.ropes = nc.dram_tensor("ropes", [32, T], F32, kind="ExternalInput")
        self.ropekc = nc.dram_tensor("ropekc", [32, 2, 128], F32, kind="ExternalInput")
        self.selc = nc.dram_tensor("selc", [128, 2, 16, 32], F32, kind="ExternalInput")
        sk = "ExternalOutput" if self.dbg.get("dump") else "Internal"
        self.s_fm = nc.dram_tensor("s_fm", [16, 128, T], BF16, kind=sk)
        self.s_tm = nc.dram_tensor("s_tm", [12, 128, 16, 128], BF16, kind=sk)
        self.s_kc = nc.dram_tensor("s_kc", [2, 128, 128], BF16, kind=sk)
        self.s_vc = nc.dram_tensor("s_vc", [2, 128, 128], BF16, kind=sk)
        self.s_ab = nc.dram_tensor("s_ab", [8, 2, 6, T], BF16, kind=sk)
        ASZ = 206 * 1024
        self.arena_t = nc.alloc_sbuf_tensor("arena", [128, ASZ], U8)
        self.A = Arena(self.arena_t.ap(), ASZ)
        self.ps = nc.alloc_psum_tensor("ps", [128, 8, 512], F32).ap()
        A = self.A
        self.cb_sb = A.take(NCB * 2, BF16)
        self.vec_sb = A.take(L * VW * 4, F32)
        self.base_mark = A.mark()
        self.wslot_ctr = 0

    def cview(self, name, rows=128):
        o, n = CB[name]
        return self.cb_sb[0:rows, o:o + n]

    def vcol(self, li, c, rows=128, n=1):
        return self.vec_sb[0:rows, li * VW + c: li * VW + c + n]

    def wview(self, li, name):
        o, n = PK[name]
        flat = self.wpk[li].ap().rearrange("r c -> (r c)")
        return flat[o:o + n]

    def prologue(self):
        P = self.P
        P.dma("pool", self.cb_sb, self.cbf.ap(), writes=["cb"])
        P.dma("sp", self.vec_sb, self.vec.ap(), writes=["vec"])
        self.convert(0)

    CONV_CH = 4096

    def conv_chunks(self, li):
        if li == 0:
            return [(r, min(self.CONV_CH, NPKR - r)) for r in range(0, NPKR, self.CONV_CH)]
        return [(0, NPKR)]

    def convert(self, li):
        P = self.P
        r0 = li * NPKR
        for k, (r, n) in enumerate(self.conv_chunks(li)):
            last = None
            rr = r
            while rr < r + n:
                m = min(4096, r + n - rr)
                last = P.dma("pool", self.wpk[li].ap()[rr:rr + m, :], self.wsrc.ap()[r0 + rr:r0 + rr + m, :],
                             writes=[], group=f"conv{li}_{k}")
                rr += m
            P.last_w[("w", li, k)] = last

    def wkeys(self, li, name):
        o, n = PK[name]
        lo, hi = o // 2048, (o + n - 1) // 2048
        return [("w", li, k) for k, (r, m) in enumerate(self.conv_chunks(li)) if r <= hi and r + m - 1 >= lo]

    def norm_tile(self, xt, h, sq, tmp, rs, li, gcol, xkey, hkey, psb, sqkey="sqa", tmpkey="ntmp"):
        P = self.P
        ones = self.cview("ones")
        psn = self.ps[:, psb, :]
        pk = ("ps", psb)
        for c in range(4):
            o_ = sq[:, 4 * c:4 * c + 4, :]
            i_ = xt[:, 4 * c:4 * c + 4, :]
            rk = [(xkey, 4 * c + k) for k in range(4)]
            wk = [(sqkey, 4 * c + k) for k in range(4)]
            if c in (0, 2):
                P.op("act", lambda e, o=o_, i=i_: e.activation(out=o, in_=i, func=AF.Square), reads=rk, writes=wk)
            else:
                P.op("dve", lambda e, o=o_, i=i_: e.tensor_tensor(out=o, in0=i, in1=i, op=ALU.mult), reads=rk, writes=wk)
        for dc in range(DC):
            P.op("pe", lambda e, r=sq[:, dc, :], s=(dc == 0), t=(dc == DC - 1): e.matmul(psn, lhsT=ones, rhs=r, start=s, stop=t),
                 reads=[(sqkey, dc), "cb"], writes=[pk])
        P.op("act", lambda e: e.activation(out=tmp, in_=psn, func=AF.Sqrt, bias=EPS, scale=1.0 / D),
             reads=[pk], writes=[tmpkey])
        P.op("dve", lambda e: e.reciprocal(out=rs, in_=tmp), reads=[tmpkey], writes=["nrs"])
        for dc in range(DC):
            P.op("dve", lambda e, o=h[:, dc, :], i=xt[:, dc, :], g=self.vcol(li, gcol + dc):
                 e.scalar_tensor_tensor(out=o, in0=i, scalar=g, in1=rs, op0=ALU.mult, op1=ALU.mult),
                 reads=[(xkey, dc), "nrs", "vec"], writes=[(hkey, dc)])

    def ffn_tile(self, li, f, xt, h, sq, tmp, rs, aT, sg, wring, xkey, after_chunk=None):
        P = self.P
        NSLOT = wring.shape[1]
        self.norm_tile(xt, h, sq, tmp, rs, li, {1: 0, 2: 32}[f], xkey, "h", 0)
        wg = self.wview(li, f"wg{f}").rearrange("(g p x) -> g p x", g=11, p=128)
        wu = self.wview(li, f"wu{f}").rearrange("(g p x) -> g p x", g=11, p=128)
        wd = self.wview(li, f"wd{f}").rearrange("(g p x) -> g p x", g=16, p=128)
        it = 0
        for fg in range(11):
            sa = self.wslot_ctr % NSLOT; self.wslot_ctr += 1
            sb_ = self.wslot_ctr % NSLOT; self.wslot_ctr += 1
            P.dma("sp", wring[:, sa, :], wg[fg], reads=self.wkeys(li, f"wg{f}"), writes=[("ws", sa)])
            P.dma("sp", wring[:, sb_, :], wu[fg], reads=self.wkeys(li, f"wu{f}"), writes=[("ws", sb_)])
            wa = wring[:, sa, :].rearrange("p (c x) -> p c x", c=DC)
            wb = wring[:, sb_, :].rearrange("p (c x) -> p c x", c=DC)
            for j in range(4):
                fc = fg * 4 + j
                bg = 1 + (it % 2)
                bu = 3 + (it % 2)
                sgs = it % 2
                it += 1
                psg = self.ps[:, bg, :]
                psu = self.ps[:, bu, :]
                for dc in range(DC):
                    P.op("pe", lambda e, o=psg, w=wa[:, dc, j * 128:(j + 1) * 128], r=h[:, dc, :], s=(dc == 0), t=(dc == DC - 1):
                         e.matmul(o, lhsT=w, rhs=r, start=s, stop=t),
                         reads=[("ws", sa), ("h", dc)], writes=[("ps", bg)])
                for dc in range(DC):
                    P.op("pe", lambda e, o=psu, w=wb[:, dc, j * 128:(j + 1) * 128], r=h[:, dc, :], s=(dc == 0), t=(dc == DC - 1):
                         e.matmul(o, lhsT=w, rhs=r, start=s, stop=t),
                         reads=[("ws", sb_), ("h", dc)], writes=[("ps", bu)])
                P.op("act", lambda e, o=sg[:, sgs, :], i=psg: e.activation(out=o, in_=i, func=AF.Silu),
                     reads=[("ps", bg)], writes=[("sg", sgs)])
                P.op("dve", lambda e, o=aT[:, fc, :], a=psu, b=sg[:, sgs, :]: e.tensor_tensor(out=o, in0=a, in1=b, op=ALU.mult),
                     reads=[("ps", bu), ("sg", sgs)], writes=[("aT", fc)])
        for dco in range(DC):
            s = self.wslot_ctr % NSLOT; self.wslot_ctr += 1
            P.dma("sp", wring[:, s, 0:FC * 128], wd[dco], reads=self.wkeys(li, f"wd{f}"), writes=[("ws", s)])
            w = wring[:, s, 0:FC * 128].rearrange("p (c x) -> p c x", c=FC)
            bd = 5 + (dco % 2)
            psd = self.ps[:, bd, :]
            for fc in range(FC):
                P.op("pe", lambda e, o=psd, ww=w[:, fc, :], r=aT[:, fc, :], s_=(fc == 0), t_=(fc == FC - 1):
                     e.matmul(o, lhsT=ww, rhs=r, start=s_, stop=t_),
                     reads=[("ws", s), ("aT", fc)], writes=[("ps", bd)])
            P.op("dve", lambda e, o=xt[:, dco, :], a=psd: e.scalar_tensor_tensor(out=o, in0=a, scalar=0.5, in1=o, op0=ALU.mult, op1=ALU.add),
                 reads=[("ps", bd)], writes=[(xkey, dco)])
            if after_chunk is not None:
                after_chunk(dco)

    def ffn_phase(self, s, jobs, src_is_input):
        P = self.P
        A = self.A
        P.barrier()
        A.reset(self.base_mark)
        xt = A.take(DC * TT * 4, F32, "p (c t) -> p c t", c=DC)
        h = A.take(DC * TT * 2, BF16, "p (c t) -> p c t", c=DC)
        sq = A.take(DC * TT * 2, BF16, "p (c t) -> p c t", c=DC)
        tmp = A.take(TT * 4, F32)
        rs = A.take(TT * 4, F32)
        aT = A.take(FC * TT * 2, BF16, "p (c t) -> p c t", c=FC)
        sg = A.take(2 * TT * 2, BF16, "p (c t) -> p c t", c=2)
        NSLOT = 4
        wring = A.take(NSLOT * 16384, BF16, "p (s x) -> p s x", s=NSLOT)
        src = self.xin if src_is_input else self.xs
        for tt in range(NTT):
            tsl = slice(tt * TT, (tt + 1) * TT)
            if tt == 0:
                P.dma("sp", xt, src.ap()[s, :, :, tsl].rearrange("c p t -> p c t"),
                      reads=[("X", s, tt, dc) for dc in range(DC)], writes=[("xt", dc) for dc in range(DC)])

            def after_chunk(dco, tt=tt, tsl=tsl):
                P.dma("act", self.xs.ap()[s, dco, :, tsl], xt[:, dco, :], reads=[("xt", dco)], writes=[("X", s, tt, dco)])
                if tt + 1 < NTT:
                    nsl = slice((tt + 1) * TT, (tt + 2) * TT)
                    P.dma("pool", xt[:, dco, :], src.ap()[s, dco, :, nsl], reads=[("X", s, tt + 1, dco)], writes=[("xt", dco)])
            for n_, (li, f) in enumerate(jobs):
                self.ffn_tile(li, f, xt, h, sq, tmp, rs, aT, sg, wring, "xt",
                              after_chunk=(after_chunk if n_ == len(jobs) - 1 else None))

    def build(self):
        P = self.P
        self.prologue()
        L = self.L
        mode = self.dbg.get("mode", "full")
        for s in range(self.nseq):
            first = True
            for li in range(L):
                jobs = []
                if li > 0:
                    jobs.append((li - 1, 2))
                jobs.append((li, 1))
                if mode in ("full", "ffn"):
                    self.ffn_phase(s, jobs, src_is_input=first)
                    first = False
                if s == 0 and li + 1 < L:
                    self.convert(li + 1)
                if mode in ("full", "mixer"):
                    self.mixer_phase(s, li, src_is_input=first)
                    first = False
            if mode in ("full", "ffn"):
                self.ffn_phase(s, [(L - 1, 2)], src_is_input=False)
        keys = [("X", s, tt, dc) for s in range(self.nseq) for tt in range(NTT) for dc in range(DC)]
        P.op("sp", lambda e: e.nop(), reads=keys)
        from contextlib import ExitStack
        with ExitStack() as es:
            es.enter_context(self.nc.allow_low_precision("bf16 matmul operands by design; accumulation is fp32"))
            block = es.enter_context(self.nc.Block())
            P.emit(block, es)
        return self.nc

    def mixer_phase(self, s, li, src_is_input):
        P, A = self.P, self.A
        P.barrier()
        A.reset(self.base_mark)
        M = {}
        M["xt"] = A.take(DC * TT * 4, F32, "p (c t) -> p c t", c=DC)
        M["h"] = A.take(DC * TT * 2, BF16, "p (c t) -> p c t", c=DC)
        M["tmp"] = A.take(TT * 4, F32)
        M["rs"] = A.take(TT * 4, F32)
        cm0 = A.mark()
        M["cfull"] = A.take(T * 4, F32)
        M["src"] = self.xin if src_is_input else self.xs
        cm = A.mark()
        if self.dbg.get("skip1") is None:
            self.pass1a(s, li, M)
            P.barrier()
            A.reset(cm)
            self.pass1b(s, li, M)
        if self.dbg.get("only1"):
            return
        P.barrier()
        A.reset(cm0)
        self.pass2(s, li, M)

    def load_x(self, M, s, tt):
        self.P.dma("sp", M["xt"], M["src"].ap()[s, :, :, tt * TT:(tt + 1) * TT].rearrange("c p t -> p c t"),
                   reads=[("X", s, tt, dc) for dc in range(DC)], writes=[("xt", dc) for dc in range(DC)])

    def head_norm(self, ps_in, pskey, n, gain, W, out_bf, outkey, rope=None, sumbank=3, rotbank=4):
        st = self.head_norm_B(ps_in, pskey, n, gain, W, out_bf, outkey, rope, sumbank)
        self.head_norm_C(st, rotbank)

    def head_norm_B(self, ps_in, pskey, n, gain, W, out_bf, outkey, rope=None, sumbank=3):
        P = self.P
        ones = self.cview("ones")
        a = self.hn_ctr % 2
        self.hn_ctr += 1
        hsq = W["hsq"][:, a, 0:n]
        pss = self.ps[:, sumbank, 0:n]
        P.op("act", lambda e: e.activation(out=hsq, in_=ps_in, func=AF.Square), reads=[pskey], writes=[("hsq", a)])
        P.op("pe", lambda e: e.matmul(pss, lhsT=ones, rhs=hsq, start=True, stop=True),
             reads=[("hsq", a), "cb"], writes=[("ps", sumbank)])
        hl = W["hl"][:, 0:n]
        hr = W["hr"][:, 0:n]
        P.op("act", lambda e: e.activation(out=hl, in_=pss, func=AF.Ln, bias=EPS, scale=1.0 / HD),
             reads=[("ps", sumbank)], writes=["hl"])
        P.op("act", lambda e: e.activation(out=hr, in_=hl, func=AF.Exp, scale=-0.5), reads=["hl"], writes=["hr"])
        if rope is None:
            P.op("dve", lambda e: e.scalar_tensor_tensor(out=out_bf, in0=ps_in, scalar=gain, in1=hr, op0=ALU.mult, op1=ALU.mult),
                 reads=[pskey, "hr", "vec"], writes=[outkey])
            return None
        kn = W["kn"][:, a, 0:n]
        P.op("dve", lambda e: e.scalar_tensor_tensor(out=kn, in0=ps_in, scalar=gain, in1=hr, op0=ALU.mult, op1=ALU.mult),
             reads=[pskey, "hr", "vec"], writes=[("kn", a)])
        knb = W["knb"][0:32, a, 0:n]
        P.op("pool", lambda e: e.tensor_copy(out=knb, in_=kn[0:32, :]), reads=[("kn", a)], writes=[("knb", a)])
        return (a, n, kn, knb, rope, out_bf, outkey, W)

    def head_norm_C(self, st, rotbank=4):
        if st is None:
            return
        P = self.P
        a, n, kn, knb, rope, out_bf, outkey, W = st
        C, S, rkey = rope
        psr = self.ps[0:32, rotbank, 0:n]
        rt = self.cview("rt", 32)
        P.op("pe", lambda e: e.matmul(psr, lhsT=rt, rhs=knb, start=True, stop=True), reads=[("knb", a), "cb"], writes=[("ps", rotbank)])
        t1 = W["t1"][0:32, 0:n]
        t2 = W["t2"][0:32, 0:n]
        P.op("dve", lambda e: e.tensor_tensor(out=t1, in0=psr, in1=S, op=ALU.mult), reads=[("ps", rotbank), rkey], writes=["t1"])
        P.op("pool", lambda e: e.tensor_tensor(out=t2, in0=kn[0:32, :], in1=C, op=ALU.mult), reads=[("kn", a), rkey], writes=["t2"])
        P.op("pool", lambda e: e.tensor_tensor(out=kn[0:32, :], in0=t1, in1=t2, op=ALU.add), reads=["t1", "t2"], writes=[("kn", a)])
        P.op("act", lambda e: e.activation(out=out_bf, in_=kn, func=AF.Copy), reads=[("kn", a)], writes=[outkey])

    def head_pipeline(self, heads):
        n = len(heads)
        stB = [None] * n
        res = [None] * n
        for step in range(n + 2):
            if step < n:
                res[step] = heads[step]["A"]()
            i = step - 1
            if 0 <= i < n:
                hd = heads[i]
                if hd.get("B") is not None:
                    stB[i] = self.head_norm_B(res[i][0], res[i][1], **hd["B"])
                elif hd.get("raw") is not None:
                    hd["raw"](res[i][0], res[i][1])
            i = step - 2
            if 0 <= i < n:
                self.head_norm_C(stB[i])
                if heads[i].get("post") is not None:
                    heads[i]["post"]()

    def head_temps(self, A):
        W = {}
        W["hsq"] = A.take(2 * TT * 2, BF16, "p (c t) -> p c t", c=2)
        W["hl"] = A.take(TT * 4, F32)
        W["hr"] = A.take(TT * 4, F32)
        W["kn"] = A.take(2 * TT * 4, F32, "p (c t) -> p c t", c=2)
        W["t1"] = A.take(TT * 4, F32)
        W["t2"] = A.take(TT * 4, F32)
        W["knb"] = A.take(2 * TT * 2, BF16, "p (c t) -> p c t", c=2)
        W["rc"] = A.take(TT * 4, F32)
        W["rsn"] = A.take(TT * 4, F32)
        return W

    def pass1a(self, s, li, M):
        P, A = self.P, self.A
        self.hn_ctr = 0
        xt, h = M["xt"], M["h"]
        NS1 = 3
        wring = A.take(NS1 * 16384, BF16, "p (s x) -> p s x", s=NS1)
        W = self.head_temps(A)
        M["sq"] = A.take(DC * TT * 2, BF16, "p (c t) -> p c t", c=DC)
        stage = A.take(4 * TT * 2, BF16, "p (c t) -> p c t", c=4)
        lf1 = A.take(TT * 4, F32)
        lf2 = A.take(TT * 4, F32)
        ones8 = A.take(TT * 4, F32)
        negb = A.take(64, F32)
        wfs = A.take(DC * 8 * 2, BF16, "p (c x) -> p c x", c=DC)
        wkey = None
        cfull = M["cfull"]
        wfm1 = self.wview(li, "wfm1").rearrange("(g p x) -> g p x", g=16, p=128)
        wtm = self.wview(li, "wtm").rearrange("(g p x) -> g p x", g=3, p=128)
        P.dma("sp", wfs, self.wview(li, "wf").rearrange("(p c x) -> p c x", p=128, c=DC), reads=self.wkeys(li, "wf"), writes=["wfs"])
        P.op("dve", lambda e: e.memset(ones8, 1.0), writes=["ones8"])
        P.op("dve", lambda e: e.tensor_scalar(out=negb[0:8, 0:1], in0=self.vcol(li, 70, rows=8), scalar1=-1.0, scalar2=None, op0=ALU.mult),
             reads=["vec"], writes=["negb"])
        sctr = 0
        stg = 0
        for tt in range(NTT):
            tsl = slice(tt * TT, (tt + 1) * TT)
            if tt == 0:
                self.load_x(M, s, tt)
            self.norm_tile(xt, h, M["sq"], M["tmp"], M["rs"], li, 16, "xt", "h", 0)
            if tt + 1 < NTT:
                self.load_x(M, s, tt + 1)
            P.dma("sp", W["rc"][0:32, :], self.ropec.ap()[:, tsl], writes=["rope"])
            P.dma("sp", W["rsn"][0:32, :], self.ropes.ap()[:, tsl], writes=["rope"])
            heads = []
            PB = (1, 2, 6, 7)
            for cc in range(16):
                def A_(cc=cc):
                    nonlocal sctr
                    sl = sctr % NS1; sctr += 1
                    P.dma("sp", wring[:, sl, 0:2048], wfm1[cc], reads=self.wkeys(li, "wfm1"), writes=[("ws", sl)])
                    w = wring[:, sl, 0:2048].rearrange("p (c x) -> p c x", c=DC)
                    pb = PB[cc % 4]
                    psp = self.ps[:, pb, :]
                    for dc in range(DC):
                        P.op("pe", lambda e, o=psp, ww=w[:, dc, :], r=h[:, dc, :], s_=(dc == 0), t_=(dc == DC - 1):
                             e.matmul(o, lhsT=ww, rhs=r, start=s_, stop=t_),
                             reads=[("ws", sl), ("h", dc)], writes=[("ps", pb)])
                    return psp, ("ps", pb)
                st = stg % 4; stg += 1
                so = stage[:, st, :]
                hd = dict(A=A_)
                if cc < 4:
                    hd["raw"] = (lambda psp, pk, so=so, st=st: P.op("dve", lambda e: e.tensor_copy(out=so, in_=psp), reads=[pk], writes=[("stage", st)]))
                elif cc < 8:
                    hd["B"] = dict(n=TT, gain=self.vcol(li, 49 + (1 if cc < 6 else 2)), W=W, out_bf=so, outkey=("stage", st),
                                   rope=(W["rc"][0:32, :], W["rsn"][0:32, :], "rope"))
                else:
                    hd["B"] = dict(n=TT, gain=self.vcol(li, 53), W=W, out_bf=so, outkey=("stage", st))
                hd["post"] = (lambda cc=cc, so=so, st=st: P.dma("act", self.s_fm.ap()[cc, :, tsl], so, reads=[("stage", st)], writes=[("s_fm", cc)]))
                heads.append(hd)
            self.head_pipeline(heads)
            psf = self.ps[0:8, 5, :]
            for dc in range(DC):
                P.op("pe", lambda e, ww=wfs[:, dc, :], r=h[:, dc, :], s_=(dc == 0), t_=(dc == DC - 1):
                     e.matmul(psf, lhsT=ww, rhs=r, start=s_, stop=t_), reads=["wfs", ("h", dc)], writes=[("ps", 5)])
            P.op("act", lambda e: e.activation(out=lf1[0:8, :], in_=psf, func=AF.Exp, bias=negb[0:8, 0:1], scale=-1.0),
                 reads=[("ps", 5), "negb"], writes=["lf1"])
            P.op("act", lambda e: e.activation(out=lf2[0:8, :], in_=lf1[0:8, :], func=AF.Ln, bias=1.0, scale=1.0),
                 reads=["lf1"], writes=["lf2"])
            P.op("dve", lambda e: e.tensor_scalar(out=lf1[0:8, :], in0=lf2[0:8, :], scalar1=-1.0 / SCALE, scalar2=None, op0=ALU.mult),
                 reads=["lf2"], writes=["lf1"])
            init = 0.0 if tt == 0 else cfull[0:8, tt * TT - 1:tt * TT]
            P.op("dve", lambda e, o=cfull[0:8, tsl], i=init: e.tensor_tensor_scan(out=o, data0=ones8[0:8, :], data1=lf1[0:8, :],
                                                                                   initial=i, op0=ALU.mult, op1=ALU.add),
                 reads=["lf1", "ones8", "cfull"], writes=["cfull"])
            n2 = 0
            for cg in range(3):
                sl = sctr % NS1; sctr += 1
                P.dma("sp", wring[:, sl, :], wtm[cg], reads=self.wkeys(li, "wtm"), writes=[("ws", sl)])
                w = wring[:, sl, :].rearrange("p (c x) -> p c x", c=DC)
                for tk in range(4):
                    pb = 6 + n2 % 2; n2 += 1
                    psp = self.ps[:, pb, :]
                    for dc in range(DC):
                        P.op("pe", lambda e, o=psp, l_=h[:, dc, tk * 128:(tk + 1) * 128], r=w[:, dc, :], s_=(dc == 0), t_=(dc == DC - 1):
                             e.matmul(o, lhsT=l_, rhs=r, start=s_, stop=t_),
                             reads=[("ws", sl), ("h", dc)], writes=[("ps", pb)])
                    st = stg % 4; stg += 1
                    so = stage[:, st, :]
                    if n2 % 2:
                        P.op("act", lambda e, o=so, i=psp: e.activation(out=o, in_=i, func=AF.Copy), reads=[("ps", pb)], writes=[("stage", st)])
                    else:
                        P.op("dve", lambda e, o=so, i=psp: e.tensor_copy(out=o, in_=i), reads=[("ps", pb)], writes=[("stage", st)])
                    kt = tt * 4 + tk
                    P.dma("act", self.s_tm.ap()[cg * 4:(cg + 1) * 4, :, kt, :].rearrange("g p d -> p g d"),
                          so.rearrange("p (g d) -> p g d", g=4), reads=[("stage", st)], writes=[("s_tm", cg)])

    def pass1b(self, s, li, M):
        P, A = self.P, self.A
        wkey = ("w", li)
        W = self.head_temps(A)
        kcraw = A.take(4 * T * 2, BF16, "p (c t) -> p c t", c=4)
        cw1 = A.take(2 * 32 * 256 * 2, BF16, "p (k l j) -> p k l j", k=2, l=32)
        cw2 = A.take(2 * 2 * 128 * 2, BF16, "p (k c x) -> p k c x", k=2, c=2)
        posb = A.take(64 * 2, BF16, "p (k l) -> p k l", k=2)
        hid = A.take(2 * 128 * 2, BF16, "p (c x) -> p c x", c=2)
        bias = A.take(16, F32)
        stage = A.take(2 * 128 * 2, BF16, "p (c x) -> p c x", c=2)
        rkc = A.take(2 * 128 * 4, F32, "p (c x) -> p c x", c=2)
        c3 = A.take(3 * T * 2, BF16, "p (c t) -> p c t", c=3)
        n3 = A.take(3 * T * 2, BF16, "p (c t) -> p c t", c=3)
        o3 = A.take(3 * T * 2, BF16, "p (c t) -> p c t", c=3)
        r1 = A.take(T * 4, F32)
        cfull = M["cfull"]
        P.dma("sp", kcraw, self.s_fm.ap()[0:4, :, :].rearrange("c p t -> p c t"), reads=[("s_fm", c) for c in range(4)], writes=["kcraw"])
        P.dma("sp", cw1, self.wview(li, "cw1").rearrange("(k p l j) -> p k l j", k=2, p=128, l=32), reads=self.wkeys(li, "cw1"), writes=["cw1"])
        P.dma("sp", cw2, self.wview(li, "cw2").rearrange("(k p c x) -> p k c x", k=2, p=128, c=2), reads=self.wkeys(li, "cw2"), writes=["cw2"])
        P.dma("sp", posb, self.wview(li, "pos").rearrange("(p k l) -> p k l", p=128, k=2), reads=self.wkeys(li, "pos"), writes=["posb"])
        P.dma("sp", rkc[0:32, :, :], self.ropekc.ap(), writes=["rkc"])
        cf = cfull[0:8, :]
        P.op("dve", lambda e: e.tensor_copy(out=c3[0:8, 0, :], in_=cf), reads=["cfull"], writes=["c3"])
        P.op("dve", lambda e: e.tensor_tensor(out=r1[0:8, :], in0=cf, in1=c3[0:8, 0, :], op=ALU.subtract), reads=["c3", "cfull"], writes=["r1"])
        P.op("dve", lambda e: e.tensor_copy(out=c3[0:8, 1, :], in_=r1[0:8, :]), reads=["r1"], writes=["c3"])
        P.op("dve", lambda e: e.tensor_tensor(out=r1[0:8, :], in0=r1[0:8, :], in1=c3[0:8, 1, :], op=ALU.subtract), reads=["c3"], writes=["r1"])
        P.op("dve", lambda e: e.tensor_copy(out=c3[0:8, 2, :], in_=r1[0:8, :]), reads=["r1"], writes=["c3"])
        P.op("dve", lambda e: e.tensor_scalar(out=n3[0:8, :, :], in0=c3[0:8, :, :], scalar1=-1.0, scalar2=None, op0=ALU.mult), reads=["c3"], writes=["n3"])
        P.op("pool", lambda e: e.memset(o3[0:8, :, :], 1.0), writes=["o3"])
        ab = self.s_ab.ap()
        P.dma("sp", ab[:, 0, 0:3, :], o3[0:8, :, :], reads=["o3"], writes=["s_ab"])
        P.dma("sp", ab[:, 0, 3:6, :], c3[0:8, :, :], reads=["c3"], writes=["s_ab"])
        P.dma("sp", ab[:, 1, 0:3, :], n3[0:8, :, :], reads=["n3"], writes=["s_ab"])
        P.dma("sp", ab[:, 1, 3:6, :], o3[0:8, :, :], reads=["o3"], writes=["s_ab"])
        self.hn_ctr = 0
        nq = 0
        for kv in range(2):
            for jc in range(2):
                for l in range(32):
                    P.op("pe", lambda e, o=self.ps[:, 0, kv * 2 + jc:kv * 2 + jc + 1], ww=cw1[:, kv, l, jc * 128:(jc + 1) * 128], r=posb[:, kv, l:l + 1],
                         s_=(l == 0), t_=(l == 31): e.matmul(o, lhsT=ww, rhs=r, start=s_, stop=t_),
                         reads=["cw1", "posb"], writes=[("ps", 0)])
            P.op("dve", lambda e, o=bias[:, kv * 2:kv * 2 + 2], i=self.ps[:, 0, kv * 2:kv * 2 + 2]: e.tensor_copy(out=o, in_=i),
                 reads=[("ps", 0)], writes=["cbias"])
            for kvh in range(2):
                raw = kcraw[:, kv * 2 + kvh, :]
                for jc in range(2):
                    pb = 1 + jc
                    psh = self.ps[:, pb, 0:NCMP]
                    for l in range(32):
                        P.op("pe", lambda e, o=psh, ww=cw1[:, kv, l, jc * 128:(jc + 1) * 128], r=raw[:, l:l + 16 * (NCMP - 1) + 1:16],
                             s_=(l == 0), t_=(l == 31): e.matmul(o, lhsT=ww, rhs=r, start=s_, stop=t_),
                             reads=["cw1", "kcraw"], writes=[("ps", pb)])
                    P.op("act", lambda e, o=hid[:, jc, 0:NCMP], i=psh, b=bias[:, kv * 2 + jc:kv * 2 + jc + 1]:
                         e.activation(out=o, in_=i, func=AF.Gelu_apprx_tanh, bias=b),
                         reads=[("ps", pb), "cbias"], writes=[("hid", jc)])
                st = nq % 2; nq += 1
                if kv == 0:
                    psk = self.ps[:, 5, 0:NCMP]
                    for jc in range(2):
                        P.op("pe", lambda e, ww=cw2[:, 0, jc, :], r=hid[:, jc, 0:NCMP], s_=(jc == 0), t_=(jc == 1):
                             e.matmul(psk, lhsT=ww, rhs=r, start=s_, stop=t_), reads=["cw2", ("hid", jc)], writes=[("ps", 5)])
                    self.head_norm(psk, ("ps", 5), NCMP, self.vcol(li, 49), W, stage[:, st, 0:NCMP], ("stage", st),
                                   rope=(rkc[0:32, 0, 0:NCMP], rkc[0:32, 1, 0:NCMP], "rkc"))
                    P.dma("sp", self.s_kc.ap()[kvh, :, 0:NCMP], stage[:, st, 0:NCMP], reads=[("stage", st)], writes=["s_kc"])
                else:
                    psv = self.ps[0:NCMP, 6, 0:128]
                    for jc in range(2):
                        P.op("pe", lambda e, l_=hid[:, jc, 0:NCMP], r=cw2[:, 1, jc, :], s_=(jc == 0), t_=(jc == 1):
                             e.matmul(psv, lhsT=l_, rhs=r, start=s_, stop=t_), reads=["cw2", ("hid", jc)], writes=[("ps", 6)])
                    P.op("dve", lambda e, o=stage[0:NCMP, st, :]: e.tensor_copy(out=o, in_=psv), reads=[("ps", 6)], writes=[("stage", st)])
                    P.dma("sp", self.s_vc.ap()[kvh, 0:NCMP, :], stage[0:NCMP, st, :], reads=[("stage", st)], writes=["s_vc"])

    def attn_multi(self, jobs):
        P = self.P
        ones = self.cview("ones")
        flat = [(ji, ti) for ji, job in enumerate(jobs) for ti in range(len(job["tiles"]))]

        def emit_qk(ji, ti):
            job = jobs[ji]
            t = job["tiles"][ti]
            sb = self.sctr % 2; self.sctr += 1
            nk = t["nk"]
            pss = self.ps[0:nk, sb, :]
            mm = [(t["k"], job["q"], list(t["kkeys"]) + [job["qkey"]])] + list(t["extras"])
            for m, (l_, r_, keys) in enumerate(mm):
                P.op("pe", lambda e, o=pss, l_=l_, r_=r_, s_=(m == 0), t_=(m == len(mm) - 1): e.matmul(o, lhsT=l_, rhs=r_, start=s_, stop=t_),
                     reads=list(keys) + ["cb"], writes=[("ps", sb)])
            return pss, sb, nk

        def emit_rest(ji, ti, info):
            job = jobs[ji]
            t = job["tiles"][ti]
            n = len(job["tiles"])
            pss, sb, nk = info
            lb, ob = job["lo"]
            p = self.pctr % 3; self.pctr += 1
            ptile = self.pt[0:nk, p, :]
            P.op("act", lambda e, o=ptile, i_=pss: e.activation(out=o, in_=i_, func=AF.Exp, scale=SCALE),
                 reads=[("ps", sb)], writes=[("pt", p)])
            P.op("pe", lambda e, l_=ones[0:nk, :], r_=ptile, s_=(ti == 0), t_=(ti == n - 1): e.matmul(self.ps[:, lb, :], lhsT=l_, rhs=r_, start=s_, stop=t_),
                 reads=[("pt", p), "cb"], writes=[("ps", lb)])
            P.op("pe", lambda e, l_=t["v"], r_=ptile, s_=(ti == 0), t_=(ti == n - 1): e.matmul(self.ps[:, ob, :], lhsT=l_, rhs=r_, start=s_, stop=t_),
                 reads=[("pt", p)] + list(t["vkeys"]), writes=[("ps", ob)])
            if ti == n - 1 and job.get("fin") is not None:
                d = job["fin"](ptile, ("pt", p))
                for f in deferred:
                    f()
                del deferred[:]
                if d is not None:
                    deferred.append(d)

        deferred = []
        pending = None
        for (ji, ti) in flat:
            if ti == 0 and jobs[ji].get("pre") is not None:
                jobs[ji]["pre"]()
            cur = emit_qk(ji, ti)
            if pending is not None:
                emit_rest(*pending)
            pending = (ji, ti, cur)
        if pending is not None:
            emit_rest(*pending)
        for f in deferred:
            f()

    def pass2(self, s, li, M):
        P, A = self.P, self.A
        self.hn_ctr = 0
        self.sctr = 0
        self.pctr = 0
        xt, h = M["xt"], M["h"]
        o_sb = h
        wkey = ("w", li)
        W = self.head_temps(A)
        NS2 = 4
        wring = A.take(NS2 * 4096, BF16, "p (s x) -> p s x", s=NS2)
        qall = A.take(16 * TT * 2, BF16, "p (c t) -> p c t", c=16)
        gsb = A.take(TT * 2, BF16)
        xst = A.take(4 * TT * 4, F32, "p (c t) -> p c t", c=4)
        wgs = A.take(DC * 24 * 2, BF16, "p (c x) -> p c x", c=DC)
        NKV = 6
        kvr = A.take(NKV * 4096, BF16, "p (s x) -> p s x", s=NKV)
        abA = A.take(2 * TT * 2, BF16, "p (c t) -> p c t", c=2)
        kcs = A.take(2 * 2 * 128 * 2, BF16, "p (a b x) -> p a b x", a=2, b=2)
        self.pt = A.take(3 * TT * 2, BF16, "p (c t) -> p c t", c=3)
        acc = A.take(4 * TT * 4, F32, "p (c t) -> p c t", c=4)
        rl = A.take(2 * TT * 4, F32, "p (c t) -> p c t", c=2)
        wg = A.take(TT * 4, F32)
        gtmp = wg
        of = A.take(2 * TT * 4, F32, "p (c t) -> p c t", c=2)
        osq = A.take(2 * TT * 2, BF16, "p (c t) -> p c t", c=2)
        phat = A.take(2 * TT * 2, BF16, "p (c t) -> p c t", c=2)
        score = A.take(4 * 32 * 4, F32, "p (c x) -> p c x", c=4)
        mx8 = A.take(4 * 8 * 4, F32, "p (c x) -> p c x", c=4)
        selb = A.take(4 * 32 * 2, BF16, "p (c x) -> p c x", c=4)
        selbT = A.take(TT * 2, BF16)
        selct = A.take(2 * 4 * 32 * 4, F32, "p (a c x) -> p a c x", a=2, c=4)
        ssq = A.take(2 * TT * 4, F32, "p (c t) -> p c t", c=2)
        rstd = A.take(2 * TT * 4, F32, "p (c t) -> p c t", c=2)
        otmp = A.take(2 * TT * 4, F32, "p (c t) -> p c t", c=2)
        ident = self.cview("ident")
        ones = self.cview("ones")
        ovl = self.cview("ovl")
        cbv = self.cview("cb").rearrange("p (k n) -> p k n", k=4)
        wbv = self.cview("wb").rearrange("p (k n) -> p k n", k=4)
        cmask = self.cview("cmask")
        exv = self.cview("ex", 32).rearrange("p (k n) -> p k n", k=16)
        eselv = self.cview("esel", 32).rearrange("p (k n) -> p k n", k=24)
        wfm2 = self.wview(li, "wfm2").rearrange("(g p x) -> g p x", g=16, p=128)
        wout = self.wview(li, "wout").rearrange("(g p x) -> g p x", g=16, p=128)
        P.dma("sp", wgs, self.wview(li, "wgt").rearrange("(p c x) -> p c x", p=128, c=DC), reads=self.wkeys(li, "wgt"), writes=["wgs"])
        P.dma("sp", kcs[:, :, 0, :], self.s_kc.ap().rearrange("k p x -> p k x"), reads=["s_kc"], writes=["kcs"])
        P.dma("sp", kcs[:, :, 1, :], self.s_vc.ap().rearrange("k p x -> p k x"), reads=["s_vc"], writes=["kcs"])
        sctr = 0
        kvc = 0
        rctr = 0
        yctr = 0
        actr = 0
        zctr = 0
        loc = 0
        for j in range(NTT):
            tsl = slice(j * TT, (j + 1) * TT)
            nkt = 4 * (j + 1)
            nk = nkt * 128
            if j == 0:
                self.load_x(M, s, j)
            self.norm_tile(xt, h, qall, wg, M["rs"], li, 16, "xt", "h", 0, sqkey="qa", tmpkey="wg")
            if j + 1 < NTT:
                self.load_x(M, s, j + 1)
            P.dma("sp", W["rc"][0:32, :], self.ropec.ap()[:, tsl], writes=["rope"])
            P.dma("sp", W["rsn"][0:32, :], self.ropes.ap()[:, tsl], writes=["rope"])
            P.dma("sp", selct, self.selc.ap()[:, :, 4 * j:4 * j + 4, :], writes=["selct"])
            heads = []
            PB = (1, 2, 5, 6)
            for cc in range(16):
                def A_(cc=cc):
                    nonlocal sctr
                    sl = sctr % NS2; sctr += 1
                    P.dma("sp", wring[:, sl, :], wfm2[cc], reads=self.wkeys(li, "wfm2"), writes=[("ws", sl)])
                    w = wring[:, sl, :].rearrange("p (c x) -> p c x", c=DC)
                    pb = PB[cc % 4]
                    psp = self.ps[:, pb, :]
                    for dc in range(DC):
                        P.op("pe", lambda e, o=psp, ww=w[:, dc, :], r=h[:, dc, :], s_=(dc == 0), t_=(dc == DC - 1):
                             e.matmul(o, lhsT=ww, rhs=r, start=s_, stop=t_),
                             reads=[("ws", sl), ("h", dc)], writes=[("ps", pb)])
                    return psp, ("ps", pb)
                if cc < 8:
                    B = dict(n=TT, gain=self.vcol(li, 48), W=W, out_bf=qall[:, cc, :], outkey=("qa", cc),
                             rope=(W["rc"][0:32, :], W["rsn"][0:32, :], "rope"))
                else:
                    B = dict(n=TT, gain=self.vcol(li, 52), W=W, out_bf=qall[:, cc, :], outkey=("qa", cc))
                heads.append(dict(A=A_, B=B))
            self.head_pipeline(heads)
            psg = self.ps[0:24, 7, :]
            for dc in range(DC):
                P.op("pe", lambda e, ww=wgs[:, dc, :], r=h[:, dc, :], s_=(dc == 0), t_=(dc == DC - 1):
                     e.matmul(psg, lhsT=ww, rhs=r, start=s_, stop=t_), reads=["wgs", ("h", dc)], writes=[("ps", 7)])
            P.op("act", lambda e: e.activation(out=gtmp[0:24, :], in_=psg, func=AF.Exp, scale=-1.0), reads=[("ps", 7)], writes=["wg"])
            P.op("dve", lambda e: e.tensor_scalar(out=gtmp[0:24, :], in0=gtmp[0:24, :], scalar1=1.0, scalar2=None, op0=ALU.add), reads=["wg"], writes=["wg"])
            P.op("dve", lambda e: e.reciprocal(out=gsb[0:24, :], in_=gtmp[0:24, :]), reads=["wg"], writes=["gsb"])
            for kvh in range(2):
                ch = []
                for _ in range(4):
                    ch.append(kvc % NKV); kvc += 1
                c_ks, c_kw, c_vs, c_vw = ch
                P.dma("sp", kvr[:, c_ks, 0:nk], self.s_fm.ap()[4 + kvh, :, 0:nk], reads=[("s_fm", 4 + kvh)], writes=[("kv", c_ks)])
                P.dma("sp", kvr[:, c_vs, 0:nk], self.s_tm.ap()[0 + kvh, :, 0:nkt, :].rearrange("p k d -> p (k d)"), reads=[("s_tm", 0)], writes=[("kv", c_vs)])
                P.dma("sp", kvr[:, c_kw, 0:nk], self.s_fm.ap()[6 + kvh, :, 0:nk], reads=[("s_fm", 6 + kvh)], writes=[("kv", c_kw)])
                P.dma("sp", kvr[:, c_vw, 0:nk], self.s_tm.ap()[2 + kvh, :, 0:nkt, :].rearrange("p k d -> p (k d)"), reads=[("s_tm", 0)], writes=[("kv", c_vw)])
                ksT = kvr[:, c_ks, :]
                kwT = kvr[:, c_kw, :]
                vs = kvr[:, c_vs, :].rearrange("p (k d) -> p k d", d=128)
                vw = kvr[:, c_vw, :].rearrange("p (k d) -> p k d", d=128)
                kcT = kcs[:, kvh, 0, 0:NCMP]
                vc = kcs[0:NCMP, kvh, 1, :]

                def fin_branch(g, b, lo, first, kvh=kvh):
                    nonlocal rctr, yctr
                    lb, ob = lo
                    x_ = rctr % 2; rctr += 1
                    rlb = rl[:, x_, :]
                    r = ((kvh * 4 + g) * 3 + b)
                    P.op("pe", lambda e: e.matmul(self.ps[:, 7, :], lhsT=eselv[0:24, r, :], rhs=gsb[0:24, :], start=True, stop=True),
                         reads=["gsb", "cb"], writes=[("ps", 7)])
                    P.op("dve", lambda e: e.tensor_scalar(out=rlb, in0=self.ps[:, lb, :], scalar1=1e-30, scalar2=None, op0=ALU.max),
                         reads=[("ps", lb)], writes=[("rl", x_)])
                    P.op("dve", lambda e: e.reciprocal(out=rlb, in_=rlb), reads=[("rl", x_)], writes=[("rl", x_)])
                    P.op("dve", lambda e: e.tensor_tensor(out=wg, in0=self.ps[:, 7, :], in1=rlb, op=ALU.mult),
                         reads=[("ps", 7), ("rl", x_)], writes=["wg"])
                    if first:
                        P.op("dve", lambda e: e.tensor_tensor(out=acc[:, g, :], in0=self.ps[:, ob, :], in1=wg, op=ALU.mult),
                             reads=[("ps", ob), "wg"], writes=[("acc", g)])
                    else:
                        y_ = yctr % 2; yctr += 1
                        P.op("dve", lambda e: e.tensor_tensor(out=otmp[:, y_, :], in0=self.ps[:, ob, :], in1=wg, op=ALU.mult),
                             reads=[("ps", ob), "wg"], writes=[("otmp", y_)])
                        P.op("pool", lambda e: e.tensor_tensor(out=acc[:, g, :], in0=acc[:, g, :], in1=otmp[:, y_, :], op=ALU.add),
                             reads=[("otmp", y_)], writes=[("acc", g)])
                    return x_

                jobs = []
                for g in range(4):
                    hq = kvh * 4 + g
                    lo = (3, 4) if loc % 2 == 0 else (5, 6); loc += 1
                    tiles = [dict(k=kcT, kkeys=["kcs"], nk=NCMP, extras=[(ident[0:NCMP, 0:NCMP], cmask[0:NCMP, tsl], [])],
                                  v=vc, vkeys=["kcs"])]

                    def fin_cmp(ptile, pkey, g=g, lo=lo):
                        nonlocal zctr
                        x_ = fin_branch(g, 0, lo, True)
                        z_ = zctr % 2; zctr += 1
                        P.op("pool", lambda e, o=phat[0:NCMP, z_, :], a=ptile, b=rl[0:NCMP, x_, :]: e.tensor_tensor(out=o, in0=a, in1=b, op=ALU.mult),
                             reads=[pkey, ("rl", x_)], writes=[("phat", z_)])
                        def dfr():
                            for tk in range(4):
                                P.op("pe", lambda e, o=self.ps[:, 2, tk * 32:(tk + 1) * 32], l_=phat[0:NCMP, z_, tk * 128:(tk + 1) * 128], r_=ovl[0:NCMP, :],
                                     s_=(g == 0 and tk == 0), t_=(g == 3): e.matmul(o, lhsT=l_, rhs=r_, start=s_, stop=t_, skip_group_check=True),
                                     reads=[("phat", z_), "cb"], writes=[("ps", 2)])
                        return dfr
                    jobs.append(dict(q=qall[:, hq, :], qkey=("qa", hq), tiles=tiles, lo=lo, fin=fin_cmp))
                self.attn_multi(jobs)
                imp = self.ps[:, 2, 0:128].rearrange("p (c x) -> p c x", c=4)
                P.op("dve", lambda e: e.tensor_tensor(out=score, in0=imp, in1=selct[:, 0, :, :], op=ALU.mult),
                     reads=[("ps", 2), "selct"], writes=["score"])
                P.op("dve", lambda e: e.tensor_tensor(out=score, in0=score, in1=selct[:, 1, :, :], op=ALU.add),
                     reads=["selct"], writes=["score"])
                for tk in range(4):
                    P.op("dve", lambda e, o=mx8[:, tk, :], i_=score[:, tk, :]: e.max(out=o, in_=i_), reads=["score"], writes=[("mx8", tk)])
                for tk in range(4):
                    P.op("dve", lambda e, o=selb[:, tk, :], i_=score[:, tk, :], th=mx8[:, tk, 7:8]:
                         e.tensor_scalar(out=o, in0=i_, scalar1=th, scalar2=NEG, op0=ALU.is_lt, op1=ALU.mult),
                         reads=["score", ("mx8", tk)], writes=[("selb", tk)])
                for tk in range(4):
                    P.op("pe", lambda e, o=self.ps[0:32, 2, tk * 128:(tk + 1) * 128], l_=selb[:, tk, :]:
                         e.matmul(o, lhsT=l_, rhs=ident, start=True, stop=True),
                         reads=[("selb", tk), "cb"], writes=[("ps", 2)])
                P.op("dve", lambda e: e.tensor_copy(out=selbT[0:32, :], in_=self.ps[0:32, 2, :]), reads=[("ps", 2)], writes=["selbT"])
                jobs = []
                for g in range(4):
                    hq = kvh * 4 + g
                    lo = (3, 4) if loc % 2 == 0 else (5, 6); loc += 1
                    tiles = []
                    for kt in range(nkt):
                        ex = [(exv[0:32, kt, :], selbT[0:32, :], ["selbT"])]
                        if kt >= 4 * j:
                            ex.append((ident, cbv[:, kt - 4 * j, :], []))
                        tiles.append(dict(k=ksT[:, kt * 128:(kt + 1) * 128], kkeys=[("kv", c_ks)], nk=128, extras=ex,
                                          v=vs[:, kt, :], vkeys=[("kv", c_vs)]))
                    jobs.append(dict(q=qall[:, hq, :], qkey=("qa", hq), tiles=tiles, lo=lo,
                                     fin=(lambda ptile, pkey, g=g, lo=lo: (fin_branch(g, 1, lo, False), None)[1])))
                for g in range(4):
                    hq = kvh * 4 + g
                    lo = (3, 4) if loc % 2 == 0 else (5, 6); loc += 1
                    tiles = []
                    for kt in range(max(0, 4 * j - 4), nkt):
                        if kt >= 4 * j:
                            ex = [(ident, cbv[:, kt - 4 * j, :], [])]
                        else:
                            ex = [(ident, wbv[:, kt - (4 * j - 4), :], [])]
                        tiles.append(dict(k=kwT[:, kt * 128:(kt + 1) * 128], kkeys=[("kv", c_kw)], nk=128, extras=ex,
                                          v=vw[:, kt, :], vkeys=[("kv", c_vw)]))
                    jobs.append(dict(q=qall[:, hq, :], qkey=("qa", hq), tiles=tiles, lo=lo,
                                     fin=(lambda ptile, pkey, g=g, lo=lo: (fin_branch(g, 2, lo, False), None)[1])))
                self.attn_multi(jobs)
                sl_of = {}
                def fsq(g):
                    nonlocal actr
                    a_ = actr % 2; actr += 1
                    sl_of[g] = a_
                    P.op("pool", lambda e, o=osq[:, a_, :], i_=acc[:, g, :]: e.tensor_tensor(out=o, in0=i_, in1=i_, op=ALU.mult),
                         reads=[("acc", g)], writes=[("osq", a_)])
                    P.op("dve", lambda e, o=o_sb[:, kvh * 4 + g, :], i_=acc[:, g, :], gc=self.vcol(li, 54 + kvh * 4 + g):
                         e.tensor_scalar(out=o, in0=i_, scalar1=gc, scalar2=None, op0=ALU.mult),
                         reads=[("acc", g), "vec"], writes=[("h", kvh * 4 + g)])
                def fsum(g):
                    a_ = sl_of[g]
                    hq = kvh * 4 + g
                    P.op("pe", lambda e, r_=osq[:, a_, :]: e.matmul(self.ps[:, 7, :], lhsT=ones, rhs=r_, start=True, stop=True),
                         reads=[("osq", a_), "cb"], writes=[("ps", 7)])
                    if hq == 0:
                        P.op("dve", lambda e: e.tensor_copy(out=ssq[:, 0, :], in_=self.ps[:, 7, :]), reads=[("ps", 7)], writes=[("ssq", 0)])
                    else:
                        P.op("dve", lambda e: e.tensor_tensor(out=ssq[:, 0, :], in0=self.ps[:, 7, :], in1=ssq[:, 0, :], op=ALU.add),
                             reads=[("ps", 7)], writes=[("ssq", 0)])
                fsq(0)
                for g in range(4):
                    if g + 1 < 4:
                        fsq(g + 1)
                    fsum(g)
            jobs = []
            for hf in range(8):
                ch = []
                for _ in range(3):
                    ch.append(kvc % NKV); kvc += 1
                c_k, c_v, c_b = ch
                a_ = hf % 2
                def pre_fox(hf=hf, c_k=c_k, c_v=c_v, c_b=c_b, a_=a_):
                    P.dma("sp", kvr[:, c_k, 0:nk], self.s_fm.ap()[8 + hf, :, 0:nk], reads=[("s_fm", 8 + hf)], writes=[("kv", c_k)])
                    P.dma("sp", kvr[0:6, c_b, 0:nk], self.s_ab.ap()[hf, 1, :, 0:nk], reads=["s_ab"], writes=[("kv", c_b)])
                    P.dma("sp", abA[0:6, a_, :], self.s_ab.ap()[hf, 0, :, tsl], reads=["s_ab"], writes=[("abA", a_)])
                    P.dma("sp", kvr[:, c_v, 0:nk], self.s_tm.ap()[4 + hf, :, 0:nkt, :].rearrange("p k d -> p (k d)"), reads=[("s_tm", 1), ("s_tm", 2)], writes=[("kv", c_v)])
                fk = kvr[:, c_k, :]
                fv = kvr[:, c_v, :].rearrange("p (k d) -> p k d", d=128)
                Bc = kvr[0:6, c_b, :]
                lo = (3, 4) if loc % 2 == 0 else (5, 6); loc += 1
                tiles = []
                for kt in range(nkt):
                    ex = [(Bc[:, kt * 128:(kt + 1) * 128], abA[0:6, a_, :], [("kv", c_b), ("abA", a_)])]
                    if kt >= 4 * j:
                        ex.append((ident, cbv[:, kt - 4 * j, :], []))
                    tiles.append(dict(k=fk[:, kt * 128:(kt + 1) * 128], kkeys=[("kv", c_k)], nk=128, extras=ex,
                                      v=fv[:, kt, :], vkeys=[("kv", c_v)]))

                def fin_fox(ptile, pkey, hf=hf, lo=lo):
                    nonlocal rctr, actr
                    lb, ob = lo
                    x_ = rctr % 2; rctr += 1
                    rlb = rl[:, x_, :]
                    P.op("dve", lambda e, o=rlb, i_=self.ps[:, lb, :]: e.reciprocal(out=o, in_=i_), reads=[("ps", lb)], writes=[("rl", x_)])
                    f_ = hf % 2
                    P.op("dve", lambda e, o=of[:, f_, :], a=self.ps[:, ob, :], b=rlb: e.tensor_tensor(out=o, in0=a, in1=b, op=ALU.mult),
                         reads=[("ps", ob), ("rl", x_)], writes=[("of", f_)])
                    q_ = actr % 2; actr += 1
                    P.op("pool", lambda e, o=osq[:, q_, :], i_=of[:, f_, :]: e.tensor_tensor(out=o, in0=i_, in1=i_, op=ALU.mult),
                         reads=[("of", f_)], writes=[("osq", q_)])
                    P.op("dve", lambda e, o=o_sb[:, 8 + hf, :], i_=of[:, f_, :], gc=self.vcol(li, 62 + hf):
                         e.tensor_scalar(out=o, in0=i_, scalar1=gc, scalar2=None, op0=ALU.mult),
                         reads=[("of", f_), "vec"], writes=[("h", 8 + hf)])

                    def dfr():
                        P.op("pe", lambda e, r_=osq[:, q_, :]: e.matmul(self.ps[:, 7, :], lhsT=ones, rhs=r_, start=True, stop=True),
                             reads=[("osq", q_), "cb"], writes=[("ps", 7)])
                        if hf == 0:
                            P.op("dve", lambda e: e.tensor_copy(out=ssq[:, 1, :], in_=self.ps[:, 7, :]), reads=[("ps", 7)], writes=[("ssq", 1)])
                        else:
                            P.op("dve", lambda e: e.tensor_tensor(out=ssq[:, 1, :], in0=self.ps[:, 7, :], in1=ssq[:, 1, :], op=ALU.add),
                                 reads=[("ps", 7)], writes=[("ssq", 1)])
                    return dfr
                jobs.append(dict(q=qall[:, 8 + hf, :], qkey=("qa", 8 + hf), tiles=tiles, lo=lo, fin=fin_fox, pre=pre_fox))
            self.attn_multi(jobs)
            for k in range(2):
                P.op("act", lambda e, o=rstd[:, k, :], i_=ssq[:, k, :]: e.activation(out=o, in_=i_, func=AF.Ln, bias=EPS, scale=1.0 / 1024.0),
                     reads=[("ssq", k)], writes=[("rstd", k)])
                P.op("act", lambda e, o=rstd[:, k, :]: e.activation(out=o, in_=o, func=AF.Exp, scale=-0.5),
                     reads=[("rstd", k)], writes=[("rstd", k)])
            for kc in range(16):
                P.op("dve" if kc % 2 == 0 else "pool", lambda e, o=o_sb[:, kc, :], b=rstd[:, kc // 8, :]: e.tensor_tensor(out=o, in0=o, in1=b, op=ALU.mult),
                     reads=[("rstd", kc // 8)], writes=[("h", kc)])
            def issue(dco):
                nonlocal sctr
                sl = sctr % NS2; sctr += 1
                k = dco % 4
                P.dma("sp", wring[:, sl, :], wout[dco], reads=self.wkeys(li, "wout"), writes=[("ws", sl)])
                P.dma("sp", xst[:, k, :], M["src"].ap()[s, dco, :, tsl], reads=[("X", s, j, dco)], writes=[("xst", k)])
                return sl
            slots = {0: issue(0), 1: issue(1)}
            for dco in range(DC):
                if dco + 2 < DC:
                    slots[dco + 2] = issue(dco + 2)
                sl = slots[dco]
                k = dco % 4
                w = wring[:, sl, :].rearrange("p (c x) -> p c x", c=16)
                bnk = dco % 2
                for kc in range(16):
                    P.op("pe", lambda e, o=self.ps[:, bnk, :], ww=w[:, kc, :], r=o_sb[:, kc, :], s_=(kc == 0), t_=(kc == 15):
                         e.matmul(o, lhsT=ww, rhs=r, start=s_, stop=t_),
                         reads=[("ws", sl), ("h", kc)], writes=[("ps", bnk)])
                P.op("dve", lambda e, o=xst[:, k, :], a=self.ps[:, bnk, :]: e.tensor_tensor(out=o, in0=a, in1=o, op=ALU.add),
                     reads=[("ps", bnk)], writes=[("xst", k)])
                P.dma("act", self.xs.ap()[s, dco, :, tsl], xst[:, k, :], reads=[("xst", k)], writes=[("X", s, j, dco)])


def _pack_layer(w, li):
    out = np.zeros(NPK, np.float32)

    def put(name, arr):
        o, n = PK[name]
        a = np.ascontiguousarray(arr, dtype=np.float32).reshape(-1)
        assert a.size == n, (name, a.size, n)
        out[o:o + n] = a

    for f in (1, 2):
        wg = w[f"ffn{f}_w_gate"][li]
        wu = w[f"ffn{f}_w_up"][li]
        wd = w[f"ffn{f}_w_down"][li]
        put(f"wg{f}", wg.reshape(DC, 128, 11, 512).transpose(2, 1, 0, 3))
        put(f"wu{f}", wu.reshape(DC, 128, 11, 512).transpose(2, 1, 0, 3))
        put(f"wd{f}", wd.reshape(FC, 128, DC, 128).transpose(2, 1, 0, 3))
    win = w["w_in"][li]
    kv0 = 1024
    def kvcol(branch, typ, kvh):
        return kv0 + ((branch * 2 + typ) * 2 + kvh) * 128
    fq0 = 2584
    fk0 = fq0 + 1024
    fv0 = fq0 + 2048
    cols1 = ([kvcol(0, 0, 0), kvcol(0, 0, 1), kvcol(0, 1, 0), kvcol(0, 1, 1),
              kvcol(1, 0, 0), kvcol(1, 0, 1), kvcol(2, 0, 0), kvcol(2, 0, 1)]
             + [fk0 + h * 128 for h in range(8)])
    def fm(cols):
        blk = np.stack([win[:, c:c + 128] for c in cols], 0)
        return blk.reshape(len(cols), DC, 128, 128).transpose(0, 2, 1, 3)
    put("wfm1", fm(cols1))
    tmcols = np.concatenate([np.arange(kvcol(1, 1, 0), kvcol(1, 1, 0) + 256),
                             np.arange(kvcol(2, 1, 0), kvcol(2, 1, 0) + 256),
                             np.arange(fv0, fv0 + 1024)])
    wt = win[:, tmcols]
    put("wtm", wt.reshape(DC, 128, 3, 512).transpose(2, 1, 0, 3))
    cols2 = [h * 128 for h in range(8)] + [fq0 + h * 128 for h in range(8)]
    put("wfm2", fm(cols2))
    wo = w["w_out"][li]
    put("wout", wo.reshape(16, 128, DC, 128).transpose(2, 1, 0, 3))
    cw1 = w["cmp_w1"][li]
    put("cw1", cw1.reshape(2, 32, 128, 256).transpose(0, 2, 1, 3))
    cw2 = w["cmp_w2"][li]
    put("cw2", cw2.reshape(2, 2, 128, 128).transpose(0, 2, 1, 3))
    put("wf", win[:, 5656:5664].reshape(DC, 128, 8).transpose(1, 0, 2))
    put("wgt", win[:, 2560:2584].reshape(DC, 128, 24).transpose(1, 0, 2))
    pos = w["cmp_pos_emb"][li]
    put("pos", pos.transpose(2, 0, 1))
    return out


def _vec_pack(w, layers):
    v = np.zeros((128, len(layers) * VW), np.float32)
    for i, li in enumerate(layers):
        b = i * VW
        v[:, b + 0:b + 16] = w["ffn1_norm"][li].reshape(DC, 128).T
        v[:, b + 16:b + 32] = w["mix_norm"][li].reshape(DC, 128).T
        v[:, b + 32:b + 48] = w["ffn2_norm"][li].reshape(DC, 128).T
        v[:, b + 48] = w["nsa_q_norm"][li]
        v[:, b + 49:b + 52] = w["nsa_k_norm"][li].T
        v[:, b + 52] = w["fox_q_norm"][li]
        v[:, b + 53] = w["fox_k_norm"][li]
        v[:, b + 54:b + 62] = w["nsa_out_norm"][li].reshape(8, 128).T
        v[:, b + 62:b + 70] = w["fox_out_norm"][li].reshape(8, 128).T
        v[0:8, b + 70] = w["fox_forget_bias"][li]
    return v


def _consts():
    cb = np.zeros((128, NCB), np.float32)
    def put(name, arr):
        o, n = CB[name]
        cb[:arr.shape[0], o:o + n] = arr.reshape(arr.shape[0], -1)
    put("ident", np.eye(128, dtype=np.float32))
    put("ones", np.ones((128, 128), np.float32))
    rt = np.zeros((32, 32), np.float32)
    for i in range(16):
        rt[16 + i, i] = -1.0
        rt[i, 16 + i] = 1.0
    put("rt", rt)
    cstart = np.arange(NCMP) * 16
    sstart = np.arange(32) * 64
    ovl = ((cstart[:, None] < sstart[None, :] + 64) & (cstart[:, None] + 32 > sstart[None, :])).astype(np.float32)
    put("ovl", ovl)
    p = np.arange(128)[:, None]
    n = np.arange(512)[None, :]
    cbm = np.stack([np.where(128 * k + p <= n, 0.0, NEG) for k in range(4)], 1)
    wbm = np.stack([np.where(128 * k + p > n, 0.0, NEG) for k in range(4)], 1)
    put("cb", cbm.astype(np.float32))
    put("wb", wbm.astype(np.float32))
    cend = cstart + 31
    t = np.arange(T)[None, :]
    put("cmask", np.where(cend[:, None] <= t, 0.0, NEG).astype(np.float32))
    j = np.arange(32)[:, None, None]
    kt = np.arange(16)[None, :, None]
    pp = np.arange(128)[None, None, :]
    put("ex", (j == 2 * kt + pp // 64).astype(np.float32))
    r = np.arange(32)[:, None, None]
    rr = np.arange(24)[None, :, None]
    put("esel", np.broadcast_to((r == rr), (32, 24, 128)).astype(np.float32))
    inv = (np.float32(500000.0) ** (-np.arange(0, 32, 2, dtype=np.float32) / np.float32(32))).astype(np.float32)
    def tables(pos):
        ang = pos.astype(np.float32)[:, None] * inv[None, :]
        c = np.cos(ang).astype(np.float32).T
        s_ = np.sin(ang).astype(np.float32).T
        return np.concatenate([c, c], 0), np.concatenate([s_, s_], 0)
    rc, rs = tables(np.arange(T))
    kc_c, kc_s = tables(cend)
    ropekc = np.zeros((32, 2, 128), np.float32)
    ropekc[:, 0, :NCMP] = kc_c
    ropekc[:, 1, :NCMP] = kc_s
    tok = np.arange(T)
    tb = tok // 64
    jj = np.arange(32)[None, :]
    causal = jj <= tb[:, None]
    forced = (jj == 0) | (causal & (jj > tb[:, None] - 2))
    mmul = (causal & ~forced).astype(np.float32)
    badd = np.where(forced, 1e9, np.where(causal, 0.0, -1e30)).astype(np.float32)
    selc = np.stack([mmul.reshape(16, 128, 32).transpose(1, 0, 2), badd.reshape(16, 128, 32).transpose(1, 0, 2)], 1)
    return cb, rc, rs, ropekc, np.ascontiguousarray(selc)


_CACHE = {}


def _get_prog(nseq, layers_key, dbg_key=None):
    key = (nseq, layers_key, dbg_key)
    if key not in _CACHE:
        b = Builder(nseq, list(layers_key), dict(dbg_key or ()))
        _CACHE[key] = b.build()
    return _CACHE[key]


def kernel(**inputs):
    x = np.asarray(inputs["x"], np.float32)
    B = x.shape[0]
    ncores = 8
    nseq = B // ncores
    w = {k: np.asarray(v) for k, v in inputs.items() if k != "x"}
    layers = tuple(range(DEPTH))
    wsrc = np.concatenate([_pack_layer(w, li) for li in layers]).reshape(len(layers) * NPKR, 2048)
    vec = _vec_pack(w, layers)
    cb, rc, rs, ropekc, selc = _consts()
    nc = _get_prog(nseq, layers)
    in_maps = []
    for c in range(ncores):
        xc = x[c * nseq:(c + 1) * nseq]
        xT = np.ascontiguousarray(xc.transpose(0, 2, 1)).reshape(nseq, DC, 128, T)
        in_maps.append({"xin": xT, "wsrc": wsrc, "vec": vec, "cbf": cb, "ropec": rc, "ropes": rs,
                        "ropekc": ropekc, "selc": selc})
    res = run_bass_kernel_spmd(nc, in_maps, core_ids=list(range(ncores)))
    outs = []
    for c in range(ncores):
        y = res.results[c]["xout"].reshape(nseq, D, T).transpose(0, 2, 1)
        outs.append(y)
    return np.ascontiguousarray(np.concatenate(outs, 0), dtype=np.float32)
```

```python
import numpy as np
import concourse.bass as bass
import concourse.mybir as mybir
from concourse.bass_utils import run_bass_kernel_spmd

F32 = mybir.dt.float32
BF16 = mybir.dt.bfloat16
U8 = mybir.dt.uint8
AF = mybir.ActivationFunctionType
ALU = mybir.AluOpType

D = 2048
T = 2048
DEPTH = 4
HD = 128
DFF = 5632
DC = D // 128
FC = DFF // 128
TT = 512
NTT = T // TT
EPS = 1e-6
SCALE = HD ** -0.5
NEG = -32768.0
NCMP = 127
VW = 72

PK = {}
_off = 0
def _add(name, n):
    global _off
    PK[name] = (_off, n)
    _off += n
for _f in (1, 2):
    _add(f"wg{_f}", D * DFF)
    _add(f"wu{_f}", D * DFF)
    _add(f"wd{_f}", D * DFF)
_add("wfm1", 16 * 128 * 16 * 128)
_add("wtm", 3 * 128 * 16 * 512)
_add("wfm2", 16 * 128 * 16 * 128)
_add("wout", 16 * 128 * 16 * 128)
_add("cw1", 2 * 128 * 32 * 256)
_add("cw2", 2 * 128 * 2 * 128)
_add("wf", 128 * 16 * 8)
_add("wgt", 128 * 16 * 24)
_add("pos", 128 * 2 * 32)
NPK = ((_off + 2047) // 2048) * 2048
NPKR = NPK // 2048

CB = {}
_c = 0
def _addc(name, n):
    global _c
    CB[name] = (_c, n)
    _c += n
_addc("ident", 128)
_addc("ones", 128)
_addc("rt", 32)
_addc("ovl", 32)
_addc("cb", 4 * 512)
_addc("wb", 4 * 512)
_addc("cmask", 2048)
_addc("ex", 16 * 128)
_addc("esel", 24 * 128)
NCB = _c


class Prog:
    NS = 8

    def __init__(self, nc):
        self.nc = nc
        self.ops = []
        self.deps = []
        self.last_w = {}
        self.readers = {}
        self.streams = {k: [] for k in ("pe", "act", "dve", "pool", "sp")}
        self.groups = {}
        self.last_op = {k: None for k in self.streams}
        self.recent_dma = {"sp": [], "pool": [], "act": []}
        self.cur_barrier = None

    def _record(self, stream, fn, kind, reads, writes, group=None, is_barrier=False):
        i = len(self.ops)
        d = set()
        lw = self.last_w
        rd = self.readers
        for k in reads:
            w = lw.get(k)
            if w is not None:
                d.add(w)
            r = rd.get(k)
            if r is None:
                r = rd[k] = [{}, []]
            if kind == "c":
                r[0][stream] = i
            else:
                r[1].append(i)
        for k in writes:
            w = lw.get(k)
            if w is not None:
                d.add(w)
            r = rd.get(k)
            if r is not None:
                d.update(r[0].values())
                d.update(r[1])
                del rd[k]
            lw[k] = i
        if is_barrier:
            for st, li in self.last_op.items():
                if li is not None:
                    d.add(li)
            for q, lst in self.recent_dma.items():
                d.update(lst)
        elif self.cur_barrier is not None:
            d.add(self.cur_barrier)
        d.discard(i)
        self.ops.append((stream, fn, kind, group))
        self.deps.append(d)
        self.streams[stream].append(i)
        if kind == "c":
            self.last_op[stream] = i
        elif group is None:
            lst = self.recent_dma[stream]
            lst.append(i)
            if len(lst) > self.NS:
                lst.pop(0)
        if group is not None:
            self.groups[group] = self.groups.get(group, 0) + 1
        if is_barrier:
            self.cur_barrier = i
        return i

    def op(self, stream, fn, reads=(), writes=()):
        return self._record(stream, fn, "c", reads, writes)

    def dma(self, q, out, in_, reads=(), writes=(), group=None):
        return self._record(q, lambda e, o=out, i=in_: e.dma_start(out=o, in_=i), "d",
                            reads, writes, group)

    def barrier(self):
        self._record("sp", lambda e: e.nop(), "c", [], [], is_barrier=True)

    def emit(self, block, sems_ctx):
        nc = self.nc
        ops = self.ops
        n = len(ops)
        dsem = {}
        qcount = {"sp": 0, "pool": 0, "act": 0}
        qhist = {"sp": [], "pool": [], "act": []}
        extra = {}
        for i in range(n):
            st, fn, kind, group = ops[i]
            if kind != "d":
                continue
            if group is not None:
                dsem[i] = (("g", group), 16 * self.groups[group])
            else:
                c = qcount[st]
                dsem[i] = (("s", st, c % self.NS), 16 * (c // self.NS + 1))
                if c >= self.NS:
                    extra[i] = qhist[st][c - self.NS]
                qhist[st].append(i)
                qcount[st] = c + 1
        sig = [False] * n
        for i in range(n):
            st = ops[i][0]
            for d in self.deps[i]:
                sd, _, kd, _ = ops[d]
                if kd == "c" and not (sd == "pe" and st == "pe"):
                    sig[d] = True
        cnt = {k: 0 for k in self.streams}
        sval = [0] * n
        for i in range(n):
            st, fn, kind, group = ops[i]
            if kind == "c" and sig[i]:
                cnt[st] += 1
                sval[i] = cnt[st]
        semkeys = set(("e", k) for k in self.streams)
        for i in dsem:
            semkeys.add(dsem[i][0])
        sem = {}
        for k in sorted(semkeys, key=str):
            sem[k] = sems_ctx.enter_context(nc.semaphore("s_" + "_".join(str(x) for x in k)))
        waits = [None] * n
        waited = {k: {} for k in self.streams}
        for st in self.streams:
            wd = waited[st]
            for i in self.streams[st]:
                need = {}
                dl = self.deps[i]
                if i in extra:
                    dl = set(dl)
                    dl.add(extra[i])
                for d in dl:
                    sd, _, kd, _ = ops[d]
                    if kd == "d":
                        k, v = dsem[d]
                    else:
                        if sd == "pe" and st == "pe":
                            continue
                        k, v = ("e", sd), sval[d]
                    if need.get(k, 0) < v:
                        need[k] = v
                w = []
                for k, v in need.items():
                    if wd.get(k, 0) < v:
                        wd[k] = v
                        w.append((sem[k], v))
                waits[i] = w
        self.n_waits = sum(len(w) for w in waits)
        self.n_sig = sum(sig)

        def run_stream(st, eng):
            for i in self.streams[st]:
                _, fn, kind, group = ops[i]
                for s, v in waits[i]:
                    eng.wait_ge(s, v)
                ins = fn(eng)
                if kind == "d":
                    ins.then_inc(sem[dsem[i][0]], 16)
                elif sig[i]:
                    ins.then_inc(sem[("e", st)], 1)

        @block.tensor
        def _(e):
            run_stream("pe", e)

        @block.scalar
        def _(e):
            run_stream("act", e)

        @block.vector
        def _(e):
            run_stream("dve", e)

        @block.gpsimd
        def _(e):
            run_stream("pool", e)

        @block.sync
        def _(e):
            run_stream("sp", e)


class Arena:
    def __init__(self, ap, size):
        self.ap = ap
        self.size = size
        self.off = 0

    def take(self, nbytes, dtype, pattern=None, **kw):
        rb = (nbytes + 63) // 64 * 64
        assert self.off + rb <= self.size, ("arena overflow", self.off, rb, self.size)
        v = self.ap[:, self.off:self.off + nbytes].bitcast(dtype)
        self.off += rb
        if pattern:
            v = v.rearrange(pattern, **kw)
        return v

    def mark(self):
        return self.off

    def reset(self, m):
        self.off = m


class Builder:
    def __init__(self, nseq, layers, dbg=None):
        self.nseq = nseq
        self.layers = layers
        self.dbg = dbg or {}
        nc = bass.Bass("TRN2", target_bir_lowering=False)
        self.nc = nc
        self.P = Prog(nc)
        L = len(layers)
        self.L = L
        self.xin = nc.dram_tensor("xin", [nseq, DC, 128, T], F32, kind="ExternalInput")
        self.xs = nc.dram_tensor("xout", [nseq, DC, 128, T], F32, kind="ExternalOutput")
        self.wsrc = nc.dram_tensor("wsrc", [L * NPKR, 2048], F32, kind="ExternalInput")
        self.wpk = [nc.dram_tensor(f"wpk{i}", [NPKR, 2048], BF16, kind="Internal") for i in range(L)]
        self.vec = nc.dram_tensor("vec", [128, L * VW], F32, kind="ExternalInput")
        self.cbf = nc.dram_tensor("cbf", [128, NCB], F32, kind="ExternalInput")
        self.ropec = nc.dram_tensor("ropec", [32, T], F32, kind="ExternalInput")
        self.ropes = nc.dram_tensor("ropes", [32, T], F32, kind="ExternalInput")
        self.ropekc = nc.dram_tensor("ropekc", [32, 2, 128], F32, kind="ExternalInput")
        self.selc = nc.dram_tensor("selc", [128, 2, 16, 32], F32, kind="ExternalInput")
        sk = "ExternalOutput" if self.dbg.get("dump") else "Internal"
        self.s_fm = nc.dram_tensor("s_fm", [16, 128, T], BF16, kind=sk)
        self.s_tm = nc.dram_tensor("s_tm", [12, 128, 16, 128], BF16, kind=sk)
        self.s_kc = nc.dram_tensor("s_kc", [2, 128, 128], BF16, kind=sk)
        self.s_vc = nc.dram_tensor("s_vc", [2, 128, 128], BF16, kind=sk)
        self.s_ab = nc.dram_tensor("s_ab", [8, 2, 6, T], BF16, kind=sk)
        ASZ = 206 * 1024
        self.arena_t = nc.alloc_sbuf_tensor("arena", [128, ASZ], U8)
        self.A = Arena(self.arena_t.ap(), ASZ)
        self.ps = nc.alloc_psum_tensor("ps", [128, 8, 512], F32).ap()
        A = self.A
        self.cb_sb = A.take(NCB * 2, BF16)
        self.vec_sb = A.take(L * VW * 4, F32)
        self.base_mark = A.mark()
        self.wslot_ctr = 0

    def cview(self, name, rows=128):
        o, n = CB[name]
        return self.cb_sb[0:rows, o:o + n]

    def vcol(self, li, c, rows=128, n=1):
        return self.vec_sb[0:rows, li * VW + c: li * VW + c + n]

    def wview(self, li, name):
        o, n = PK[name]
        flat = self.wpk[li].ap().rearrange("r c -> (r c)")
        return flat[o:o + n]

    def prologue(self):
        P = self.P
        P.dma("pool", self.cb_sb, self.cbf.ap(), writes=["cb"])
        P.dma("sp", self.vec_sb, self.vec.ap(), writes=["vec"])
        self.convert(0)

    CONV_CH = 4096

    def conv_chunks(self, li):
        if li == 0:
            return [(r, min(self.CONV_CH, NPKR - r)) for r in range(0, NPKR, self.CONV_CH)]
        return [(0, NPKR)]

    def convert(self, li):
        P = self.P
        r0 = li * NPKR
        for k, (r, n) in enumerate(self.conv_chunks(li)):
            last = None
            rr = r
            while rr < r + n:
                m = min(4096, r + n - rr)
                last = P.dma("pool", self.wpk[li].ap()[rr:rr + m, :], self.wsrc.ap()[r0 + rr:r0 + rr + m, :],
                             writes=[], group=f"conv{li}_{k}")
                rr += m
            P.last_w[("w", li, k)] = last

    def wkeys(self, li, name):
        o, n = PK[name]
        lo, hi = o // 2048, (o + n - 1) // 2048
        return [("w", li, k) for k, (r, m) in enumerate(self.conv_chunks(li)) if r <= hi and r + m - 1 >= lo]

    def norm_tile(self, xt, h, sq, tmp, rs, li, gcol, xkey, hkey, psb, sqkey="sqa", tmpkey="ntmp"):
        self.norm_sq(xt, sq, xkey, sqkey)
        self.norm_sum(sq, tmp, rs, psb, sqkey, tmpkey)
        self.norm_apply(xt, h, rs, li, gcol, xkey, hkey)

    def norm_sq(self, xt, sq, xkey, sqkey="sqa"):
        P = self.P
        for c in range(4):
            o_ = sq[:, 4 * c:4 * c + 4, :]
            i_ = xt[:, 4 * c:4 * c + 4, :]
            rk = [(xkey, 4 * c + k) for k in range(4)]
            wk = [(sqkey, 4 * c + k) for k in range(4)]
            if c in (0, 2):
                P.op("act", lambda e, o=o_, i=i_: e.activation(out=o, in_=i, func=AF.Square), reads=rk, writes=wk)
            else:
                P.op("dve", lambda e, o=o_, i=i_: e.tensor_tensor(out=o, in0=i, in1=i, op=ALU.mult), reads=rk, writes=wk)

    def norm_sum(self, sq, tmp, rs, psb, sqkey="sqa", tmpkey="ntmp"):
        P = self.P
        ones = self.cview("ones")
        psn = self.ps[:, psb, :]
        pk = ("ps", psb)
        for dc in range(DC):
            P.op("pe", lambda e, r=sq[:, dc, :], s=(dc == 0), t=(dc == DC - 1): e.matmul(psn, lhsT=ones, rhs=r, start=s, stop=t),
                 reads=[(sqkey, dc), "cb"], writes=[pk])
        P.op("act", lambda e: e.activation(out=tmp, in_=psn, func=AF.Sqrt, bias=EPS, scale=1.0 / D),
             reads=[pk], writes=[tmpkey])
        P.op("dve", lambda e: e.reciprocal(out=rs, in_=tmp), reads=[tmpkey], writes=["nrs"])

    def norm_apply(self, xt, h, rs, li, gcol, xkey, hkey):
        P = self.P
        for dc in range(DC):
            P.op("dve", lambda e, o=h[:, dc, :], i=xt[:, dc, :], g=self.vcol(li, gcol + dc):
                 e.scalar_tensor_tensor(out=o, in0=i, scalar=g, in1=rs, op0=ALU.mult, op1=ALU.mult),
                 reads=[(xkey, dc), "nrs", "vec"], writes=[(hkey, dc)])

    def ffn_tile(self, li, f, xt, h, sq, tmp, rs, aT, sg, wring, xkey, after_chunk=None):
        P = self.P
        NSLOT = wring.shape[1]
        self.norm_tile(xt, h, sq, tmp, rs, li, {1: 0, 2: 32}[f], xkey, "h", 0)
        wg = self.wview(li, f"wg{f}").rearrange("(g p x) -> g p x", g=11, p=128)
        wu = self.wview(li, f"wu{f}").rearrange("(g p x) -> g p x", g=11, p=128)
        wd = self.wview(li, f"wd{f}").rearrange("(g p x) -> g p x", g=16, p=128)
        it = 0
        for fg in range(11):
            sa = self.wslot_ctr % NSLOT; self.wslot_ctr += 1
            sb_ = self.wslot_ctr % NSLOT; self.wslot_ctr += 1
            P.dma("sp", wring[:, sa, :], wg[fg], reads=self.wkeys(li, f"wg{f}"), writes=[("ws", sa)])
            P.dma("sp", wring[:, sb_, :], wu[fg], reads=self.wkeys(li, f"wu{f}"), writes=[("ws", sb_)])
            wa = wring[:, sa, :].rearrange("p (c x) -> p c x", c=DC)
            wb = wring[:, sb_, :].rearrange("p (c x) -> p c x", c=DC)
            for j in range(4):
                fc = fg * 4 + j
                bg = 1 + (it % 2)
                bu = 3 + (it % 2)
                sgs = it % 2
                it += 1
                psg = self.ps[:, bg, :]
                psu = self.ps[:, bu, :]
                for dc in range(DC):
                    P.op("pe", lambda e, o=psg, w=wa[:, dc, j * 128:(j + 1) * 128], r=h[:, dc, :], s=(dc == 0), t=(dc == DC - 1):
                         e.matmul(o, lhsT=w, rhs=r, start=s, stop=t),
                         reads=[("ws", sa), ("h", dc)], writes=[("ps", bg)])
                for dc in range(DC):
                    P.op("pe", lambda e, o=psu, w=wb[:, dc, j * 128:(j + 1) * 128], r=h[:, dc, :], s=(dc == 0), t=(dc == DC - 1):
                         e.matmul(o, lhsT=w, rhs=r, start=s, stop=t),
                         reads=[("ws", sb_), ("h", dc)], writes=[("ps", bu)])
                P.op("act", lambda e, o=sg[:, sgs, :], i=psg: e.activation(out=o, in_=i, func=AF.Silu),
                     reads=[("ps", bg)], writes=[("sg", sgs)])
                P.op("dve", lambda e, o=aT[:, fc, :], a=psu, b=sg[:, sgs, :]: e.tensor_tensor(out=o, in0=a, in1=b, op=ALU.mult),
                     reads=[("ps", bu), ("sg", sgs)], writes=[("aT", fc)])
        for dco in range(DC):
            s = self.wslot_ctr % NSLOT; self.wslot_ctr += 1
            P.dma("sp", wring[:, s, 0:FC * 128], wd[dco], reads=self.wkeys(li, f"wd{f}"), writes=[("ws", s)])
            w = wring[:, s, 0:FC * 128].rearrange("p (c x) -> p c x", c=FC)
            bd = 5 + (dco % 2)
            psd = self.ps[:, bd, :]
            for fc in range(FC):
                P.op("pe", lambda e, o=psd, ww=w[:, fc, :], r=aT[:, fc, :], s_=(fc == 0), t_=(fc == FC - 1):
                     e.matmul(o, lhsT=ww, rhs=r, start=s_, stop=t_),
                     reads=[("ws", s), ("aT", fc)], writes=[("ps", bd)])
            P.op("dve", lambda e, o=xt[:, dco, :], a=psd: e.scalar_tensor_tensor(out=o, in0=a, scalar=0.5, in1=o, op0=ALU.mult, op1=ALU.add),
                 reads=[("ps", bd)], writes=[(xkey, dco)])
            if after_chunk is not None:
                after_chunk(dco)

    def ffn_phase(self, s, jobs, src_is_input):
        P = self.P
        A = self.A
        P.barrier()
        A.reset(self.base_mark)
        xt = A.take(DC * TT * 4, F32, "p (c t) -> p c t", c=DC)
        h = A.take(DC * TT * 2, BF16, "p (c t) -> p c t", c=DC)
        sq = A.take(DC * TT * 2, BF16, "p (c t) -> p c t", c=DC)
        tmp = A.take(TT * 4, F32)
        rs = A.take(TT * 4, F32)
        aT = A.take(FC * TT * 2, BF16, "p (c t) -> p c t", c=FC)
        sg = A.take(2 * TT * 2, BF16, "p (c t) -> p c t", c=2)
        NSLOT = 4
        wring = A.take(NSLOT * 16384, BF16, "p (s x) -> p s x", s=NSLOT)
        src = self.xin if src_is_input else self.xs
        for tt in range(NTT):
            tsl = slice(tt * TT, (tt + 1) * TT)
            if tt == 0:
                P.dma("sp", xt, src.ap()[s, :, :, tsl].rearrange("c p t -> p c t"),
                      reads=[("X", s, tt, dc) for dc in range(DC)], writes=[("xt", dc) for dc in range(DC)])

            def after_chunk(dco, tt=tt, tsl=tsl):
                P.dma("act", self.xs.ap()[s, dco, :, tsl], xt[:, dco, :], reads=[("xt", dco)], writes=[("X", s, tt, dco)])
                if tt + 1 < NTT:
                    nsl = slice((tt + 1) * TT, (tt + 2) * TT)
                    P.dma("pool", xt[:, dco, :], src.ap()[s, dco, :, nsl], reads=[("X", s, tt + 1, dco)], writes=[("xt", dco)])
            for n_, (li, f) in enumerate(jobs):
                self.ffn_tile(li, f, xt, h, sq, tmp, rs, aT, sg, wring, "xt",
                              after_chunk=(after_chunk if n_ == len(jobs) - 1 else None))

    def build(self):
        P = self.P
        self.prologue()
        L = self.L
        mode = self.dbg.get("mode", "full")
        for s in range(self.nseq):
            first = True
            for li in range(L):
                jobs = []
                if li > 0:
                    jobs.append((li - 1, 2))
                jobs.append((li, 1))
                if mode in ("full", "ffn"):
                    self.ffn_phase(s, jobs, src_is_input=first)
                    first = False
                if s == 0 and li + 1 < L:
                    self.convert(li + 1)
                if mode in ("full", "mixer"):
                    self.mixer_phase(s, li, src_is_input=first)
                    first = False
            if mode in ("full", "ffn"):
                self.ffn_phase(s, [(L - 1, 2)], src_is_input=False)
        keys = [("X", s, tt, dc) for s in range(self.nseq) for tt in range(NTT) for dc in range(DC)]
        P.op("sp", lambda e: e.nop(), reads=keys)
        from contextlib import ExitStack
        with ExitStack() as es:
            es.enter_context(self.nc.allow_low_precision("bf16 matmul operands by design; accumulation is fp32"))
            block = es.enter_context(self.nc.Block())
            P.emit(block, es)
        return self.nc

    def mixer_phase(self, s, li, src_is_input):
        P, A = self.P, self.A
        P.barrier()
        A.reset(self.base_mark)
        M = {}
        M["xt"] = A.take(DC * TT * 4, F32, "p (c t) -> p c t", c=DC)
        M["h"] = A.take(DC * TT * 2, BF16, "p (c t) -> p c t", c=DC)
        M["tmp"] = A.take(TT * 4, F32)
        M["rs"] = A.take(TT * 4, F32)
        cm0 = A.mark()
        M["cfull"] = A.take(T * 4, F32)
        M["src"] = self.xin if src_is_input else self.xs
        cm = A.mark()
        if self.dbg.get("skip1") is None:
            self.pass1a(s, li, M)
            P.barrier()
            A.reset(cm)
            self.pass1b(s, li, M)
        if self.dbg.get("only1"):
            return
        P.barrier()
        A.reset(cm0)
        self.pass2(s, li, M)

    def load_x(self, M, s, tt):
        self.P.dma("sp", M["xt"], M["src"].ap()[s, :, :, tt * TT:(tt + 1) * TT].rearrange("c p t -> p c t"),
                   reads=[("X", s, tt, dc) for dc in range(DC)], writes=[("xt", dc) for dc in range(DC)])

    def head_norm(self, ps_in, pskey, n, gain, W, out_bf, outkey, rope=None, sumbank=3, rotbank=4):
        st = self.head_norm_B(ps_in, pskey, n, gain, W, out_bf, outkey, rope, sumbank)
        self.head_norm_C(st, rotbank)

    def head_norm_B(self, ps_in, pskey, n, gain, W, out_bf, outkey, rope=None, sumbank=3):
        P = self.P
        ones = self.cview("ones")
        a = self.hn_ctr % 2
        self.hn_ctr += 1
        hsq = W["hsq"][:, a, 0:n]
        pss = self.ps[:, sumbank, 0:n]
        P.op("act", lambda e: e.activation(out=hsq, in_=ps_in, func=AF.Square), reads=[pskey], writes=[("hsq", a)])
        P.op("pe", lambda e: e.matmul(pss, lhsT=ones, rhs=hsq, start=True, stop=True),
             reads=[("hsq", a), "cb"], writes=[("ps", sumbank)])
        hl = W["hl"][:, 0:n]
        hr = W["hr"][:, 0:n]
        P.op("act", lambda e: e.activation(out=hl, in_=pss, func=AF.Ln, bias=EPS, scale=1.0 / HD),
             reads=[("ps", sumbank)], writes=["hl"])
        P.op("act", lambda e: e.activation(out=hr, in_=hl, func=AF.Exp, scale=-0.5), reads=["hl"], writes=["hr"])
        if rope is None:
            P.op("dve", lambda e: e.scalar_tensor_tensor(out=out_bf, in0=ps_in, scalar=gain, in1=hr, op0=ALU.mult, op1=ALU.mult),
                 reads=[pskey, "hr", "vec"], writes=[outkey])
            return None
        kn = W["kn"][:, a, 0:n]
        P.op("dve", lambda e: e.scalar_tensor_tensor(out=kn, in0=ps_in, scalar=gain, in1=hr, op0=ALU.mult, op1=ALU.mult),
             reads=[pskey, "hr", "vec"], writes=[("kn", a)])
        knb = W["knb"][0:32, a, 0:n]
        P.op("pool", lambda e: e.tensor_copy(out=knb, in_=kn[0:32, :]), reads=[("kn", a)], writes=[("knb", a)])
        return (a, n, kn, knb, rope, out_bf, outkey, W)

    def head_norm_C(self, st, rotbank=4):
        if st is None:
            return
        P = self.P
        a, n, kn, knb, rope, out_bf, outkey, W = st
        C, S, rkey = rope
        psr = self.ps[0:32, rotbank, 0:n]
        rt = self.cview("rt", 32)
        P.op("pe", lambda e: e.matmul(psr, lhsT=rt, rhs=knb, start=True, stop=True), reads=[("knb", a), "cb"], writes=[("ps", rotbank)])
        t1 = W["t1"][0:32, 0:n]
        t2 = W["t2"][0:32, 0:n]
        P.op("dve", lambda e: e.tensor_tensor(out=t1, in0=psr, in1=S, op=ALU.mult), reads=[("ps", rotbank), rkey], writes=["t1"])
        P.op("pool", lambda e: e.tensor_tensor(out=t2, in0=kn[0:32, :], in1=C, op=ALU.mult), reads=[("kn", a), rkey], writes=["t2"])
        P.op("pool", lambda e: e.tensor_tensor(out=kn[0:32, :], in0=t1, in1=t2, op=ALU.add), reads=["t1", "t2"], writes=[("kn", a)])
        P.op("act", lambda e: e.activation(out=out_bf, in_=kn, func=AF.Copy), reads=[("kn", a)], writes=[outkey])

    def head_pipeline(self, heads):
        n = len(heads)
        stB = [None] * n
        res = [None] * n
        for step in range(n + 2):
            if step < n:
                res[step] = heads[step]["A"]()
            i = step - 1
            if 0 <= i < n:
                hd = heads[i]
                if hd.get("B") is not None:
                    stB[i] = self.head_norm_B(res[i][0], res[i][1], **hd["B"])
                elif hd.get("raw") is not None:
                    hd["raw"](res[i][0], res[i][1])
            i = step - 2
            if 0 <= i < n:
                self.head_norm_C(stB[i])
                if heads[i].get("post") is not None:
                    heads[i]["post"]()

    def head_temps(self, A):
        W = {}
        W["hsq"] = A.take(2 * TT * 2, BF16, "p (c t) -> p c t", c=2)
        W["hl"] = A.take(TT * 4, F32)
        W["hr"] = A.take(TT * 4, F32)
        W["kn"] = A.take(2 * TT * 4, F32, "p (c t) -> p c t", c=2)
        W["t1"] = A.take(TT * 4, F32)
        W["t2"] = A.take(TT * 4, F32)
        W["knb"] = A.take(2 * TT * 2, BF16, "p (c t) -> p c t", c=2)
        W["rc"] = A.take(TT * 4, F32)
        W["rsn"] = A.take(TT * 4, F32)
        return W

    def pass1a(self, s, li, M):
        P, A = self.P, self.A
        self.hn_ctr = 0
        xt, h = M["xt"], M["h"]
        NS1 = 3
        wring = A.take(NS1 * 16384, BF16, "p (s x) -> p s x", s=NS1)
        W = self.head_temps(A)
        M["sq"] = A.take(DC * TT * 2, BF16, "p (c t) -> p c t", c=DC)
        stage = A.take(4 * TT * 2, BF16, "p (c t) -> p c t", c=4)
        lf1 = A.take(TT * 4, F32)
        lf2 = A.take(TT * 4, F32)
        ones8 = A.take(TT * 4, F32)
        negb = A.take(64, F32)
        wfs = A.take(DC * 8 * 2, BF16, "p (c x) -> p c x", c=DC)
        wkey = None
        cfull = M["cfull"]
        wfm1 = self.wview(li, "wfm1").rearrange("(g p x) -> g p x", g=16, p=128)
        wtm = self.wview(li, "wtm").rearrange("(g p x) -> g p x", g=3, p=128)
        P.dma("sp", wfs, self.wview(li, "wf").rearrange("(p c x) -> p c x", p=128, c=DC), reads=self.wkeys(li, "wf"), writes=["wfs"])
        P.op("dve", lambda e: e.memset(ones8, 1.0), writes=["ones8"])
        P.op("dve", lambda e: e.tensor_scalar(out=negb[0:8, 0:1], in0=self.vcol(li, 70, rows=8), scalar1=-1.0, scalar2=None, op0=ALU.mult),
             reads=["vec"], writes=["negb"])
        sctr = 0
        stg = 0
        for tt in range(NTT):
            tsl = slice(tt * TT, (tt + 1) * TT)
            if tt == 0:
                self.load_x(M, s, tt)
                self.norm_sq(xt, M["sq"], "xt")
                self.norm_sum(M["sq"], M["tmp"], M["rs"], 0)
            self.norm_apply(xt, h, M["rs"], li, 16, "xt", "h")
            if tt + 1 < NTT:
                self.load_x(M, s, tt + 1)
            P.dma("sp", W["rc"][0:32, :], self.ropec.ap()[:, tsl], writes=["rope"])
            P.dma("sp", W["rsn"][0:32, :], self.ropes.ap()[:, tsl], writes=["rope"])
            heads = []
            PB = (1, 2, 6, 7)
            for cc in range(16):
                def A_(cc=cc):
                    nonlocal sctr
                    sl = sctr % NS1; sctr += 1
                    P.dma("sp", wring[:, sl, 0:2048], wfm1[cc], reads=self.wkeys(li, "wfm1"), writes=[("ws", sl)])
                    w = wring[:, sl, 0:2048].rearrange("p (c x) -> p c x", c=DC)
                    pb = PB[cc % 4]
                    psp = self.ps[:, pb, :]
                    for dc in range(DC):
                        P.op("pe", lambda e, o=psp, ww=w[:, dc, :], r=h[:, dc, :], s_=(dc == 0), t_=(dc == DC - 1):
                             e.matmul(o, lhsT=ww, rhs=r, start=s_, stop=t_),
                             reads=[("ws", sl), ("h", dc)], writes=[("ps", pb)])
                    return psp, ("ps", pb)
                st = stg % 4; stg += 1
                so = stage[:, st, :]
                hd = dict(A=A_)
                if cc < 4:
                    hd["raw"] = (lambda psp, pk, so=so, st=st: P.op("dve", lambda e: e.tensor_copy(out=so, in_=psp), reads=[pk], writes=[("stage", st)]))
                elif cc < 8:
                    hd["B"] = dict(n=TT, gain=self.vcol(li, 49 + (1 if cc < 6 else 2)), W=W, out_bf=so, outkey=("stage", st),
                                   rope=(W["rc"][0:32, :], W["rsn"][0:32, :], "rope"))
                else:
                    hd["B"] = dict(n=TT, gain=self.vcol(li, 53), W=W, out_bf=so, outkey=("stage", st))
                hd["post"] = (lambda cc=cc, so=so, st=st: P.dma("act", self.s_fm.ap()[cc, :, tsl], so, reads=[("stage", st)], writes=[("s_fm", cc)]))
                heads.append(hd)
            self.head_pipeline(heads)
            psf = self.ps[0:8, 5, :]
            for dc in range(DC):
                P.op("pe", lambda e, ww=wfs[:, dc, :], r=h[:, dc, :], s_=(dc == 0), t_=(dc == DC - 1):
                     e.matmul(psf, lhsT=ww, rhs=r, start=s_, stop=t_), reads=["wfs", ("h", dc)], writes=[("ps", 5)])
            P.op("act", lambda e: e.activation(out=lf1[0:8, :], in_=psf, func=AF.Exp, bias=negb[0:8, 0:1], scale=-1.0),
                 reads=[("ps", 5), "negb"], writes=["lf1"])
            P.op("act", lambda e: e.activation(out=lf2[0:8, :], in_=lf1[0:8, :], func=AF.Ln, bias=1.0, scale=1.0),
                 reads=["lf1"], writes=["lf2"])
            P.op("dve", lambda e: e.tensor_scalar(out=lf1[0:8, :], in0=lf2[0:8, :], scalar1=-1.0 / SCALE, scalar2=None, op0=ALU.mult),
                 reads=["lf2"], writes=["lf1"])
            init = 0.0 if tt == 0 else cfull[0:8, tt * TT - 1:tt * TT]
            P.op("dve", lambda e, o=cfull[0:8, tsl], i=init: e.tensor_tensor_scan(out=o, data0=ones8[0:8, :], data1=lf1[0:8, :],
                                                                                   initial=i, op0=ALU.mult, op1=ALU.add),
                 reads=["lf1", "ones8", "cfull"], writes=["cfull"])
            if tt + 1 < NTT:
                self.norm_sq(xt, M["sq"], "xt")
            n2 = 0
            for cg in range(3):
                sl = sctr % NS1; sctr += 1
                P.dma("sp", wring[:, sl, :], wtm[cg], reads=self.wkeys(li, "wtm"), writes=[("ws", sl)])
                w = wring[:, sl, :].rearrange("p (c x) -> p c x", c=DC)
                for tk in range(4):
                    pb = 6 + n2 % 2; n2 += 1
                    psp = self.ps[:, pb, :]
                    for dc in range(DC):
                        P.op("pe", lambda e, o=psp, l_=h[:, dc, tk * 128:(tk + 1) * 128], r=w[:, dc, :], s_=(dc == 0), t_=(dc == DC - 1):
                             e.matmul(o, lhsT=l_, rhs=r, start=s_, stop=t_),
                             reads=[("ws", sl), ("h", dc)], writes=[("ps", pb)])
                    st = stg % 4; stg += 1
                    so = stage[:, st, :]
                    if n2 % 2:
                        P.op("act", lambda e, o=so, i=psp: e.activation(out=o, in_=i, func=AF.Copy), reads=[("ps", pb)], writes=[("stage", st)])
                    else:
                        P.op("dve", lambda e, o=so, i=psp: e.tensor_copy(out=o, in_=i), reads=[("ps", pb)], writes=[("stage", st)])
                    kt = tt * 4 + tk
                    P.dma("act", self.s_tm.ap()[cg * 4:(cg + 1) * 4, :, kt, :].rearrange("g p d -> p g d"),
                          so.rearrange("p (g d) -> p g d", g=4), reads=[("stage", st)], writes=[("s_tm", cg)])
            if tt + 1 < NTT:
                self.norm_sum(M["sq"], M["tmp"], M["rs"], 0)

    def pass1b(self, s, li, M):
        P, A = self.P, self.A
        wkey = ("w", li)
        W = self.head_temps(A)
        kcraw = A.take(4 * T * 2, BF16, "p (c t) -> p c t", c=4)
        cw1 = A.take(2 * 32 * 256 * 2, BF16, "p (k l j) -> p k l j", k=2, l=32)
        cw2 = A.take(2 * 2 * 128 * 2, BF16, "p (k c x) -> p k c x", k=2, c=2)
        posb = A.take(64 * 2, BF16, "p (k l) -> p k l", k=2)
        hid = A.take(2 * 128 * 2, BF16, "p (c x) -> p c x", c=2)
        bias = A.take(16, F32)
        stage = A.take(2 * 128 * 2, BF16, "p (c x) -> p c x", c=2)
        rkc = A.take(2 * 128 * 4, F32, "p (c x) -> p c x", c=2)
        c3 = A.take(3 * T * 2, BF16, "p (c t) -> p c t", c=3)
        n3 = A.take(3 * T * 2, BF16, "p (c t) -> p c t", c=3)
        o3 = A.take(3 * T * 2, BF16, "p (c t) -> p c t", c=3)
        r1 = A.take(T * 4, F32)
        cfull = M["cfull"]
        P.dma("sp", kcraw, self.s_fm.ap()[0:4, :, :].rearrange("c p t -> p c t"), reads=[("s_fm", c) for c in range(4)], writes=["kcraw"])
        P.dma("sp", cw1, self.wview(li, "cw1").rearrange("(k p l j) -> p k l j", k=2, p=128, l=32), reads=self.wkeys(li, "cw1"), writes=["cw1"])
        P.dma("sp", cw2, self.wview(li, "cw2").rearrange("(k p c x) -> p k c x", k=2, p=128, c=2), reads=self.wkeys(li, "cw2"), writes=["cw2"])
        P.dma("sp", posb, self.wview(li, "pos").rearrange("(p k l) -> p k l", p=128, k=2), reads=self.wkeys(li, "pos"), writes=["posb"])
        P.dma("sp", rkc[0:32, :, :], self.ropekc.ap(), writes=["rkc"])
        cf = cfull[0:8, :]
        P.op("dve", lambda e: e.tensor_copy(out=c3[0:8, 0, :], in_=cf), reads=["cfull"], writes=["c3"])
        P.op("dve", lambda e: e.tensor_tensor(out=r1[0:8, :], in0=cf, in1=c3[0:8, 0, :], op=ALU.subtract), reads=["c3", "cfull"], writes=["r1"])
        P.op("dve", lambda e: e.tensor_copy(out=c3[0:8, 1, :], in_=r1[0:8, :]), reads=["r1"], writes=["c3"])
        P.op("dve", lambda e: e.tensor_tensor(out=r1[0:8, :], in0=r1[0:8, :], in1=c3[0:8, 1, :], op=ALU.subtract), reads=["c3"], writes=["r1"])
        P.op("dve", lambda e: e.tensor_copy(out=c3[0:8, 2, :], in_=r1[0:8, :]), reads=["r1"], writes=["c3"])
        P.op("dve", lambda e: e.tensor_scalar(out=n3[0:8, :, :], in0=c3[0:8, :, :], scalar1=-1.0, scalar2=None, op0=ALU.mult), reads=["c3"], writes=["n3"])
        P.op("pool", lambda e: e.memset(o3[0:8, :, :], 1.0), writes=["o3"])
        ab = self.s_ab.ap()
        P.dma("sp", ab[:, 0, 0:3, :], o3[0:8, :, :], reads=["o3"], writes=["s_ab"])
        P.dma("sp", ab[:, 0, 3:6, :], c3[0:8, :, :], reads=["c3"], writes=["s_ab"])
        P.dma("sp", ab[:, 1, 0:3, :], n3[0:8, :, :], reads=["n3"], writes=["s_ab"])
        P.dma("sp", ab[:, 1, 3:6, :], o3[0:8, :, :], reads=["o3"], writes=["s_ab"])
        self.hn_ctr = 0
        nq = 0
        for kv in range(2):
            for jc in range(2):
                for l in range(32):
                    P.op("pe", lambda e, o=self.ps[:, 0, kv * 2 + jc:kv * 2 + jc + 1], ww=cw1[:, kv, l, jc * 128:(jc + 1) * 128], r=posb[:, kv, l:l + 1],
                         s_=(l == 0), t_=(l == 31): e.matmul(o, lhsT=ww, rhs=r, start=s_, stop=t_),
                         reads=["cw1", "posb"], writes=[("ps", 0)])
            P.op("dve", lambda e, o=bias[:, kv * 2:kv * 2 + 2], i=self.ps[:, 0, kv * 2:kv * 2 + 2]: e.tensor_copy(out=o, in_=i),
                 reads=[("ps", 0)], writes=["cbias"])
            for kvh in range(2):
                raw = kcraw[:, kv * 2 + kvh, :]
                for jc in range(2):
                    pb = 1 + jc
                    psh = self.ps[:, pb, 0:NCMP]
                    for l in range(32):
                        P.op("pe", lambda e, o=psh, ww=cw1[:, kv, l, jc * 128:(jc + 1) * 128], r=raw[:, l:l + 16 * (NCMP - 1) + 1:16],
                             s_=(l == 0), t_=(l == 31): e.matmul(o, lhsT=ww, rhs=r, start=s_, stop=t_),
                             reads=["cw1", "kcraw"], writes=[("ps", pb)])
                    P.op("act", lambda e, o=hid[:, jc, 0:NCMP], i=psh, b=bias[:, kv * 2 + jc:kv * 2 + jc + 1]:
                         e.activation(out=o, in_=i, func=AF.Gelu_apprx_tanh, bias=b),
                         reads=[("ps", pb), "cbias"], writes=[("hid", jc)])
                st = nq % 2; nq += 1
                if kv == 0:
                    psk = self.ps[:, 5, 0:NCMP]
                    for jc in range(2):
                        P.op("pe", lambda e, ww=cw2[:, 0, jc, :], r=hid[:, jc, 0:NCMP], s_=(jc == 0), t_=(jc == 1):
                             e.matmul(psk, lhsT=ww, rhs=r, start=s_, stop=t_), reads=["cw2", ("hid", jc)], writes=[("ps", 5)])
                    self.head_norm(psk, ("ps", 5), NCMP, self.vcol(li, 49), W, stage[:, st, 0:NCMP], ("stage", st),
                                   rope=(rkc[0:32, 0, 0:NCMP], rkc[0:32, 1, 0:NCMP], "rkc"))
                    P.dma("sp", self.s_kc.ap()[kvh, :, 0:NCMP], stage[:, st, 0:NCMP], reads=[("stage", st)], writes=["s_kc"])
                else:
                    psv = self.ps[0:NCMP, 6, 0:128]
                    for jc in range(2):
                        P.op("pe", lambda e, l_=hid[:, jc, 0:NCMP], r=cw2[:, 1, jc, :], s_=(jc == 0), t_=(jc == 1):
                             e.matmul(psv, lhsT=l_, rhs=r, start=s_, stop=t_), reads=["cw2", ("hid", jc)], writes=[("ps", 6)])
                    P.op("dve", lambda e, o=stage[0:NCMP, st, :]: e.tensor_copy(out=o, in_=psv), reads=[("ps", 6)], writes=[("stage", st)])
                    P.dma("sp", self.s_vc.ap()[kvh, 0:NCMP, :], stage[0:NCMP, st, :], reads=[("stage", st)], writes=["s_vc"])

    def attn_multi(self, jobs):
        P = self.P
        ones = self.cview("ones")
        flat = [(ji, ti) for ji, job in enumerate(jobs) for ti in range(len(job["tiles"]))]

        def emit_qk(ji, ti):
            job = jobs[ji]
            t = job["tiles"][ti]
            sb = self.sctr % 2; self.sctr += 1
            nk = t["nk"]
            pss = self.ps[0:nk, sb, :]
            mm = [(t["k"], job["q"], list(t["kkeys"]) + [job["qkey"]])] + list(t["extras"])
            for m, (l_, r_, keys) in enumerate(mm):
                P.op("pe", lambda e, o=pss, l_=l_, r_=r_, s_=(m == 0), t_=(m == len(mm) - 1): e.matmul(o, lhsT=l_, rhs=r_, start=s_, stop=t_),
                     reads=list(keys) + ["cb"], writes=[("ps", sb)])
            return pss, sb, nk

        def emit_rest(ji, ti, info):
            job = jobs[ji]
            t = job["tiles"][ti]
            n = len(job["tiles"])
            pss, sb, nk = info
            lb, ob = job["lo"]
            p = self.pctr % 3; self.pctr += 1
            ptile = self.pt[0:nk, p, :]
            P.op("act", lambda e, o=ptile, i_=pss: e.activation(out=o, in_=i_, func=AF.Exp, scale=SCALE),
                 reads=[("ps", sb)], writes=[("pt", p)])
            P.op("pe", lambda e, l_=ones[0:nk, :], r_=ptile, s_=(ti == 0), t_=(ti == n - 1): e.matmul(self.ps[:, lb, :], lhsT=l_, rhs=r_, start=s_, stop=t_),
                 reads=[("pt", p), "cb"], writes=[("ps", lb)])
            P.op("pe", lambda e, l_=t["v"], r_=ptile, s_=(ti == 0), t_=(ti == n - 1): e.matmul(self.ps[:, ob, :], lhsT=l_, rhs=r_, start=s_, stop=t_),
                 reads=[("pt", p)] + list(t["vkeys"]), writes=[("ps", ob)])
            if ti == n - 1 and job.get("fin") is not None:
                d = job["fin"](ptile, ("pt", p))
                for f in deferred:
                    f()
                del deferred[:]
                if d is not None:
                    deferred.append(d)

        deferred = []
        pending = None
        for (ji, ti) in flat:
            if ti == 0 and jobs[ji].get("pre") is not None:
                jobs[ji]["pre"]()
            cur = emit_qk(ji, ti)
            if pending is not None:
                emit_rest(*pending)
            pending = (ji, ti, cur)
        if pending is not None:
            emit_rest(*pending)
        for f in deferred:
            f()

    def pass2(self, s, li, M):
        P, A = self.P, self.A
        self.hn_ctr = 0
        self.sctr = 0
        self.pctr = 0
        xt, h = M["xt"], M["h"]
        o_sb = h
        wkey = ("w", li)
        W = self.head_temps(A)
        NS2 = 4
        wring = A.take(NS2 * 4096, BF16, "p (s x) -> p s x", s=NS2)
        qall = A.take(16 * TT * 2, BF16, "p (c t) -> p c t", c=16)
        gsb = A.take(TT * 2, BF16)
        xst = A.take(4 * TT * 4, F32, "p (c t) -> p c t", c=4)
        wgs = A.take(DC * 24 * 2, BF16, "p (c x) -> p c x", c=DC)
        NKV = 6
        kvr = A.take(NKV * 4096, BF16, "p (s x) -> p s x", s=NKV)
        abA = A.take(2 * TT * 2, BF16, "p (c t) -> p c t", c=2)
        kcs = A.take(2 * 2 * 128 * 2, BF16, "p (a b x) -> p a b x", a=2, b=2)
        self.pt = A.take(3 * TT * 2, BF16, "p (c t) -> p c t", c=3)
        acc = A.take(4 * TT * 4, F32, "p (c t) -> p c t", c=4)
        rl = A.take(2 * TT * 4, F32, "p (c t) -> p c t", c=2)
        wg = A.take(TT * 4, F32)
        gtmp = wg
        of = A.take(2 * TT * 4, F32, "p (c t) -> p c t", c=2)
        osq = A.take(2 * TT * 2, BF16, "p (c t) -> p c t", c=2)
        phat = A.take(2 * TT * 2, BF16, "p (c t) -> p c t", c=2)
        score = A.take(4 * 32 * 4, F32, "p (c x) -> p c x", c=4)
        mx8 = A.take(4 * 8 * 4, F32, "p (c x) -> p c x", c=4)
        selb = A.take(4 * 32 * 2, BF16, "p (c x) -> p c x", c=4)
        selbT = A.take(TT * 2, BF16)
        selct = A.take(2 * 4 * 32 * 4, F32, "p (a c x) -> p a c x", a=2, c=4)
        ssq = A.take(2 * TT * 4, F32, "p (c t) -> p c t", c=2)
        rstd = A.take(2 * TT * 4, F32, "p (c t) -> p c t", c=2)
        otmp = A.take(2 * TT * 4, F32, "p (c t) -> p c t", c=2)
        ident = self.cview("ident")
        ones = self.cview("ones")
        ovl = self.cview("ovl")
        cbv = self.cview("cb").rearrange("p (k n) -> p k n", k=4)
        wbv = self.cview("wb").rearrange("p (k n) -> p k n", k=4)
        cmask = self.cview("cmask")
        exv = self.cview("ex", 32).rearrange("p (k n) -> p k n", k=16)
        eselv = self.cview("esel", 32).rearrange("p (k n) -> p k n", k=24)
        wfm2 = self.wview(li, "wfm2").rearrange("(g p x) -> g p x", g=16, p=128)
        wout = self.wview(li, "wout").rearrange("(g p x) -> g p x", g=16, p=128)
        P.dma("sp", wgs, self.wview(li, "wgt").rearrange("(p c x) -> p c x", p=128, c=DC), reads=self.wkeys(li, "wgt"), writes=["wgs"])
        P.dma("sp", kcs[:, :, 0, :], self.s_kc.ap().rearrange("k p x -> p k x"), reads=["s_kc"], writes=["kcs"])
        P.dma("sp", kcs[:, :, 1, :], self.s_vc.ap().rearrange("k p x -> p k x"), reads=["s_vc"], writes=["kcs"])
        sctr = 0
        kvc = 0
        rctr = 0
        yctr = 0
        actr = 0
        zctr = 0
        loc = 0
        for j in range(NTT):
            tsl = slice(j * TT, (j + 1) * TT)
            nkt = 4 * (j + 1)
            nk = nkt * 128
            if j == 0:
                self.load_x(M, s, j)
                self.norm_sq(xt, qall, "xt", sqkey="qa")
                self.norm_sum(qall, wg, M["rs"], 0, sqkey="qa", tmpkey="wg")
            self.norm_apply(xt, h, M["rs"], li, 16, "xt", "h")
            if j + 1 < NTT:
                self.load_x(M, s, j + 1)
            P.dma("sp", W["rc"][0:32, :], self.ropec.ap()[:, tsl], writes=["rope"])
            P.dma("sp", W["rsn"][0:32, :], self.ropes.ap()[:, tsl], writes=["rope"])
            P.dma("sp", selct, self.selc.ap()[:, :, 4 * j:4 * j + 4, :], writes=["selct"])
            heads = []
            PB = (1, 2, 5, 6)
            for cc in range(16):
                def A_(cc=cc):
                    nonlocal sctr
                    sl = sctr % NS2; sctr += 1
                    P.dma("sp", wring[:, sl, :], wfm2[cc], reads=self.wkeys(li, "wfm2"), writes=[("ws", sl)])
                    w = wring[:, sl, :].rearrange("p (c x) -> p c x", c=DC)
                    pb = PB[cc % 4]
                    psp = self.ps[:, pb, :]
                    for dc in range(DC):
                        P.op("pe", lambda e, o=psp, ww=w[:, dc, :], r=h[:, dc, :], s_=(dc == 0), t_=(dc == DC - 1):
                             e.matmul(o, lhsT=ww, rhs=r, start=s_, stop=t_),
                             reads=[("ws", sl), ("h", dc)], writes=[("ps", pb)])
                    return psp, ("ps", pb)
                if cc < 8:
                    B = dict(n=TT, gain=self.vcol(li, 48), W=W, out_bf=qall[:, cc, :], outkey=("qa", cc),
                             rope=(W["rc"][0:32, :], W["rsn"][0:32, :], "rope"))
                else:
                    B = dict(n=TT, gain=self.vcol(li, 52), W=W, out_bf=qall[:, cc, :], outkey=("qa", cc))
                heads.append(dict(A=A_, B=B))
            self.head_pipeline(heads)
            psg = self.ps[0:24, 7, :]
            for dc in range(DC):
                P.op("pe", lambda e, ww=wgs[:, dc, :], r=h[:, dc, :], s_=(dc == 0), t_=(dc == DC - 1):
                     e.matmul(psg, lhsT=ww, rhs=r, start=s_, stop=t_), reads=["wgs", ("h", dc)], writes=[("ps", 7)])
            P.op("act", lambda e: e.activation(out=gtmp[0:24, :], in_=psg, func=AF.Exp, scale=-1.0), reads=[("ps", 7)], writes=["wg"])
            P.op("dve", lambda e: e.tensor_scalar(out=gtmp[0:24, :], in0=gtmp[0:24, :], scalar1=1.0, scalar2=None, op0=ALU.add), reads=["wg"], writes=["wg"])
            P.op("dve", lambda e: e.reciprocal(out=gsb[0:24, :], in_=gtmp[0:24, :]), reads=["wg"], writes=["gsb"])
            for kvh in range(2):
                ch = []
                for _ in range(4):
                    ch.append(kvc % NKV); kvc += 1
                c_ks, c_kw, c_vs, c_vw = ch
                P.dma("sp", kvr[:, c_ks, 0:nk], self.s_fm.ap()[4 + kvh, :, 0:nk], reads=[("s_fm", 4 + kvh)], writes=[("kv", c_ks)])
                P.dma("sp", kvr[:, c_vs, 0:nk], self.s_tm.ap()[0 + kvh, :, 0:nkt, :].rearrange("p k d -> p (k d)"), reads=[("s_tm", 0)], writes=[("kv", c_vs)])
                P.dma("sp", kvr[:, c_kw, 0:nk], self.s_fm.ap()[6 + kvh, :, 0:nk], reads=[("s_fm", 6 + kvh)], writes=[("kv", c_kw)])
                P.dma("sp", kvr[:, c_vw, 0:nk], self.s_tm.ap()[2 + kvh, :, 0:nkt, :].rearrange("p k d -> p (k d)"), reads=[("s_tm", 0)], writes=[("kv", c_vw)])
                ksT = kvr[:, c_ks, :]
                kwT = kvr[:, c_kw, :]
                vs = kvr[:, c_vs, :].rearrange("p (k d) -> p k d", d=128)
                vw = kvr[:, c_vw, :].rearrange("p (k d) -> p k d", d=128)
                kcT = kcs[:, kvh, 0, 0:NCMP]
                vc = kcs[0:NCMP, kvh, 1, :]

                def fin_branch(g, b, lo, first, kvh=kvh):
                    nonlocal rctr, yctr
                    lb, ob = lo
                    x_ = rctr % 2; rctr += 1
                    rlb = rl[:, x_, :]
                    r = ((kvh * 4 + g) * 3 + b)
                    P.op("pe", lambda e: e.matmul(self.ps[:, 7, :], lhsT=eselv[0:24, r, :], rhs=gsb[0:24, :], start=True, stop=True),
                         reads=["gsb", "cb"], writes=[("ps", 7)])
                    P.op("dve", lambda e: e.tensor_scalar(out=rlb, in0=self.ps[:, lb, :], scalar1=1e-30, scalar2=None, op0=ALU.max),
                         reads=[("ps", lb)], writes=[("rl", x_)])
                    P.op("dve", lambda e: e.reciprocal(out=rlb, in_=rlb), reads=[("rl", x_)], writes=[("rl", x_)])
                    P.op("dve", lambda e: e.tensor_tensor(out=wg, in0=self.ps[:, 7, :], in1=rlb, op=ALU.mult),
                         reads=[("ps", 7), ("rl", x_)], writes=["wg"])
                    if first:
                        P.op("dve", lambda e: e.tensor_tensor(out=acc[:, g, :], in0=self.ps[:, ob, :], in1=wg, op=ALU.mult),
                             reads=[("ps", ob), "wg"], writes=[("acc", g)])
                    else:
                        y_ = yctr % 2; yctr += 1
                        P.op("dve", lambda e: e.tensor_tensor(out=otmp[:, y_, :], in0=self.ps[:, ob, :], in1=wg, op=ALU.mult),
                             reads=[("ps", ob), "wg"], writes=[("otmp", y_)])
                        P.op("pool", lambda e: e.tensor_tensor(out=acc[:, g, :], in0=acc[:, g, :], in1=otmp[:, y_, :], op=ALU.add),
                             reads=[("otmp", y_)], writes=[("acc", g)])
                    return x_

                jobs = []
                for g in range(4):
                    hq = kvh * 4 + g
                    lo = (3, 4) if loc % 2 == 0 else (5, 6); loc += 1
                    tiles = [dict(k=kcT, kkeys=["kcs"], nk=NCMP, extras=[(ident[0:NCMP, 0:NCMP], cmask[0:NCMP, tsl], [])],
                                  v=vc, vkeys=["kcs"])]

                    def fin_cmp(ptile, pkey, g=g, lo=lo):
                        nonlocal zctr
                        x_ = fin_branch(g, 0, lo, True)
                        z_ = zctr % 2; zctr += 1
                        P.op("pool", lambda e, o=phat[0:NCMP, z_, :], a=ptile, b=rl[0:NCMP, x_, :]: e.tensor_tensor(out=o, in0=a, in1=b, op=ALU.mult),
                             reads=[pkey, ("rl", x_)], writes=[("phat", z_)])
                        def dfr():
                            for tk in range(4):
                                P.op("pe", lambda e, o=self.ps[:, 2, tk * 32:(tk + 1) * 32], l_=phat[0:NCMP, z_, tk * 128:(tk + 1) * 128], r_=ovl[0:NCMP, :],
                                     s_=(g == 0 and tk == 0), t_=(g == 3): e.matmul(o, lhsT=l_, rhs=r_, start=s_, stop=t_, skip_group_check=True),
                                     reads=[("phat", z_), "cb"], writes=[("ps", 2)])
                        return dfr
                    jobs.append(dict(q=qall[:, hq, :], qkey=("qa", hq), tiles=tiles, lo=lo, fin=fin_cmp))
                self.attn_multi(jobs)
                imp = self.ps[:, 2, 0:128].rearrange("p (c x) -> p c x", c=4)
                P.op("dve", lambda e: e.tensor_tensor(out=score, in0=imp, in1=selct[:, 0, :, :], op=ALU.mult),
                     reads=[("ps", 2), "selct"], writes=["score"])
                P.op("dve", lambda e: e.tensor_tensor(out=score, in0=score, in1=selct[:, 1, :, :], op=ALU.add),
                     reads=["selct"], writes=["score"])
                for tk in range(4):
                    P.op("dve", lambda e, o=mx8[:, tk, :], i_=score[:, tk, :]: e.max(out=o, in_=i_), reads=["score"], writes=[("mx8", tk)])
                for tk in range(4):
                    P.op("dve", lambda e, o=selb[:, tk, :], i_=score[:, tk, :], th=mx8[:, tk, 7:8]:
                         e.tensor_scalar(out=o, in0=i_, scalar1=th, scalar2=NEG, op0=ALU.is_lt, op1=ALU.mult),
                         reads=["score", ("mx8", tk)], writes=[("selb", tk)])
                for tk in range(4):
                    P.op("pe", lambda e, o=self.ps[0:32, 2, tk * 128:(tk + 1) * 128], l_=selb[:, tk, :]:
                         e.matmul(o, lhsT=l_, rhs=ident, start=True, stop=True),
                         reads=[("selb", tk), "cb"], writes=[("ps", 2)])
                P.op("dve", lambda e: e.tensor_copy(out=selbT[0:32, :], in_=self.ps[0:32, 2, :]), reads=[("ps", 2)], writes=["selbT"])
                jobs = []
                for g in range(4):
                    hq = kvh * 4 + g
                    lo = (3, 4) if loc % 2 == 0 else (5, 6); loc += 1
                    tiles = []
                    for kt in range(nkt):
                        ex = [(exv[0:32, kt, :], selbT[0:32, :], ["selbT"])]
                        if kt >= 4 * j:
                            ex.append((ident, cbv[:, kt - 4 * j, :], []))
                        tiles.append(dict(k=ksT[:, kt * 128:(kt + 1) * 128], kkeys=[("kv", c_ks)], nk=128, extras=ex,
                                          v=vs[:, kt, :], vkeys=[("kv", c_vs)]))
                    jobs.append(dict(q=qall[:, hq, :], qkey=("qa", hq), tiles=tiles, lo=lo,
                                     fin=(lambda ptile, pkey, g=g, lo=lo: (fin_branch(g, 1, lo, False), None)[1])))
                for g in range(4):
                    hq = kvh * 4 + g
                    lo = (3, 4) if loc % 2 == 0 else (5, 6); loc += 1
                    tiles = []
                    for kt in range(max(0, 4 * j - 4), nkt):
                        if kt >= 4 * j:
                            ex = [(ident, cbv[:, kt - 4 * j, :], [])]
                        else:
                            ex = [(ident, wbv[:, kt - (4 * j - 4), :], [])]
                        tiles.append(dict(k=kwT[:, kt * 128:(kt + 1) * 128], kkeys=[("kv", c_kw)], nk=128, extras=ex,
                                          v=vw[:, kt, :], vkeys=[("kv", c_vw)]))
                    jobs.append(dict(q=qall[:, hq, :], qkey=("qa", hq), tiles=tiles, lo=lo,
                                     fin=(lambda ptile, pkey, g=g, lo=lo: (fin_branch(g, 2, lo, False), None)[1])))
                self.attn_multi(jobs)
                sl_of = {}
                def fsq(g):
                    nonlocal actr
                    a_ = actr % 2; actr += 1
                    sl_of[g] = a_
                    P.op("pool", lambda e, o=osq[:, a_, :], i_=acc[:, g, :]: e.tensor_tensor(out=o, in0=i_, in1=i_, op=ALU.mult),
                         reads=[("acc", g)], writes=[("osq", a_)])
                    P.op("dve", lambda e, o=o_sb[:, kvh * 4 + g, :], i_=acc[:, g, :], gc=self.vcol(li, 54 + kvh * 4 + g):
                         e.tensor_scalar(out=o, in0=i_, scalar1=gc, scalar2=None, op0=ALU.mult),
                         reads=[("acc", g), "vec"], writes=[("h", kvh * 4 + g)])
                def fsum(g):
                    a_ = sl_of[g]
                    hq = kvh * 4 + g
                    P.op("pe", lambda e, r_=osq[:, a_, :]: e.matmul(self.ps[:, 7, :], lhsT=ones, rhs=r_, start=True, stop=True),
                         reads=[("osq", a_), "cb"], writes=[("ps", 7)])
                    if hq == 0:
                        P.op("dve", lambda e: e.tensor_copy(out=ssq[:, 0, :], in_=self.ps[:, 7, :]), reads=[("ps", 7)], writes=[("ssq", 0)])
                    else:
                        P.op("dve", lambda e: e.tensor_tensor(out=ssq[:, 0, :], in0=self.ps[:, 7, :], in1=ssq[:, 0, :], op=ALU.add),
                             reads=[("ps", 7)], writes=[("ssq", 0)])
                fsq(0)
                for g in range(4):
                    if g + 1 < 4:
                        fsq(g + 1)
                    fsum(g)
            jobs = []
            for hf in range(8):
                ch = []
                for _ in range(3):
                    ch.append(kvc % NKV); kvc += 1
                c_k, c_v, c_b = ch
                a_ = hf % 2
                def pre_fox(hf=hf, c_k=c_k, c_v=c_v, c_b=c_b, a_=a_):
                    P.dma("sp", kvr[:, c_k, 0:nk], self.s_fm.ap()[8 + hf, :, 0:nk], reads=[("s_fm", 8 + hf)], writes=[("kv", c_k)])
                    P.dma("sp", kvr[0:6, c_b, 0:nk], self.s_ab.ap()[hf, 1, :, 0:nk], reads=["s_ab"], writes=[("kv", c_b)])
                    P.dma("sp", abA[0:6, a_, :], self.s_ab.ap()[hf, 0, :, tsl], reads=["s_ab"], writes=[("abA", a_)])
                    P.dma("sp", kvr[:, c_v, 0:nk], self.s_tm.ap()[4 + hf, :, 0:nkt, :].rearrange("p k d -> p (k d)"), reads=[("s_tm", 1), ("s_tm", 2)], writes=[("kv", c_v)])
                fk = kvr[:, c_k, :]
                fv = kvr[:, c_v, :].rearrange("p (k d) -> p k d", d=128)
                Bc = kvr[0:6, c_b, :]
                lo = (3, 4) if loc % 2 == 0 else (5, 6); loc += 1
                tiles = []
                for kt in range(nkt):
                    ex = [(Bc[:, kt * 128:(kt + 1) * 128], abA[0:6, a_, :], [("kv", c_b), ("abA", a_)])]
                    if kt >= 4 * j:
                        ex.append((ident, cbv[:, kt - 4 * j, :], []))
                    tiles.append(dict(k=fk[:, kt * 128:(kt + 1) * 128], kkeys=[("kv", c_k)], nk=128, extras=ex,
                                      v=fv[:, kt, :], vkeys=[("kv", c_v)]))

                def fin_fox(ptile, pkey, hf=hf, lo=lo):
                    nonlocal rctr, actr
                    lb, ob = lo
                    x_ = rctr % 2; rctr += 1
                    rlb = rl[:, x_, :]
                    P.op("dve", lambda e, o=rlb, i_=self.ps[:, lb, :]: e.reciprocal(out=o, in_=i_), reads=[("ps", lb)], writes=[("rl", x_)])
                    f_ = hf % 2
                    P.op("dve", lambda e, o=of[:, f_, :], a=self.ps[:, ob, :], b=rlb: e.tensor_tensor(out=o, in0=a, in1=b, op=ALU.mult),
                         reads=[("ps", ob), ("rl", x_)], writes=[("of", f_)])
                    q_ = actr % 2; actr += 1
                    P.op("pool", lambda e, o=osq[:, q_, :], i_=of[:, f_, :]: e.tensor_tensor(out=o, in0=i_, in1=i_, op=ALU.mult),
                         reads=[("of", f_)], writes=[("osq", q_)])
                    P.op("dve", lambda e, o=o_sb[:, 8 + hf, :], i_=of[:, f_, :], gc=self.vcol(li, 62 + hf):
                         e.tensor_scalar(out=o, in0=i_, scalar1=gc, scalar2=None, op0=ALU.mult),
                         reads=[("of", f_), "vec"], writes=[("h", 8 + hf)])

                    def dfr():
                        P.op("pe", lambda e, r_=osq[:, q_, :]: e.matmul(self.ps[:, 7, :], lhsT=ones, rhs=r_, start=True, stop=True),
                             reads=[("osq", q_), "cb"], writes=[("ps", 7)])
                        if hf == 0:
                            P.op("dve", lambda e: e.tensor_copy(out=ssq[:, 1, :], in_=self.ps[:, 7, :]), reads=[("ps", 7)], writes=[("ssq", 1)])
                        else:
                            P.op("dve", lambda e: e.tensor_tensor(out=ssq[:, 1, :], in0=self.ps[:, 7, :], in1=ssq[:, 1, :], op=ALU.add),
                                 reads=[("ps", 7)], writes=[("ssq", 1)])
                    return dfr
                jobs.append(dict(q=qall[:, 8 + hf, :], qkey=("qa", 8 + hf), tiles=tiles, lo=lo, fin=fin_fox, pre=pre_fox))
            self.attn_multi(jobs)
            for k in range(2):
                P.op("act", lambda e, o=rstd[:, k, :], i_=ssq[:, k, :]: e.activation(out=o, in_=i_, func=AF.Ln, bias=EPS, scale=1.0 / 1024.0),
                     reads=[("ssq", k)], writes=[("rstd", k)])
                P.op("act", lambda e, o=rstd[:, k, :]: e.activation(out=o, in_=o, func=AF.Exp, scale=-0.5),
                     reads=[("rstd", k)], writes=[("rstd", k)])
            for kc in range(16):
                P.op("dve" if kc % 2 == 0 else "pool", lambda e, o=o_sb[:, kc, :], b=rstd[:, kc // 8, :]: e.tensor_tensor(out=o, in0=o, in1=b, op=ALU.mult),
                     reads=[("rstd", kc // 8)], writes=[("h", kc)])
            if j + 1 < NTT:
                self.norm_sq(xt, qall, "xt", sqkey="qa")

            def issue(dco):
                nonlocal sctr
                sl = sctr % NS2; sctr += 1
                k = dco % 4
                P.dma("sp", wring[:, sl, :], wout[dco], reads=self.wkeys(li, "wout"), writes=[("ws", sl)])
                P.dma("sp", xst[:, k, :], M["src"].ap()[s, dco, :, tsl], reads=[("X", s, j, dco)], writes=[("xst", k)])
                return sl
            slots = {0: issue(0), 1: issue(1)}
            for dco in range(DC):
                if dco + 2 < DC:
                    slots[dco + 2] = issue(dco + 2)
                sl = slots[dco]
                k = dco % 4
                w = wring[:, sl, :].rearrange("p (c x) -> p c x", c=16)
                bnk = dco % 2
                for kc in range(16):
                    P.op("pe", lambda e, o=self.ps[:, bnk, :], ww=w[:, kc, :], r=o_sb[:, kc, :], s_=(kc == 0), t_=(kc == 15):
                         e.matmul(o, lhsT=ww, rhs=r, start=s_, stop=t_),
                         reads=[("ws", sl), ("h", kc)], writes=[("ps", bnk)])
                P.op("dve", lambda e, o=xst[:, k, :], a=self.ps[:, bnk, :]: e.tensor_tensor(out=o, in0=a, in1=o, op=ALU.add),
                     reads=[("ps", bnk)], writes=[("xst", k)])
                P.dma("act", self.xs.ap()[s, dco, :, tsl], xst[:, k, :], reads=[("xst", k)], writes=[("X", s, j, dco)])
            if j + 1 < NTT:
                self.norm_sum(qall, wg, M["rs"], 2, sqkey="qa", tmpkey="wg")


def _pack_layer(w, li):
    out = np.zeros(NPK, np.float32)

    def put(name, arr):
        o, n = PK[name]
        a = np.ascontiguousarray(arr, dtype=np.float32).reshape(-1)
        assert a.size == n, (name, a.size, n)
        out[o:o + n] = a

    for f in (1, 2):
        wg = w[f"ffn{f}_w_gate"][li]
        wu = w[f"ffn{f}_w_up"][li]
        wd = w[f"ffn{f}_w_down"][li]
        put(f"wg{f}", wg.reshape(DC, 128, 11, 512).transpose(2, 1, 0, 3))
        put(f"wu{f}", wu.reshape(DC, 128, 11, 512).transpose(2, 1, 0, 3))
        put(f"wd{f}", wd.reshape(FC, 128, DC, 128).transpose(2, 1, 0, 3))
    win = w["w_in"][li]
    kv0 = 1024
    def kvcol(branch, typ, kvh):
        return kv0 + ((branch * 2 + typ) * 2 + kvh) * 128
    fq0 = 2584
    fk0 = fq0 + 1024
    fv0 = fq0 + 2048
    cols1 = ([kvcol(0, 0, 0), kvcol(0, 0, 1), kvcol(0, 1, 0), kvcol(0, 1, 1),
              kvcol(1, 0, 0), kvcol(1, 0, 1), kvcol(2, 0, 0), kvcol(2, 0, 1)]
             + [fk0 + h * 128 for h in range(8)])
    def fm(cols):
        blk = np.stack([win[:, c:c + 128] for c in cols], 0)
        return blk.reshape(len(cols), DC, 128, 128).transpose(0, 2, 1, 3)
    put("wfm1", fm(cols1))
    tmcols = np.concatenate([np.arange(kvcol(1, 1, 0), kvcol(1, 1, 0) + 256),
                             np.arange(kvcol(2, 1, 0), kvcol(2, 1, 0) + 256),
                             np.arange(fv0, fv0 + 1024)])
    wt = win[:, tmcols]
    put("wtm", wt.reshape(DC, 128, 3, 512).transpose(2, 1, 0, 3))
    cols2 = [h * 128 for h in range(8)] + [fq0 + h * 128 for h in range(8)]
    put("wfm2", fm(cols2))
    wo = w["w_out"][li]
    put("wout", wo.reshape(16, 128, DC, 128).transpose(2, 1, 0, 3))
    cw1 = w["cmp_w1"][li]
    put("cw1", cw1.reshape(2, 32, 128, 256).transpose(0, 2, 1, 3))
    cw2 = w["cmp_w2"][li]
    put("cw2", cw2.reshape(2, 2, 128, 128).transpose(0, 2, 1, 3))
    put("wf", win[:, 5656:5664].reshape(DC, 128, 8).transpose(1, 0, 2))
    put("wgt", win[:, 2560:2584].reshape(DC, 128, 24).transpose(1, 0, 2))
    pos = w["cmp_pos_emb"][li]
    put("pos", pos.transpose(2, 0, 1))
    return out


def _vec_pack(w, layers):
    v = np.zeros((128, len(layers) * VW), np.float32)
    for i, li in enumerate(layers):
        b = i * VW
        v[:, b + 0:b + 16] = w["ffn1_norm"][li].reshape(DC, 128).T
        v[:, b + 16:b + 32] = w["mix_norm"][li].reshape(DC, 128).T
        v[:, b + 32:b + 48] = w["ffn2_norm"][li].reshape(DC, 128).T
        v[:, b + 48] = w["nsa_q_norm"][li]
        v[:, b + 49:b + 52] = w["nsa_k_norm"][li].T
        v[:, b + 52] = w["fox_q_norm"][li]
        v[:, b + 53] = w["fox_k_norm"][li]
        v[:, b + 54:b + 62] = w["nsa_out_norm"][li].reshape(8, 128).T
        v[:, b + 62:b + 70] = w["fox_out_norm"][li].reshape(8, 128).T
        v[0:8, b + 70] = w["fox_forget_bias"][li]
    return v


def _consts():
    cb = np.zeros((128, NCB), np.float32)
    def put(name, arr):
        o, n = CB[name]
        cb[:arr.shape[0], o:o + n] = arr.reshape(arr.shape[0], -1)
    put("ident", np.eye(128, dtype=np.float32))
    put("ones", np.ones((128, 128), np.float32))
    rt = np.zeros((32, 32), np.float32)
    for i in range(16):
        rt[16 + i, i] = -1.0
        rt[i, 16 + i] = 1.0
    put("rt", rt)
    cstart = np.arange(NCMP) * 16
    sstart = np.arange(32) * 64
    ovl = ((cstart[:, None] < sstart[None, :] + 64) & (cstart[:, None] + 32 > sstart[None, :])).astype(np.float32)
    put("ovl", ovl)
    p = np.arange(128)[:, None]
    n = np.arange(512)[None, :]
    cbm = np.stack([np.where(128 * k + p <= n, 0.0, NEG) for k in range(4)], 1)
    wbm = np.stack([np.where(128 * k + p > n, 0.0, NEG) for k in range(4)], 1)
    put("cb", cbm.astype(np.float32))
    put("wb", wbm.astype(np.float32))
    cend = cstart + 31
    t = np.arange(T)[None, :]
    put("cmask", np.where(cend[:, None] <= t, 0.0, NEG).astype(np.float32))
    j = np.arange(32)[:, None, None]
    kt = np.arange(16)[None, :, None]
    pp = np.arange(128)[None, None, :]
    put("ex", (j == 2 * kt + pp // 64).astype(np.float32))
    r = np.arange(32)[:, None, None]
    rr = np.arange(24)[None, :, None]
    put("esel", np.broadcast_to((r == rr), (32, 24, 128)).astype(np.float32))
    inv = (np.float32(500000.0) ** (-np.arange(0, 32, 2, dtype=np.float32) / np.float32(32))).astype(np.float32)
    def tables(pos):
        ang = pos.astype(np.float32)[:, None] * inv[None, :]
        c = np.cos(ang).astype(np.float32).T
        s_ = np.sin(ang).astype(np.float32).T
        return np.concatenate([c, c], 0), np.concatenate([s_, s_], 0)
    rc, rs = tables(np.arange(T))
    kc_c, kc_s = tables(cend)
    ropekc = np.zeros((32, 2, 128), np.float32)
    ropekc[:, 0, :NCMP] = kc_c
    ropekc[:, 1, :NCMP] = kc_s
    tok = np.arange(T)
    tb = tok // 64
    jj = np.arange(32)[None, :]
    causal = jj <= tb[:, None]
    forced = (jj == 0) | (causal & (jj > tb[:, None] - 2))
    mmul = (causal & ~forced).astype(np.float32)
    badd = np.where(forced, 1e9, np.where(causal, 0.0, -1e30)).astype(np.float32)
    selc = np.stack([mmul.reshape(16, 128, 32).transpose(1, 0, 2), badd.reshape(16, 128, 32).transpose(1, 0, 2)], 1)
    return cb, rc, rs, ropekc, np.ascontiguousarray(selc)


_CACHE = {}


def _get_prog(nseq, layers_key, dbg_key=None):
    key = (nseq, layers_key, dbg_key)
    if key not in _CACHE:
        b = Builder(nseq, list(layers_key), dict(dbg_key or ()))
        _CACHE[key] = b.build()
    return _CACHE[key]


def kernel(**inputs):
    x = np.asarray(inputs["x"], np.float32)
    B = x.shape[0]
    ncores = 8
    nseq = B // ncores
    w = {k: np.asarray(v) for k, v in inputs.items() if k != "x"}
    layers = tuple(range(DEPTH))
    wsrc = np.concatenate([_pack_layer(w, li) for li in layers]).reshape(len(layers) * NPKR, 2048)
    vec = _vec_pack(w, layers)
    cb, rc, rs, ropekc, selc = _consts()
    nc = _get_prog(nseq, layers)
    in_maps = []
    for c in range(ncores):
        xc = x[c * nseq:(c + 1) * nseq]
        xT = np.ascontiguousarray(xc.transpose(0, 2, 1)).reshape(nseq, DC, 128, T)
        in_maps.append({"xin": xT, "wsrc": wsrc, "vec": vec, "cbf": cb, "ropec": rc, "ropes": rs,
                        "ropekc": ropekc, "selc": selc})
    res = run_bass_kernel_spmd(nc, in_maps, core_ids=list(range(ncores)))
    outs = []
    for c in range(ncores):
        y = res.results[c]["xout"].reshape(nseq, D, T).transpose(0, 2, 1)
        outs.append(y)
    return np.ascontiguousarray(np.concatenate(outs, 0), dtype=np.float32)
```

```python
import numpy as np
import concourse.bass as bass
import concourse.mybir as mybir
from concourse.bass_utils import run_bass_kernel_spmd

F32 = mybir.dt.float32
BF16 = mybir.dt.bfloat16
U8 = mybir.dt.uint8
AF = mybir.ActivationFunctionType
ALU = mybir.AluOpType

D = 2048
T = 2048
DEPTH = 4
HD = 128
DFF = 5632
DC = D // 128
FC = DFF // 128
TT = 512
NTT = T // TT
EPS = 1e-6
SCALE = HD ** -0.5
NEG = -32768.0
NCMP = 127
VW = 72

PK = {}
_off = 0
def _add(name, n):
    global _off
    PK[name] = (_off, n)
    _off += n
for _f in (1, 2):
    _add(f"wg{_f}", D * DFF)
    _add(f"wu{_f}", D * DFF)
    _add(f"wd{_f}", D * DFF)
_add("wfm1", 16 * 128 * 16 * 128)
_add("wtm", 3 * 128 * 16 * 512)
_add("wfm2", 16 * 128 * 16 * 128)
_add("wout", 16 * 128 * 16 * 128)
_add("cw1", 2 * 128 * 32 * 256)
_add("cw2", 2 * 128 * 2 * 128)
_add("wf", 128 * 16 * 8)
_add("wgt", 128 * 16 * 24)
_add("pos", 128 * 2 * 32)
NPK = ((_off + 2047) // 2048) * 2048
NPKR = NPK // 2048

CB = {}
_c = 0
def _addc(name, n):
    global _c
    CB[name] = (_c, n)
    _c += n
_addc("ident", 128)
_addc("ones", 128)
_addc("rt", 32)
_addc("ovl", 32)
_addc("cb", 4 * 512)
_addc("wb", 4 * 512)
_addc("cmask", 2048)
_addc("ex", 16 * 128)
_addc("esel", 24 * 128)
NCB = _c


class Prog:
    NS = 8

    def __init__(self, nc):
        self.nc = nc
        self.ops = []
        self.deps = []
        self.last_w = {}
        self.readers = {}
        self.streams = {k: [] for k in ("pe", "act", "dve", "pool", "sp")}
        self.groups = {}
        self.last_op = {k: None for k in self.streams}
        self.recent_dma = {"sp": [], "pool": [], "act": []}
        self.cur_barrier = None

    def _record(self, stream, fn, kind, reads, writes, group=None, is_barrier=False):
        i = len(self.ops)
        d = set()
        lw = self.last_w
        rd = self.readers
        for k in reads:
            w = lw.get(k)
            if w is not None:
                d.add(w)
            r = rd.get(k)
            if r is None:
                r = rd[k] = [{}, []]
            if kind == "c":
                r[0][stream] = i
            else:
                r[1].append(i)
        for k in writes:
            w = lw.get(k)
            if w is not None:
                d.add(w)
            r = rd.get(k)
            if r is not None:
                d.update(r[0].values())
                d.update(r[1])
                del rd[k]
            lw[k] = i
        if is_barrier:
            for st, li in self.last_op.items():
                if li is not None:
                    d.add(li)
            for q, lst in self.recent_dma.items():
                d.update(lst)
        elif self.cur_barrier is not None:
            d.add(self.cur_barrier)
        d.discard(i)
        self.ops.append((stream, fn, kind, group))
        self.deps.append(d)
        self.streams[stream].append(i)
        if kind == "c":
            self.last_op[stream] = i
        elif group is None:
            lst = self.recent_dma[stream]
            lst.append(i)
            if len(lst) > self.NS:
                lst.pop(0)
        if group is not None:
            self.groups[group] = self.groups.get(group, 0) + 1
        if is_barrier:
            self.cur_barrier = i
        return i

    def op(self, stream, fn, reads=(), writes=()):
        return self._record(stream, fn, "c", reads, writes)

    def dma(self, q, out, in_, reads=(), writes=(), group=None):
        return self._record(q, lambda e, o=out, i=in_: e.dma_start(out=o, in_=i), "d",
                            reads, writes, group)

    def barrier(self):
        self._record("sp", lambda e: e.nop(), "c", [], [], is_barrier=True)

    def emit(self, block, sems_ctx):
        nc = self.nc
        ops = self.ops
        n = len(ops)
        dsem = {}
        qcount = {"sp": 0, "pool": 0, "act": 0}
        qhist = {"sp": [], "pool": [], "act": []}
        extra = {}
        for i in range(n):
            st, fn, kind, group = ops[i]
            if kind != "d":
                continue
            if group is not None:
                dsem[i] = (("g", group), 16 * self.groups[group])
            else:
                c = qcount[st]
                dsem[i] = (("s", st, c % self.NS), 16 * (c // self.NS + 1))
                if c >= self.NS:
                    extra[i] = qhist[st][c - self.NS]
                qhist[st].append(i)
                qcount[st] = c + 1
        sig = [False] * n
        for i in range(n):
            st = ops[i][0]
            for d in self.deps[i]:
                sd, _, kd, _ = ops[d]
                if kd == "c" and not (sd == "pe" and st == "pe"):
                    sig[d] = True
        cnt = {k: 0 for k in self.streams}
        sval = [0] * n
        for i in range(n):
            st, fn, kind, group = ops[i]
            if kind == "c" and sig[i]:
                cnt[st] += 1
                sval[i] = cnt[st]
        semkeys = set(("e", k) for k in self.streams)
        for i in dsem:
            semkeys.add(dsem[i][0])
        sem = {}
        for k in sorted(semkeys, key=str):
            sem[k] = sems_ctx.enter_context(nc.semaphore("s_" + "_".join(str(x) for x in k)))
        waits = [None] * n
        waited = {k: {} for k in self.streams}
        for st in self.streams:
            wd = waited[st]
            for i in self.streams[st]:
                need = {}
                dl = self.deps[i]
                if i in extra:
                    dl = set(dl)
                    dl.add(extra[i])
                for d in dl:
                    sd, _, kd, _ = ops[d]
                    if kd == "d":
                        k, v = dsem[d]
                    else:
                        if sd == "pe" and st == "pe":
                            continue
                        k, v = ("e", sd), sval[d]
                    if need.get(k, 0) < v:
                        need[k] = v
                w = []
                for k, v in need.items():
                    if wd.get(k, 0) < v:
                        wd[k] = v
                        w.append((sem[k], v))
                waits[i] = w
        self.n_waits = sum(len(w) for w in waits)
        self.n_sig = sum(sig)

        def run_stream(st, eng):
            for i in self.streams[st]:
                _, fn, kind, group = ops[i]
                for s, v in waits[i]:
                    eng.wait_ge(s, v)
                ins = fn(eng)
                if kind == "d":
                    ins.then_inc(sem[dsem[i][0]], 16)
                elif sig[i]:
                    ins.then_inc(sem[("e", st)], 1)

        @block.tensor
        def _(e):
            run_stream("pe", e)

        @block.scalar
        def _(e):
            run_stream("act", e)

        @block.vector
        def _(e):
            run_stream("dve", e)

        @block.gpsimd
        def _(e):
            run_stream("pool", e)

        @block.sync
        def _(e):
            run_stream("sp", e)


class Arena:
    def __init__(self, ap, size):
        self.ap = ap
        self.size = size
        self.off = 0

    def take(self, nbytes, dtype, pattern=None, **kw):
        rb = (nbytes + 63) // 64 * 64
        assert self.off + rb <= self.size, ("arena overflow", self.off, rb, self.size)
        v = self.ap[:, self.off:self.off + nbytes].bitcast(dtype)
        self.off += rb
        if pattern:
            v = v.rearrange(pattern, **kw)
        return v

    def mark(self):
        return self.off

    def reset(self, m):
        self.off = m


class Builder:
    def __init__(self, nseq, layers, dbg=None):
        self.nseq = nseq
        self.layers = layers
        self.dbg = dbg or {}
        nc = bass.Bass("TRN2", target_bir_lowering=False)
        self.nc = nc
        self.P = Prog(nc)
        L = len(layers)
        self.L = L
        self.xin = nc.dram_tensor("xin", [nseq, DC, 128, T], F32, kind="ExternalInput")
        self.xs = nc.dram_tensor("xout", [nseq, DC, 128, T], F32, kind="ExternalOutput")
        self.wsrc = nc.dram_tensor("wsrc", [L * NPKR, 2048], F32, kind="ExternalInput")
        self.wpk = [nc.dram_tensor(f"wpk{i}", [NPKR, 2048], BF16, kind="Internal") for i in range(L)]
        self.vec = nc.dram_tensor("vec", [128, L * VW], F32, kind="ExternalInput")
        self.cbf = nc.dram_tensor("cbf", [128, NCB], F32, kind="ExternalInput")
        self.ropec = nc.dram_tensor("ropec", [32, T], F32, kind="ExternalInput")
        self.ropes = nc.dram_tensor("ropes", [32, T], F32, kind="ExternalInput")
        self.ropekc = nc.dram_tensor("ropekc", [32, 2, 128], F32, kind="ExternalInput")
        self.selc = nc.dram_tensor("selc", [128, 2, 16, 32], F32, kind="ExternalInput")
        sk = "ExternalOutput" if self.dbg.get("dump") else "Internal"
        self.s_fm = nc.dram_tensor("s_fm", [16, 128, T], BF16, kind=sk)
        self.s_tm = nc.dram_tensor("s_tm", [12, 128, 16, 128], BF16, kind=sk)
        self.s_kc = nc.dram_tensor("s_kc", [2, 128, 128], BF16, kind=sk)
        self.s_vc = nc.dram_tensor("s_vc", [2, 128, 128], BF16, kind=sk)
        self.s_ab = nc.dram_tensor("s_ab", [8, 2, 6, T], BF16, kind=sk)
        ASZ = 206 * 1024
        self.arena_t = nc.alloc_sbuf_tensor("arena", [128, ASZ], U8)
        self.A = Arena(self.arena_t.ap(), ASZ)
        self.ps = nc.alloc_psum_tensor("ps", [128, 8, 512], F32).ap()
        A = self.A
        self.cb_sb = A.take(NCB * 2, BF16)
        self.vec_sb = A.take(L * VW * 4, F32)
        self.base_mark = A.mark()
        self.wslot_ctr = 0

    def cview(self, name, rows=128):
        o, n = CB[name]
        return self.cb_sb[0:rows, o:o + n]

    def vcol(self, li, c, rows=128, n=1):
        return self.vec_sb[0:rows, li * VW + c: li * VW + c + n]

    def wview(self, li, name):
        o, n = PK[name]
        flat = self.wpk[li].ap().rearrange("r c -> (r c)")
        return flat[o:o + n]

    def prologue(self):
        P = self.P
        P.dma("pool", self.cb_sb, self.cbf.ap(), writes=["cb"])
        P.dma("sp", self.vec_sb, self.vec.ap(), writes=["vec"])
        self.convert(0)

    CONV_CH = 4096

    def conv_chunks(self, li):
        if li == 0:
            return [(r, min(self.CONV_CH, NPKR - r)) for r in range(0, NPKR, self.CONV_CH)]
        return [(0, NPKR)]

    def convert(self, li):
        P = self.P
        r0 = li * NPKR
        for k, (r, n) in enumerate(self.conv_chunks(li)):
            last = None
            rr = r
            while rr < r + n:
                m = min(4096, r + n - rr)
                last = P.dma("pool", self.wpk[li].ap()[rr:rr + m, :], self.wsrc.ap()[r0 + rr:r0 + rr + m, :],
                             writes=[], group=f"conv{li}_{k}")
                rr += m
            P.last_w[("w", li, k)] = last

    def wkeys(self, li, name):
        o, n = PK[name]
        lo, hi = o // 2048, (o + n - 1) // 2048
        return [("w", li, k) for k, (r, m) in enumerate(self.conv_chunks(li)) if r <= hi and r + m - 1 >= lo]

    def norm_tile(self, xt, h, sq, tmp, rs, li, gcol, xkey, hkey, psb, sqkey="sqa", tmpkey="ntmp"):
        self.norm_sq(xt, sq, xkey, sqkey)
        self.norm_sum(sq, tmp, rs, psb, sqkey, tmpkey)
        self.norm_apply(xt, h, rs, li, gcol, xkey, hkey)

    def norm_sq(self, xt, sq, xkey, sqkey="sqa"):
        P = self.P
        for c in range(4):
            o_ = sq[:, 4 * c:4 * c + 4, :]
            i_ = xt[:, 4 * c:4 * c + 4, :]
            rk = [(xkey, 4 * c + k) for k in range(4)]
            wk = [(sqkey, 4 * c + k) for k in range(4)]
            if c in (0, 2):
                P.op("act", lambda e, o=o_, i=i_: e.activation(out=o, in_=i, func=AF.Square), reads=rk, writes=wk)
            else:
                P.op("dve", lambda e, o=o_, i=i_: e.tensor_tensor(out=o, in0=i, in1=i, op=ALU.mult), reads=rk, writes=wk)

    def norm_sum(self, sq, tmp, rs, psb, sqkey="sqa", tmpkey="ntmp"):
        P = self.P
        ones = self.cview("ones")
        psn = self.ps[:, psb, :]
        pk = ("ps", psb)
        for dc in range(DC):
            P.op("pe", lambda e, r=sq[:, dc, :], s=(dc == 0), t=(dc == DC - 1): e.matmul(psn, lhsT=ones, rhs=r, start=s, stop=t),
                 reads=[(sqkey, dc), "cb"], writes=[pk])
        P.op("act", lambda e: e.activation(out=tmp, in_=psn, func=AF.Sqrt, bias=EPS, scale=1.0 / D),
             reads=[pk], writes=[tmpkey])
        P.op("dve", lambda e: e.reciprocal(out=rs, in_=tmp), reads=[tmpkey], writes=["nrs"])

    def norm_apply(self, xt, h, rs, li, gcol, xkey, hkey):
        P = self.P
        for dc in range(DC):
            P.op("dve", lambda e, o=h[:, dc, :], i=xt[:, dc, :], g=self.vcol(li, gcol + dc):
                 e.scalar_tensor_tensor(out=o, in0=i, scalar=g, in1=rs, op0=ALU.mult, op1=ALU.mult),
                 reads=[(xkey, dc), "nrs", "vec"], writes=[(hkey, dc)])

    def ffn_tile(self, li, f, xt, h, sq, tmp, rs, aT, sg, wring, xkey, after_chunk=None):
        P = self.P
        NSLOT = wring.shape[1]
        self.norm_tile(xt, h, sq, tmp, rs, li, {1: 0, 2: 32}[f], xkey, "h", 0)
        wg = self.wview(li, f"wg{f}").rearrange("(g p x) -> g p x", g=11, p=128)
        wu = self.wview(li, f"wu{f}").rearrange("(g p x) -> g p x", g=11, p=128)
        wd = self.wview(li, f"wd{f}").rearrange("(g p x) -> g p x", g=16, p=128)
        it = 0
        for fg in range(11):
            sa = self.wslot_ctr % NSLOT; self.wslot_ctr += 1
            sb_ = self.wslot_ctr % NSLOT; self.wslot_ctr += 1
            P.dma("sp", wring[:, sa, :], wg[fg], reads=self.wkeys(li, f"wg{f}"), writes=[("ws", sa)])
            P.dma("sp", wring[:, sb_, :], wu[fg], reads=self.wkeys(li, f"wu{f}"), writes=[("ws", sb_)])
            wa = wring[:, sa, :].rearrange("p (c x) -> p c x", c=DC)
            wb = wring[:, sb_, :].rearrange("p (c x) -> p c x", c=DC)
            for j in range(4):
                fc = fg * 4 + j
                bg = 1 + (it % 2)
                bu = 3 + (it % 2)
                sgs = it % 2
                it += 1
                psg = self.ps[:, bg, :]
                psu = self.ps[:, bu, :]
                for dc in range(DC):
                    P.op("pe", lambda e, o=psg, w=wa[:, dc, j * 128:(j + 1) * 128], r=h[:, dc, :], s=(dc == 0), t=(dc == DC - 1):
                         e.matmul(o, lhsT=w, rhs=r, start=s, stop=t),
                         reads=[("ws", sa), ("h", dc)], writes=[("ps", bg)])
                for dc in range(DC):
                    P.op("pe", lambda e, o=psu, w=wb[:, dc, j * 128:(j + 1) * 128], r=h[:, dc, :], s=(dc == 0), t=(dc == DC - 1):
                         e.matmul(o, lhsT=w, rhs=r, start=s, stop=t),
                         reads=[("ws", sb_), ("h", dc)], writes=[("ps", bu)])
                P.op("act", lambda e, o=sg[:, sgs, :], i=psg: e.activation(out=o, in_=i, func=AF.Silu),
                     reads=[("ps", bg)], writes=[("sg", sgs)])
                P.op("dve", lambda e, o=aT[:, fc, :], a=psu, b=sg[:, sgs, :]: e.tensor_tensor(out=o, in0=a, in1=b, op=ALU.mult),
                     reads=[("ps", bu), ("sg", sgs)], writes=[("aT", fc)])
        for dco in range(DC):
            s = self.wslot_ctr % NSLOT; self.wslot_ctr += 1
            P.dma("sp", wring[:, s, 0:FC * 128], wd[dco], reads=self.wkeys(li, f"wd{f}"), writes=[("ws", s)])
            w = wring[:, s, 0:FC * 128].rearrange("p (c x) -> p c x", c=FC)
            bd = 5 + (dco % 2)
            psd = self.ps[:, bd, :]
            for fc in range(FC):
                P.op("pe", lambda e, o=psd, ww=w[:, fc, :], r=aT[:, fc, :], s_=(fc == 0), t_=(fc == FC - 1):
                     e.matmul(o, lhsT=ww, rhs=r, start=s_, stop=t_),
                     reads=[("ws", s), ("aT", fc)], writes=[("ps", bd)])
            P.op("dve", lambda e, o=xt[:, dco, :], a=psd: e.scalar_tensor_tensor(out=o, in0=a, scalar=0.5, in1=o, op0=ALU.mult, op1=ALU.add),
                 reads=[("ps", bd)], writes=[(xkey, dco)])
            if after_chunk is not None:
                after_chunk(dco)

    def ffn_phase(self, s, jobs, src_is_input):
        P = self.P
        A = self.A
        P.barrier()
        A.reset(self.base_mark)
        xt = A.take(DC * TT * 4, F32, "p (c t) -> p c t", c=DC)
        h = A.take(DC * TT * 2, BF16, "p (c t) -> p c t", c=DC)
        sq = A.take(DC * TT * 2, BF16, "p (c t) -> p c t", c=DC)
        tmp = A.take(TT * 4, F32)
        rs = A.take(TT * 4, F32)
        aT = A.take(FC * TT * 2, BF16, "p (c t) -> p c t", c=FC)
        sg = A.take(2 * TT * 2, BF16, "p (c t) -> p c t", c=2)
        NSLOT = 4
        wring = A.take(NSLOT * 16384, BF16, "p (s x) -> p s x", s=NSLOT)
        src = self.xin if src_is_input else self.xs
        for tt in range(NTT):
            tsl = slice(tt * TT, (tt + 1) * TT)
            if tt == 0:
                P.dma("sp", xt, src.ap()[s, :, :, tsl].rearrange("c p t -> p c t"),
                      reads=[("X", s, tt, dc) for dc in range(DC)], writes=[("xt", dc) for dc in range(DC)])

            def after_chunk(dco, tt=tt, tsl=tsl):
                P.dma("act", self.xs.ap()[s, dco, :, tsl], xt[:, dco, :], reads=[("xt", dco)], writes=[("X", s, tt, dco)])
                if tt + 1 < NTT:
                    nsl = slice((tt + 1) * TT, (tt + 2) * TT)
                    P.dma("pool", xt[:, dco, :], src.ap()[s, dco, :, nsl], reads=[("X", s, tt + 1, dco)], writes=[("xt", dco)])
            for n_, (li, f) in enumerate(jobs):
                self.ffn_tile(li, f, xt, h, sq, tmp, rs, aT, sg, wring, "xt",
                              after_chunk=(after_chunk if n_ == len(jobs) - 1 else None))

    def build(self):
        P = self.P
        self.prologue()
        L = self.L
        mode = self.dbg.get("mode", "full")
        for s in range(self.nseq):
            first = True
            for li in range(L):
                jobs = []
                if li > 0:
                    jobs.append((li - 1, 2))
                jobs.append((li, 1))
                if mode in ("full", "ffn"):
                    self.ffn_phase(s, jobs, src_is_input=first)
                    first = False
                if s == 0 and li + 1 < L:
                    self.convert(li + 1)
                if mode in ("full", "mixer"):
                    self.mixer_phase(s, li, src_is_input=first)
                    first = False
            if mode in ("full", "ffn"):
                self.ffn_phase(s, [(L - 1, 2)], src_is_input=False)
        keys = [("X", s, tt, dc) for s in range(self.nseq) for tt in range(NTT) for dc in range(DC)]
        P.op("sp", lambda e: e.nop(), reads=keys)
        from contextlib import ExitStack
        with ExitStack() as es:
            es.enter_context(self.nc.allow_low_precision("bf16 matmul operands by design; accumulation is fp32"))
            block = es.enter_context(self.nc.Block())
            P.emit(block, es)
        return self.nc

    def mixer_phase(self, s, li, src_is_input):
        P, A = self.P, self.A
        P.barrier()
        A.reset(self.base_mark)
        M = {}
        M["xt"] = A.take(DC * TT * 4, F32, "p (c t) -> p c t", c=DC)
        M["h"] = A.take(DC * TT * 2, BF16, "p (c t) -> p c t", c=DC)
        M["tmp"] = A.take(TT * 4, F32)
        M["rs"] = A.take(TT * 4, F32)
        cm0 = A.mark()
        M["cfull"] = A.take(T * 4, F32)
        M["src"] = self.xin if src_is_input else self.xs
        cm = A.mark()
        if self.dbg.get("skip1") is None:
            self.pass1a(s, li, M)
            P.barrier()
            A.reset(cm)
            self.pass1b(s, li, M)
        if self.dbg.get("only1"):
            return
        P.barrier()
        A.reset(cm0)
        self.pass2(s, li, M)

    def load_x(self, M, s, tt):
        self.P.dma("sp", M["xt"], M["src"].ap()[s, :, :, tt * TT:(tt + 1) * TT].rearrange("c p t -> p c t"),
                   reads=[("X", s, tt, dc) for dc in range(DC)], writes=[("xt", dc) for dc in range(DC)])

    def head_norm(self, ps_in, pskey, n, gain, W, out_bf, outkey, rope=None, sumbank=3, rotbank=4):
        st = self.head_norm_B(ps_in, pskey, n, gain, W, out_bf, outkey, rope, sumbank)
        self.head_norm_C(st, rotbank)

    def head_norm_B(self, ps_in, pskey, n, gain, W, out_bf, outkey, rope=None, sumbank=3):
        P = self.P
        ones = self.cview("ones")
        a = self.hn_ctr % 2
        self.hn_ctr += 1
        hsq = W["hsq"][:, a, 0:n]
        pss = self.ps[:, sumbank, 0:n]
        P.op("act", lambda e: e.activation(out=hsq, in_=ps_in, func=AF.Square), reads=[pskey], writes=[("hsq", a)])
        P.op("pe", lambda e: e.matmul(pss, lhsT=ones, rhs=hsq, start=True, stop=True),
             reads=[("hsq", a), "cb"], writes=[("ps", sumbank)])
        hl = W["hl"][:, 0:n]
        hr = W["hr"][:, 0:n]
        P.op("act", lambda e: e.activation(out=hl, in_=pss, func=AF.Ln, bias=EPS, scale=1.0 / HD),
             reads=[("ps", sumbank)], writes=["hl"])
        P.op("act", lambda e: e.activation(out=hr, in_=hl, func=AF.Exp, scale=-0.5), reads=["hl"], writes=["hr"])
        if rope is None:
            P.op("dve", lambda e: e.scalar_tensor_tensor(out=out_bf, in0=ps_in, scalar=gain, in1=hr, op0=ALU.mult, op1=ALU.mult),
                 reads=[pskey, "hr", "vec"], writes=[outkey])
            return None
        kn = W["kn"][:, a, 0:n]
        P.op("dve", lambda e: e.scalar_tensor_tensor(out=kn, in0=ps_in, scalar=gain, in1=hr, op0=ALU.mult, op1=ALU.mult),
             reads=[pskey, "hr", "vec"], writes=[("kn", a)])
        knb = W["knb"][0:32, a, 0:n]
        P.op("act", lambda e: e.activation(out=knb, in_=kn[0:32, :], func=AF.Copy), reads=[("kn", a)], writes=[("knb", a)])
        return (a, n, kn, knb, rope, out_bf, outkey, W)

    def head_norm_C(self, st, rotbank=4):
        if st is None:
            return
        P = self.P
        a, n, kn, knb, rope, out_bf, outkey, W = st
        C, S, rkey = rope
        psr = self.ps[0:32, rotbank, 0:n]
        rt = self.cview("rt", 32)
        P.op("pe", lambda e: e.matmul(psr, lhsT=rt, rhs=knb, start=True, stop=True), reads=[("knb", a), "cb"], writes=[("ps", rotbank)])
        t1 = W["t1"][0:32, 0:n]
        t2 = W["t2"][0:32, 0:n]
        P.op("dve", lambda e: e.tensor_tensor(out=t1, in0=psr, in1=S, op=ALU.mult), reads=[("ps", rotbank), rkey], writes=["t1"])
        P.op("dve", lambda e: e.tensor_tensor(out=t2, in0=kn[0:32, :], in1=C, op=ALU.mult), reads=[("kn", a), rkey], writes=["t2"])
        P.op("pool", lambda e: e.tensor_tensor(out=kn[0:32, :], in0=t1, in1=t2, op=ALU.add), reads=["t1", "t2"], writes=[("kn", a)])
        P.op("act", lambda e: e.activation(out=out_bf, in_=kn, func=AF.Copy), reads=[("kn", a)], writes=[outkey])

    def head_pipeline(self, heads):
        n = len(heads)
        stB = [None] * n
        res = [None] * n
        for step in range(n + 2):
            if step < n:
                res[step] = heads[step]["A"]()
            i = step - 1
            if 0 <= i < n:
                hd = heads[i]
                if hd.get("B") is not None:
                    stB[i] = self.head_norm_B(res[i][0], res[i][1], **hd["B"])
                elif hd.get("raw") is not None:
                    hd["raw"](res[i][0], res[i][1])
            i = step - 2
            if 0 <= i < n:
                self.head_norm_C(stB[i])
                if heads[i].get("post") is not None:
                    heads[i]["post"]()

    def head_temps(self, A):
        W = {}
        W["hsq"] = A.take(2 * TT * 2, BF16, "p (c t) -> p c t", c=2)
        W["hl"] = A.take(TT * 4, F32)
        W["hr"] = A.take(TT * 4, F32)
        W["kn"] = A.take(2 * TT * 4, F32, "p (c t) -> p c t", c=2)
        W["t1"] = A.take(TT * 4, F32)
        W["t2"] = A.take(TT * 4, F32)
        W["knb"] = A.take(2 * TT * 2, BF16, "p (c t) -> p c t", c=2)
        W["rc"] = A.take(TT * 4, F32)
        W["rsn"] = A.take(TT * 4, F32)
        return W

    def pass1a(self, s, li, M):
        P, A = self.P, self.A
        self.hn_ctr = 0
        xt, h = M["xt"], M["h"]
        NS1 = 3
        wring = A.take(NS1 * 16384, BF16, "p (s x) -> p s x", s=NS1)
        W = self.head_temps(A)
        M["sq"] = A.take(DC * TT * 2, BF16, "p (c t) -> p c t", c=DC)
        stage = A.take(4 * TT * 2, BF16, "p (c t) -> p c t", c=4)
        lf1 = A.take(TT * 4, F32)
        lf2 = A.take(TT * 4, F32)
        ones8 = A.take(TT * 4, F32)
        negb = A.take(64, F32)
        wfs = A.take(DC * 8 * 2, BF16, "p (c x) -> p c x", c=DC)
        wkey = None
        cfull = M["cfull"]
        wfm1 = self.wview(li, "wfm1").rearrange("(g p x) -> g p x", g=16, p=128)
        wtm = self.wview(li, "wtm").rearrange("(g p x) -> g p x", g=3, p=128)
        P.dma("sp", wfs, self.wview(li, "wf").rearrange("(p c x) -> p c x", p=128, c=DC), reads=self.wkeys(li, "wf"), writes=["wfs"])
        P.op("dve", lambda e: e.memset(ones8, 1.0), writes=["ones8"])
        P.op("dve", lambda e: e.tensor_scalar(out=negb[0:8, 0:1], in0=self.vcol(li, 70, rows=8), scalar1=-1.0, scalar2=None, op0=ALU.mult),
             reads=["vec"], writes=["negb"])
        sctr = 0
        stg = 0
        for tt in range(NTT):
            tsl = slice(tt * TT, (tt + 1) * TT)
            if tt == 0:
                self.load_x(M, s, tt)
                self.norm_sq(xt, M["sq"], "xt")
                self.norm_sum(M["sq"], M["tmp"], M["rs"], 0)
            self.norm_apply(xt, h, M["rs"], li, 16, "xt", "h")
            if tt + 1 < NTT:
                self.load_x(M, s, tt + 1)
            P.dma("sp", W["rc"][0:32, :], self.ropec.ap()[:, tsl], writes=["rope"])
            P.dma("sp", W["rsn"][0:32, :], self.ropes.ap()[:, tsl], writes=["rope"])
            heads = []
            PB = (1, 2, 6, 7)
            for cc in range(16):
                def A_(cc=cc):
                    nonlocal sctr
                    sl = sctr % NS1; sctr += 1
                    P.dma("sp", wring[:, sl, 0:2048], wfm1[cc], reads=self.wkeys(li, "wfm1"), writes=[("ws", sl)])
                    w = wring[:, sl, 0:2048].rearrange("p (c x) -> p c x", c=DC)
                    pb = PB[cc % 4]
                    psp = self.ps[:, pb, :]
                    for dc in range(DC):
                        P.op("pe", lambda e, o=psp, ww=w[:, dc, :], r=h[:, dc, :], s_=(dc == 0), t_=(dc == DC - 1):
                             e.matmul(o, lhsT=ww, rhs=r, start=s_, stop=t_),
                             reads=[("ws", sl), ("h", dc)], writes=[("ps", pb)])
                    return psp, ("ps", pb)
                st = stg % 4; stg += 1
                so = stage[:, st, :]
                hd = dict(A=A_)
                if cc < 4:
                    hd["raw"] = (lambda psp, pk, so=so, st=st: P.op("dve", lambda e: e.tensor_copy(out=so, in_=psp), reads=[pk], writes=[("stage", st)]))
                elif cc < 8:
                    hd["B"] = dict(n=TT, gain=self.vcol(li, 49 + (1 if cc < 6 else 2)), W=W, out_bf=so, outkey=("stage", st),
                                   rope=(W["rc"][0:32, :], W["rsn"][0:32, :], "rope"))
                else:
                    hd["B"] = dict(n=TT, gain=self.vcol(li, 53), W=W, out_bf=so, outkey=("stage", st))
                hd["post"] = (lambda cc=cc, so=so, st=st: P.dma("act", self.s_fm.ap()[cc, :, tsl], so, reads=[("stage", st)], writes=[("s_fm", cc)]))
                heads.append(hd)
            self.head_pipeline(heads)
            psf = self.ps[0:8, 5, :]
            for dc in range(DC):
                P.op("pe", lambda e, ww=wfs[:, dc, :], r=h[:, dc, :], s_=(dc == 0), t_=(dc == DC - 1):
                     e.matmul(psf, lhsT=ww, rhs=r, start=s_, stop=t_), reads=["wfs", ("h", dc)], writes=[("ps", 5)])
            P.op("act", lambda e: e.activation(out=lf1[0:8, :], in_=psf, func=AF.Exp, bias=negb[0:8, 0:1], scale=-1.0),
                 reads=[("ps", 5), "negb"], writes=["lf1"])
            P.op("act", lambda e: e.activation(out=lf2[0:8, :], in_=lf1[0:8, :], func=AF.Ln, bias=1.0, scale=1.0),
                 reads=["lf1"], writes=["lf2"])
            P.op("dve", lambda e: e.tensor_scalar(out=lf1[0:8, :], in0=lf2[0:8, :], scalar1=-1.0 / SCALE, scalar2=None, op0=ALU.mult),
                 reads=["lf2"], writes=["lf1"])
            init = 0.0 if tt == 0 else cfull[0:8, tt * TT - 1:tt * TT]
            P.op("dve", lambda e, o=cfull[0:8, tsl], i=init: e.tensor_tensor_scan(out=o, data0=ones8[0:8, :], data1=lf1[0:8, :],
                                                                                   initial=i, op0=ALU.mult, op1=ALU.add),
                 reads=["lf1", "ones8", "cfull"], writes=["cfull"])
            if tt + 1 < NTT:
                self.norm_sq(xt, M["sq"], "xt")
            n2 = 0
            for cg in range(3):
                sl = sctr % NS1; sctr += 1
                P.dma("sp", wring[:, sl, :], wtm[cg], reads=self.wkeys(li, "wtm"), writes=[("ws", sl)])
                w = wring[:, sl, :].rearrange("p (c x) -> p c x", c=DC)
                for tk in range(4):
                    pb = 6 + n2 % 2; n2 += 1
                    psp = self.ps[:, pb, :]
                    for dc in range(DC):
                        P.op("pe", lambda e, o=psp, l_=h[:, dc, tk * 128:(tk + 1) * 128], r=w[:, dc, :], s_=(dc == 0), t_=(dc == DC - 1):
                             e.matmul(o, lhsT=l_, rhs=r, start=s_, stop=t_),
                             reads=[("ws", sl), ("h", dc)], writes=[("ps", pb)])
                    st = stg % 4; stg += 1
                    so = stage[:, st, :]
                    if n2 % 2:
                        P.op("act", lambda e, o=so, i=psp: e.activation(out=o, in_=i, func=AF.Copy), reads=[("ps", pb)], writes=[("stage", st)])
                    else:
                        P.op("dve", lambda e, o=so, i=psp: e.tensor_copy(out=o, in_=i), reads=[("ps", pb)], writes=[("stage", st)])
                    kt = tt * 4 + tk
                    P.dma("act", self.s_tm.ap()[cg * 4:(cg + 1) * 4, :, kt, :].rearrange("g p d -> p g d"),
                          so.rearrange("p (g d) -> p g d", g=4), reads=[("stage", st)], writes=[("s_tm", cg)])
            if tt + 1 < NTT:
                self.norm_sum(M["sq"], M["tmp"], M["rs"], 0)

    def pass1b(self, s, li, M):
        P, A = self.P, self.A
        wkey = ("w", li)
        W = self.head_temps(A)
        kcraw = A.take(4 * T * 2, BF16, "p (c t) -> p c t", c=4)
        cw1 = A.take(2 * 32 * 256 * 2, BF16, "p (k l j) -> p k l j", k=2, l=32)
        cw2 = A.take(2 * 2 * 128 * 2, BF16, "p (k c x) -> p k c x", k=2, c=2)
        posb = A.take(64 * 2, BF16, "p (k l) -> p k l", k=2)
        hid = A.take(2 * 128 * 2, BF16, "p (c x) -> p c x", c=2)
        bias = A.take(16, F32)
        stage = A.take(2 * 128 * 2, BF16, "p (c x) -> p c x", c=2)
        rkc = A.take(2 * 128 * 4, F32, "p (c x) -> p c x", c=2)
        c3 = A.take(3 * T * 2, BF16, "p (c t) -> p c t", c=3)
        n3 = A.take(3 * T * 2, BF16, "p (c t) -> p c t", c=3)
        o3 = A.take(3 * T * 2, BF16, "p (c t) -> p c t", c=3)
        r1 = A.take(T * 4, F32)
        cfull = M["cfull"]
        P.dma("sp", kcraw, self.s_fm.ap()[0:4, :, :].rearrange("c p t -> p c t"), reads=[("s_fm", c) for c in range(4)], writes=["kcraw"])
        P.dma("sp", cw1, self.wview(li, "cw1").rearrange("(k p l j) -> p k l j", k=2, p=128, l=32), reads=self.wkeys(li, "cw1"), writes=["cw1"])
        P.dma("sp", cw2, self.wview(li, "cw2").rearrange("(k p c x) -> p k c x", k=2, p=128, c=2), reads=self.wkeys(li, "cw2"), writes=["cw2"])
        P.dma("sp", posb, self.wview(li, "pos").rearrange("(p k l) -> p k l", p=128, k=2), reads=self.wkeys(li, "pos"), writes=["posb"])
        P.dma("sp", rkc[0:32, :, :], self.ropekc.ap(), writes=["rkc"])
        cf = cfull[0:8, :]
        P.op("dve", lambda e: e.tensor_copy(out=c3[0:8, 0, :], in_=cf), reads=["cfull"], writes=["c3"])
        P.op("dve", lambda e: e.tensor_tensor(out=r1[0:8, :], in0=cf, in1=c3[0:8, 0, :], op=ALU.subtract), reads=["c3", "cfull"], writes=["r1"])
        P.op("dve", lambda e: e.tensor_copy(out=c3[0:8, 1, :], in_=r1[0:8, :]), reads=["r1"], writes=["c3"])
        P.op("dve", lambda e: e.tensor_tensor(out=r1[0:8, :], in0=r1[0:8, :], in1=c3[0:8, 1, :], op=ALU.subtract), reads=["c3"], writes=["r1"])
        P.op("dve", lambda e: e.tensor_copy(out=c3[0:8, 2, :], in_=r1[0:8, :]), reads=["r1"], writes=["c3"])
        P.op("dve", lambda e: e.tensor_scalar(out=n3[0:8, :, :], in0=c3[0:8, :, :], scalar1=-1.0, scalar2=None, op0=ALU.mult), reads=["c3"], writes=["n3"])
        P.op("pool", lambda e: e.memset(o3[0:8, :, :], 1.0), writes=["o3"])
        ab = self.s_ab.ap()
        P.dma("sp", ab[:, 0, 0:3, :], o3[0:8, :, :], reads=["o3"], writes=["s_ab"])
        P.dma("sp", ab[:, 0, 3:6, :], c3[0:8, :, :], reads=["c3"], writes=["s_ab"])
        P.dma("sp", ab[:, 1, 0:3, :], n3[0:8, :, :], reads=["n3"], writes=["s_ab"])
        P.dma("sp", ab[:, 1, 3:6, :], o3[0:8, :, :], reads=["o3"], writes=["s_ab"])
        self.hn_ctr = 0
        nq = 0
        for kv in range(2):
            for jc in range(2):
                for l in range(32):
                    P.op("pe", lambda e, o=self.ps[:, 0, kv * 2 + jc:kv * 2 + jc + 1], ww=cw1[:, kv, l, jc * 128:(jc + 1) * 128], r=posb[:, kv, l:l + 1],
                         s_=(l == 0), t_=(l == 31): e.matmul(o, lhsT=ww, rhs=r, start=s_, stop=t_),
                         reads=["cw1", "posb"], writes=[("ps", 0)])
            P.op("dve", lambda e, o=bias[:, kv * 2:kv * 2 + 2], i=self.ps[:, 0, kv * 2:kv * 2 + 2]: e.tensor_copy(out=o, in_=i),
                 reads=[("ps", 0)], writes=["cbias"])
            for kvh in range(2):
                raw = kcraw[:, kv * 2 + kvh, :]
                for jc in range(2):
                    pb = 1 + jc
                    psh = self.ps[:, pb, 0:NCMP]
                    for l in range(32):
                        P.op("pe", lambda e, o=psh, ww=cw1[:, kv, l, jc * 128:(jc + 1) * 128], r=raw[:, l:l + 16 * (NCMP - 1) + 1:16],
                             s_=(l == 0), t_=(l == 31): e.matmul(o, lhsT=ww, rhs=r, start=s_, stop=t_),
                             reads=["cw1", "kcraw"], writes=[("ps", pb)])
                    P.op("act", lambda e, o=hid[:, jc, 0:NCMP], i=psh, b=bias[:, kv * 2 + jc:kv * 2 + jc + 1]:
                         e.activation(out=o, in_=i, func=AF.Gelu_apprx_tanh, bias=b),
                         reads=[("ps", pb), "cbias"], writes=[("hid", jc)])
                st = nq % 2; nq += 1
                if kv == 0:
                    psk = self.ps[:, 5, 0:NCMP]
                    for jc in range(2):
                        P.op("pe", lambda e, ww=cw2[:, 0, jc, :], r=hid[:, jc, 0:NCMP], s_=(jc == 0), t_=(jc == 1):
                             e.matmul(psk, lhsT=ww, rhs=r, start=s_, stop=t_), reads=["cw2", ("hid", jc)], writes=[("ps", 5)])
                    self.head_norm(psk, ("ps", 5), NCMP, self.vcol(li, 49), W, stage[:, st, 0:NCMP], ("stage", st),
                                   rope=(rkc[0:32, 0, 0:NCMP], rkc[0:32, 1, 0:NCMP], "rkc"))
                    P.dma("sp", self.s_kc.ap()[kvh, :, 0:NCMP], stage[:, st, 0:NCMP], reads=[("stage", st)], writes=["s_kc"])
                else:
                    psv = self.ps[0:NCMP, 6, 0:128]
                    for jc in range(2):
                        P.op("pe", lambda e, l_=hid[:, jc, 0:NCMP], r=cw2[:, 1, jc, :], s_=(jc == 0), t_=(jc == 1):
                             e.matmul(psv, lhsT=l_, rhs=r, start=s_, stop=t_), reads=["cw2", ("hid", jc)], writes=[("ps", 6)])
                    P.op("dve", lambda e, o=stage[0:NCMP, st, :]: e.tensor_copy(out=o, in_=psv), reads=[("ps", 6)], writes=[("stage", st)])
                    P.dma("sp", self.s_vc.ap()[kvh, 0:NCMP, :], stage[0:NCMP, st, :], reads=[("stage", st)], writes=["s_vc"])

    def attn_multi(self, jobs):
        P = self.P
        ones = self.cview("ones")
        flat = [(ji, ti) for ji, job in enumerate(jobs) for ti in range(len(job["tiles"]))]

        def emit_qk(ji, ti):
            job = jobs[ji]
            t = job["tiles"][ti]
            sb = self.sctr % 2; self.sctr += 1
            nk = t["nk"]
            pss = self.ps[0:nk, sb, :]
            mm = [(t["k"], job["q"], list(t["kkeys"]) + [job["qkey"]])] + list(t["extras"])
            for m, (l_, r_, keys) in enumerate(mm):
                P.op("pe", lambda e, o=pss, l_=l_, r_=r_, s_=(m == 0), t_=(m == len(mm) - 1): e.matmul(o, lhsT=l_, rhs=r_, start=s_, stop=t_),
                     reads=list(keys) + ["cb"], writes=[("ps", sb)])
            return pss, sb, nk

        def emit_rest(ji, ti, info):
            job = jobs[ji]
            t = job["tiles"][ti]
            n = len(job["tiles"])
            pss, sb, nk = info
            lb, ob = job["lo"]
            p = self.pctr % 3; self.pctr += 1
            ptile = self.pt[0:nk, p, :]
            P.op("act", lambda e, o=ptile, i_=pss: e.activation(out=o, in_=i_, func=AF.Exp, scale=SCALE),
                 reads=[("ps", sb)], writes=[("pt", p)])
            P.op("pe", lambda e, l_=ones[0:nk, :], r_=ptile, s_=(ti == 0), t_=(ti == n - 1): e.matmul(self.ps[:, lb, :], lhsT=l_, rhs=r_, start=s_, stop=t_),
                 reads=[("pt", p), "cb"], writes=[("ps", lb)])
            P.op("pe", lambda e, l_=t["v"], r_=ptile, s_=(ti == 0), t_=(ti == n - 1): e.matmul(self.ps[:, ob, :], lhsT=l_, rhs=r_, start=s_, stop=t_),
                 reads=[("pt", p)] + list(t["vkeys"]), writes=[("ps", ob)])
            if ti == n - 1 and job.get("fin") is not None:
                d = job["fin"](ptile, ("pt", p))
                for f in deferred:
                    f()
                del deferred[:]
                if d is not None:
                    deferred.append(d)

        deferred = []
        pending = None
        for (ji, ti) in flat:
            if ti == 0 and jobs[ji].get("pre") is not None:
                jobs[ji]["pre"]()
            cur = emit_qk(ji, ti)
            if pending is not None:
                emit_rest(*pending)
            pending = (ji, ti, cur)
        if pending is not None:
            emit_rest(*pending)
        for f in deferred:
            f()

    def pass2(self, s, li, M):
        P, A = self.P, self.A
        self.hn_ctr = 0
        self.sctr = 0
        self.pctr = 0
        xt, h = M["xt"], M["h"]
        o_sb = h
        wkey = ("w", li)
        W = self.head_temps(A)
        NS2 = 4
        wring = A.take(NS2 * 4096, BF16, "p (s x) -> p s x", s=NS2)
        qall = A.take(16 * TT * 2, BF16, "p (c t) -> p c t", c=16)
        gsb = A.take(TT * 2, BF16)
        xst = A.take(4 * TT * 4, F32, "p (c t) -> p c t", c=4)
        wgs = A.take(DC * 24 * 2, BF16, "p (c x) -> p c x", c=DC)
        NKV = 6
        kvr = A.take(NKV * 4096, BF16, "p (s x) -> p s x", s=NKV)
        abA = A.take(2 * TT * 2, BF16, "p (c t) -> p c t", c=2)
        kcs = A.take(2 * 2 * 128 * 2, BF16, "p (a b x) -> p a b x", a=2, b=2)
        self.pt = A.take(3 * TT * 2, BF16, "p (c t) -> p c t", c=3)
        acc = A.take(4 * TT * 4, F32, "p (c t) -> p c t", c=4)
        rl = A.take(2 * TT * 4, F32, "p (c t) -> p c t", c=2)
        wg = A.take(TT * 4, F32)
        gtmp = wg
        of = A.take(2 * TT * 4, F32, "p (c t) -> p c t", c=2)
        osq = A.take(2 * TT * 2, BF16, "p (c t) -> p c t", c=2)
        phat = A.take(2 * TT * 2, BF16, "p (c t) -> p c t", c=2)
        score = A.take(4 * 32 * 4, F32, "p (c x) -> p c x", c=4)
        mx8 = A.take(4 * 8 * 4, F32, "p (c x) -> p c x", c=4)
        selb = A.take(4 * 32 * 2, BF16, "p (c x) -> p c x", c=4)
        selbT = A.take(TT * 2, BF16)
        selct = A.take(2 * 4 * 32 * 4, F32, "p (a c x) -> p a c x", a=2, c=4)
        ssq = A.take(2 * TT * 4, F32, "p (c t) -> p c t", c=2)
        rstd = A.take(2 * TT * 4, F32, "p (c t) -> p c t", c=2)
        otmp = A.take(2 * TT * 4, F32, "p (c t) -> p c t", c=2)
        ident = self.cview("ident")
        ones = self.cview("ones")
        ovl = self.cview("ovl")
        cbv = self.cview("cb").rearrange("p (k n) -> p k n", k=4)
        wbv = self.cview("wb").rearrange("p (k n) -> p k n", k=4)
        cmask = self.cview("cmask")
        exv = self.cview("ex", 32).rearrange("p (k n) -> p k n", k=16)
        eselv = self.cview("esel", 32).rearrange("p (k n) -> p k n", k=24)
        wfm2 = self.wview(li, "wfm2").rearrange("(g p x) -> g p x", g=16, p=128)
        wout = self.wview(li, "wout").rearrange("(g p x) -> g p x", g=16, p=128)
        P.dma("sp", wgs, self.wview(li, "wgt").rearrange("(p c x) -> p c x", p=128, c=DC), reads=self.wkeys(li, "wgt"), writes=["wgs"])
        P.dma("sp", kcs[:, :, 0, :], self.s_kc.ap().rearrange("k p x -> p k x"), reads=["s_kc"], writes=["kcs"])
        P.dma("sp", kcs[:, :, 1, :], self.s_vc.ap().rearrange("k p x -> p k x"), reads=["s_vc"], writes=["kcs"])
        sctr = 0
        kvc = 0
        rctr = 0
        yctr = 0
        actr = 0
        zctr = 0
        loc = 0
        for j in range(NTT):
            tsl = slice(j * TT, (j + 1) * TT)
            nkt = 4 * (j + 1)
            nk = nkt * 128
            if j == 0:
                self.load_x(M, s, j)
                self.norm_sq(xt, qall, "xt", sqkey="qa")
                self.norm_sum(qall, wg, M["rs"], 0, sqkey="qa", tmpkey="wg")
            self.norm_apply(xt, h, M["rs"], li, 16, "xt", "h")
            if j + 1 < NTT:
                self.load_x(M, s, j + 1)
            P.dma("sp", W["rc"][0:32, :], self.ropec.ap()[:, tsl], writes=["rope"])
            P.dma("sp", W["rsn"][0:32, :], self.ropes.ap()[:, tsl], writes=["rope"])
            P.dma("sp", selct, self.selc.ap()[:, :, 4 * j:4 * j + 4, :], writes=["selct"])
            heads = []
            PB = (1, 2, 5, 6)
            for cc in range(16):
                def A_(cc=cc):
                    nonlocal sctr
                    sl = sctr % NS2; sctr += 1
                    P.dma("sp", wring[:, sl, :], wfm2[cc], reads=self.wkeys(li, "wfm2"), writes=[("ws", sl)])
                    w = wring[:, sl, :].rearrange("p (c x) -> p c x", c=DC)
                    pb = PB[cc % 4]
                    psp = self.ps[:, pb, :]
                    for dc in range(DC):
                        P.op("pe", lambda e, o=psp, ww=w[:, dc, :], r=h[:, dc, :], s_=(dc == 0), t_=(dc == DC - 1):
                             e.matmul(o, lhsT=ww, rhs=r, start=s_, stop=t_),
                             reads=[("ws", sl), ("h", dc)], writes=[("ps", pb)])
                    return psp, ("ps", pb)
                if cc < 8:
                    B = dict(n=TT, gain=self.vcol(li, 48), W=W, out_bf=qall[:, cc, :], outkey=("qa", cc),
                             rope=(W["rc"][0:32, :], W["rsn"][0:32, :], "rope"))
                else:
                    B = dict(n=TT, gain=self.vcol(li, 52), W=W, out_bf=qall[:, cc, :], outkey=("qa", cc))
                heads.append(dict(A=A_, B=B))
            self.head_pipeline(heads)
            psg = self.ps[0:24, 7, :]
            for dc in range(DC):
                P.op("pe", lambda e, ww=wgs[:, dc, :], r=h[:, dc, :], s_=(dc == 0), t_=(dc == DC - 1):
                     e.matmul(psg, lhsT=ww, rhs=r, start=s_, stop=t_), reads=["wgs", ("h", dc)], writes=[("ps", 7)])
            P.op("act", lambda e: e.activation(out=gtmp[0:24, :], in_=psg, func=AF.Exp, scale=-1.0), reads=[("ps", 7)], writes=["wg"])
            P.op("dve", lambda e: e.tensor_scalar(out=gtmp[0:24, :], in0=gtmp[0:24, :], scalar1=1.0, scalar2=None, op0=ALU.add), reads=["wg"], writes=["wg"])
            P.op("dve", lambda e: e.reciprocal(out=gsb[0:24, :], in_=gtmp[0:24, :]), reads=["wg"], writes=["gsb"])
            for kvh in range(2):
                ch = []
                for _ in range(4):
                    ch.append(kvc % NKV); kvc += 1
                c_ks, c_kw, c_vs, c_vw = ch
                P.dma("sp", kvr[:, c_ks, 0:nk], self.s_fm.ap()[4 + kvh, :, 0:nk], reads=[("s_fm", 4 + kvh)], writes=[("kv", c_ks)])
                P.dma("sp", kvr[:, c_vs, 0:nk], self.s_tm.ap()[0 + kvh, :, 0:nkt, :].rearrange("p k d -> p (k d)"), reads=[("s_tm", 0)], writes=[("kv", c_vs)])
                P.dma("sp", kvr[:, c_kw, 0:nk], self.s_fm.ap()[6 + kvh, :, 0:nk], reads=[("s_fm", 6 + kvh)], writes=[("kv", c_kw)])
                P.dma("sp", kvr[:, c_vw, 0:nk], self.s_tm.ap()[2 + kvh, :, 0:nkt, :].rearrange("p k d -> p (k d)"), reads=[("s_tm", 0)], writes=[("kv", c_vw)])
                ksT = kvr[:, c_ks, :]
                kwT = kvr[:, c_kw, :]
                vs = kvr[:, c_vs, :].rearrange("p (k d) -> p k d", d=128)
                vw = kvr[:, c_vw, :].rearrange("p (k d) -> p k d", d=128)
                kcT = kcs[:, kvh, 0, 0:NCMP]
                vc = kcs[0:NCMP, kvh, 1, :]

                def fin_branch(g, b, lo, first, kvh=kvh):
                    nonlocal rctr, yctr
                    lb, ob = lo
                    x_ = rctr % 2; rctr += 1
                    rlb = rl[:, x_, :]
                    r = ((kvh * 4 + g) * 3 + b)
                    P.op("pe", lambda e: e.matmul(self.ps[:, 7, :], lhsT=eselv[0:24, r, :], rhs=gsb[0:24, :], start=True, stop=True),
                         reads=["gsb", "cb"], writes=[("ps", 7)])
                    P.op("dve", lambda e: e.tensor_scalar(out=rlb, in0=self.ps[:, lb, :], scalar1=1e-30, scalar2=None, op0=ALU.max),
                         reads=[("ps", lb)], writes=[("rl", x_)])
                    P.op("dve", lambda e: e.reciprocal(out=rlb, in_=rlb), reads=[("rl", x_)], writes=[("rl", x_)])
                    P.op("dve", lambda e: e.tensor_tensor(out=wg, in0=self.ps[:, 7, :], in1=rlb, op=ALU.mult),
                         reads=[("ps", 7), ("rl", x_)], writes=["wg"])
                    if first:
                        P.op("dve", lambda e: e.tensor_tensor(out=acc[:, g, :], in0=self.ps[:, ob, :], in1=wg, op=ALU.mult),
                             reads=[("ps", ob), "wg"], writes=[("acc", g)])
                    else:
                        y_ = yctr % 2; yctr += 1
                        P.op("dve", lambda e: e.tensor_tensor(out=otmp[:, y_, :], in0=self.ps[:, ob, :], in1=wg, op=ALU.mult),
                             reads=[("ps", ob), "wg"], writes=[("otmp", y_)])
                        P.op("pool", lambda e: e.tensor_tensor(out=acc[:, g, :], in0=acc[:, g, :], in1=otmp[:, y_, :], op=ALU.add),
                             reads=[("otmp", y_)], writes=[("acc", g)])
                    return x_

                jobs = []
                for g in range(4):
                    hq = kvh * 4 + g
                    lo = (3, 4) if loc % 2 == 0 else (5, 6); loc += 1
                    tiles = [dict(k=kcT, kkeys=["kcs"], nk=NCMP, extras=[(ident[0:NCMP, 0:NCMP], cmask[0:NCMP, tsl], [])],
                                  v=vc, vkeys=["kcs"])]

                    def fin_cmp(ptile, pkey, g=g, lo=lo):
                        nonlocal zctr
                        x_ = fin_branch(g, 0, lo, True)
                        z_ = zctr % 2; zctr += 1
                        P.op("pool", lambda e, o=phat[0:NCMP, z_, :], a=ptile, b=rl[0:NCMP, x_, :]: e.tensor_tensor(out=o, in0=a, in1=b, op=ALU.mult),
                             reads=[pkey, ("rl", x_)], writes=[("phat", z_)])
                        def dfr():
                            for tk in range(4):
                                P.op("pe", lambda e, o=self.ps[:, 2, tk * 32:(tk + 1) * 32], l_=phat[0:NCMP, z_, tk * 128:(tk + 1) * 128], r_=ovl[0:NCMP, :],
                                     s_=(g == 0 and tk == 0), t_=(g == 3): e.matmul(o, lhsT=l_, rhs=r_, start=s_, stop=t_, skip_group_check=True),
                                     reads=[("phat", z_), "cb"], writes=[("ps", 2)])
                        return dfr
                    jobs.append(dict(q=qall[:, hq, :], qkey=("qa", hq), tiles=tiles, lo=lo, fin=fin_cmp))
                self.attn_multi(jobs)
                imp = self.ps[:, 2, 0:128].rearrange("p (c x) -> p c x", c=4)
                P.op("dve", lambda e: e.tensor_tensor(out=score, in0=imp, in1=selct[:, 0, :, :], op=ALU.mult),
                     reads=[("ps", 2), "selct"], writes=["score"])
                P.op("dve", lambda e: e.tensor_tensor(out=score, in0=score, in1=selct[:, 1, :, :], op=ALU.add),
                     reads=["selct"], writes=["score"])
                for tk in range(4):
                    P.op("dve", lambda e, o=mx8[:, tk, :], i_=score[:, tk, :]: e.max(out=o, in_=i_), reads=["score"], writes=[("mx8", tk)])
                for tk in range(4):
                    P.op("dve", lambda e, o=selb[:, tk, :], i_=score[:, tk, :], th=mx8[:, tk, 7:8]:
                         e.tensor_scalar(out=o, in0=i_, scalar1=th, scalar2=NEG, op0=ALU.is_lt, op1=ALU.mult),
                         reads=["score", ("mx8", tk)], writes=[("selb", tk)])
                for tk in range(4):
                    P.op("pe", lambda e, o=self.ps[0:32, 2, tk * 128:(tk + 1) * 128], l_=selb[:, tk, :]:
                         e.matmul(o, lhsT=l_, rhs=ident, start=True, stop=True),
                         reads=[("selb", tk), "cb"], writes=[("ps", 2)])
                P.op("dve", lambda e: e.tensor_copy(out=selbT[0:32, :], in_=self.ps[0:32, 2, :]), reads=[("ps", 2)], writes=["selbT"])
                jobs = []
                for g in range(4):
                    hq = kvh * 4 + g
                    lo = (3, 4) if loc % 2 == 0 else (5, 6); loc += 1
                    tiles = []
                    for kt in range(nkt):
                        ex = [(exv[0:32, kt, :], selbT[0:32, :], ["selbT"])]
                        if kt >= 4 * j:
                            ex.append((ident, cbv[:, kt - 4 * j, :], []))
                        tiles.append(dict(k=ksT[:, kt * 128:(kt + 1) * 128], kkeys=[("kv", c_ks)], nk=128, extras=ex,
                                          v=vs[:, kt, :], vkeys=[("kv", c_vs)]))
                    jobs.append(dict(q=qall[:, hq, :], qkey=("qa", hq), tiles=tiles, lo=lo,
                                     fin=(lambda ptile, pkey, g=g, lo=lo: (fin_branch(g, 1, lo, False), None)[1])))
                for g in range(4):
                    hq = kvh * 4 + g
                    lo = (3, 4) if loc % 2 == 0 else (5, 6); loc += 1
                    tiles = []
                    for kt in range(max(0, 4 * j - 4), nkt):
                        if kt >= 4 * j:
                            ex = [(ident, cbv[:, kt - 4 * j, :], [])]
                        else:
                            ex = [(ident, wbv[:, kt - (4 * j - 4), :], [])]
                        tiles.append(dict(k=kwT[:, kt * 128:(kt + 1) * 128], kkeys=[("kv", c_kw)], nk=128, extras=ex,
                                          v=vw[:, kt, :], vkeys=[("kv", c_vw)]))
                    jobs.append(dict(q=qall[:, hq, :], qkey=("qa", hq), tiles=tiles, lo=lo,
                                     fin=(lambda ptile, pkey, g=g, lo=lo: (fin_branch(g, 2, lo, False), None)[1])))
                self.attn_multi(jobs)
                sl_of = {}
                def fsq(g):
                    nonlocal actr
                    a_ = actr % 2; actr += 1
                    sl_of[g] = a_
                    P.op("pool", lambda e, o=osq[:, a_, :], i_=acc[:, g, :]: e.tensor_tensor(out=o, in0=i_, in1=i_, op=ALU.mult),
                         reads=[("acc", g)], writes=[("osq", a_)])
                    P.op("dve", lambda e, o=o_sb[:, kvh * 4 + g, :], i_=acc[:, g, :], gc=self.vcol(li, 54 + kvh * 4 + g):
                         e.tensor_scalar(out=o, in0=i_, scalar1=gc, scalar2=None, op0=ALU.mult),
                         reads=[("acc", g), "vec"], writes=[("h", kvh * 4 + g)])
                def fsum(g):
                    a_ = sl_of[g]
                    hq = kvh * 4 + g
                    P.op("pe", lambda e, r_=osq[:, a_, :]: e.matmul(self.ps[:, 7, :], lhsT=ones, rhs=r_, start=True, stop=True),
                         reads=[("osq", a_), "cb"], writes=[("ps", 7)])
                    if hq == 0:
                        P.op("dve", lambda e: e.tensor_copy(out=ssq[:, 0, :], in_=self.ps[:, 7, :]), reads=[("ps", 7)], writes=[("ssq", 0)])
                    else:
                        P.op("dve", lambda e: e.tensor_tensor(out=ssq[:, 0, :], in0=self.ps[:, 7, :], in1=ssq[:, 0, :], op=ALU.add),
                             reads=[("ps", 7)], writes=[("ssq", 0)])
                fsq(0)
                for g in range(4):
                    if g + 1 < 4:
                        fsq(g + 1)
                    fsum(g)
            jobs = []
            for hf in range(8):
                ch = []
                for _ in range(3):
                    ch.append(kvc % NKV); kvc += 1
                c_k, c_v, c_b = ch
                a_ = hf % 2
                def pre_fox(hf=hf, c_k=c_k, c_v=c_v, c_b=c_b, a_=a_):
                    P.dma("sp", kvr[:, c_k, 0:nk], self.s_fm.ap()[8 + hf, :, 0:nk], reads=[("s_fm", 8 + hf)], writes=[("kv", c_k)])
                    P.dma("sp", kvr[0:6, c_b, 0:nk], self.s_ab.ap()[hf, 1, :, 0:nk], reads=["s_ab"], writes=[("kv", c_b)])
                    P.dma("sp", abA[0:6, a_, :], self.s_ab.ap()[hf, 0, :, tsl], reads=["s_ab"], writes=[("abA", a_)])
                    P.dma("sp", kvr[:, c_v, 0:nk], self.s_tm.ap()[4 + hf, :, 0:nkt, :].rearrange("p k d -> p (k d)"), reads=[("s_tm", 1), ("s_tm", 2)], writes=[("kv", c_v)])
                fk = kvr[:, c_k, :]
                fv = kvr[:, c_v, :].rearrange("p (k d) -> p k d", d=128)
                Bc = kvr[0:6, c_b, :]
                lo = (3, 4) if loc % 2 == 0 else (5, 6); loc += 1
                tiles = []
                for kt in range(nkt):
                    ex = [(Bc[:, kt * 128:(kt + 1) * 128], abA[0:6, a_, :], [("kv", c_b), ("abA", a_)])]
                    if kt >= 4 * j:
                        ex.append((ident, cbv[:, kt - 4 * j, :], []))
                    tiles.append(dict(k=fk[:, kt * 128:(kt + 1) * 128], kkeys=[("kv", c_k)], nk=128, extras=ex,
                                      v=fv[:, kt, :], vkeys=[("kv", c_v)]))

                def fin_fox(ptile, pkey, hf=hf, lo=lo):
                    nonlocal rctr, actr
                    lb, ob = lo
                    x_ = rctr % 2; rctr += 1
                    rlb = rl[:, x_, :]
                    P.op("dve", lambda e, o=rlb, i_=self.ps[:, lb, :]: e.reciprocal(out=o, in_=i_), reads=[("ps", lb)], writes=[("rl", x_)])
                    f_ = hf % 2
                    P.op("dve", lambda e, o=of[:, f_, :], a=self.ps[:, ob, :], b=rlb: e.tensor_tensor(out=o, in0=a, in1=b, op=ALU.mult),
                         reads=[("ps", ob), ("rl", x_)], writes=[("of", f_)])
                    q_ = actr % 2; actr += 1
                    P.op("pool", lambda e, o=osq[:, q_, :], i_=of[:, f_, :]: e.tensor_tensor(out=o, in0=i_, in1=i_, op=ALU.mult),
                         reads=[("of", f_)], writes=[("osq", q_)])
                    P.op("dve", lambda e, o=o_sb[:, 8 + hf, :], i_=of[:, f_, :], gc=self.vcol(li, 62 + hf):
                         e.tensor_scalar(out=o, in0=i_, scalar1=gc, scalar2=None, op0=ALU.mult),
                         reads=[("of", f_), "vec"], writes=[("h", 8 + hf)])

                    def dfr():
                        P.op("pe", lambda e, r_=osq[:, q_, :]: e.matmul(self.ps[:, 7, :], lhsT=ones, rhs=r_, start=True, stop=True),
                             reads=[("osq", q_), "cb"], writes=[("ps", 7)])
                        if hf == 0:
                            P.op("dve", lambda e: e.tensor_copy(out=ssq[:, 1, :], in_=self.ps[:, 7, :]), reads=[("ps", 7)], writes=[("ssq", 1)])
                        else:
                            P.op("dve", lambda e: e.tensor_tensor(out=ssq[:, 1, :], in0=self.ps[:, 7, :], in1=ssq[:, 1, :], op=ALU.add),
                                 reads=[("ps", 7)], writes=[("ssq", 1)])
                    return dfr
                jobs.append(dict(q=qall[:, 8 + hf, :], qkey=("qa", 8 + hf), tiles=tiles, lo=lo, fin=fin_fox, pre=pre_fox))
            self.attn_multi(jobs)
            for k in range(2):
                P.op("act", lambda e, o=rstd[:, k, :], i_=ssq[:, k, :]: e.activation(out=o, in_=i_, func=AF.Ln, bias=EPS, scale=1.0 / 1024.0),
                     reads=[("ssq", k)], writes=[("rstd", k)])
                P.op("act", lambda e, o=rstd[:, k, :]: e.activation(out=o, in_=o, func=AF.Exp, scale=-0.5),
                     reads=[("rstd", k)], writes=[("rstd", k)])
            for kc in range(16):
                P.op("dve" if kc % 2 == 0 else "pool", lambda e, o=o_sb[:, kc, :], b=rstd[:, kc // 8, :]: e.tensor_tensor(out=o, in0=o, in1=b, op=ALU.mult),
                     reads=[("rstd", kc // 8)], writes=[("h", kc)])
            if j + 1 < NTT:
                self.norm_sq(xt, qall, "xt", sqkey="qa")

            def issue(dco):
                nonlocal sctr
                sl = sctr % NS2; sctr += 1
                k = dco % 4
                P.dma("sp", wring[:, sl, :], wout[dco], reads=self.wkeys(li, "wout"), writes=[("ws", sl)])
                P.dma("sp", xst[:, k, :], M["src"].ap()[s, dco, :, tsl], reads=[("X", s, j, dco)], writes=[("xst", k)])
                return sl
            slots = {0: issue(0), 1: issue(1)}
            for dco in range(DC):
                if dco + 2 < DC:
                    slots[dco + 2] = issue(dco + 2)
                sl = slots[dco]
                k = dco % 4
                w = wring[:, sl, :].rearrange("p (c x) -> p c x", c=16)
                bnk = dco % 2
                for kc in range(16):
                    P.op("pe", lambda e, o=self.ps[:, bnk, :], ww=w[:, kc, :], r=o_sb[:, kc, :], s_=(kc == 0), t_=(kc == 15):
                         e.matmul(o, lhsT=ww, rhs=r, start=s_, stop=t_),
                         reads=[("ws", sl), ("h", kc)], writes=[("ps", bnk)])
                P.op("dve", lambda e, o=xst[:, k, :], a=self.ps[:, bnk, :]: e.tensor_tensor(out=o, in0=a, in1=o, op=ALU.add),
                     reads=[("ps", bnk)], writes=[("xst", k)])
                P.dma("act", self.xs.ap()[s, dco, :, tsl], xst[:, k, :], reads=[("xst", k)], writes=[("X", s, j, dco)])
            if j + 1 < NTT:
                self.norm_sum(qall, wg, M["rs"], 2, sqkey="qa", tmpkey="wg")


def _pack_layer(w, li):
    out = np.zeros(NPK, np.float32)

    def put(name, arr):
        o, n = PK[name]
        a = np.ascontiguousarray(arr, dtype=np.float32).reshape(-1)
        assert a.size == n, (name, a.size, n)
        out[o:o + n] = a

    for f in (1, 2):
        wg = w[f"ffn{f}_w_gate"][li]
        wu = w[f"ffn{f}_w_up"][li]
        wd = w[f"ffn{f}_w_down"][li]
        put(f"wg{f}", wg.reshape(DC, 128, 11, 512).transpose(2, 1, 0, 3))
        put(f"wu{f}", wu.reshape(DC, 128, 11, 512).transpose(2, 1, 0, 3))
        put(f"wd{f}", wd.reshape(FC, 128, DC, 128).transpose(2, 1, 0, 3))
    win = w["w_in"][li]
    kv0 = 1024
    def kvcol(branch, typ, kvh):
        return kv0 + ((branch * 2 + typ) * 2 + kvh) * 128
    fq0 = 2584
    fk0 = fq0 + 1024
    fv0 = fq0 + 2048
    cols1 = ([kvcol(0, 0, 0), kvcol(0, 0, 1), kvcol(0, 1, 0), kvcol(0, 1, 1),
              kvcol(1, 0, 0), kvcol(1, 0, 1), kvcol(2, 0, 0), kvcol(2, 0, 1)]
             + [fk0 + h * 128 for h in range(8)])
    def fm(cols):
        blk = np.stack([win[:, c:c + 128] for c in cols], 0)
        return blk.reshape(len(cols), DC, 128, 128).transpose(0, 2, 1, 3)
    put("wfm1", fm(cols1))
    tmcols = np.concatenate([np.arange(kvcol(1, 1, 0), kvcol(1, 1, 0) + 256),
                             np.arange(kvcol(2, 1, 0), kvcol(2, 1, 0) + 256),
                             np.arange(fv0, fv0 + 1024)])
    wt = win[:, tmcols]
    put("wtm", wt.reshape(DC, 128, 3, 512).transpose(2, 1, 0, 3))
    cols2 = [h * 128 for h in range(8)] + [fq0 + h * 128 for h in range(8)]
    put("wfm2", fm(cols2))
    wo = w["w_out"][li]
    put("wout", wo.reshape(16, 128, DC, 128).transpose(2, 1, 0, 3))
    cw1 = w["cmp_w1"][li]
    put("cw1", cw1.reshape(2, 32, 128, 256).transpose(0, 2, 1, 3))
    cw2 = w["cmp_w2"][li]
    put("cw2", cw2.reshape(2, 2, 128, 128).transpose(0, 2, 1, 3))
    put("wf", win[:, 5656:5664].reshape(DC, 128, 8).transpose(1, 0, 2))
    put("wgt", win[:, 2560:2584].reshape(DC, 128, 24).transpose(1, 0, 2))
    pos = w["cmp_pos_emb"][li]
    put("pos", pos.transpose(2, 0, 1))
    return out


def _vec_pack(w, layers):
    v = np.zeros((128, len(layers) * VW), np.float32)
    for i, li in enumerate(layers):
        b = i * VW
        v[:, b + 0:b + 16] = w["ffn1_norm"][li].reshape(DC, 128).T
        v[:, b + 16:b + 32] = w["mix_norm"][li].reshape(DC, 128).T
        v[:, b + 32:b + 48] = w["ffn2_norm"][li].reshape(DC, 128).T
        v[:, b + 48] = w["nsa_q_norm"][li]
        v[:, b + 49:b + 52] = w["nsa_k_norm"][li].T
        v[:, b + 52] = w["fox_q_norm"][li]
        v[:, b + 53] = w["fox_k_norm"][li]
        v[:, b + 54:b + 62] = w["nsa_out_norm"][li].reshape(8, 128).T
        v[:, b + 62:b + 70] = w["fox_out_norm"][li].reshape(8, 128).T
        v[0:8, b + 70] = w["fox_forget_bias"][li]
    return v


def _consts():
    cb = np.zeros((128, NCB), np.float32)
    def put(name, arr):
        o, n = CB[name]
        cb[:arr.shape[0], o:o + n] = arr.reshape(arr.shape[0], -1)
    put("ident", np.eye(128, dtype=np.float32))
    put("ones", np.ones((128, 128), np.float32))
    rt = np.zeros((32, 32), np.float32)
    for i in range(16):
        rt[16 + i, i] = -1.0
        rt[i, 16 + i] = 1.0
    put("rt", rt)
    cstart = np.arange(NCMP) * 16
    sstart = np.arange(32) * 64
    ovl = ((cstart[:, None] < sstart[None, :] + 64) & (cstart[:, None] + 32 > sstart[None, :])).astype(np.float32)
    put("ovl", ovl)
    p = np.arange(128)[:, None]
    n = np.arange(512)[None, :]
    cbm = np.stack([np.where(128 * k + p <= n, 0.0, NEG) for k in range(4)], 1)
    wbm = np.stack([np.where(128 * k + p > n, 0.0, NEG) for k in range(4)], 1)
    put("cb", cbm.astype(np.float32))
    put("wb", wbm.astype(np.float32))
    cend = cstart + 31
    t = np.arange(T)[None, :]
    put("cmask", np.where(cend[:, None] <= t, 0.0, NEG).astype(np.float32))
    j = np.arange(32)[:, None, None]
    kt = np.arange(16)[None, :, None]
    pp = np.arange(128)[None, None, :]
    put("ex", (j == 2 * kt + pp // 64).astype(np.float32))
    r = np.arange(32)[:, None, None]
    rr = np.arange(24)[None, :, None]
    put("esel", np.broadcast_to((r == rr), (32, 24, 128)).astype(np.float32))
    inv = (np.float32(500000.0) ** (-np.arange(0, 32, 2, dtype=np.float32) / np.float32(32))).astype(np.float32)
    def tables(pos):
        ang = pos.astype(np.float32)[:, None] * inv[None, :]
        c = np.cos(ang).astype(np.float32).T
        s_ = np.sin(ang).astype(np.float32).T
        return np.concatenate([c, c], 0), np.concatenate([s_, s_], 0)
    rc, rs = tables(np.arange(T))
    kc_c, kc_s = tables(cend)
    ropekc = np.zeros((32, 2, 128), np.float32)
    ropekc[:, 0, :NCMP] = kc_c
    ropekc[:, 1, :NCMP] = kc_s
    tok = np.arange(T)
    tb = tok // 64
    jj = np.arange(32)[None, :]
    causal = jj <= tb[:, None]
    forced = (jj == 0) | (causal & (jj > tb[:, None] - 2))
    mmul = (causal & ~forced).astype(np.float32)
    badd = np.where(forced, 1e9, np.where(causal, 0.0, -1e30)).astype(np.float32)
    selc = np.stack([mmul.reshape(16, 128, 32).transpose(1, 0, 2), badd.reshape(16, 128, 32).transpose(1, 0, 2)], 1)
    return cb, rc, rs, ropekc, np.ascontiguousarray(selc)


_CACHE = {}


def _get_prog(nseq, layers_key, dbg_key=None):
    key = (nseq, layers_key, dbg_key)
    if key not in _CACHE:
        b = Builder(nseq, list(layers_key), dict(dbg_key or ()))
        _CACHE[key] = b.build()
    return _CACHE[key]


def kernel(**inputs):
    x = np.asarray(inputs["x"], np.float32)
    B = x.shape[0]
    ncores = 8
    nseq = B // ncores
    w = {k: np.asarray(v) for k, v in inputs.items() if k != "x"}
    layers = tuple(range(DEPTH))
    wsrc = np.concatenate([_pack_layer(w, li) for li in layers]).reshape(len(layers) * NPKR, 2048)
    vec = _vec_pack(w, layers)
    cb, rc, rs, ropekc, selc = _consts()
    nc = _get_prog(nseq, layers)
    in_maps = []
    for c in range(ncores):
        xc = x[c * nseq:(c + 1) * nseq]
        xT = np.ascontiguousarray(xc.transpose(0, 2, 1)).reshape(nseq, DC, 128, T)
        in_maps.append({"xin": xT, "wsrc": wsrc, "vec": vec, "cbf": cb, "ropec": rc, "ropes": rs,
                        "ropekc": ropekc, "selc": selc})
    res = run_bass_kernel_spmd(nc, in_maps, core_ids=list(range(ncores)))
    outs = []
    for c in range(ncores):
        y = res.results[c]["xout"].reshape(nseq, D, T).transpose(0, 2, 1)
        outs.append(y)
    return np.ascontiguousarray(np.concatenate(outs, 0), dtype=np.float32)
```
